# Optimizing a Trainium2 kernel written in Bass

```python
import math
import jax, jax.numpy as jnp
from jax import lax
import numpy as np


D_MODEL = 1024
BATCH = 2
SEQ = 8192
DEPTH = 2

CHUNK = 64
N_META = 16
Q_BLOCK = 128

SB_HEADS = 8
SB_HEAD_DIM = 64
SB_WIDTH = SB_HEADS * SB_HEAD_DIM

RWKV_HEADS = 8
RWKV_HEAD_DIM = 64
RWKV_WIDTH = RWKV_HEADS * RWKV_HEAD_DIM
W_LORA = 64
A_LORA = 64
G_LORA = 128
RWKV_SHIFT_WIDTH = 3 * RWKV_WIDTH + W_LORA + A_LORA + G_LORA
DECAY_SCALE = math.exp(-0.5)
LNX_EPS = 64e-5

C_IN = 3 * SB_WIDTH + RWKV_SHIFT_WIDTH + 2 * D_MODEL
SPLITS = [int(s) for s in np.cumsum([SB_WIDTH, SB_WIDTH, SB_WIDTH, RWKV_SHIFT_WIDTH, D_MODEL])]
RWKV_SPLITS = [int(s) for s in np.cumsum([RWKV_WIDTH, RWKV_WIDTH, RWKV_WIDTH, W_LORA, A_LORA])]

N_EXPERTS = 16
N_GROUPS = 4
EXPERTS_PER_GROUP = N_EXPERTS // N_GROUPS
TOP_K = 2
D_EXPERT = 512

ALPHA = (2 * DEPTH) ** 0.25
BETA_INIT = (8 * DEPTH) ** -0.25
LN_EPS = 1e-5

kernel_name = "hybrid_stickbreak_rwkv7_moe_encoder"


def layer_norm(x, g, b):
    xf = x.astype(jnp.float32)
    mu = jnp.mean(xf, axis=-1, keepdims=True)
    var = jnp.mean(jnp.square(xf - mu), axis=-1, keepdims=True)
    return ((xf - mu) * lax.rsqrt(var + LN_EPS) * g + b).astype(x.dtype)


def token_shift(p):
    return jnp.pad(p[:, :-1], ((0, 0), (1, 0), (0, 0)))


def stick_breaking_attention(q, k, v):
    B, H, L, N = q.shape
    nblk = -(-L // Q_BLOCK)
    Lp = nblk * Q_BLOCK
    pad = ((0, 0), (0, 0), (0, Lp - L), (0, 0))
    q, k, v = jnp.pad(q, pad), jnp.pad(k, pad), jnp.pad(v, pad)
    scale = N ** -0.5
    key_pos = jnp.arange(Lp)

    def block(i):
        q_blk = lax.dynamic_slice_in_dim(q, i * Q_BLOCK, Q_BLOCK, axis=2)
        z = jnp.einsum('bhqn,bhkn->bhqk', q_blk, k) * scale
        q_pos = i * Q_BLOCK + jnp.arange(Q_BLOCK)
        mask = key_pos[None, :] < q_pos[:, None]
        u = jnp.where(mask, jax.nn.log_sigmoid(-z), 0.0)
        rest = lax.cumsum(u, axis=3, reverse=True) - u
        A = jnp.where(mask, jnp.exp(jax.nn.log_sigmoid(z) + rest), 0.0)
        return jnp.einsum('bhqk,bhkn->bhqn', A, v)

    out = lax.map(block, jnp.arange(nblk))
    out = jnp.moveaxis(out, 0, 2).reshape(B, H, Lp, N)
    return out[:, :, :L]


def rwkv7_scan(r, w, k, v, kk, a):
    B, L, H, N = r.shape

    def step(S, inp):
        r_t, w_t, k_t, v_t, kk_t, a_t = inp
        sa = jnp.einsum('bhvk,bhk->bhv', S, -kk_t)
        S = (S * w_t[:, :, None, :]
             + sa[..., None] * (kk_t * a_t)[:, :, None, :]
             + v_t[..., None] * k_t[:, :, None, :])
        y = jnp.einsum('bhvk,bhk->bhv', S, r_t)
        return S, y

    xs = tuple(jnp.moveaxis(t, 1, 0) for t in (r, w, k, v, kk, a))
    S0 = jnp.zeros((B, H, N, N), jnp.float32)
    _, ys = lax.scan(step, S0, xs)
    return jnp.moveaxis(ys, 0, 1)


def rwkv7_time_mix(p_rw, mu, w0, w_up, a0, a_up, g_up, k_k, k_a, r_k, lnx_g, lnx_b):
    B, L, _ = p_rw.shape
    x = p_rw + (token_shift(p_rw) - p_rw) * mu
    r, k, v, wd, ad, gd = jnp.split(x, RWKV_SPLITS, axis=-1)
    w = jnp.exp(-DECAY_SCALE * jax.nn.sigmoid(w0 + jnp.tanh(wd) @ w_up))
    a = jax.nn.sigmoid(a0 + ad @ a_up)
    g = jax.nn.sigmoid(gd) @ g_up

    def heads(t):
        return t.reshape(B, L, RWKV_HEADS, RWKV_HEAD_DIM)

    kk = heads(k * k_k)
    kk = kk / jnp.maximum(jnp.sqrt(jnp.sum(kk * kk, axis=-1, keepdims=True)), 1e-12)
    k = k * (1.0 + (a - 1.0) * k_a)
    r, k, v, w, a = heads(r), heads(k), heads(v), heads(w), heads(a)
    y = rwkv7_scan(r, w, k, v, kk, a)
    mean = jnp.mean(y, axis=-1, keepdims=True)
    var = jnp.mean(jnp.square(y - mean), axis=-1, keepdims=True)
    yn = ((y - mean) * lax.rsqrt(var + LNX_EPS)).reshape(B, L, RWKV_WIDTH) * lnx_g + lnx_b
    bonus = (jnp.sum(r * k * r_k, axis=-1, keepdims=True) * v).reshape(B, L, RWKV_WIDTH)
    return (yn + bonus) * g


def hybrid_mixer(h, w_in, mu, w0, w_up, a0, a_up, g_up, k_k, k_a, r_k,
                 lnx_g, lnx_b, p_sb, p_rwkv, w_out):
    B, L, _ = h.shape
    p = jnp.einsum('bld,dc->blc', h, w_in).astype(jnp.float32)
    q_sb, k_sb, v_sb, p_rw, gate_sb, gate_rw = jnp.split(p, SPLITS, axis=-1)

    def heads(t):
        return t.reshape(B, L, SB_HEADS, SB_HEAD_DIM).transpose(0, 2, 1, 3)

    o_sb = stick_breaking_attention(heads(q_sb), heads(k_sb), heads(v_sb))
    o_sb = o_sb.transpose(0, 2, 1, 3).reshape(B, L, SB_WIDTH)
    o_rw = rwkv7_time_mix(p_rw, mu, w0, w_up, a0, a_up, g_up, k_k, k_a, r_k, lnx_g, lnx_b)
    merged = (jax.nn.sigmoid(gate_sb) * (o_sb @ p_sb)
              + jax.nn.sigmoid(gate_rw) * (o_rw @ p_rwkv))
    return (merged @ w_out).astype(h.dtype)


def moe_ffn(h, router_w, router_b, wg, wu, wd):
    B, L, D = h.shape
    t = h.reshape(B * L, D)
    logits = (t @ router_w).astype(jnp.float32) + router_b.astype(jnp.float32)
    probs = jax.nn.softmax(logits, axis=-1)
    grouped = probs.reshape(-1, N_GROUPS, EXPERTS_PER_GROUP)
    group_score = jnp.sum(lax.top_k(grouped, TOP_K)[0], axis=-1)
    g_sel = jnp.argmax(group_score, axis=-1)
    in_group = (jnp.arange(N_EXPERTS) // EXPERTS_PER_GROUP)[None, :] == g_sel[:, None]
    top_val, top_idx = lax.top_k(jnp.where(in_group, probs, -1.0), TOP_K)
    gate = top_val / jnp.sum(top_val, axis=-1, keepdims=True)
    combine = jnp.sum(jax.nn.one_hot(top_idx, N_EXPERTS, dtype=jnp.float32) * gate[..., None], axis=1)
    out = jnp.zeros((B * L, D), jnp.float32)
    for e in range(N_EXPERTS):
        hid = jax.nn.silu(t @ wg[e]) * (t @ wu[e])
        out = out + combine[:, e:e + 1] * (hid @ wd[e])
    return out.astype(h.dtype).reshape(B, L, D)


def setup_inputs(seed: int = 0) -> dict:
    key = jax.random.key(seed)
    ks = jax.random.split(key, 32)
    f32 = jnp.float32

    def nrm(k, shape, scale):
        return jax.random.normal(k, shape, f32) * scale

    return {
        "x": nrm(ks[0], (BATCH, SEQ, D_MODEL), 1.0),
        "meta": nrm(ks[1], (N_META, D_MODEL), 1.0),
        "emb_ln_g": 1.0 + nrm(ks[2], (D_MODEL,), 0.02),
        "emb_ln_b": nrm(ks[3], (D_MODEL,), 0.02),
        "w_in": nrm(ks[4], (DEPTH, D_MODEL, C_IN), D_MODEL ** -0.5),
        "rwkv_mu": jax.random.uniform(ks[5], (DEPTH, RWKV_SHIFT_WIDTH), f32),
        "w0": nrm(ks[6], (DEPTH, RWKV_WIDTH), 0.5),
        "w_up": nrm(ks[7], (DEPTH, W_LORA, RWKV_WIDTH), W_LORA ** -0.5),
        "a0": nrm(ks[8], (DEPTH, RWKV_WIDTH), 0.5),
        "a_up": nrm(ks[9], (DEPTH, A_LORA, RWKV_WIDTH), A_LORA ** -0.5),
        "g_up": nrm(ks[10], (DEPTH, G_LORA, RWKV_WIDTH), G_LORA ** -0.5),
        "k_k": 0.85 + nrm(ks[11], (DEPTH, RWKV_WIDTH), 0.05),
        "k_a": 1.0 + nrm(ks[12], (DEPTH, RWKV_WIDTH), 0.05),
        "r_k": nrm(ks[13], (DEPTH, RWKV_HEADS, RWKV_HEAD_DIM), 0.1),
        "lnx_g": 1.0 + nrm(ks[14], (DEPTH, RWKV_WIDTH), 0.02),
        "lnx_b": nrm(ks[15], (DEPTH, RWKV_WIDTH), 0.02),
        "p_sb": nrm(ks[16], (DEPTH, SB_WIDTH, D_MODEL), SB_WIDTH ** -0.5),
        "p_rwkv": nrm(ks[17], (DEPTH, RWKV_WIDTH, D_MODEL), RWKV_WIDTH ** -0.5),
        "w_out": nrm(ks[18], (DEPTH, D_MODEL, D_MODEL), D_MODEL ** -0.5 * BETA_INIT),
        "ln1_g": 1.0 + nrm(ks[19], (DEPTH, D_MODEL), 0.02),
        "ln1_b": nrm(ks[20], (DEPTH, D_MODEL), 0.02),
        "router_w": nrm(ks[21], (D_MODEL, N_EXPERTS), D_MODEL ** -0.5),
        "router_b": nrm(ks[22], (N_EXPERTS,), 0.01),
        "exp_w_gate": nrm(ks[23], (DEPTH, N_EXPERTS, D_MODEL, D_EXPERT), D_MODEL ** -0.5),
        "exp_w_up": nrm(ks[24], (DEPTH, N_EXPERTS, D_MODEL, D_EXPERT), D_MODEL ** -0.5),
        "exp_w_down": nrm(ks[25], (DEPTH, N_EXPERTS, D_EXPERT, D_MODEL), D_EXPERT ** -0.5 * BETA_INIT),
        "ln2_g": 1.0 + nrm(ks[26], (DEPTH, D_MODEL), 0.02),
        "ln2_b": nrm(ks[27], (DEPTH, D_MODEL), 0.02),
    }


def reference(x, meta, emb_ln_g, emb_ln_b, w_in, rwkv_mu, w0, w_up, a0, a_up, g_up,
              k_k, k_a, r_k, lnx_g, lnx_b, p_sb, p_rwkv, w_out, ln1_g, ln1_b,
              router_w, router_b, exp_w_gate, exp_w_up, exp_w_down, ln2_g, ln2_b):
    B = x.shape[0]
    meta_b = jnp.broadcast_to(meta.astype(x.dtype)[None], (B, N_META, D_MODEL))
    h = layer_norm(jnp.concatenate([meta_b, x], axis=1), emb_ln_g, emb_ln_b)
    for l in range(DEPTH):
        mix = hybrid_mixer(h, w_in[l], rwkv_mu[l], w0[l], w_up[l], a0[l], a_up[l], g_up[l],
                           k_k[l], k_a[l], r_k[l], lnx_g[l], lnx_b[l], p_sb[l], p_rwkv[l], w_out[l])
        h = layer_norm(ALPHA * h + mix, ln1_g[l], ln1_b[l])
        ff = moe_ffn(h, router_w, router_b, exp_w_gate[l], exp_w_up[l], exp_w_down[l])
        h = layer_norm(ALPHA * h + ff, ln2_g[l], ln2_b[l])
    return h[:, N_META:]
```

```python
import numpy as np
from contextlib import ExitStack
import concourse.bass as bass
import concourse.mybir as mybir
from concourse.bass_utils import run_bass_kernel_spmd

F32 = mybir.dt.float32
BF16 = mybir.dt.bfloat16
AF = mybir.ActivationFunctionType
ALU = mybir.AluOpType
AX = mybir.AxisListType

import os
BUDGET = int(os.environ["KBUDGET"]) if "KBUDGET" in os.environ else None
ENGS = ["pe", "act", "dve", "pool", "sp"]
DMA_R = 8


class T:
    __slots__ = ("name", "lw", "rd", "const", "excl")

    def __init__(self, name, const=False, excl=False):
        self.name = name
        self.lw = None
        self.rd = []
        self.const = const
        self.excl = excl


class Prog:
    def __init__(self, nc, es):
        self.nc = nc
        self.es = es
        self.items = {e: [] for e in ENGS}
        self.sem = {e: es.enter_context(nc.semaphore("s_" + e)) for e in ENGS}
        self.cnt = {e: 0 for e in ENGS}
        self.seen = {e: {} for e in ENGS}
        self.dsem = {}
        self.dma_n = {}
        self.dma_final = {}
        self.ntile = 0

    def sb(self, name, shape, dt=F32):
        return self.es.enter_context(self.nc.sbuf_tensor("sb_" + name, list(shape), dt))

    def ps(self, name, shape=(128, 512), dt=F32):
        return self.es.enter_context(self.nc.psum_tensor("pp_" + name, list(shape), dt))

    def T(self, name=None, const=False, excl=False):
        self.ntile += 1
        return T(name or f"t{self.ntile}", const, excl)

    def _deps(self, eng, reads, writes):
        deps = {}

        def add(p):
            if p is None:
                return
            k, v = p
            if deps.get(k, 0) < v:
                deps[k] = v

        for t in reads:
            add(t.lw)
        for t in writes:
            add(t.lw)
            for p in t.rd:
                add(p)
        waits = []
        for k, v in deps.items():
            if k == eng and eng == "pe":
                continue
            if self.seen[eng].get(k, 0) >= v:
                continue
            self.seen[eng][k] = v
            waits.append((k, v))
        return waits

    def _commit(self, pid, reads, writes):
        for t in writes:
            t.lw = pid
            t.rd = []
        for t in reads:
            if not t.const:
                t.rd.append(pid)
                if len(t.rd) > 64:
                    m = {}
                    for k, v in t.rd:
                        if m.get(k, 0) < v:
                            m[k] = v
                    t.rd = list(m.items())

    def op(self, eng, fn, reads=(), writes=()):
        self.nops = getattr(self, "nops", 0) + 1
        if BUDGET is not None and self.nops > BUDGET:
            return
        ex = [t for t in reads if t.excl]
        if ex:
            reads = [t for t in reads if not t.excl]
            writes = list(writes) + ex
        waits = self._deps(eng, reads, writes)
        self.cnt[eng] += 1
        pid = (eng, self.cnt[eng])
        self.items[eng].append((waits, fn, True))
        self._commit(pid, reads, writes)

    def dma(self, q, out_ap, in_ap, reads=(), writes=()):
        self.nops = getattr(self, "nops", 0) + 1
        if BUDGET is not None and self.nops > BUDGET:
            return
        if q not in self.dsem:
            self.dsem[q] = [self.es.enter_context(self.nc.semaphore(f"d_{q}_{i}")) for i in range(DMA_R)]
            self.dma_n[q] = 0
        n = self.dma_n[q]
        self.dma_n[q] += 1
        i = n % DMA_R
        rnd = n // DMA_R
        key = ("d", q, i)
        waits = self._deps(q, reads, writes)
        if rnd > 0 and self.seen[q].get(key, 0) < 16 * rnd:
            self.seen[q][key] = 16 * rnd
            waits.append((key, 16 * rnd))
        sem = self.dsem[q][i]

        def fn(e, out_ap=out_ap, in_ap=in_ap, sem=sem):
            return e.dma_start(out=out_ap, in_=in_ap).then_inc(sem, 16)

        self.items[q].append((waits, fn, False))
        pid = (key, 16 * (rnd + 1))
        self.dma_final[key] = 16 * (rnd + 1)
        self._commit(pid, reads, writes)

    def _semobj(self, k):
        if isinstance(k, tuple):
            return self.dsem[k[1]][k[2]]
        return self.sem[k]

    def finish(self):
        waits = []
        for key, v in self.dma_final.items():
            waits.append((key, v))
        for e in ENGS:
            if e != "sp" and self.cnt[e] > 0:
                waits.append((e, self.cnt[e]))
        self.items["sp"].append((waits, None, False))
        nc = self.nc
        with nc.Block() as block:
            def replay(name, e):
                for waits, fn, inc in self.items[name]:
                    for k, v in waits:
                        e.wait_ge(self._semobj(k), v)
                    if fn is None:
                        continue
                    ins = fn(e)
                    if inc:
                        ins.then_inc(self.sem[name], 1)

            @block.tensor
            def _(e):
                replay("pe", e)

            @block.scalar
            def _(e):
                replay("act", e)

            @block.vector
            def _(e):
                replay("dve", e)

            @block.gpsimd
            def _(e):
                replay("pool", e)

            @block.sync
            def _(e):
                replay("sp", e)


N_META = 16


def build_attn(nc, NH, NG):
    NB = 4 * NG
    L = N_META + 512 * NG
    qT = nc.dram_tensor("qT", [NH, 64, L], F32, kind="ExternalInput").ap()
    kT = nc.dram_tensor("kT", [NH, 64, L], F32, kind="ExternalInput").ap()
    vx = nc.dram_tensor("vx", [NH, 128, NB, 64], F32, kind="ExternalInput").ap()
    vm = nc.dram_tensor("vm", [NH, 16, 64], F32, kind="ExternalInput").ap()
    msk = nc.dram_tensor("msk", [128, 3, 128], F32, kind="ExternalInput").ap()
    oT = nc.dram_tensor("oT", [NH, 64, L], F32, kind="ExternalOutput").ap()
    with ExitStack() as es:
        P = Prog(nc, es)
        emit_attn(P, NH, NG, qT, kT, vx, vm, msk, oT)
        P.finish()
    return nc


def emit_attn(P, NH, NG, qT, kT, vx, vm, msk, oT):
    NB = 4 * NG
    L = N_META + 512 * NG
    mstage = P.sb("mstage", [128, 3, 128], F32)
    t_mstage = P.T()
    P.dma("sp", mstage[:], msk, writes=[t_mstage])
    cm = P.sb("cm", [128, 3, 128], BF16)
    t_cm = P.T(const=True)
    P.op("dve", lambda e: e.tensor_copy(cm[:], mstage[:]), reads=[t_mstage], writes=[t_cm])
    zeros = P.sb("zeros", [128, 64], BF16)
    t_zeros = P.T(const=True)
    P.op("pool", lambda e: e.memset(zeros[:], 0.0), writes=[t_zeros])

    QT = P.sb("QT", [64, L], BF16)
    KT = P.sb("KT", [64, L], BF16)
    V = P.sb("V", [128, NB, 64], BF16)
    VM = P.sb("VM", [16, 64], BF16)
    t_Q, t_K, t_V = P.T(), P.T(), P.T()
    CH = 2064 if L > 2064 else L
    stg = [P.sb(f"stg{i}", [128, CH], F32) for i in range(2)]
    t_stg = [P.T(), P.T()]
    vstg = P.sb("vstg", [128, NB, 64], F32)
    t_vstg = P.T()
    vmstg = P.sb("vmstg", [16, 64], F32)
    t_vmstg = P.T()

    zA = [P.ps(f"zA{i}") for i in range(2)]
    zB = [P.ps(f"zB{i}") for i in range(2)]
    Ops = [P.ps(f"Ops{i}") for i in range(2)]
    t_zA = [P.T(excl=True), P.T(excl=True)]
    t_zB = [P.T(excl=True), P.T(excl=True)]
    t_O = [P.T(excl=True), P.T(excl=True)]
    e_sb = [P.sb(f"e{i}", [128, 512], F32) for i in range(2)]
    t_e = [P.T(), P.T()]
    sp_sb = [P.sb(f"sp{i}", [128, 512], BF16) for i in range(2)]
    t_sp = [P.T(), P.T()]
    A_sb = [P.sb(f"A{i}", [128, 512], BF16) for i in range(2)]
    t_A = [P.T(), P.T()]
    acc = P.sb("acc", [128, 512], F32)
    t_acc = P.T()
    accb = [P.sb(f"accb{i}", [128, 512], BF16) for i in range(2)]
    t_accb = [P.T(), P.T()]
    ost = [P.sb(f"ost{i}", [64, 512], F32) for i in range(2)]
    t_ost = [P.T(), P.T()]

    nstg = 0
    gcount = 0
    for h in range(NH):
        for (src, dst, tt, scale) in ((qT, QT, t_Q, 0.125), (kT, KT, t_K, 1.0)):
            first = True
            for c0 in range(0, L, CH):
                w = min(CH, L - c0)
                s = nstg % 2
                nstg += 1
                P.dma("sp", stg[s][0:64, 0:w], src[h, :, c0:c0 + w], writes=[t_stg[s]])
                if scale != 1.0:
                    P.op("dve", lambda e, s=s, w=w, c0=c0, dst=dst, scale=scale:
                         e.tensor_scalar(dst[:, c0:c0 + w], stg[s][0:64, 0:w], scale, None, ALU.mult),
                         reads=[t_stg[s]], writes=[tt])
                else:
                    P.op("pool", lambda e, s=s, w=w, c0=c0, dst=dst:
                         e.tensor_copy(dst[:, c0:c0 + w], stg[s][0:64, 0:w]),
                         reads=[t_stg[s]], writes=[tt])
        P.dma("sp", vstg[:], vx[h], writes=[t_vstg])
        P.op("pool", lambda e: e.tensor_copy(V[:], vstg[:]), reads=[t_vstg], writes=[t_V])
        P.dma("sp", vmstg[:], vm[h], writes=[t_vmstg])
        P.op("pool", lambda e: e.tensor_copy(VM[:], vmstg[:]), reads=[t_vmstg], writes=[t_V])

        for g in [-1] + list(range(NG)):
            if g < 0:
                q0, QW = 0, N_META
                blocks = [("m", 0)]
            else:
                q0, QW = N_META + 512 * g, 512
                blocks = [("x", j) for j in range(4 * g + 3, -1, -1)] + [("m", 0)]
            ob = gcount % 2
            gcount += 1
            P.op("pe", lambda e, ob=ob, QW=QW, q0=q0:
                 e.matmul(Ops[ob][0:64, 0:QW], zeros[0:64, 0:64], QT[:, q0:q0 + QW], start=True, stop=False),
                 reads=[t_zeros, t_Q], writes=[t_O[ob]])
            P.op("pool", lambda e: e.memset(acc[:], 0.0), writes=[t_acc])
            nblk = len(blocks)
            info = []
            for it, (kind, j) in enumerate(blocks):
                if kind == "m":
                    kp, k0 = N_META, 0
                    c0 = 0
                    diag = (g < 0)
                else:
                    kp, k0 = 128, N_META + 128 * j
                    jl = j - 4 * g
                    diag = jl >= 0
                    c0 = 128 * jl if diag else 0
                info.append((kind, j, kp, k0, c0, diag))

            def stage1(it, info=info, q0=q0, QW=QW, ob=ob, nblk=nblk):
                kind, j, kp, k0, c0, diag = info[it]
                b = it % 2
                W = QW - c0
                qs = slice(q0 + c0, q0 + QW)
                dw = min(128, W)
                P.op("pe", lambda e: e.matmul(zA[b][0:kp, 0:W], KT[:, k0:k0 + kp], QT[:, qs], start=True, stop=True),
                     reads=[t_K, t_Q], writes=[t_zA[b]])
                P.op("act", lambda e: e.activation(e_sb[b][0:kp, 0:W], zA[b][0:kp, 0:W], AF.Exp),
                     reads=[t_zA[b]], writes=[t_e[b]])
                P.op("act", lambda e: e.activation(sp_sb[b][0:kp, 0:W], e_sb[b][0:kp, 0:W], AF.Ln, bias=1.0),
                     reads=[t_e[b]], writes=[t_sp[b]])
                if diag:
                    P.op("pool", lambda e: e.tensor_tensor(sp_sb[b][0:kp, 0:dw], sp_sb[b][0:kp, 0:dw],
                                                           cm[0:kp, 0, 0:dw], ALU.mult),
                         reads=[t_sp[b], t_cm], writes=[t_sp[b]])
                last = (it == 0)
                P.op("pe", lambda e: e.matmul(zB[b][0:kp, 0:W], KT[:, k0:k0 + kp], QT[:, qs], start=True, stop=False),
                     reads=[t_K, t_Q], writes=[t_zB[b]])
                P.op("pe", lambda e: e.matmul(zB[b][0:kp, 0:W], cm[0:kp, 1, 0:kp], sp_sb[b][0:kp, 0:W],
                                              start=False, stop=last),
                     reads=[t_cm, t_sp[b]], writes=[t_zB[b]])
                if not last:
                    ab = (it - 1) % 2
                    P.op("pe", lambda e: e.matmul(zB[b][0:kp, 0:W], cm[0:128, 2, 0:kp], accb[ab][0:128, c0:QW],
                                                  start=False, stop=True),
                         reads=[t_cm, t_accb[ab]], writes=[t_zB[b]])
                if it < nblk - 1:
                    P.op("dve", lambda e: e.tensor_tensor(acc[0:kp, c0:QW], acc[0:kp, c0:QW], sp_sb[b][0:kp, 0:W], ALU.add),
                         reads=[t_sp[b], t_acc], writes=[t_acc])
                    P.op("dve", lambda e: e.tensor_copy(accb[b][:, 0:QW], acc[:, 0:QW]),
                         reads=[t_acc], writes=[t_accb[b]])

            def stage2(it, info=info, q0=q0, QW=QW, ob=ob, nblk=nblk):
                kind, j, kp, k0, c0, diag = info[it]
                b = it % 2
                W = QW - c0
                dw = min(128, W)
                P.op("act", lambda e: e.activation(A_sb[b][0:kp, 0:W], zB[b][0:kp, 0:W], AF.Exp),
                     reads=[t_zB[b]], writes=[t_A[b]])
                if diag:
                    P.op("pool", lambda e: e.tensor_tensor(A_sb[b][0:kp, 0:dw], A_sb[b][0:kp, 0:dw],
                                                           cm[0:kp, 0, 0:dw], ALU.mult),
                         reads=[t_A[b], t_cm], writes=[t_A[b]])
                vop = (VM[0:kp, :] if kind == "m" else V[:, j, :])
                P.op("pe", lambda e: e.matmul(Ops[ob][0:64, c0:QW], vop, A_sb[b][0:kp, 0:W],
                                              start=False, stop=(it == nblk - 1)),
                     reads=[t_V, t_A[b]], writes=[t_O[ob]])

            for step in range(nblk + 1):
                if step < nblk:
                    stage1(step)
                if step >= 1:
                    stage2(step - 1)
            P.op("dve", lambda e, ob=ob, QW=QW: e.tensor_copy(ost[ob][:, 0:QW], Ops[ob][0:64, 0:QW]),
                 reads=[t_O[ob]], writes=[t_ost[ob]])
            P.dma("sp", oT[h, :, q0:q0 + QW], ost[ob][:, 0:QW], reads=[t_ost[ob]])


def attn_masks():
    s = np.arange(128)[:, None]
    t = np.arange(128)[None, :]
    m = np.zeros((128, 3, 128), np.float32)
    m[:, 0, :] = (s < t)
    m[:, 1, :] = -1.0 * (s >= t)
    m[:, 2, :] = -1.0
    return m


DECAY_SCALE = float(np.exp(-0.5))
LNX_EPS = 64e-5


def rwkv_consts():
    idx = np.arange(128)
    ch = idx // 64
    same = ch[:, None] == ch[None, :]
    le = idx[:, None] <= idx[None, :]
    lt = idx[:, None] < idx[None, :]
    c = np.zeros((128, 9, 128), np.float32)
    c[:, 0] = -DECAY_SCALE * (same & le)
    c[:, 1] = -DECAY_SCALE * same
    c[:, 2] = same & lt
    c[:, 3] = same & le
    c[:, 4] = same & lt
    c[:, 5] = same & le
    c[:, 6] = (same & lt).T
    c[:, 7] = np.eye(128)
    c[:, 8, 0] = -DECAY_SCALE * (ch == 0)
    c[:, 8, 1] = -DECAY_SCALE * (ch == 1)
    c[:, 8, 2] = (ch == 0)
    c[:, 8, 3] = (ch == 1)
    return c


def build_rwkv(nc, NTX):
    L = N_META + 128 * NTX
    D = {}
    D["rkv"] = nc.dram_tensor("rkv", [L + 1, 384], F32, kind="ExternalInput").ap()
    D["lo"] = nc.dram_tensor("lo", [256, L + 1], F32, kind="ExternalInput").ap()
    D["mu_tm"] = nc.dram_tensor("mu_tm", [128, 384], F32, kind="ExternalInput").ap()
    D["mu_fm"] = nc.dram_tensor("mu_fm", [128, 3], F32, kind="ExternalInput").ap()
    D["bc"] = nc.dram_tensor("bc", [128, 7, 128], F32, kind="ExternalInput").ap()
    D["wa_up"] = nc.dram_tensor("wa_up", [128, 128], F32, kind="ExternalInput").ap()
    D["g_up"] = nc.dram_tensor("g_up", [128, 128], F32, kind="ExternalInput").ap()
    D["cst"] = nc.dram_tensor("cst", [128, 9, 128], F32, kind="ExternalInput").ap()
    D["o"] = nc.dram_tensor("o_rw", [L, 128], F32, kind="ExternalOutput").ap()
    with ExitStack() as es:
        P = Prog(nc, es)
        emit_rwkv(P, NTX, D)
        P.finish()
    return nc


def emit_rwkv(P, NTX, D, pfx="rw"):
    DS = DECAY_SCALE
    NCH = 1 + 2 * NTX

    def const_load(name, shape, src):
        t = P.sb(pfx + name, shape, F32)
        tt = P.T(const=True)
        P.dma("sp", t[:], src, writes=[tt])
        return t, tt

    cst, t_cst = const_load("cst", [128, 9, 128], D["cst"])
    bc, t_bc = const_load("bc", [128, 7, 128], D["bc"])
    mu_tm, t_mutm = const_load("mu_tm", [128, 384], D["mu_tm"])
    mu_fm, t_mufm = const_load("mu_fm", [128, 3], D["mu_fm"])
    wup, t_wup = const_load("wup", [64, 128], D["wa_up"][0:64, :])
    aup, t_aup = const_load("aup", [64, 128], D["wa_up"][64:128, :])
    gup, t_gup = const_load("gup", [128, 128], D["g_up"])

    S_all = [P.sb(pfx + f"S_all{h}", [64, NCH + 1, 64], F32) for h in range(2)]
    t_S = [[P.T() for _ in range(NCH + 1)] for h in range(2)]
    for h in range(2):
        P.op("pool", lambda e, h=h: e.memset(S_all[h][:, 0, :], 0.0), writes=[t_S[h][0]])

    pb = [P.ps(pfx + f"pb{i}", [128, 4, 128]) for i in range(8)]
    t_bank = [P.T("bank%d" % i, excl=True) for i in range(8)]

    names_sb = {
        "xa": [128, 384], "xs": [128, 384], "x": [128, 384],
        "la": [64, 128], "ls": [64, 128], "la2": [64, 128], "ls2": [64, 128], "ga": [128, 128], "gs": [128, 128],
        "lx": [64, 128], "lx2": [64, 128], "gx": [128, 128], "tw": [64, 128], "sg": [128, 128],
        "sigw": [128, 128], "a": [128, 128], "g": [128, 128],
        "kk": [128, 128], "sq": [128, 128], "ss": [128, 2], "rn": [128, 2],
        "kp": [128, 128], "t1": [128, 128], "bs": [128, 2],
        "cum": [128, 128], "c1": [128, 128], "c2": [128, 128],
        "ep": [128, 128], "em": [128, 128], "epm": [128, 128], "ebar": [128, 128], "ebz": [128, 2, 128],
        "PC": [64, 2, 2],
        "TM": [128, 4, 128],
        "kka": [128, 128], "bbz": [128, 2, 128], "kbz": [128, 2, 128],
        "FM0": [64, 4, 128], "FM1": [64, 4, 128],
        "G0": [128, 4, 128], "G1": [128, 4, 128],
        "NP0": [128, 6, 2, 128], "NP1": [128, 6, 2, 128],
        "X0a": [128, 128], "X0b": [128, 128], "X1a": [128, 128], "X1b": [128, 128],
        "M": [64, 4, 64], "RHz0": [64, 2, 128], "RHz1": [64, 2, 128],
        "y": [128, 128], "ysq": [128, 128], "st": [128, 8], "yn": [128, 128], "o": [128, 128],
    }
    bufs = []
    for p in range(2):
        d = {}
        for nm, shp in names_sb.items():
            d[nm] = (P.sb(f"{pfx}{nm}_{p}", shp, F32), P.T(f"{nm}{p}"))
        bufs.append(d)
        for nm in ("RHz0", "RHz1"):
            t, tt = d[nm]
            P.op("pool", lambda e, t=t: e.memset(t[:], 0.0), writes=[tt])

    rkv, lo, o_d = D["rkv"], D["lo"], D["o"]
    B_LORA, B_CUM, B_FM0, B_G0, B_G1, B_FM1, B_PM, B_Y = range(8)
    B_NP = [B_LORA, B_CUM]
    B_XW = [B_FM0, B_FM1]
    B_CH = B_G0
    B_RH = B_G1

    def tile_body(ti):
        n = 16 if ti == 0 else 128
        t0 = 0 if ti == 0 else N_META + 128 * (ti - 1)
        nch = 1 if ti == 0 else 2
        cb = 0 if ti == 0 else 1 + 2 * (ti - 1)
        B = bufs[ti % 2]

        def b(nm):
            return B[nm]

        xa, t_xa = b("xa"); xs, t_xs = b("xs"); x, t_x = b("x")
        la, t_la = b("la"); ls, t_ls = b("ls"); la2, t_la2 = b("la2"); ls2, t_ls2 = b("ls2")
        ga, t_ga = b("ga"); gs, t_gs = b("gs")
        lx, t_lx = b("lx"); lx2, t_lx2 = b("lx2"); gx, t_gx = b("gx"); tw, t_tw = b("tw"); sg, t_sg = b("sg")
        sigw, t_sigw = b("sigw"); a_, t_a = b("a"); g_, t_g = b("g")
        kk, t_kk = b("kk"); sq, t_sq = b("sq"); ss, t_ss = b("ss"); rn, t_rn = b("rn")
        kp, t_kp = b("kp"); t1, t_t1 = b("t1"); bs, t_bs = b("bs")
        cum, t_cum = b("cum"); c1, t_c1 = b("c1"); c2, t_c2 = b("c2")
        ep, t_ep = b("ep"); em, t_em = b("em"); epm, t_epm = b("epm"); ebar, t_ebar = b("ebar")
        ebz, t_ebz = b("ebz")
        PC, t_PC = b("PC"); TM, t_TM = b("TM"); kka, t_kka = b("kka")
        bbz, t_bbz = b("bbz"); kbz, t_kbz = b("kbz")
        FMh = [b("FM0"), b("FM1")]
        M_, t_M = b("M"); RHz = [b("RHz0"), b("RHz1")]
        y_, t_y = b("y"); ysq, t_ysq = b("ysq"); st, t_st = b("st"); yn, t_yn = b("yn"); o_, t_o = b("o")

        P.dma("sp", xa[0:n, :], rkv[1 + t0:1 + t0 + n, :], writes=[t_xa])
        P.dma("sp", xs[0:n, :], rkv[t0:t0 + n, :], writes=[t_xs])
        P.dma("sp", la[:, 0:n], lo[0:64, 1 + t0:1 + t0 + n], writes=[t_la])
        P.dma("sp", ls[:, 0:n], lo[0:64, t0:t0 + n], writes=[t_ls])
        P.dma("sp", la2[:, 0:n], lo[64:128, 1 + t0:1 + t0 + n], writes=[t_la2])
        P.dma("sp", ls2[:, 0:n], lo[64:128, t0:t0 + n], writes=[t_ls2])
        P.dma("sp", ga[:, 0:n], lo[128:256, 1 + t0:1 + t0 + n], writes=[t_ga])
        P.dma("sp", gs[:, 0:n], lo[128:256, t0:t0 + n], writes=[t_gs])
        P.op("pool", lambda e: e.tensor_tensor(xs[0:n, :], xs[0:n, :], xa[0:n, :], ALU.subtract),
             reads=[t_xa, t_xs], writes=[t_xs])
        P.op("pool", lambda e: e.tensor_tensor(xs[0:n, :], xs[0:n, :], mu_tm[0:n, :], ALU.mult),
             reads=[t_xs, t_mutm], writes=[t_xs])
        P.op("pool", lambda e: e.tensor_tensor(x[0:n, :], xs[0:n, :], xa[0:n, :], ALU.add),
             reads=[t_xa, t_xs], writes=[t_x])
        for (A_, tA, S_, tS, O_, tO, col, np_) in ((la, t_la, ls, t_ls, lx, t_lx, 0, 64), (la2, t_la2, ls2, t_ls2, lx2, t_lx2, 1, 64),
                                                  (ga, t_ga, gs, t_gs, gx, t_gx, 2, 128)):
            P.op("pool", lambda e, A_=A_, S_=S_: e.tensor_tensor(S_[:, 0:n], S_[:, 0:n], A_[:, 0:n], ALU.subtract),
                 reads=[tA, tS], writes=[tS])
            P.op("dve", lambda e, A_=A_, S_=S_, O_=O_, col=col, np_=np_: e.scalar_tensor_tensor(O_[:, 0:n], S_[:, 0:n], mu_fm[0:np_, col:col + 1], A_[:, 0:n], ALU.mult, ALU.add),
                 reads=[tA, tS, t_mufm], writes=[tO])
        xr = x[0:n, 0:128]
        xk = x[0:n, 128:256]
        P.op("act", lambda e: e.activation(tw[:, 0:n], lx[:, 0:n], AF.Exp, scale=-2.0), reads=[t_lx], writes=[t_tw])
        P.op("dve", lambda e: e.tensor_scalar(tw[:, 0:n], tw[:, 0:n], 1.0, None, ALU.add), reads=[t_tw], writes=[t_tw])
        P.op("dve", lambda e: e.reciprocal(tw[:, 0:n], tw[:, 0:n]), reads=[t_tw], writes=[t_tw])
        P.op("dve", lambda e: e.tensor_scalar(tw[:, 0:n], tw[:, 0:n], 2.0, -1.0, ALU.mult, ALU.add), reads=[t_tw], writes=[t_tw])
        P.op("act", lambda e: e.activation(sg[:, 0:n], gx[:, 0:n], AF.Exp, scale=-1.0), reads=[t_gx], writes=[t_sg])
        P.op("dve", lambda e: e.tensor_scalar(sg[:, 0:n], sg[:, 0:n], 1.0, None, ALU.add), reads=[t_sg], writes=[t_sg])
        P.op("dve", lambda e: e.reciprocal(sg[:, 0:n], sg[:, 0:n]), reads=[t_sg], writes=[t_sg])
        tb = t_bank[B_LORA]
        P.op("pe", lambda e: e.matmul(pb[B_LORA][0:n, 0, :], tw[:, 0:n], wup[:, :], start=True, stop=True),
             reads=[t_tw, t_wup], writes=[tb])
        P.op("pe", lambda e: e.matmul(pb[B_LORA][0:n, 1, :], lx2[:, 0:n], aup[:, :], start=True, stop=True),
             reads=[t_lx2, t_aup], writes=[tb])
        P.op("pe", lambda e: e.matmul(pb[B_LORA][0:n, 2, :], sg[:, 0:n], gup[:, :], start=True, stop=True),
             reads=[t_sg, t_gup], writes=[tb])
        P.op("dve", lambda e: e.tensor_tensor(sigw[0:n, :], pb[B_LORA][0:n, 0, :], bc[0:n, 0, :], ALU.add),
             reads=[tb, t_bc], writes=[t_sigw])
        P.op("dve", lambda e: e.tensor_tensor(a_[0:n, :], pb[B_LORA][0:n, 1, :], bc[0:n, 1, :], ALU.add),
             reads=[tb, t_bc], writes=[t_a])
        P.op("act", lambda e: e.activation(g_[0:n, :], pb[B_LORA][0:n, 2, :], AF.Identity), reads=[tb], writes=[t_g])
        for (Z, tZ) in ((sigw, t_sigw), (a_, t_a)):
            P.op("act", lambda e, Z=Z: e.activation(Z[0:n, :], Z[0:n, :], AF.Exp, scale=-1.0), reads=[tZ], writes=[tZ])
            P.op("dve", lambda e, Z=Z: e.tensor_scalar(Z[0:n, :], Z[0:n, :], 1.0, None, ALU.add), reads=[tZ], writes=[tZ])
            P.op("dve", lambda e, Z=Z: e.reciprocal(Z[0:n, :], Z[0:n, :]), reads=[tZ], writes=[tZ])
        P.op("pool", lambda e: e.tensor_tensor(kk[0:n, :], xk, bc[0:n, 2, :], ALU.mult), reads=[t_x, t_bc], writes=[t_kk])
        P.op("pool", lambda e: e.tensor_tensor(sq[0:n, :], kk[0:n, :], kk[0:n, :], ALU.mult), reads=[t_kk], writes=[t_sq])
        for h in range(2):
            P.op("dve", lambda e, h=h: e.reduce_sum(ss[0:n, h:h + 1], sq[0:n, 64 * h:64 * h + 64], axis=AX.X),
                 reads=[t_sq], writes=[t_ss])
        P.op("dve", lambda e: e.tensor_scalar(rn[0:n, :], ss[0:n, :], 1e-24, None, ALU.max), reads=[t_ss], writes=[t_rn])
        P.op("act", lambda e: e.activation(rn[0:n, :], rn[0:n, :], AF.Ln), reads=[t_rn], writes=[t_rn])
        P.op("act", lambda e: e.activation(rn[0:n, :], rn[0:n, :], AF.Exp, scale=-0.5), reads=[t_rn], writes=[t_rn])
        for h in range(2):
            P.op("dve", lambda e, h=h: e.tensor_scalar(kk[0:n, 64 * h:64 * h + 64], kk[0:n, 64 * h:64 * h + 64],
                                                       rn[0:n, h:h + 1], None, ALU.mult),
                 reads=[t_kk, t_rn], writes=[t_kk])
        P.op("dve", lambda e: e.scalar_tensor_tensor(t1[0:n, :], a_[0:n, :], -1.0, bc[0:n, 3, :], ALU.add, ALU.mult),
             reads=[t_a, t_bc], writes=[t_t1])
        P.op("dve", lambda e: e.scalar_tensor_tensor(kp[0:n, :], t1[0:n, :], 1.0, xk, ALU.add, ALU.mult),
             reads=[t_t1, t_x], writes=[t_kp])
        P.op("pool", lambda e: e.tensor_tensor(t1[0:n, :], xr, kp[0:n, :], ALU.mult), reads=[t_x, t_kp, t_t1], writes=[t_t1])
        P.op("pool", lambda e: e.tensor_tensor(t1[0:n, :], t1[0:n, :], bc[0:n, 4, :], ALU.mult), reads=[t_t1, t_bc], writes=[t_t1])
        for h in range(2):
            P.op("dve", lambda e, h=h: e.reduce_sum(bs[0:n, h:h + 1], t1[0:n, 64 * h:64 * h + 64], axis=AX.X),
                 reads=[t_t1], writes=[t_bs])
        tb = t_bank[B_CUM]
        P.op("pe", lambda e: e.matmul(pb[B_CUM][0:n, 0, :], cst[0:n, 0, 0:n], sigw[0:n, :], start=True, stop=True),
             reads=[t_cst, t_sigw], writes=[tb])
        P.op("pe", lambda e: e.matmul(pb[B_CUM][0:n, 1, :], cst[0:n, 1, 0:n], sigw[0:n, :], start=True, stop=True),
             reads=[t_cst, t_sigw], writes=[tb])
        for h in range(2):
            P.op("pe", lambda e, h=h: e.matmul(pb[B_CUM][0:64, 2 + h, 0:2], sigw[0:n, 64 * h:64 * h + 64], cst[0:n, 8, 0:2], start=True, stop=True),
                 reads=[t_cst, t_sigw], writes=[tb])
        P.op("dve", lambda e: e.tensor_copy(cum[0:n, :], pb[B_CUM][0:n, 0, :]), reads=[tb], writes=[t_cum])
        P.op("dve", lambda e: e.scalar_tensor_tensor(c1[0:n, :], sigw[0:n, :], DS, cum[0:n, :], ALU.mult, ALU.add),
             reads=[t_sigw, t_cum], writes=[t_c1])
        P.op("dve", lambda e: e.tensor_tensor(c2[0:n, :], pb[B_CUM][0:n, 1, :], cum[0:n, :], ALU.subtract),
             reads=[tb, t_cum], writes=[t_c2])
        P.op("act", lambda e: e.activation(ep[0:n, :], cum[0:n, :], AF.Exp), reads=[t_cum], writes=[t_ep])
        P.op("act", lambda e: e.activation(em[0:n, :], cum[0:n, :], AF.Exp, scale=-1.0), reads=[t_cum], writes=[t_em])
        P.op("act", lambda e: e.activation(epm[0:n, :], c1[0:n, :], AF.Exp), reads=[t_c1], writes=[t_epm])
        P.op("act", lambda e: e.activation(ebar[0:n, :], c2[0:n, :], AF.Exp), reads=[t_c2], writes=[t_ebar])
        P.op("act", lambda e: e.activation(PC[:, :, :], pb[B_CUM][0:64, 2:4, 0:2], AF.Exp), reads=[tb], writes=[t_PC])
        P.op("dve", lambda e: e.tensor_tensor(kka[0:n, :], kk[0:n, :], a_[0:n, :], ALU.mult), reads=[t_kk, t_a], writes=[t_kka])
        P.op("dve", lambda e: e.tensor_tensor(TM[0:n, 0, :], kka[0:n, :], em[0:n, :], ALU.mult), reads=[t_kka, t_em], writes=[t_TM])
        P.op("dve", lambda e: e.tensor_tensor(TM[0:n, 1, :], kp[0:n, :], em[0:n, :], ALU.mult), reads=[t_kp, t_em], writes=[t_TM])
        P.op("dve", lambda e: e.scalar_tensor_tensor(TM[0:n, 2, :], kk[0:n, :], -1.0, epm[0:n, :], ALU.mult, ALU.mult),
             reads=[t_kk, t_epm], writes=[t_TM])
        P.op("dve", lambda e: e.tensor_tensor(TM[0:n, 3, :], xr, ep[0:n, :], ALU.mult), reads=[t_x, t_ep], writes=[t_TM])
        for c in range(nch):
            P.op("pool", lambda e, c=c: e.tensor_scalar(ebz[0:n, c, :], ebar[0:n, :], cst[0:n, 8, 2 + c:3 + c], None, ALU.mult),
                 reads=[t_ebar, t_cst], writes=[t_ebz])
            P.op("pool", lambda e, c=c: e.tensor_tensor(bbz[0:n, c, :], kka[0:n, :], ebz[0:n, c, :], ALU.mult),
                 reads=[t_kka, t_ebz], writes=[t_bbz])
            P.op("pool", lambda e, c=c: e.tensor_tensor(kbz[0:n, c, :], kp[0:n, :], ebz[0:n, c, :], ALU.mult),
                 reads=[t_kp, t_ebz], writes=[t_kbz])
        for h in range(2):
            FM, t_FM = FMh[h]
            bk = B_XW[h]
            for s_ in range(4):
                P.op("pe", lambda e, s_=s_, h=h, bk=bk: e.transpose(pb[bk][0:64, s_, 0:n], TM[0:n, s_, 64 * h:64 * h + 64], cst[0:n, 7, 0:n]),
                     reads=[t_TM, t_cst], writes=[t_bank[bk]])
            P.op("act", lambda e, FM=FM, bk=bk: e.activation(FM[:, :, 0:n], pb[bk][0:64, :, 0:n], AF.Identity),
                 reads=[t_bank[bk]], writes=[t_FM])

        G = [b("G0"), b("G1")]
        NP = [b("NP0"), b("NP1")]
        Xb = [[b("X0a"), b("X0b")], [b("X1a"), b("X1b")]]
        for h in range(2):
            FM, t_FM = FMh[h]
            Gh, t_G = G[h]
            NPh, t_NP = NP[h]
            gbk = [B_G0, B_G1][h]
            gb = pb[gbk]
            for (so, sl, sr) in ((0, 0, 2), (1, 0, 3), (2, 1, 2), (3, 1, 3)):
                P.op("pe", lambda e, gb=gb, so=so, sl=sl, sr=sr, FM=FM: e.matmul(gb[0:n, so, 0:n], FM[:, sl, 0:n], FM[:, sr, 0:n], start=True, stop=True),
                     reads=[t_FM], writes=[t_bank[gbk]])
            nbk = B_NP[h]
            P.op("pe", lambda e, nbk=nbk, FM=FM: e.matmul(pb[nbk][0:n, 1, 0:n], FM[:, 2, 0:n], FM[:, 0, 0:n], start=True, stop=True),
                 reads=[t_FM], writes=[t_bank[nbk]])
            P.op("dve", lambda e, gb=gb, Gh=Gh: e.tensor_tensor(Gh[0:n, :, 0:n], gb[0:n, :, 0:n], cst[0:n, 2:6, 0:n], ALU.mult),
                 reads=[t_bank[gbk], t_cst], writes=[t_G])
            P.op("dve", lambda e, nbk=nbk, NPh=NPh: e.tensor_tensor(NPh[0:n, 0, 1, 0:n], pb[nbk][0:n, 1, 0:n], cst[0:n, 6, 0:n], ALU.mult),
                 reads=[t_bank[nbk], t_cst], writes=[t_NP])
            P.op("pool", lambda e, Gh=Gh, NPh=NPh: e.tensor_copy(NPh[0:n, 0, 0, 0:n], Gh[0:n, 0, 0:n]),
                 reads=[t_G], writes=[t_NP])
        for h in range(2):
            hs = slice(64 * h, 64 * h + 64)
            vs = slice(256 + 64 * h, 256 + 64 * h + 64)
            Gh, t_G = G[h]
            xbk = B_XW[h]
            P.op("pe", lambda e, hs=hs, xbk=xbk: e.matmul(pb[xbk][0:n, 0, 0:64], cst[0:n, 7, 0:n], TM[0:n, 2, hs], start=True, stop=True),
                 reads=[t_TM, t_cst], writes=[t_bank[xbk]])
            P.op("pe", lambda e, vs=vs, xbk=xbk, Gh=Gh: e.matmul(pb[xbk][0:n, 0, 64:128], Gh[0:n, 2, 0:n], x[0:n, vs], start=True, stop=True),
                 reads=[t_G, t_x], writes=[t_bank[xbk]])
            X0, t_X0 = Xb[h][0]
            P.op("act", lambda e, xbk=xbk, X0=X0: e.activation(X0[0:n, :], pb[xbk][0:n, 0, :], AF.Identity),
                 reads=[t_bank[xbk]], writes=[t_X0])
        NPW = 6
        cur = [0, 0]
        for i in range(NPW):
            for h in range(2):
                NPh, t_NP = NP[h]
                nbk = B_NP[h]
                xbk = B_XW[h]
                Xc, t_Xc = Xb[h][cur[h]]
                Xn, t_Xn = Xb[h][1 - cur[h]]
                P.op("pe", lambda e, i=i, NPh=NPh, Xc=Xc, xbk=xbk: e.matmul(pb[xbk][0:n, 0, :], NPh[0:n, i, 0, 0:n], Xc[0:n, :], start=True, stop=True),
                     reads=[t_NP, t_Xc], writes=[t_bank[xbk]])
                P.op("dve", lambda e, Xc=Xc, Xn=Xn, xbk=xbk: e.tensor_tensor(Xn[0:n, :], pb[xbk][0:n, 0, :], Xc[0:n, :], ALU.add),
                     reads=[t_bank[xbk], t_Xc], writes=[t_Xn])
                cur[h] = 1 - cur[h]
                if i < NPW - 1:
                    P.op("pe", lambda e, i=i, NPh=NPh, nbk=nbk: e.matmul(pb[nbk][0:n, 0, 0:n], NPh[0:n, i, 1, 0:n], NPh[0:n, i, 0, 0:n], start=True, stop=True),
                         reads=[t_NP], writes=[t_bank[nbk]])
                    P.op("pe", lambda e, i=i, NPh=NPh, nbk=nbk: e.matmul(pb[nbk][0:n, 1, 0:n], NPh[0:n, i, 0, 0:n], NPh[0:n, i, 1, 0:n], start=True, stop=True),
                         reads=[t_NP], writes=[t_bank[nbk]])
                    P.op("act", lambda e, i=i, NPh=NPh, nbk=nbk: e.activation(NPh[0:n, i + 1, :, 0:n], pb[nbk][0:n, 0:2, 0:n], AF.Identity),
                         reads=[t_bank[nbk]], writes=[t_NP])
        Xf = [Xb[h][cur[h]] for h in range(2)]
        tb = t_bank[B_PM]
        for h in range(2):
            hs = slice(64 * h, 64 * h + 64)
            Xh, t_Xh = Xf[h]
            for c in range(nch):
                P.op("pe", lambda e, hs=hs, c=c, h=h, Xh=Xh: e.matmul(pb[B_PM][0:64, 2 * h + c, 0:64], Xh[0:n, 0:64], bbz[0:n, c, hs], start=True, stop=True),
                     reads=[t_Xh, t_bbz], writes=[tb])
        for h in range(2):
            for c in range(nch):
                P.op("dve", lambda e, h=h, c=c: e.scalar_tensor_tensor(M_[:, 2 * h + c, :], cst[0:64, 7, 0:64], PC[:, h, c:c + 1], pb[B_PM][0:64, 2 * h + c, 0:64], ALU.mult, ALU.add),
                     reads=[tb, t_PC, t_cst], writes=[t_M])
        tb = t_bank[B_CH]
        for c in range(nch):
            ci = cb + c
            for h in range(2):
                hs = slice(64 * h, 64 * h + 64)
                vs = slice(256 + 64 * h, 256 + 64 * h + 64)
                Xh, t_Xh = Xf[h]
                P.op("pe", lambda e, h=h, c=c, ci=ci: e.matmul(pb[B_CH][0:64, h, 0:64], M_[:, 2 * h + c, :], S_all[h][:, ci, :], start=True, stop=False),
                     reads=[t_M, t_S[h][ci]], writes=[tb])
                P.op("pe", lambda e, h=h, hs=hs, c=c, Xh=Xh: e.matmul(pb[B_CH][0:64, h, 0:64], bbz[0:n, c, hs], Xh[0:n, 64:128], start=False, stop=False),
                     reads=[t_bbz, t_Xh], writes=[tb])
                P.op("pe", lambda e, h=h, hs=hs, vs=vs, c=c: e.matmul(pb[B_CH][0:64, h, 0:64], kbz[0:n, c, hs], x[0:n, vs], start=False, stop=True),
                     reads=[t_kbz, t_x], writes=[tb])
                P.op("act", lambda e, h=h, ci=ci: e.activation(S_all[h][:, ci + 1, :], pb[B_CH][0:64, h, 0:64], AF.Identity),
                     reads=[tb], writes=[t_S[h][ci + 1]])
        tb = t_bank[B_RH]
        for h in range(2):
            Xh, t_Xh = Xf[h]
            Gh, t_G = G[h]
            FM, t_FM = FMh[h]
            Rz, t_Rz = RHz[h]
            P.op("pe", lambda e, h=h, Xh=Xh, Gh=Gh: e.matmul(pb[B_RH][0:64, h, 0:n], Xh[0:n, 0:64], Gh[0:n, 1, 0:n], start=True, stop=True),
                 reads=[t_Xh, t_G], writes=[tb])
            for c in range(nch):
                cs = slice(64 * c, min(64 * c + 64, n))
                P.op("dve", lambda e, h=h, c=c, cs=cs, Rz=Rz, FM=FM: e.tensor_tensor(Rz[:, c, cs], pb[B_RH][0:64, h, cs], FM[:, 3, cs], ALU.add),
                     reads=[tb, t_FM], writes=[t_Rz])
        tb = t_bank[B_Y]
        for h in range(2):
            hs = slice(64 * h, 64 * h + 64)
            vs = slice(256 + 64 * h, 256 + 64 * h + 64)
            Xh, t_Xh = Xf[h]
            Gh, t_G = G[h]
            Rz, t_Rz = RHz[h]
            P.op("pe", lambda e, hs=hs, Xh=Xh, Gh=Gh: e.matmul(pb[B_Y][0:n, 0, hs], Gh[0:n, 1, 0:n], Xh[0:n, 64:128], start=True, stop=False),
                 reads=[t_G, t_Xh], writes=[tb])
            P.op("pe", lambda e, hs=hs, vs=vs, Gh=Gh: e.matmul(pb[B_Y][0:n, 0, hs], Gh[0:n, 3, 0:n], x[0:n, vs], start=False, stop=False),
                 reads=[t_G, t_x], writes=[tb])
            for c in range(nch):
                ci = cb + c
                P.op("pe", lambda e, h=h, hs=hs, ci=ci, c=c, Rz=Rz: e.matmul(pb[B_Y][0:n, 0, hs], Rz[:, c, 0:n], S_all[h][:, ci, :], start=False, stop=(c == nch - 1)),
                     reads=[t_Rz, t_S[h][ci]], writes=[tb])
        P.op("act", lambda e: e.activation(y_[0:n, :], pb[B_Y][0:n, 0, :], AF.Identity), reads=[tb], writes=[t_y])
        P.op("pool", lambda e: e.tensor_tensor(ysq[0:n, :], y_[0:n, :], y_[0:n, :], ALU.mult), reads=[t_y], writes=[t_ysq])
        for h in range(2):
            hs = slice(64 * h, 64 * h + 64)
            P.op("dve", lambda e, h=h, hs=hs: e.reduce_sum(st[0:n, h:h + 1], y_[0:n, hs], axis=AX.X), reads=[t_y], writes=[t_st])
            P.op("dve", lambda e, h=h, hs=hs: e.reduce_sum(st[0:n, 2 + h:3 + h], ysq[0:n, hs], axis=AX.X), reads=[t_ysq], writes=[t_st])
        P.op("dve", lambda e: e.tensor_scalar(st[0:n, 4:6], st[0:n, 0:2], 1.0 / 64, None, ALU.mult), reads=[t_st], writes=[t_st])
        P.op("dve", lambda e: e.tensor_tensor(st[0:n, 6:8], st[0:n, 4:6], st[0:n, 4:6], ALU.mult), reads=[t_st], writes=[t_st])
        P.op("dve", lambda e: e.scalar_tensor_tensor(st[0:n, 6:8], st[0:n, 2:4], 1.0 / 64, st[0:n, 6:8], ALU.mult, ALU.subtract), reads=[t_st], writes=[t_st])
        P.op("dve", lambda e: e.tensor_scalar(st[0:n, 6:8], st[0:n, 6:8], LNX_EPS, None, ALU.add), reads=[t_st], writes=[t_st])
        P.op("act", lambda e: e.activation(st[0:n, 6:8], st[0:n, 6:8], AF.Ln), reads=[t_st], writes=[t_st])
        P.op("act", lambda e: e.activation(st[0:n, 6:8], st[0:n, 6:8], AF.Exp, scale=-0.5), reads=[t_st], writes=[t_st])
        for h in range(2):
            hs = slice(64 * h, 64 * h + 64)
            P.op("dve", lambda e, h=h, hs=hs: e.tensor_scalar(yn[0:n, hs], y_[0:n, hs], st[0:n, 4 + h:5 + h], st[0:n, 6 + h:7 + h], ALU.subtract, ALU.mult),
                 reads=[t_y, t_st], writes=[t_yn])
        P.op("pool", lambda e: e.tensor_tensor(yn[0:n, :], yn[0:n, :], bc[0:n, 5, :], ALU.mult), reads=[t_yn, t_bc], writes=[t_yn])
        P.op("pool", lambda e: e.tensor_tensor(yn[0:n, :], yn[0:n, :], bc[0:n, 6, :], ALU.add), reads=[t_yn, t_bc], writes=[t_yn])
        for h in range(2):
            hs = slice(64 * h, 64 * h + 64)
            vs = slice(256 + 64 * h, 256 + 64 * h + 64)
            P.op("dve", lambda e, h=h, hs=hs, vs=vs: e.scalar_tensor_tensor(yn[0:n, hs], x[0:n, vs], bs[0:n, h:h + 1], yn[0:n, hs], ALU.mult, ALU.add),
                 reads=[t_x, t_bs, t_yn], writes=[t_yn])
        P.op("dve", lambda e: e.tensor_tensor(o_[0:n, :], yn[0:n, :], g_[0:n, :], ALU.mult), reads=[t_yn, t_g], writes=[t_o])
        P.dma("sp", o_d[t0:t0 + n, :], o_[0:n, :], reads=[t_o])

    for ti in range(NTX + 1):
        tile_body(ti)


def rwkv_host_inputs(p_rw, hp, prm):
    L = p_rw.shape[0]
    cs = slice(128 * hp, 128 * hp + 128)
    rkv = np.zeros((L + 1, 384), np.float32)
    rkv[1:, 0:128] = p_rw[:, 0:512][:, cs]
    rkv[1:, 128:256] = p_rw[:, 512:1024][:, cs]
    rkv[1:, 256:384] = p_rw[:, 1024:1536][:, cs]
    lo = np.zeros((256, L + 1), np.float32)
    lo[:, 1:] = p_rw[:, 1536:1792].T
    mu = prm["rwkv_mu"]
    mu_tm = np.concatenate([mu[0:512][cs], mu[512:1024][cs], mu[1024:1536][cs]])
    mu_tm = np.ascontiguousarray(np.broadcast_to(mu_tm[None, :], (128, 384)))
    mu_fm = np.zeros((128, 3), np.float32)
    mu_fm[0:64, 0] = mu[1536:1600]
    mu_fm[0:64, 1] = mu[1600:1664]
    mu_fm[:, 2] = mu[1664:1792]
    rows = [prm["w0"][cs], prm["a0"][cs], prm["k_k"][cs], prm["k_a"][cs], prm["r_k"].reshape(-1)[cs],
            prm["lnx_g"][cs], prm["lnx_b"][cs]]
    bc = np.ascontiguousarray(np.broadcast_to(np.stack(rows)[None], (128, 7, 128)))
    wa_up = np.ascontiguousarray(np.concatenate([prm["w_up"][:, cs], prm["a_up"][:, cs]], axis=0))
    g_up = np.ascontiguousarray(prm["g_up"][:, cs])
    return {"rkv": rkv, "lo": lo, "mu_tm": mu_tm, "mu_fm": mu_fm, "bc": bc, "wa_up": wa_up, "g_up": g_up,
            "cst": rwkv_consts()}


ALPHA = float((2 * 2) ** 0.25)
LN_EPS = 1e-5
NTOK = N_META + 2048
HALVES = [(0, [(0, 16), (16, 512), (528, 512)], [(0, 16)] + [(16 + 128 * i, 128) for i in range(8)]),
          (1040, [(0, 512), (512, 512)], [(128 * i, 128) for i in range(8)])]
C_IN = 5376


def tok_consts():
    c = np.zeros((128, 2, 128), np.float32)
    c[:, 0, :] = 1.0 / 1024
    c[:, 1, :] = np.eye(128)
    sel = np.zeros((16, 16, 128), np.float32)
    for e in range(16):
        sel[e, e, :] = 1.0
    return c, sel


def build_tok(nc, mode, do_proj, ntok=NTOK, halves=HALVES, n_exp=16):
    D = {}

    def inp(name, shape):
        D[name] = nc.dram_tensor(name, list(shape), F32, kind="ExternalInput").ap()

    def outp(name, shape):
        D[name] = nc.dram_tensor(name, list(shape), F32, kind="ExternalOutput").ap()

    inp("xT", [1024, ntok])
    inp("tc", [128, 2, 128])
    inp("lnA", [128, 2, 8])
    if mode == "C":
        inp("osbT", [512, ntok]); inp("orwT", [512, ntok]); inp("gT", [2048, ntok])
        inp("p_sb", [512, 1024]); inp("p_rw", [512, 1024]); inp("w_out", [1024, 1024])
        inp("lnB", [128, 2, 8])
        inp("router_w", [1024, 16]); inp("rb", [128, 16]); inp("sel", [16, 16, 128])
        inp("wg", [16, 1024, 512]); inp("wu", [16, 1024, 512]); inp("wd", [16, 512, 1024])
    if do_proj:
        inp("w_in", [1024, C_IN])
        outp("pT", [C_IN, ntok])
    outp("hT", [1024, ntok])
    with ExitStack() as es:
        P = Prog(nc, es)
        emit_tok(P, D, mode, do_proj, halves, n_exp)
        P.finish()
    return nc


def emit_tok(P, D, mode, do_proj, halves, n_exp=16):
    WMAX = 1040
    tc = P.sb("tc", [128, 2, 128]); t_tc = P.T(const=True)
    P.dma("sp", tc[:], D["tc"], writes=[t_tc])
    lnA = P.sb("lnA", [128, 2, 8]); t_lnA = P.T(const=True)
    P.dma("sp", lnA[:], D["lnA"], writes=[t_lnA])
    hT = P.sb("hT", [128, 8, WMAX]); hbf = P.sb("hbf", [128, 8, WMAX], BF16)
    NG = 3
    t_h = [P.T(f"h{g}") for g in range(NG)]
    t_hb = [P.T(f"hb{g}") for g in range(NG)]
    stg = [P.sb(f"stg{i}", [128, 2048]) for i in range(2)]
    t_stg = [P.T(), P.T()]
    ps = [P.ps(f"ps{i}") for i in range(8)]
    t_ps = [P.T(f"psb{i}", excl=True) for i in range(8)]
    mean_sb = P.sb("mean_sb", [128, 512]); t_mean = P.T()
    rstd_sb = P.sb("rstd_sb", [128, 512]); t_rstd = P.T()
    tmp = [P.sb(f"tmp{i}", [128, 512]) for i in range(2)]
    t_tmp = [P.T(), P.T()]
    cnt = {"stg": 0, "tmp": 0, "ev": 0}

    def ln_group(gi, c0, w, lnp, t_lnp):
        PM, PX = 6, 7
        for k in range(8):
            s = cnt["tmp"] % 2; cnt["tmp"] += 1
            P.op("pool", lambda e, k=k, s=s: e.tensor_tensor(tmp[s][:, 0:w], hT[:, k, c0:c0 + w], hT[:, k, c0:c0 + w], ALU.mult),
                 reads=[t_h[gi]], writes=[t_tmp[s]])
            P.op("pe", lambda e, k=k: e.matmul(ps[PM][:, 0:w], tc[:, 0, :], hT[:, k, c0:c0 + w], start=(k == 0), stop=(k == 7)),
                 reads=[t_tc, t_h[gi]], writes=[t_ps[PM]])
            P.op("pe", lambda e, k=k, s=s: e.matmul(ps[PX][:, 0:w], tc[:, 0, :], tmp[s][:, 0:w], start=(k == 0), stop=(k == 7)),
                 reads=[t_tc, t_tmp[s]], writes=[t_ps[PX]])
        P.op("act", lambda e: e.activation(mean_sb[:, 0:w], ps[PM][:, 0:w], AF.Identity), reads=[t_ps[PM]], writes=[t_mean])
        P.op("dve", lambda e: e.tensor_tensor(rstd_sb[:, 0:w], mean_sb[:, 0:w], mean_sb[:, 0:w], ALU.mult), reads=[t_mean], writes=[t_rstd])
        P.op("dve", lambda e: e.tensor_tensor(rstd_sb[:, 0:w], ps[PX][:, 0:w], rstd_sb[:, 0:w], ALU.subtract), reads=[t_ps[PX], t_rstd], writes=[t_rstd])
        P.op("dve", lambda e: e.tensor_scalar(rstd_sb[:, 0:w], rstd_sb[:, 0:w], LN_EPS, None, ALU.add), reads=[t_rstd], writes=[t_rstd])
        P.op("act", lambda e: e.activation(rstd_sb[:, 0:w], rstd_sb[:, 0:w], AF.Ln), reads=[t_rstd], writes=[t_rstd])
        P.op("act", lambda e: e.activation(rstd_sb[:, 0:w], rstd_sb[:, 0:w], AF.Exp, scale=-0.5), reads=[t_rstd], writes=[t_rstd])
        for k in range(8):
            s = cnt["tmp"] % 2; cnt["tmp"] += 1
            P.op("dve", lambda e, k=k, s=s: e.tensor_tensor(tmp[s][:, 0:w], hT[:, k, c0:c0 + w], mean_sb[:, 0:w], ALU.subtract),
                 reads=[t_h[gi], t_mean], writes=[t_tmp[s]])
            P.op("pool", lambda e, s=s: e.tensor_tensor(tmp[s][:, 0:w], tmp[s][:, 0:w], rstd_sb[:, 0:w], ALU.mult),
                 reads=[t_tmp[s], t_rstd], writes=[t_tmp[s]])
            P.op("act", lambda e, k=k, s=s: e.activation(hT[:, k, c0:c0 + w], tmp[s][:, 0:w], AF.Identity, bias=lnp[:, 1, k:k + 1], scale=lnp[:, 0, k:k + 1]),
                 reads=[t_tmp[s], t_lnp], writes=[t_h[gi]])
            P.op("pool", lambda e, k=k: e.tensor_copy(hbf[:, k, c0:c0 + w], hT[:, k, c0:c0 + w]),
                 reads=[t_h[gi]], writes=[t_hb[gi]])

    if do_proj:
        wpj = [P.sb(f"wpj{i}", [128, 8, 128], BF16) for i in range(2)]
        t_wpj = [P.T(), P.T()]
        ost = [P.sb(f"ost{i}", [128, 512]) for i in range(2)]
        t_ost = [P.T(), P.T()]
        w_in_v = D["w_in"].rearrange("(k p) c -> p k c", p=128)

    def proj(groups, tok0):
        for j in range(C_IN // 128):
            s = cnt["stg"] % 2; cnt["stg"] += 1
            wb = j % 2
            P.dma("sp", stg[s][:, 0:1024].rearrange("p (k c) -> p k c", k=8), w_in_v[:, :, 128 * j:128 * j + 128], writes=[t_stg[s]])
            P.op("pool", lambda e, s=s, wb=wb: e.tensor_copy(wpj[wb][:], stg[s][:, 0:1024].rearrange("p (k c) -> p k c", k=8)),
                 reads=[t_stg[s]], writes=[t_wpj[wb]])
            for gi, (c0, w) in enumerate(groups):
                bk = cnt["ev"] % 2
                for k in range(8):
                    P.op("pe", lambda e, k=k, wb=wb, bk=bk, c0=c0, w=w: e.matmul(ps[bk][:, 0:w], wpj[wb][:, k, :], hbf[:, k, c0:c0 + w], start=(k == 0), stop=(k == 7)),
                         reads=[t_wpj[wb], t_hb[gi]], writes=[t_ps[bk]])
                eng = "act" if cnt["ev"] % 2 == 0 else "dve"
                cnt["ev"] += 1
                if eng == "act":
                    P.op("act", lambda e, bk=bk, w=w: e.activation(ost[bk][:, 0:w], ps[bk][:, 0:w], AF.Identity), reads=[t_ps[bk]], writes=[t_ost[bk]])
                else:
                    P.op("dve", lambda e, bk=bk, w=w: e.tensor_copy(ost[bk][:, 0:w], ps[bk][:, 0:w]), reads=[t_ps[bk]], writes=[t_ost[bk]])
                P.dma("sp", D["pT"][128 * j:128 * j + 128, tok0 + c0:tok0 + c0 + w], ost[bk][:, 0:w], reads=[t_ost[bk]])

    if mode == "C":
        lnB = P.sb("lnB", [128, 2, 8]); t_lnB = P.T(const=True)
        P.dma("sp", lnB[:], D["lnB"], writes=[t_lnB])
        rw = P.sb("rw", [128, 8, 16]); t_rw = P.T(const=True)
        P.dma("sp", rw[:], D["router_w"].rearrange("(k p) e -> p k e", p=128), writes=[t_rw])
        rb = P.sb("rb", [128, 16]); t_rb = P.T(const=True)
        P.dma("sp", rb[:], D["rb"], writes=[t_rb])
        sel = P.sb("sel", [16, 16, 128]); t_sel = P.T(const=True)
        P.dma("sp", sel[:], D["sel"], writes=[t_sel])
        arena = [P.sb(f"arena{i}", [128, 8192], BF16) for i in range(2)]
        t_ar = [P.T("arena0"), P.T("arena1")]
        ob = [P.sb(f"ob{i}", [128, 4, 512], BF16) for i in range(2)]
        t_ob = [P.T(), P.T()]
        gts = [P.sb(f"gts{i}", [128, 512]) for i in range(4)]
        t_gts = [P.T() for _ in range(4)]
        merged = P.sb("merged", [128, 8, 512], BF16); t_merged = P.T()
        combT = P.sb("combT", [16, WMAX]); t_combT = P.T()
        cbc = [P.sb(f"cbc{i}", [128, WMAX]) for i in range(2)]
        t_cbc = [P.T(), P.T()]
        hid = [P.sb(f"hid{i}", [128, 2, 512], BF16) for i in range(2)]
        t_hid = [P.T(), P.T()]
        sgl = [P.sb(f"sgl{i}", [128, 512]) for i in range(2)]
        t_sgl = [P.T(), P.T()]
        rt = P.sb("rt", [128, 16, 16]); t_rt = P.T()
        rs = P.sb("rs", [128, 16]); t_rs = P.T()
        osb_v = D["osbT"].rearrange("(k p) t -> p k t", p=128)
        orw_v = D["orwT"].rearrange("(k p) t -> p k t", p=128)
        psb_v = D["p_sb"].rearrange("(k p) c -> p k c", p=128)
        prw_v = D["p_rw"].rearrange("(k p) c -> p k c", p=128)
        wout_v = D["w_out"].rearrange("(k p) c -> p k c", p=128)
        psb_bf = arena[0][:, 0:4096].rearrange("p (k c) -> p k c", k=4)
        prw_bf = arena[0][:, 4096:8192].rearrange("p (k c) -> p k c", k=4)
        wout_bf = arena[1][:, 0:8192].rearrange("p (k c) -> p k c", k=8)

    xT_v = D["xT"].rearrange("(k p) t -> p k t", p=128)
    hTo_v = D["hT"].rearrange("(k p) t -> p k t", p=128)

    for (tok0, groups, rtiles) in halves:
        for gi, (c0, w) in enumerate(groups):
            P.dma("sp", hT[:, :, c0:c0 + w], xT_v[:, :, tok0 + c0:tok0 + c0 + w], writes=[t_h[gi]])
        if mode == "A":
            for gi, (c0, w) in enumerate(groups):
                ln_group(gi, c0, w, lnA, t_lnA)
        else:
            for (src, dst, ai, nk) in ((psb_v, psb_bf, 0, 4), (prw_v, prw_bf, 0, 4), (wout_v, wout_bf, 1, 8)):
                for k0 in range(0, nk, 2):
                    s = cnt["stg"] % 2; cnt["stg"] += 1
                    P.dma("sp", stg[s][:, 0:2048].rearrange("p (k c) -> p k c", k=2), src[:, k0:k0 + 2, :], writes=[t_stg[s]])
                    P.op("pool", lambda e, s=s, dst=dst, k0=k0: e.tensor_copy(dst[:, k0:k0 + 2, :], stg[s][:, 0:2048].rearrange("p (k c) -> p k c", k=2)),
                         reads=[t_stg[s]], writes=[t_ar[ai]])
            for gi, (c0, w) in enumerate(groups):
                for (src, oi) in ((osb_v, 0), (orw_v, 1)):
                    s = cnt["stg"] % 2; cnt["stg"] += 1
                    P.dma("sp", stg[s][:, 0:4 * w].rearrange("p (k c) -> p k c", k=4), src[:, :, tok0 + c0:tok0 + c0 + w], writes=[t_stg[s]])
                    P.op("dve", lambda e, s=s, oi=oi, w=w: e.tensor_copy(ob[oi][:, :, 0:w], stg[s][:, 0:4 * w].rearrange("p (k c) -> p k c", k=4)),
                         reads=[t_stg[s]], writes=[t_ob[oi]])
                for m in range(8):
                    ms = slice(128 * m, 128 * m + 128)
                    ba, bb = 0 + (m % 2) * 2, 1 + (m % 2) * 2
                    for k in range(4):
                        P.op("pe", lambda e, k=k, ms=ms, ba=ba, w=w: e.matmul(ps[ba][:, 0:w], psb_bf[:, k, ms], ob[0][:, k, 0:w], start=(k == 0), stop=(k == 3)),
                             reads=[t_ar[0], t_ob[0]], writes=[t_ps[ba]])
                    for k in range(4):
                        P.op("pe", lambda e, k=k, ms=ms, bb=bb, w=w: e.matmul(ps[bb][:, 0:w], prw_bf[:, k, ms], ob[1][:, k, 0:w], start=(k == 0), stop=(k == 3)),
                             reads=[t_ar[0], t_ob[1]], writes=[t_ps[bb]])
                    g0, g1 = (m % 2) * 2, (m % 2) * 2 + 1
                    P.dma("sp", gts[g0][:, 0:w], D["gT"][128 * m:128 * m + 128, tok0 + c0:tok0 + c0 + w], writes=[t_gts[g0]])
                    P.dma("sp", gts[g1][:, 0:w], D["gT"][1024 + 128 * m:1024 + 128 * m + 128, tok0 + c0:tok0 + c0 + w], writes=[t_gts[g1]])
                    P.op("act", lambda e, g0=g0, w=w: e.activation(gts[g0][:, 0:w], gts[g0][:, 0:w], AF.Sigmoid), reads=[t_gts[g0]], writes=[t_gts[g0]])
                    P.op("act", lambda e, g1=g1, w=w: e.activation(gts[g1][:, 0:w], gts[g1][:, 0:w], AF.Sigmoid), reads=[t_gts[g1]], writes=[t_gts[g1]])
                    P.op("dve", lambda e, g0=g0, ba=ba, w=w: e.tensor_tensor(gts[g0][:, 0:w], ps[ba][:, 0:w], gts[g0][:, 0:w], ALU.mult),
                         reads=[t_ps[ba], t_gts[g0]], writes=[t_gts[g0]])
                    P.op("dve", lambda e, g1=g1, bb=bb, w=w: e.tensor_tensor(gts[g1][:, 0:w], ps[bb][:, 0:w], gts[g1][:, 0:w], ALU.mult),
                         reads=[t_ps[bb], t_gts[g1]], writes=[t_gts[g1]])
                    P.op("pool", lambda e, g0=g0, g1=g1, m=m, w=w: e.tensor_tensor(merged[:, m, 0:w], gts[g0][:, 0:w], gts[g1][:, 0:w], ALU.add),
                         reads=[t_gts[g0], t_gts[g1]], writes=[t_merged])
                for m in range(8):
                    ms = slice(128 * m, 128 * m + 128)
                    bk = 4 + (m % 2)
                    for k in range(8):
                        P.op("pe", lambda e, k=k, ms=ms, bk=bk, w=w: e.matmul(ps[bk][:, 0:w], wout_bf[:, k, ms], merged[:, k, 0:w], start=(k == 0), stop=(k == 7)),
                             reads=[t_ar[1], t_merged], writes=[t_ps[bk]])
                    P.op("dve", lambda e, m=m, bk=bk, c0=c0, w=w: e.scalar_tensor_tensor(hT[:, m, c0:c0 + w], hT[:, m, c0:c0 + w], ALPHA, ps[bk][:, 0:w], ALU.mult, ALU.add),
                         reads=[t_ps[bk], t_h[gi]], writes=[t_h[gi]])
                ln_group(gi, c0, w, lnA, t_lnA)
            for (r0, nt) in rtiles:
                gi = [i for i, (c0, w) in enumerate(groups) if c0 <= r0 < c0 + w][0]
                RB = 5
                for k in range(8):
                    P.op("pe", lambda e, k=k, r0=r0, nt=nt: e.matmul(ps[RB][0:nt, 0:16], hT[:, k, r0:r0 + nt], rw[:, k, :], start=(k == 0), stop=(k == 7)),
                         reads=[t_h[gi], t_rw], writes=[t_ps[RB]])
                R = lambda i: rt[0:nt, i, :]
                S = lambda i: rs[0:nt, i:i + 1]

                def dv(fn, nt=nt):
                    P.op("dve", fn, reads=[t_rt, t_rs], writes=[t_rt, t_rs])
                P.op("dve", lambda e, nt=nt: e.tensor_tensor(rt[0:nt, 0, :], ps[RB][0:nt, 0:16], rb[0:nt, :], ALU.add),
                     reads=[t_ps[RB], t_rb], writes=[t_rt])
                dv(lambda e, nt=nt: e.reduce_max(rs[0:nt, 0:1], rt[0:nt, 0, :], axis=AX.X))
                dv(lambda e, nt=nt: e.tensor_scalar(rs[0:nt, 0:1], rs[0:nt, 0:1], -1.0, None, ALU.mult))
                P.op("act", lambda e, nt=nt: e.activation(rt[0:nt, 1, :], rt[0:nt, 0, :], AF.Exp, bias=rs[0:nt, 0:1]),
                     reads=[t_rt, t_rs], writes=[t_rt])
                dv(lambda e, nt=nt: e.reduce_sum(rs[0:nt, 1:2], rt[0:nt, 1, :], axis=AX.X))
                dv(lambda e, nt=nt: e.reciprocal(rs[0:nt, 1:2], rs[0:nt, 1:2]))
                dv(lambda e, nt=nt: e.tensor_scalar(rt[0:nt, 2, :], rt[0:nt, 1, :], rs[0:nt, 1:2], None, ALU.mult))
                for g in range(4):
                    dv(lambda e, nt=nt, g=g: e.reduce_max(rt[0:nt, 3, g:g + 1], rt[0:nt, 2, 4 * g:4 * g + 4], axis=AX.X))
                for g in range(4):
                    dv(lambda e, nt=nt, g=g: e.tensor_scalar(rt[0:nt, 4, 4 * g:4 * g + 4], rt[0:nt, 2, 4 * g:4 * g + 4], rt[0:nt, 3, g:g + 1], None, ALU.is_equal))
                dv(lambda e, nt=nt: e.scalar_tensor_tensor(rt[0:nt, 5, :], rt[0:nt, 4, :], -2.0, rt[0:nt, 2, :], ALU.mult, ALU.add))
                for g in range(4):
                    dv(lambda e, nt=nt, g=g: e.reduce_max(rt[0:nt, 3, 4 + g:5 + g], rt[0:nt, 5, 4 * g:4 * g + 4], axis=AX.X))
                dv(lambda e, nt=nt: e.tensor_tensor(rt[0:nt, 3, 8:12], rt[0:nt, 3, 0:4], rt[0:nt, 3, 4:8], ALU.add))
                dv(lambda e, nt=nt: e.reduce_max(rs[0:nt, 2:3], rt[0:nt, 3, 8:12], axis=AX.X))
                dv(lambda e, nt=nt: e.tensor_scalar(rt[0:nt, 3, 12:16], rt[0:nt, 3, 8:12], rs[0:nt, 2:3], None, ALU.is_equal))
                for g in range(4):
                    dv(lambda e, nt=nt, g=g: e.tensor_scalar(rt[0:nt, 6, 4 * g:4 * g + 4], rt[0:nt, 2, 4 * g:4 * g + 4], 1.0, rt[0:nt, 3, 12 + g:13 + g], ALU.add, ALU.mult))
                dv(lambda e, nt=nt: e.tensor_scalar(rt[0:nt, 6, :], rt[0:nt, 6, :], -1.0, None, ALU.add))
                dv(lambda e, nt=nt: e.reduce_max(rs[0:nt, 3:4], rt[0:nt, 6, :], axis=AX.X))
                dv(lambda e, nt=nt: e.tensor_scalar(rt[0:nt, 7, :], rt[0:nt, 6, :], rs[0:nt, 3:4], None, ALU.is_equal))
                dv(lambda e, nt=nt: e.scalar_tensor_tensor(rt[0:nt, 8, :], rt[0:nt, 7, :], -2.0, rt[0:nt, 6, :], ALU.mult, ALU.add))
                dv(lambda e, nt=nt: e.reduce_max(rs[0:nt, 4:5], rt[0:nt, 8, :], axis=AX.X))
                dv(lambda e, nt=nt: e.tensor_scalar(rt[0:nt, 9, :], rt[0:nt, 8, :], rs[0:nt, 4:5], None, ALU.is_equal))
                dv(lambda e, nt=nt: e.tensor_tensor(rs[0:nt, 5:6], rs[0:nt, 3:4], rs[0:nt, 4:5], ALU.add))
                dv(lambda e, nt=nt: e.reciprocal(rs[0:nt, 5:6], rs[0:nt, 5:6]))
                dv(lambda e, nt=nt: e.tensor_tensor(rs[0:nt, 6:7], rs[0:nt, 3:4], rs[0:nt, 5:6], ALU.mult))
                dv(lambda e, nt=nt: e.tensor_tensor(rs[0:nt, 7:8], rs[0:nt, 4:5], rs[0:nt, 5:6], ALU.mult))
                dv(lambda e, nt=nt: e.tensor_scalar(rt[0:nt, 10, :], rt[0:nt, 7, :], rs[0:nt, 6:7], None, ALU.mult))
                dv(lambda e, nt=nt: e.scalar_tensor_tensor(rt[0:nt, 11, :], rt[0:nt, 9, :], rs[0:nt, 7:8], rt[0:nt, 10, :], ALU.mult, ALU.add))
                P.op("pe", lambda e, nt=nt: e.transpose(ps[RB][0:16, 256:256 + nt], rt[0:nt, 11, :], tc[0:nt, 1, 0:nt]),
                     reads=[t_rt, t_tc], writes=[t_ps[RB]])
                P.op("act", lambda e, nt=nt, r0=r0: e.activation(combT[:, r0:r0 + nt], ps[RB][0:16, 256:256 + nt], AF.Identity),
                     reads=[t_ps[RB]], writes=[t_combT])
            for gi, (c0, w) in enumerate(groups):
                P.op("pool", lambda e, c0=c0, w=w: e.tensor_scalar(hT[:, :, c0:c0 + w], hT[:, :, c0:c0 + w], ALPHA, None, ALU.mult),
                     reads=[t_h[gi]], writes=[t_h[gi]])
            nhe = 0
            for ex in range(n_exp):
                cb_ = ex % 2
                for gi, (c0, w) in enumerate(groups):
                    P.op("pe", lambda e, ex=ex, c0=c0, w=w: e.matmul(ps[6][:, 0:w], sel[:, ex, :], combT[:, c0:c0 + w], start=True, stop=True),
                         reads=[t_sel, t_combT], writes=[t_ps[6]])
                    P.op("act", lambda e, cb_=cb_, c0=c0, w=w: e.activation(cbc[cb_][:, c0:c0 + w], ps[6][:, 0:w], AF.Identity),
                         reads=[t_ps[6]], writes=[t_cbc[cb_]])
                for hf in range(2):
                    ai = nhe % 2
                    nhe += 1
                    wg_bf = arena[ai][:, 0:2048].rearrange("p (k c) -> p k c", k=8)
                    wu_bf = arena[ai][:, 2048:4096].rearrange("p (k c) -> p k c", k=8)
                    wd_bf = arena[ai][:, 4096:6144].rearrange("p (k c) -> p k c", k=2)
                    fs = slice(256 * hf, 256 * hf + 256)
                    for (src, dst, kk_) in ((D["wg"][ex].rearrange("(k p) f -> p k f", p=128)[:, :, fs], wg_bf, 8),
                                            (D["wu"][ex].rearrange("(k p) f -> p k f", p=128)[:, :, fs], wu_bf, 8),
                                            (D["wd"][ex, 256 * hf:256 * hf + 256, :].rearrange("(k p) d -> p k d", p=128), wd_bf, 2)):
                        s = cnt["stg"] % 2; cnt["stg"] += 1
                        P.dma("sp", stg[s][:, 0:2048].rearrange("p (k c) -> p k c", k=kk_), src, writes=[t_stg[s]])
                        P.op("pool", lambda e, s=s, dst=dst, kk_=kk_: e.tensor_copy(dst, stg[s][:, 0:2048].rearrange("p (k c) -> p k c", k=kk_)),
                             reads=[t_stg[s]], writes=[t_ar[ai]])
                    for gi, (c0, w) in enumerate(groups):
                        hb_ = cnt["ev"] % 2
                        cnt["ev"] += 1
                        for fc in range(2):
                            bg, bu = fc, 2 + fc
                            fcs = slice(128 * fc, 128 * fc + 128)
                            for k in range(8):
                                P.op("pe", lambda e, k=k, bg=bg, fcs=fcs, c0=c0, w=w, wg_bf=wg_bf: e.matmul(ps[bg][:, 0:w], wg_bf[:, k, fcs], hbf[:, k, c0:c0 + w], start=(k == 0), stop=(k == 7)),
                                     reads=[t_ar[ai], t_hb[gi]], writes=[t_ps[bg]])
                            for k in range(8):
                                P.op("pe", lambda e, k=k, bu=bu, fcs=fcs, c0=c0, w=w, wu_bf=wu_bf: e.matmul(ps[bu][:, 0:w], wu_bf[:, k, fcs], hbf[:, k, c0:c0 + w], start=(k == 0), stop=(k == 7)),
                                     reads=[t_ar[ai], t_hb[gi]], writes=[t_ps[bu]])
                            P.op("act", lambda e, fc=fc, bg=bg, w=w: e.activation(sgl[fc][:, 0:w], ps[bg][:, 0:w], AF.Silu),
                                 reads=[t_ps[bg]], writes=[t_sgl[fc]])
                            P.op("dve", lambda e, fc=fc, bu=bu, w=w: e.tensor_tensor(sgl[fc][:, 0:w], ps[bu][:, 0:w], sgl[fc][:, 0:w], ALU.mult),
                                 reads=[t_ps[bu], t_sgl[fc]], writes=[t_sgl[fc]])
                            P.op("pool", lambda e, fc=fc, hb_=hb_, cb_=cb_, c0=c0, w=w: e.tensor_tensor(hid[hb_][:, fc, 0:w], sgl[fc][:, 0:w], cbc[cb_][:, c0:c0 + w], ALU.mult),
                                 reads=[t_sgl[fc], t_cbc[cb_]], writes=[t_hid[hb_]])
                        for m in range(8):
                            bd = 4 + (m % 2)
                            ms = slice(128 * m, 128 * m + 128)
                            for fc in range(2):
                                P.op("pe", lambda e, fc=fc, bd=bd, ms=ms, hb_=hb_, w=w, wd_bf=wd_bf: e.matmul(ps[bd][:, 0:w], wd_bf[:, fc, ms], hid[hb_][:, fc, 0:w], start=(fc == 0), stop=(fc == 1)),
                                     reads=[t_ar[ai], t_hid[hb_]], writes=[t_ps[bd]])
                            P.op("dve", lambda e, m=m, bd=bd, c0=c0, w=w: e.tensor_tensor(hT[:, m, c0:c0 + w], ps[bd][:, 0:w], hT[:, m, c0:c0 + w], ALU.add),
                                 reads=[t_ps[bd], t_h[gi]], writes=[t_h[gi]])
            for gi, (c0, w) in enumerate(groups):
                ln_group(gi, c0, w, lnB, t_lnB)
        for gi, (c0, w) in enumerate(groups):
            P.dma("sp", hTo_v[:, :, tok0 + c0:tok0 + c0 + w], hT[:, :, c0:c0 + w], reads=[t_h[gi]])
        if do_proj:
            proj(groups, tok0)


NCORES = 8
SEQ = 8192
LFULL = N_META + SEQ


def _fm(v):
    return np.ascontiguousarray(np.asarray(v, np.float32).reshape(8, 128).T)


def _run(nc, in_maps):
    res = run_bass_kernel_spmd(nc, in_maps, core_ids=list(range(NCORES)))
    return res.results


def _new_nc():
    return bass.Bass("TRN2", target_bir_lowering=False)


def _mixers(pT_cores, prm):
    amask = attn_masks()
    attn_maps, rwkv_maps = [], []
    for c in range(NCORES):
        b, hp = c // 4, c % 4
        pb_ = np.concatenate([pT_cores[4 * b][:, 0:N_META]] + [pT_cores[4 * b + r][:, N_META:] for r in range(4)], axis=1)
        q = pb_[128 * hp:128 * hp + 128].reshape(2, 64, LFULL)
        k = pb_[512 + 128 * hp:512 + 128 * hp + 128].reshape(2, 64, LFULL)
        v = pb_[1024 + 128 * hp:1024 + 128 * hp + 128].reshape(2, 64, LFULL)
        vtm = v.transpose(0, 2, 1)
        vm = np.ascontiguousarray(vtm[:, 0:N_META])
        vx = np.ascontiguousarray(vtm[:, N_META:].reshape(2, SEQ // 128, 128, 64).transpose(0, 2, 1, 3))
        attn_maps.append({"qT": np.ascontiguousarray(q), "kT": np.ascontiguousarray(k), "vx": vx, "vm": vm, "msk": amask})
        p_rw = np.ascontiguousarray(pb_[1536:3328].T)
        rwkv_maps.append(rwkv_host_inputs(p_rw, hp, prm))
    nc = _new_nc()
    build_attn(nc, 2, SEQ // 512)
    ares = _run(nc, attn_maps)
    nc = _new_nc()
    build_rwkv(nc, SEQ // 128)
    rres = _run(nc, rwkv_maps)
    osbT, orwT = [], []
    for b in range(2):
        osbT.append(np.concatenate([ares[4 * b + hp]["oT"].reshape(128, LFULL) for hp in range(4)], axis=0))
        orwT.append(np.concatenate([rres[4 * b + hp]["o_rw"].T for hp in range(4)], axis=0))
    return osbT, orwT


def _core_cols(full, r):
    return np.ascontiguousarray(np.concatenate([full[:, 0:N_META], full[:, N_META + 2048 * r:N_META + 2048 * (r + 1)]], axis=1))


def kernel(**inputs):
    inp = {k: np.asarray(v) for k, v in inputs.items()}
    x, meta = inp["x"].astype(np.float32), inp["meta"].astype(np.float32)
    tcc, sel = tok_consts()
    maps = []
    for c in range(NCORES):
        b, r = c // 4, c % 4
        xT = np.ascontiguousarray(np.concatenate([meta.T, x[b, 2048 * r:2048 * (r + 1)].T], axis=1))
        maps.append({"xT": xT, "tc": tcc, "lnA": np.stack([_fm(inp["emb_ln_g"]), _fm(inp["emb_ln_b"])], axis=1),
                     "w_in": np.ascontiguousarray(inp["w_in"][0])})
    nc = _new_nc()
    build_tok(nc, "A", True)
    res = _run(nc, maps)
    hT_c = [r_["hT"] for r_ in res]
    pT_c = [r_["pT"] for r_ in res]
    for l in range(2):
        prm = {k: inp[k][l] for k in ["rwkv_mu", "w0", "w_up", "a0", "a_up", "g_up", "k_k", "k_a", "r_k", "lnx_g", "lnx_b"]}
        osbT, orwT = _mixers(pT_c, prm)
        last = (l == 1)
        maps = []
        for c in range(NCORES):
            b, r = c // 4, c % 4
            m = {"xT": hT_c[c], "tc": tcc, "lnA": np.stack([_fm(inp["ln1_g"][l]), _fm(inp["ln1_b"][l])], axis=1),
                 "osbT": _core_cols(osbT[b], r), "orwT": _core_cols(orwT[b], r),
                 "gT": np.ascontiguousarray(pT_c[c][3328:5376]),
                 "p_sb": np.ascontiguousarray(inp["p_sb"][l]), "p_rw": np.ascontiguousarray(inp["p_rwkv"][l]),
                 "w_out": np.ascontiguousarray(inp["w_out"][l]),
                 "lnB": np.stack([_fm(inp["ln2_g"][l]), _fm(inp["ln2_b"][l])], axis=1),
                 "router_w": np.ascontiguousarray(inp["router_w"]),
                 "rb": np.ascontiguousarray(np.broadcast_to(inp["router_b"][None].astype(np.float32), (128, 16))),
                 "sel": sel,
                 "wg": np.ascontiguousarray(inp["exp_w_gate"][l]), "wu": np.ascontiguousarray(inp["exp_w_up"][l]),
                 "wd": np.ascontiguousarray(inp["exp_w_down"][l])}
            if not last:
                m["w_in"] = np.ascontiguousarray(inp["w_in"][l + 1])
            maps.append(m)
        nc = _new_nc()
        build_tok(nc, "C", not last)
        res = _run(nc, maps)
        hT_c = [r_["hT"] for r_ in res]
        if not last:
            pT_c = [r_["pT"] for r_ in res]
    out = np.zeros((2, SEQ, 1024), np.float32)
    for c in range(NCORES):
        b, r = c // 4, c % 4
        out[b, 2048 * r:2048 * (r + 1)] = hT_c[c][:, N_META:].T
    return out
```

```python
import numpy as np
from contextlib import ExitStack
import concourse.bass as bass
import concourse.mybir as mybir
from concourse.bass_utils import run_bass_kernel_spmd

F32 = mybir.dt.float32
BF16 = mybir.dt.bfloat16
AF = mybir.ActivationFunctionType
ALU = mybir.AluOpType
AX = mybir.AxisListType

import os
BUDGET = int(os.environ["KBUDGET"]) if "KBUDGET" in os.environ else None
ENGS = ["pe", "act", "dve", "pool", "sp"]
DMA_R = 8


class T:
    __slots__ = ("name", "lw", "rd", "const", "excl")

    def __init__(self, name, const=False, excl=False):
        self.name = name
        self.lw = None
        self.rd = []
        self.const = const
        self.excl = excl


class Prog:
    def __init__(self, nc, es):
        self.nc = nc
        self.es = es
        self.items = {e: [] for e in ENGS}
        self.sem = {e: es.enter_context(nc.semaphore("s_" + e)) for e in ENGS}
        self.cnt = {e: 0 for e in ENGS}
        self.seen = {e: {} for e in ENGS}
        self.dsem = {}
        self.dma_n = {}
        self.dma_final = {}
        self.ntile = 0

    def sb(self, name, shape, dt=F32):
        return self.es.enter_context(self.nc.sbuf_tensor("sb_" + name, list(shape), dt))

    def ps(self, name, shape=(128, 512), dt=F32):
        return self.es.enter_context(self.nc.psum_tensor("pp_" + name, list(shape), dt))

    def T(self, name=None, const=False, excl=False):
        self.ntile += 1
        return T(name or f"t{self.ntile}", const, excl)

    def _deps(self, eng, reads, writes):
        deps = {}

        def add(p):
            if p is None:
                return
            k, v = p
            if deps.get(k, 0) < v:
                deps[k] = v

        for t in reads:
            add(t.lw)
        for t in writes:
            add(t.lw)
            for p in t.rd:
                add(p)
        waits = []
        for k, v in deps.items():
            if k == eng and eng == "pe":
                continue
            if self.seen[eng].get(k, 0) >= v:
                continue
            self.seen[eng][k] = v
            waits.append((k, v))
        return waits

    def _commit(self, pid, reads, writes):
        for t in writes:
            t.lw = pid
            t.rd = []
        for t in reads:
            if not t.const:
                t.rd.append(pid)
                if len(t.rd) > 64:
                    m = {}
                    for k, v in t.rd:
                        if m.get(k, 0) < v:
                            m[k] = v
                    t.rd = list(m.items())

    def begin_stream(self):
        self._rec = []

    def end_stream(self):
        r, self._rec = self._rec, None
        return r

    def mark(self):
        if getattr(self, "_rec", None) is not None:
            self._rec.append(("mark", ()))

    def run_interleaved(self, streams, max_active=2):
        streams = list(streams)
        active = []
        nxt = 0
        while nxt < len(streams) or active:
            if nxt < len(streams) and len(active) < max_active and (not active or active[0][2]):
                active.append([streams[nxt], 0, False])
                nxt += 1
            for idx, a in enumerate(list(active)):
                if a[1] >= len(a[0]):
                    continue
                kind, args = a[0][a[1]]
                if kind == "mark":
                    if idx > 0 and active[0] is not a and active[0][1] < len(active[0][0]):
                        continue
                    a[2] = True
                    a[1] += 1
                    continue
                a[1] += 1
                (self.op if kind == "op" else self.dma)(*args)
            active = [a for a in active if a[1] < len(a[0])]

    def op(self, eng, fn, reads=(), writes=()):
        if getattr(self, "_rec", None) is not None:
            self._rec.append(("op", (eng, fn, tuple(reads), tuple(writes))))
            return
        self.nops = getattr(self, "nops", 0) + 1
        if BUDGET is not None and self.nops > BUDGET:
            return
        ex = [t for t in reads if t.excl]
        if ex:
            reads = [t for t in reads if not t.excl]
            writes = list(writes) + ex
        waits = self._deps(eng, reads, writes)
        self.cnt[eng] += 1
        pid = (eng, self.cnt[eng])
        self.items[eng].append((waits, fn, True))
        self._commit(pid, reads, writes)

    def dma(self, q, out_ap, in_ap, reads=(), writes=()):
        if getattr(self, "_rec", None) is not None:
            self._rec.append(("dma", (q, out_ap, in_ap, tuple(reads), tuple(writes))))
            return
        self.nops = getattr(self, "nops", 0) + 1
        if BUDGET is not None and self.nops > BUDGET:
            return
        if q not in self.dsem:
            self.dsem[q] = [self.es.enter_context(self.nc.semaphore(f"d_{q}_{i}")) for i in range(DMA_R)]
            self.dma_n[q] = 0
        n = self.dma_n[q]
        self.dma_n[q] += 1
        i = n % DMA_R
        rnd = n // DMA_R
        key = ("d", q, i)
        waits = self._deps(q, reads, writes)
        if rnd > 0 and self.seen[q].get(key, 0) < 16 * rnd:
            self.seen[q][key] = 16 * rnd
            waits.append((key, 16 * rnd))
        sem = self.dsem[q][i]

        def fn(e, out_ap=out_ap, in_ap=in_ap, sem=sem):
            return e.dma_start(out=out_ap, in_=in_ap).then_inc(sem, 16)

        self.items[q].append((waits, fn, False))
        pid = (key, 16 * (rnd + 1))
        self.dma_final[key] = 16 * (rnd + 1)
        self._commit(pid, reads, writes)

    def _semobj(self, k):
        if isinstance(k, tuple):
            return self.dsem[k[1]][k[2]]
        return self.sem[k]

    def finish(self):
        waits = []
        for key, v in self.dma_final.items():
            waits.append((key, v))
        for e in ENGS:
            if e != "sp" and self.cnt[e] > 0:
                waits.append((e, self.cnt[e]))
        self.items["sp"].append((waits, None, False))
        nc = self.nc
        with nc.Block() as block:
            def replay(name, e):
                for waits, fn, inc in self.items[name]:
                    for k, v in waits:
                        e.wait_ge(self._semobj(k), v)
                    if fn is None:
                        continue
                    ins = fn(e)
                    if inc:
                        ins.then_inc(self.sem[name], 1)

            @block.tensor
            def _(e):
                replay("pe", e)

            @block.scalar
            def _(e):
                replay("act", e)

            @block.vector
            def _(e):
                replay("dve", e)

            @block.gpsimd
            def _(e):
                replay("pool", e)

            @block.sync
            def _(e):
                replay("sp", e)


N_META = 16


def build_attn(nc, NH, NG):
    NB = 4 * NG
    L = N_META + 512 * NG
    qT = nc.dram_tensor("qT", [NH, 64, L], F32, kind="ExternalInput").ap()
    kT = nc.dram_tensor("kT", [NH, 64, L], F32, kind="ExternalInput").ap()
    vx = nc.dram_tensor("vx", [NH, 128, NB, 64], F32, kind="ExternalInput").ap()
    vm = nc.dram_tensor("vm", [NH, 16, 64], F32, kind="ExternalInput").ap()
    msk = nc.dram_tensor("msk", [128, 3, 128], F32, kind="ExternalInput").ap()
    oT = nc.dram_tensor("oT", [NH, 64, L], F32, kind="ExternalOutput").ap()
    with ExitStack() as es:
        P = Prog(nc, es)
        emit_attn(P, NH, NG, qT, kT, vx, vm, msk, oT)
        P.finish()
    return nc


def emit_attn(P, NH, NG, qT, kT, vx, vm, msk, oT):
    NB = 4 * NG
    L = N_META + 512 * NG
    mstage = P.sb("mstage", [128, 3, 128], F32)
    t_mstage = P.T()
    P.dma("sp", mstage[:], msk, writes=[t_mstage])
    cm = P.sb("cm", [128, 3, 128], BF16)
    t_cm = P.T(const=True)
    P.op("dve", lambda e: e.tensor_copy(cm[:], mstage[:]), reads=[t_mstage], writes=[t_cm])
    zeros = P.sb("zeros", [128, 64], BF16)
    t_zeros = P.T(const=True)
    P.op("pool", lambda e: e.memset(zeros[:], 0.0), writes=[t_zeros])

    QT = P.sb("QT", [64, L], BF16)
    KT = P.sb("KT", [64, L], BF16)
    V = P.sb("V", [128, NB, 64], BF16)
    VM = P.sb("VM", [16, 64], BF16)
    t_Q, t_K, t_V = P.T(), P.T(), P.T()
    CH = 2064 if L > 2064 else L
    stg = [P.sb(f"stg{i}", [128, CH], F32) for i in range(2)]
    t_stg = [P.T(), P.T()]
    vstg = P.sb("vstg", [128, NB, 64], F32)
    t_vstg = P.T()
    vmstg = P.sb("vmstg", [16, 64], F32)
    t_vmstg = P.T()

    zA = [P.ps(f"zA{i}") for i in range(2)]
    zB = [P.ps(f"zB{i}") for i in range(2)]
    Ops = [P.ps(f"Ops{i}") for i in range(2)]
    t_zA = [P.T(excl=True), P.T(excl=True)]
    t_zB = [P.T(excl=True), P.T(excl=True)]
    t_O = [P.T(excl=True), P.T(excl=True)]
    e_sb = [P.sb(f"e{i}", [128, 512], F32) for i in range(2)]
    t_e = [P.T(), P.T()]
    sp_sb = [P.sb(f"sp{i}", [128, 512], BF16) for i in range(2)]
    t_sp = [P.T(), P.T()]
    A_sb = [P.sb(f"A{i}", [128, 512], BF16) for i in range(2)]
    t_A = [P.T(), P.T()]
    acc = P.sb("acc", [128, 512], F32)
    t_acc = P.T()
    accb = [P.sb(f"accb{i}", [128, 512], BF16) for i in range(3)]
    t_accb = [P.T(), P.T(), P.T()]
    ost = [P.sb(f"ost{i}", [64, 512], F32) for i in range(2)]
    t_ost = [P.T(), P.T()]

    nstg = 0
    gcount = 0
    for h in range(NH):
        for (src, dst, tt, scale) in ((qT, QT, t_Q, 0.125), (kT, KT, t_K, 1.0)):
            first = True
            for c0 in range(0, L, CH):
                w = min(CH, L - c0)
                s = nstg % 2
                nstg += 1
                P.dma("sp", stg[s][0:64, 0:w], src[h, :, c0:c0 + w], writes=[t_stg[s]])
                if scale != 1.0:
                    P.op("dve", lambda e, s=s, w=w, c0=c0, dst=dst, scale=scale:
                         e.tensor_scalar(dst[:, c0:c0 + w], stg[s][0:64, 0:w], scale, None, ALU.mult),
                         reads=[t_stg[s]], writes=[tt])
                else:
                    P.op("pool", lambda e, s=s, w=w, c0=c0, dst=dst:
                         e.tensor_copy(dst[:, c0:c0 + w], stg[s][0:64, 0:w]),
                         reads=[t_stg[s]], writes=[tt])
        P.dma("sp", vstg[:], vx[h], writes=[t_vstg])
        P.op("pool", lambda e: e.tensor_copy(V[:], vstg[:]), reads=[t_vstg], writes=[t_V])
        P.dma("sp", vmstg[:], vm[h], writes=[t_vmstg])
        P.op("pool", lambda e: e.tensor_copy(VM[:], vmstg[:]), reads=[t_vmstg], writes=[t_V])

        for g in [-1] + list(range(NG)):
            if g < 0:
                q0, QW = 0, N_META
                blocks = [("m", 0)]
            else:
                q0, QW = N_META + 512 * g, 512
                blocks = [("x", j) for j in range(4 * g + 3, -1, -1)] + [("m", 0)]
            ob = gcount % 2
            gcount += 1
            P.op("pe", lambda e, ob=ob, QW=QW, q0=q0:
                 e.matmul(Ops[ob][0:64, 0:QW], zeros[0:64, 0:64], QT[:, q0:q0 + QW], start=True, stop=False),
                 reads=[t_zeros, t_Q], writes=[t_O[ob]])
            P.op("pool", lambda e: e.memset(acc[:], 0.0), writes=[t_acc])
            nblk = len(blocks)
            info = []
            for it, (kind, j) in enumerate(blocks):
                if kind == "m":
                    kp, k0 = N_META, 0
                    c0 = 0
                    diag = (g < 0)
                else:
                    kp, k0 = 128, N_META + 128 * j
                    jl = j - 4 * g
                    diag = jl >= 0
                    c0 = 128 * jl if diag else 0
                info.append((kind, j, kp, k0, c0, diag))

            def prm(it, info=info, q0=q0, QW=QW):
                kind, j, kp, k0, c0, diag = info[it]
                W = QW - c0
                return kind, j, kp, k0, c0, diag, it % 2, W, slice(q0 + c0, q0 + QW), min(128, W)

            def pe_zA(it):
                kind, j, kp, k0, c0, diag, b, W, qs, dw = prm(it)
                P.op("pe", lambda e: e.matmul(zA[b][0:kp, 0:W], KT[:, k0:k0 + kp], QT[:, qs], start=True, stop=True),
                     reads=[t_K, t_Q], writes=[t_zA[b]])

            def act_e(it):
                kind, j, kp, k0, c0, diag, b, W, qs, dw = prm(it)
                P.op("act", lambda e: e.activation(e_sb[b][0:kp, 0:W], zA[b][0:kp, 0:W], AF.Exp),
                     reads=[t_zA[b]], writes=[t_e[b]])

            def act_sp(it, nblk=nblk, QW=QW):
                kind, j, kp, k0, c0, diag, b, W, qs, dw = prm(it)
                P.op("act", lambda e: e.activation(sp_sb[b][0:kp, 0:W], e_sb[b][0:kp, 0:W], AF.Ln, bias=1.0),
                     reads=[t_e[b]], writes=[t_sp[b]])
                if diag:
                    P.op("pool", lambda e: e.tensor_tensor(sp_sb[b][0:kp, 0:dw], sp_sb[b][0:kp, 0:dw],
                                                           cm[0:kp, 0, 0:dw], ALU.mult),
                         reads=[t_sp[b], t_cm], writes=[t_sp[b]])
                if it < nblk - 1:
                    a3 = it % 3
                    P.op("dve", lambda e: e.tensor_tensor(acc[0:kp, c0:QW], acc[0:kp, c0:QW], sp_sb[b][0:kp, 0:W], ALU.add),
                         reads=[t_sp[b], t_acc], writes=[t_acc])
                    P.op("dve", lambda e: e.tensor_copy(accb[a3][:, 0:QW], acc[:, 0:QW]),
                         reads=[t_acc], writes=[t_accb[a3]])

            def pe_zB(it, QW=QW):
                kind, j, kp, k0, c0, diag, b, W, qs, dw = prm(it)
                last = (it == 0)
                P.op("pe", lambda e: e.matmul(zB[b][0:kp, 0:W], KT[:, k0:k0 + kp], QT[:, qs], start=True, stop=False),
                     reads=[t_K, t_Q], writes=[t_zB[b]])
                P.op("pe", lambda e: e.matmul(zB[b][0:kp, 0:W], cm[0:kp, 1, 0:kp], sp_sb[b][0:kp, 0:W],
                                              start=False, stop=last),
                     reads=[t_cm, t_sp[b]], writes=[t_zB[b]])
                if not last:
                    ab = (it - 1) % 3
                    P.op("pe", lambda e: e.matmul(zB[b][0:kp, 0:W], cm[0:128, 2, 0:kp], accb[ab][0:128, c0:QW],
                                                  start=False, stop=True),
                         reads=[t_cm, t_accb[ab]], writes=[t_zB[b]])

            def act_A(it):
                kind, j, kp, k0, c0, diag, b, W, qs, dw = prm(it)
                P.op("act", lambda e: e.activation(A_sb[b][0:kp, 0:W], zB[b][0:kp, 0:W], AF.Exp),
                     reads=[t_zB[b]], writes=[t_A[b]])
                if diag:
                    P.op("pool", lambda e: e.tensor_tensor(A_sb[b][0:kp, 0:dw], A_sb[b][0:kp, 0:dw],
                                                           cm[0:kp, 0, 0:dw], ALU.mult),
                         reads=[t_A[b], t_cm], writes=[t_A[b]])

            def pe_AV(it, ob=ob, nblk=nblk, QW=QW):
                kind, j, kp, k0, c0, diag, b, W, qs, dw = prm(it)
                vop = (VM[0:kp, :] if kind == "m" else V[:, j, :])
                P.op("pe", lambda e: e.matmul(Ops[ob][0:64, c0:QW], vop, A_sb[b][0:kp, 0:W],
                                              start=False, stop=(it == nblk - 1)),
                     reads=[t_V, t_A[b]], writes=[t_O[ob]])

            for s_ in range(-2, nblk + 2):
                if 0 <= s_ + 2 < nblk:
                    pe_zA(s_ + 2)
                if 0 <= s_ < nblk:
                    pe_zB(s_)
                if 0 <= s_ - 2 < nblk:
                    pe_AV(s_ - 2)
                if 0 <= s_ + 1 < nblk:
                    act_e(s_ + 1)
                if 0 <= s_ - 1 < nblk:
                    act_A(s_ - 1)
                if 0 <= s_ + 1 < nblk:
                    act_sp(s_ + 1)
            P.op("dve", lambda e, ob=ob, QW=QW: e.tensor_copy(ost[ob][:, 0:QW], Ops[ob][0:64, 0:QW]),
                 reads=[t_O[ob]], writes=[t_ost[ob]])
            P.dma("sp", oT[h, :, q0:q0 + QW], ost[ob][:, 0:QW], reads=[t_ost[ob]])


def attn_masks():
    s = np.arange(128)[:, None]
    t = np.arange(128)[None, :]
    m = np.zeros((128, 3, 128), np.float32)
    m[:, 0, :] = (s < t)
    m[:, 1, :] = -1.0 * (s >= t)
    m[:, 2, :] = -1.0
    return m


DECAY_SCALE = float(np.exp(-0.5))
LNX_EPS = 64e-5


def rwkv_consts():
    idx = np.arange(128)
    ch = idx // 64
    same = ch[:, None] == ch[None, :]
    le = idx[:, None] <= idx[None, :]
    lt = idx[:, None] < idx[None, :]
    c = np.zeros((128, 9, 128), np.float32)
    c[:, 0] = -DECAY_SCALE * (same & le)
    c[:, 1] = -DECAY_SCALE * same
    c[:, 2] = same & lt
    c[:, 3] = same & le
    c[:, 4] = same & lt
    c[:, 5] = same & le
    c[:, 6] = (same & lt).T
    c[:, 7] = np.eye(128)
    c[:, 8, 0] = -DECAY_SCALE * (ch == 0)
    c[:, 8, 1] = -DECAY_SCALE * (ch == 1)
    c[:, 8, 2] = (ch == 0)
    c[:, 8, 3] = (ch == 1)
    return c


def build_rwkv(nc, NTX):
    L = N_META + 128 * NTX
    D = {}
    D["rkv"] = nc.dram_tensor("rkv", [L + 1, 384], F32, kind="ExternalInput").ap()
    D["lo"] = nc.dram_tensor("lo", [256, L + 1], F32, kind="ExternalInput").ap()
    D["mu_tm"] = nc.dram_tensor("mu_tm", [128, 384], F32, kind="ExternalInput").ap()
    D["mu_fm"] = nc.dram_tensor("mu_fm", [128, 3], F32, kind="ExternalInput").ap()
    D["bc"] = nc.dram_tensor("bc", [128, 7, 128], F32, kind="ExternalInput").ap()
    D["wa_up"] = nc.dram_tensor("wa_up", [128, 128], F32, kind="ExternalInput").ap()
    D["g_up"] = nc.dram_tensor("g_up", [128, 128], F32, kind="ExternalInput").ap()
    D["cst"] = nc.dram_tensor("cst", [128, 9, 128], F32, kind="ExternalInput").ap()
    D["o"] = nc.dram_tensor("o_rw", [L, 128], F32, kind="ExternalOutput").ap()
    with ExitStack() as es:
        P = Prog(nc, es)
        emit_rwkv(P, NTX, D)
        P.finish()
    return nc


def emit_rwkv(P, NTX, D, pfx="rw"):
    DS = DECAY_SCALE
    NCH = 1 + 2 * NTX

    def const_load(name, shape, src):
        t = P.sb(pfx + name, shape, F32)
        tt = P.T(const=True)
        P.dma("sp", t[:], src, writes=[tt])
        return t, tt

    cst, t_cst = const_load("cst", [128, 9, 128], D["cst"])
    bc, t_bc = const_load("bc", [128, 7, 128], D["bc"])
    mu_tm, t_mutm = const_load("mu_tm", [128, 384], D["mu_tm"])
    mu_fm, t_mufm = const_load("mu_fm", [128, 3], D["mu_fm"])
    wup, t_wup = const_load("wup", [64, 128], D["wa_up"][0:64, :])
    aup, t_aup = const_load("aup", [64, 128], D["wa_up"][64:128, :])
    gup, t_gup = const_load("gup", [128, 128], D["g_up"])

    S_all = [P.sb(pfx + f"S_all{h}", [64, NCH + 1, 64], F32) for h in range(2)]
    t_S = [[P.T() for _ in range(NCH + 1)] for h in range(2)]
    for h in range(2):
        P.op("pool", lambda e, h=h: e.memset(S_all[h][:, 0, :], 0.0), writes=[t_S[h][0]])

    pb = [P.ps(pfx + f"pb{i}", [128, 4, 128]) for i in range(8)]
    t_bank = [P.T("bank%d" % i, excl=True) for i in range(8)]

    names_sb = {
        "xa": [128, 384], "xs": [128, 384], "x": [128, 384],
        "la": [64, 128], "ls": [64, 128], "la2": [64, 128], "ls2": [64, 128], "ga": [128, 128], "gs": [128, 128],
        "lx": [64, 128], "lx2": [64, 128], "gx": [128, 128], "tw": [64, 128], "sg": [128, 128],
        "sigw": [128, 128], "a": [128, 128], "g": [128, 128],
        "kk": [128, 128], "sq": [128, 128], "ss": [128, 2], "rn": [128, 2],
        "kp": [128, 128], "t1": [128, 128], "bs": [128, 2],
        "cum": [128, 128], "c1": [128, 128], "c2": [128, 128],
        "ep": [128, 128], "em": [128, 128], "epm": [128, 128], "ebar": [128, 128], "ebz": [128, 2, 128],
        "PC": [64, 2, 2],
        "TM": [128, 4, 128],
        "kka": [128, 128], "bbz": [128, 2, 128], "kbz": [128, 2, 128],
        "FM0": [64, 4, 128], "FM1": [64, 4, 128],
        "G0": [128, 4, 128], "G1": [128, 4, 128],
        "NP0": [128, 6, 2, 128], "NP1": [128, 6, 2, 128],
        "X0a": [128, 128], "X0b": [128, 128], "X1a": [128, 128], "X1b": [128, 128],
        "M": [64, 4, 64], "RHz0": [64, 2, 128], "RHz1": [64, 2, 128],
        "y": [128, 128], "ysq": [128, 128], "st": [128, 8], "yn": [128, 128], "o": [128, 128],
    }
    bufs = []
    for p in range(2):
        d = {}
        for nm, shp in names_sb.items():
            d[nm] = (P.sb(f"{pfx}{nm}_{p}", shp, F32), P.T(f"{nm}{p}"))
        bufs.append(d)
        for nm in ("RHz0", "RHz1"):
            t, tt = d[nm]
            P.op("pool", lambda e, t=t: e.memset(t[:], 0.0), writes=[tt])

    rkv, lo, o_d = D["rkv"], D["lo"], D["o"]
    B_LORA, B_CUM, B_FM, B_G, B_H0, B_H1, B_PM, B_Y = range(8)
    B_HH = [B_H0, B_H1]

    def tile_body(ti):
        n = 16 if ti == 0 else 128
        t0 = 0 if ti == 0 else N_META + 128 * (ti - 1)
        nch = 1 if ti == 0 else 2
        cb = 0 if ti == 0 else 1 + 2 * (ti - 1)
        B = bufs[ti % 2]

        def b(nm):
            return B[nm]

        xa, t_xa = b("xa"); xs, t_xs = b("xs"); x, t_x = b("x")
        la, t_la = b("la"); ls, t_ls = b("ls"); la2, t_la2 = b("la2"); ls2, t_ls2 = b("ls2")
        ga, t_ga = b("ga"); gs, t_gs = b("gs")
        lx, t_lx = b("lx"); lx2, t_lx2 = b("lx2"); gx, t_gx = b("gx"); tw, t_tw = b("tw"); sg, t_sg = b("sg")
        sigw, t_sigw = b("sigw"); a_, t_a = b("a"); g_, t_g = b("g")
        kk, t_kk = b("kk"); sq, t_sq = b("sq"); ss, t_ss = b("ss"); rn, t_rn = b("rn")
        kp, t_kp = b("kp"); t1, t_t1 = b("t1"); bs, t_bs = b("bs")
        cum, t_cum = b("cum"); c1, t_c1 = b("c1"); c2, t_c2 = b("c2")
        ep, t_ep = b("ep"); em, t_em = b("em"); epm, t_epm = b("epm"); ebar, t_ebar = b("ebar")
        ebz, t_ebz = b("ebz")
        PC, t_PC = b("PC"); TM, t_TM = b("TM"); kka, t_kka = b("kka")
        bbz, t_bbz = b("bbz"); kbz, t_kbz = b("kbz")
        FMh = [b("FM0"), b("FM1")]
        M_, t_M = b("M"); RHz = [b("RHz0"), b("RHz1")]
        y_, t_y = b("y"); ysq, t_ysq = b("ysq"); st, t_st = b("st"); yn, t_yn = b("yn"); o_, t_o = b("o")

        P.dma("sp", xa[0:n, :], rkv[1 + t0:1 + t0 + n, :], writes=[t_xa])
        P.dma("sp", xs[0:n, :], rkv[t0:t0 + n, :], writes=[t_xs])
        P.dma("sp", la[:, 0:n], lo[0:64, 1 + t0:1 + t0 + n], writes=[t_la])
        P.dma("sp", ls[:, 0:n], lo[0:64, t0:t0 + n], writes=[t_ls])
        P.dma("sp", la2[:, 0:n], lo[64:128, 1 + t0:1 + t0 + n], writes=[t_la2])
        P.dma("sp", ls2[:, 0:n], lo[64:128, t0:t0 + n], writes=[t_ls2])
        P.dma("sp", ga[:, 0:n], lo[128:256, 1 + t0:1 + t0 + n], writes=[t_ga])
        P.dma("sp", gs[:, 0:n], lo[128:256, t0:t0 + n], writes=[t_gs])
        P.op("pool", lambda e: e.tensor_tensor(xs[0:n, :], xs[0:n, :], xa[0:n, :], ALU.subtract),
             reads=[t_xa, t_xs], writes=[t_xs])
        P.op("pool", lambda e: e.tensor_tensor(xs[0:n, :], xs[0:n, :], mu_tm[0:n, :], ALU.mult),
             reads=[t_xs, t_mutm], writes=[t_xs])
        P.op("pool", lambda e: e.tensor_tensor(x[0:n, :], xs[0:n, :], xa[0:n, :], ALU.add),
             reads=[t_xa, t_xs], writes=[t_x])
        for (A_, tA, S_, tS, O_, tO, col, np_) in ((la, t_la, ls, t_ls, lx, t_lx, 0, 64), (la2, t_la2, ls2, t_ls2, lx2, t_lx2, 1, 64),
                                                  (ga, t_ga, gs, t_gs, gx, t_gx, 2, 128)):
            P.op("pool", lambda e, A_=A_, S_=S_: e.tensor_tensor(S_[:, 0:n], S_[:, 0:n], A_[:, 0:n], ALU.subtract),
                 reads=[tA, tS], writes=[tS])
            P.op("dve", lambda e, A_=A_, S_=S_, O_=O_, col=col, np_=np_: e.scalar_tensor_tensor(O_[:, 0:n], S_[:, 0:n], mu_fm[0:np_, col:col + 1], A_[:, 0:n], ALU.mult, ALU.add),
                 reads=[tA, tS, t_mufm], writes=[tO])
        xr = x[0:n, 0:128]
        xk = x[0:n, 128:256]
        P.op("act", lambda e: e.activation(tw[:, 0:n], lx[:, 0:n], AF.Exp, scale=-2.0), reads=[t_lx], writes=[t_tw])
        P.op("dve", lambda e: e.tensor_scalar(tw[:, 0:n], tw[:, 0:n], 1.0, None, ALU.add), reads=[t_tw], writes=[t_tw])
        P.op("dve", lambda e: e.reciprocal(tw[:, 0:n], tw[:, 0:n]), reads=[t_tw], writes=[t_tw])
        P.op("dve", lambda e: e.tensor_scalar(tw[:, 0:n], tw[:, 0:n], 2.0, -1.0, ALU.mult, ALU.add), reads=[t_tw], writes=[t_tw])
        P.op("act", lambda e: e.activation(sg[:, 0:n], gx[:, 0:n], AF.Exp, scale=-1.0), reads=[t_gx], writes=[t_sg])
        P.op("dve", lambda e: e.tensor_scalar(sg[:, 0:n], sg[:, 0:n], 1.0, None, ALU.add), reads=[t_sg], writes=[t_sg])
        P.op("dve", lambda e: e.reciprocal(sg[:, 0:n], sg[:, 0:n]), reads=[t_sg], writes=[t_sg])
        tb = t_bank[B_LORA]
        P.op("pe", lambda e: e.matmul(pb[B_LORA][0:n, 0, :], tw[:, 0:n], wup[:, :], start=True, stop=True),
             reads=[t_tw, t_wup], writes=[tb])
        P.op("pe", lambda e: e.matmul(pb[B_LORA][0:n, 1, :], lx2[:, 0:n], aup[:, :], start=True, stop=True),
             reads=[t_lx2, t_aup], writes=[tb])
        P.op("pe", lambda e: e.matmul(pb[B_LORA][0:n, 2, :], sg[:, 0:n], gup[:, :], start=True, stop=True),
             reads=[t_sg, t_gup], writes=[tb])
        P.op("dve", lambda e: e.tensor_tensor(sigw[0:n, :], pb[B_LORA][0:n, 0, :], bc[0:n, 0, :], ALU.add),
             reads=[tb, t_bc], writes=[t_sigw])
        P.op("dve", lambda e: e.tensor_tensor(a_[0:n, :], pb[B_LORA][0:n, 1, :], bc[0:n, 1, :], ALU.add),
             reads=[tb, t_bc], writes=[t_a])
        P.op("act", lambda e: e.activation(g_[0:n, :], pb[B_LORA][0:n, 2, :], AF.Identity), reads=[tb], writes=[t_g])
        for (Z, tZ) in ((sigw, t_sigw), (a_, t_a)):
            P.op("act", lambda e, Z=Z: e.activation(Z[0:n, :], Z[0:n, :], AF.Exp, scale=-1.0), reads=[tZ], writes=[tZ])
            P.op("dve", lambda e, Z=Z: e.tensor_scalar(Z[0:n, :], Z[0:n, :], 1.0, None, ALU.add), reads=[tZ], writes=[tZ])
            P.op("dve", lambda e, Z=Z: e.reciprocal(Z[0:n, :], Z[0:n, :]), reads=[tZ], writes=[tZ])
        P.op("pool", lambda e: e.tensor_tensor(kk[0:n, :], xk, bc[0:n, 2, :], ALU.mult), reads=[t_x, t_bc], writes=[t_kk])
        P.op("pool", lambda e: e.tensor_tensor(sq[0:n, :], kk[0:n, :], kk[0:n, :], ALU.mult), reads=[t_kk], writes=[t_sq])
        for h in range(2):
            P.op("dve", lambda e, h=h: e.reduce_sum(ss[0:n, h:h + 1], sq[0:n, 64 * h:64 * h + 64], axis=AX.X),
                 reads=[t_sq], writes=[t_ss])
        P.op("dve", lambda e: e.tensor_scalar(rn[0:n, :], ss[0:n, :], 1e-24, None, ALU.max), reads=[t_ss], writes=[t_rn])
        P.op("act", lambda e: e.activation(rn[0:n, :], rn[0:n, :], AF.Ln), reads=[t_rn], writes=[t_rn])
        P.op("act", lambda e: e.activation(rn[0:n, :], rn[0:n, :], AF.Exp, scale=-0.5), reads=[t_rn], writes=[t_rn])
        for h in range(2):
            P.op("dve", lambda e, h=h: e.tensor_scalar(kk[0:n, 64 * h:64 * h + 64], kk[0:n, 64 * h:64 * h + 64],
                                                       rn[0:n, h:h + 1], None, ALU.mult),
                 reads=[t_kk, t_rn], writes=[t_kk])
        P.op("dve", lambda e: e.scalar_tensor_tensor(t1[0:n, :], a_[0:n, :], -1.0, bc[0:n, 3, :], ALU.add, ALU.mult),
             reads=[t_a, t_bc], writes=[t_t1])
        P.op("dve", lambda e: e.scalar_tensor_tensor(kp[0:n, :], t1[0:n, :], 1.0, xk, ALU.add, ALU.mult),
             reads=[t_t1, t_x], writes=[t_kp])
        P.op("pool", lambda e: e.tensor_tensor(t1[0:n, :], xr, kp[0:n, :], ALU.mult), reads=[t_x, t_kp, t_t1], writes=[t_t1])
        P.op("pool", lambda e: e.tensor_tensor(t1[0:n, :], t1[0:n, :], bc[0:n, 4, :], ALU.mult), reads=[t_t1, t_bc], writes=[t_t1])
        for h in range(2):
            P.op("dve", lambda e, h=h: e.reduce_sum(bs[0:n, h:h + 1], t1[0:n, 64 * h:64 * h + 64], axis=AX.X),
                 reads=[t_t1], writes=[t_bs])
        tb = t_bank[B_CUM]
        P.op("pe", lambda e: e.matmul(pb[B_CUM][0:n, 0, :], cst[0:n, 0, 0:n], sigw[0:n, :], start=True, stop=True),
             reads=[t_cst, t_sigw], writes=[tb])
        P.op("pe", lambda e: e.matmul(pb[B_CUM][0:n, 1, :], cst[0:n, 1, 0:n], sigw[0:n, :], start=True, stop=True),
             reads=[t_cst, t_sigw], writes=[tb])
        for h in range(2):
            P.op("pe", lambda e, h=h: e.matmul(pb[B_CUM][0:64, 2 + h, 0:2], sigw[0:n, 64 * h:64 * h + 64], cst[0:n, 8, 0:2], start=True, stop=True),
                 reads=[t_cst, t_sigw], writes=[tb])
        P.op("dve", lambda e: e.tensor_copy(cum[0:n, :], pb[B_CUM][0:n, 0, :]), reads=[tb], writes=[t_cum])
        P.op("dve", lambda e: e.scalar_tensor_tensor(c1[0:n, :], sigw[0:n, :], DS, cum[0:n, :], ALU.mult, ALU.add),
             reads=[t_sigw, t_cum], writes=[t_c1])
        P.op("dve", lambda e: e.tensor_tensor(c2[0:n, :], pb[B_CUM][0:n, 1, :], cum[0:n, :], ALU.subtract),
             reads=[tb, t_cum], writes=[t_c2])
        P.op("act", lambda e: e.activation(ep[0:n, :], cum[0:n, :], AF.Exp), reads=[t_cum], writes=[t_ep])
        P.op("act", lambda e: e.activation(em[0:n, :], cum[0:n, :], AF.Exp, scale=-1.0), reads=[t_cum], writes=[t_em])
        P.op("act", lambda e: e.activation(epm[0:n, :], c1[0:n, :], AF.Exp), reads=[t_c1], writes=[t_epm])
        P.op("act", lambda e: e.activation(ebar[0:n, :], c2[0:n, :], AF.Exp), reads=[t_c2], writes=[t_ebar])
        P.op("act", lambda e: e.activation(PC[:, :, :], pb[B_CUM][0:64, 2:4, 0:2], AF.Exp), reads=[tb], writes=[t_PC])
        P.op("dve", lambda e: e.tensor_tensor(kka[0:n, :], kk[0:n, :], a_[0:n, :], ALU.mult), reads=[t_kk, t_a], writes=[t_kka])
        P.op("dve", lambda e: e.tensor_tensor(TM[0:n, 0, :], kka[0:n, :], em[0:n, :], ALU.mult), reads=[t_kka, t_em], writes=[t_TM])
        P.op("dve", lambda e: e.tensor_tensor(TM[0:n, 1, :], kp[0:n, :], em[0:n, :], ALU.mult), reads=[t_kp, t_em], writes=[t_TM])
        P.op("dve", lambda e: e.scalar_tensor_tensor(TM[0:n, 2, :], kk[0:n, :], -1.0, epm[0:n, :], ALU.mult, ALU.mult),
             reads=[t_kk, t_epm], writes=[t_TM])
        P.op("dve", lambda e: e.tensor_tensor(TM[0:n, 3, :], xr, ep[0:n, :], ALU.mult), reads=[t_x, t_ep], writes=[t_TM])
        for c in range(nch):
            P.op("pool", lambda e, c=c: e.tensor_scalar(ebz[0:n, c, :], ebar[0:n, :], cst[0:n, 8, 2 + c:3 + c], None, ALU.mult),
                 reads=[t_ebar, t_cst], writes=[t_ebz])
            P.op("pool", lambda e, c=c: e.tensor_tensor(bbz[0:n, c, :], kka[0:n, :], ebz[0:n, c, :], ALU.mult),
                 reads=[t_kka, t_ebz], writes=[t_bbz])
            P.op("pool", lambda e, c=c: e.tensor_tensor(kbz[0:n, c, :], kp[0:n, :], ebz[0:n, c, :], ALU.mult),
                 reads=[t_kp, t_ebz], writes=[t_kbz])
        for h in range(2):
            FM, t_FM = FMh[h]
            bk = B_FM
            for s_ in range(4):
                P.op("pe", lambda e, s_=s_, h=h, bk=bk: e.transpose(pb[bk][0:64, s_, 0:n], TM[0:n, s_, 64 * h:64 * h + 64], cst[0:n, 7, 0:n]),
                     reads=[t_TM, t_cst], writes=[t_bank[bk]])
            P.op("act", lambda e, FM=FM, bk=bk: e.activation(FM[:, :, 0:n], pb[bk][0:64, :, 0:n], AF.Identity),
                 reads=[t_bank[bk]], writes=[t_FM])

        G = [b("G0"), b("G1")]
        NP = [b("NP0"), b("NP1")]
        Xb = [[b("X0a"), b("X0b")], [b("X1a"), b("X1b")]]
        for h in range(2):
            FM, t_FM = FMh[h]
            Gh, t_G = G[h]
            NPh, t_NP = NP[h]
            gbk = B_G
            gb = pb[gbk]
            for (so, sl, sr) in ((0, 0, 2), (1, 0, 3), (2, 1, 2), (3, 1, 3)):
                P.op("pe", lambda e, gb=gb, so=so, sl=sl, sr=sr, FM=FM: e.matmul(gb[0:n, so, 0:n], FM[:, sl, 0:n], FM[:, sr, 0:n], start=True, stop=True),
                     reads=[t_FM], writes=[t_bank[gbk]])
            nbk = B_CUM
            P.op("pe", lambda e, nbk=nbk, FM=FM, h=h: e.matmul(pb[nbk][0:n, 2 + h, 0:n], FM[:, 2, 0:n], FM[:, 0, 0:n], start=True, stop=True),
                 reads=[t_FM], writes=[t_bank[nbk]])
            P.op("dve", lambda e, gb=gb, Gh=Gh: e.tensor_tensor(Gh[0:n, :, 0:n], gb[0:n, :, 0:n], cst[0:n, 2:6, 0:n], ALU.mult),
                 reads=[t_bank[gbk], t_cst], writes=[t_G])
            P.op("dve", lambda e, nbk=nbk, NPh=NPh, h=h: e.tensor_tensor(NPh[0:n, 0, 1, 0:n], pb[nbk][0:n, 2 + h, 0:n], cst[0:n, 6, 0:n], ALU.mult),
                 reads=[t_bank[nbk], t_cst], writes=[t_NP])
            P.op("pool", lambda e, Gh=Gh, NPh=NPh: e.tensor_copy(NPh[0:n, 0, 0, 0:n], Gh[0:n, 0, 0:n]),
                 reads=[t_G], writes=[t_NP])
        P.mark()
        for h in range(2):
            hs = slice(64 * h, 64 * h + 64)
            vs = slice(256 + 64 * h, 256 + 64 * h + 64)
            Gh, t_G = G[h]
            xbk = B_HH[h]
            P.op("pe", lambda e, hs=hs, xbk=xbk: e.matmul(pb[xbk][0:n, 2, 0:64], cst[0:n, 7, 0:n], TM[0:n, 2, hs], start=True, stop=True),
                 reads=[t_TM, t_cst], writes=[t_bank[xbk]])
            P.op("pe", lambda e, vs=vs, xbk=xbk, Gh=Gh: e.matmul(pb[xbk][0:n, 2, 64:128], Gh[0:n, 2, 0:n], x[0:n, vs], start=True, stop=True),
                 reads=[t_G, t_x], writes=[t_bank[xbk]])
            X0, t_X0 = Xb[h][0]
            P.op("act", lambda e, xbk=xbk, X0=X0: e.activation(X0[0:n, :], pb[xbk][0:n, 2, :], AF.Identity),
                 reads=[t_bank[xbk]], writes=[t_X0])
        NPW = 6
        cur = [0, 0]
        for i in range(NPW):
            for h in range(2):
                NPh, t_NP = NP[h]
                nbk = B_HH[h]
                xbk = B_HH[h]
                Xc, t_Xc = Xb[h][cur[h]]
                Xn, t_Xn = Xb[h][1 - cur[h]]
                P.op("pe", lambda e, i=i, NPh=NPh, Xc=Xc, xbk=xbk: e.matmul(pb[xbk][0:n, 2, :], NPh[0:n, i, 0, 0:n], Xc[0:n, :], start=True, stop=True),
                     reads=[t_NP, t_Xc], writes=[t_bank[xbk]])
                P.op("dve", lambda e, Xc=Xc, Xn=Xn, xbk=xbk: e.tensor_tensor(Xn[0:n, :], pb[xbk][0:n, 2, :], Xc[0:n, :], ALU.add),
                     reads=[t_bank[xbk], t_Xc], writes=[t_Xn])
                cur[h] = 1 - cur[h]
                if i < NPW - 1:
                    P.op("pe", lambda e, i=i, NPh=NPh, nbk=nbk: e.matmul(pb[nbk][0:n, 0, 0:n], NPh[0:n, i, 1, 0:n], NPh[0:n, i, 0, 0:n], start=True, stop=True),
                         reads=[t_NP], writes=[t_bank[nbk]])
                    P.op("pe", lambda e, i=i, NPh=NPh, nbk=nbk: e.matmul(pb[nbk][0:n, 1, 0:n], NPh[0:n, i, 0, 0:n], NPh[0:n, i, 1, 0:n], start=True, stop=True),
                         reads=[t_NP], writes=[t_bank[nbk]])
                    P.op("act", lambda e, i=i, NPh=NPh, nbk=nbk: e.activation(NPh[0:n, i + 1, :, 0:n], pb[nbk][0:n, 0:2, 0:n], AF.Identity),
                         reads=[t_bank[nbk]], writes=[t_NP])
        Xf = [Xb[h][cur[h]] for h in range(2)]
        tb = t_bank[B_PM]
        for h in range(2):
            hs = slice(64 * h, 64 * h + 64)
            Xh, t_Xh = Xf[h]
            for c in range(nch):
                P.op("pe", lambda e, hs=hs, c=c, h=h, Xh=Xh: e.matmul(pb[B_PM][0:64, h, 64 * c:64 * c + 64], Xh[0:n, 0:64], bbz[0:n, c, hs], start=True, stop=True),
                     reads=[t_Xh, t_bbz], writes=[tb])
        for h in range(2):
            for c in range(nch):
                P.op("dve", lambda e, h=h, c=c: e.scalar_tensor_tensor(M_[:, 2 * h + c, :], cst[0:64, 7, 0:64], PC[:, h, c:c + 1], pb[B_PM][0:64, h, 64 * c:64 * c + 64], ALU.mult, ALU.add),
                     reads=[tb, t_PC, t_cst], writes=[t_M])
        tb = t_bank[B_PM]
        for c in range(nch):
            ci = cb + c
            for h in range(2):
                hs = slice(64 * h, 64 * h + 64)
                vs = slice(256 + 64 * h, 256 + 64 * h + 64)
                Xh, t_Xh = Xf[h]
                P.op("pe", lambda e, h=h, c=c, ci=ci: e.matmul(pb[B_PM][0:64, 2 + h, 0:64], M_[:, 2 * h + c, :], S_all[h][:, ci, :], start=True, stop=False),
                     reads=[t_M, t_S[h][ci]], writes=[tb])
                P.op("pe", lambda e, h=h, hs=hs, c=c, Xh=Xh: e.matmul(pb[B_PM][0:64, 2 + h, 0:64], bbz[0:n, c, hs], Xh[0:n, 64:128], start=False, stop=False),
                     reads=[t_bbz, t_Xh], writes=[tb])
                P.op("pe", lambda e, h=h, hs=hs, vs=vs, c=c: e.matmul(pb[B_PM][0:64, 2 + h, 0:64], kbz[0:n, c, hs], x[0:n, vs], start=False, stop=True),
                     reads=[t_kbz, t_x], writes=[tb])
                P.op("act", lambda e, h=h, ci=ci: e.activation(S_all[h][:, ci + 1, :], pb[B_PM][0:64, 2 + h, 0:64], AF.Identity),
                     reads=[tb], writes=[t_S[h][ci + 1]])
        tb = t_bank[B_Y]
        for h in range(2):
            Xh, t_Xh = Xf[h]
            Gh, t_G = G[h]
            FM, t_FM = FMh[h]
            Rz, t_Rz = RHz[h]
            P.op("pe", lambda e, h=h, Xh=Xh, Gh=Gh: e.matmul(pb[B_Y][0:64, 1 + h, 0:n], Xh[0:n, 0:64], Gh[0:n, 1, 0:n], start=True, stop=True),
                 reads=[t_Xh, t_G], writes=[tb])
            for c in range(nch):
                cs = slice(64 * c, min(64 * c + 64, n))
                P.op("dve", lambda e, h=h, c=c, cs=cs, Rz=Rz, FM=FM: e.tensor_tensor(Rz[:, c, cs], pb[B_Y][0:64, 1 + h, cs], FM[:, 3, cs], ALU.add),
                     reads=[tb, t_FM], writes=[t_Rz])
        tb = t_bank[B_Y]
        for h in range(2):
            hs = slice(64 * h, 64 * h + 64)
            vs = slice(256 + 64 * h, 256 + 64 * h + 64)
            Xh, t_Xh = Xf[h]
            Gh, t_G = G[h]
            Rz, t_Rz = RHz[h]
            P.op("pe", lambda e, hs=hs, Xh=Xh, Gh=Gh: e.matmul(pb[B_Y][0:n, 0, hs], Gh[0:n, 1, 0:n], Xh[0:n, 64:128], start=True, stop=False),
                 reads=[t_G, t_Xh], writes=[tb])
            P.op("pe", lambda e, hs=hs, vs=vs, Gh=Gh: e.matmul(pb[B_Y][0:n, 0, hs], Gh[0:n, 3, 0:n], x[0:n, vs], start=False, stop=False),
                 reads=[t_G, t_x], writes=[tb])
            for c in range(nch):
                ci = cb + c
                P.op("pe", lambda e, h=h, hs=hs, ci=ci, c=c, Rz=Rz: e.matmul(pb[B_Y][0:n, 0, hs], Rz[:, c, 0:n], S_all[h][:, ci, :], start=False, stop=(c == nch - 1)),
                     reads=[t_Rz, t_S[h][ci]], writes=[tb])
        P.op("act", lambda e: e.activation(y_[0:n, :], pb[B_Y][0:n, 0, :], AF.Identity), reads=[tb], writes=[t_y])
        P.op("pool", lambda e: e.tensor_tensor(ysq[0:n, :], y_[0:n, :], y_[0:n, :], ALU.mult), reads=[t_y], writes=[t_ysq])
        for h in range(2):
            hs = slice(64 * h, 64 * h + 64)
            P.op("dve", lambda e, h=h, hs=hs: e.reduce_sum(st[0:n, h:h + 1], y_[0:n, hs], axis=AX.X), reads=[t_y], writes=[t_st])
            P.op("dve", lambda e, h=h, hs=hs: e.reduce_sum(st[0:n, 2 + h:3 + h], ysq[0:n, hs], axis=AX.X), reads=[t_ysq], writes=[t_st])
        P.op("dve", lambda e: e.tensor_scalar(st[0:n, 4:6], st[0:n, 0:2], 1.0 / 64, None, ALU.mult), reads=[t_st], writes=[t_st])
        P.op("dve", lambda e: e.tensor_tensor(st[0:n, 6:8], st[0:n, 4:6], st[0:n, 4:6], ALU.mult), reads=[t_st], writes=[t_st])
        P.op("dve", lambda e: e.scalar_tensor_tensor(st[0:n, 6:8], st[0:n, 2:4], 1.0 / 64, st[0:n, 6:8], ALU.mult, ALU.subtract), reads=[t_st], writes=[t_st])
        P.op("dve", lambda e: e.tensor_scalar(st[0:n, 6:8], st[0:n, 6:8], LNX_EPS, None, ALU.add), reads=[t_st], writes=[t_st])
        P.op("act", lambda e: e.activation(st[0:n, 6:8], st[0:n, 6:8], AF.Ln), reads=[t_st], writes=[t_st])
        P.op("act", lambda e: e.activation(st[0:n, 6:8], st[0:n, 6:8], AF.Exp, scale=-0.5), reads=[t_st], writes=[t_st])
        for h in range(2):
            hs = slice(64 * h, 64 * h + 64)
            P.op("dve", lambda e, h=h, hs=hs: e.tensor_scalar(yn[0:n, hs], y_[0:n, hs], st[0:n, 4 + h:5 + h], st[0:n, 6 + h:7 + h], ALU.subtract, ALU.mult),
                 reads=[t_y, t_st], writes=[t_yn])
        P.op("pool", lambda e: e.tensor_tensor(yn[0:n, :], yn[0:n, :], bc[0:n, 5, :], ALU.mult), reads=[t_yn, t_bc], writes=[t_yn])
        P.op("pool", lambda e: e.tensor_tensor(yn[0:n, :], yn[0:n, :], bc[0:n, 6, :], ALU.add), reads=[t_yn, t_bc], writes=[t_yn])
        for h in range(2):
            hs = slice(64 * h, 64 * h + 64)
            vs = slice(256 + 64 * h, 256 + 64 * h + 64)
            P.op("dve", lambda e, h=h, hs=hs, vs=vs: e.scalar_tensor_tensor(yn[0:n, hs], x[0:n, vs], bs[0:n, h:h + 1], yn[0:n, hs], ALU.mult, ALU.add),
                 reads=[t_x, t_bs, t_yn], writes=[t_yn])
        P.op("dve", lambda e: e.tensor_tensor(o_[0:n, :], yn[0:n, :], g_[0:n, :], ALU.mult), reads=[t_yn, t_g], writes=[t_o])
        P.dma("sp", o_d[t0:t0 + n, :], o_[0:n, :], reads=[t_o])

    streams = []
    for ti in range(NTX + 1):
        P.begin_stream()
        tile_body(ti)
        streams.append(P.end_stream())
    P.run_interleaved(streams, max_active=int(os.environ.get("RW_ACT", "2")))


def rwkv_host_inputs(p_rw, hp, prm):
    L = p_rw.shape[0]
    cs = slice(128 * hp, 128 * hp + 128)
    rkv = np.zeros((L + 1, 384), np.float32)
    rkv[1:, 0:128] = p_rw[:, 0:512][:, cs]
    rkv[1:, 128:256] = p_rw[:, 512:1024][:, cs]
    rkv[1:, 256:384] = p_rw[:, 1024:1536][:, cs]
    lo = np.zeros((256, L + 1), np.float32)
    lo[:, 1:] = p_rw[:, 1536:1792].T
    mu = prm["rwkv_mu"]
    mu_tm = np.concatenate([mu[0:512][cs], mu[512:1024][cs], mu[1024:1536][cs]])
    mu_tm = np.ascontiguousarray(np.broadcast_to(mu_tm[None, :], (128, 384)))
    mu_fm = np.zeros((128, 3), np.float32)
    mu_fm[0:64, 0] = mu[1536:1600]
    mu_fm[0:64, 1] = mu[1600:1664]
    mu_fm[:, 2] = mu[1664:1792]
    rows = [prm["w0"][cs], prm["a0"][cs], prm["k_k"][cs], prm["k_a"][cs], prm["r_k"].reshape(-1)[cs],
            prm["lnx_g"][cs], prm["lnx_b"][cs]]
    bc = np.ascontiguousarray(np.broadcast_to(np.stack(rows)[None], (128, 7, 128)))
    wa_up = np.ascontiguousarray(np.concatenate([prm["w_up"][:, cs], prm["a_up"][:, cs]], axis=0))
    g_up = np.ascontiguousarray(prm["g_up"][:, cs])
    return {"rkv": rkv, "lo": lo, "mu_tm": mu_tm, "mu_fm": mu_fm, "bc": bc, "wa_up": wa_up, "g_up": g_up,
            "cst": rwkv_consts()}


ALPHA = float((2 * 2) ** 0.25)
LN_EPS = 1e-5
NTOK = N_META + 2048
HALVES = [(0, [(0, 16), (16, 512), (528, 512)], [(0, 16)] + [(16 + 128 * i, 128) for i in range(8)]),
          (1040, [(0, 512), (512, 512)], [(128 * i, 128) for i in range(8)])]
C_IN = 5376


def tok_consts():
    c = np.zeros((128, 2, 128), np.float32)
    c[:, 0, :] = 1.0 / 1024
    c[:, 1, :] = np.eye(128)
    sel = np.zeros((16, 16, 128), np.float32)
    for e in range(16):
        sel[e, e, :] = 1.0
    return c, sel


def build_tok(nc, mode, do_proj, ntok=NTOK, halves=HALVES, n_exp=16):
    D = {}

    def inp(name, shape):
        D[name] = nc.dram_tensor(name, list(shape), F32, kind="ExternalInput").ap()

    def outp(name, shape):
        D[name] = nc.dram_tensor(name, list(shape), F32, kind="ExternalOutput").ap()

    inp("xT", [1024, ntok])
    inp("tc", [128, 2, 128])
    inp("lnA", [128, 2, 8])
    if mode == "C":
        inp("osbT", [512, ntok]); inp("orwT", [512, ntok]); inp("gT", [2048, ntok])
        inp("p_sb", [512, 1024]); inp("p_rw", [512, 1024]); inp("w_out", [1024, 1024])
        inp("lnB", [128, 2, 8])
        inp("router_w", [1024, 16]); inp("rb", [128, 16]); inp("sel", [16, 16, 128])
        inp("wg", [16, 1024, 512]); inp("wu", [16, 1024, 512]); inp("wd", [16, 512, 1024])
    if do_proj:
        inp("w_in", [1024, C_IN])
        outp("pT", [C_IN, ntok])
    outp("hT", [1024, ntok])
    with ExitStack() as es:
        P = Prog(nc, es)
        emit_tok(P, D, mode, do_proj, halves, n_exp)
        P.finish()
    return nc


def emit_tok(P, D, mode, do_proj, halves, n_exp=16):
    WMAX = 1040
    tc = P.sb("tc", [128, 2, 128]); t_tc = P.T(const=True)
    P.dma("sp", tc[:], D["tc"], writes=[t_tc])
    lnA = P.sb("lnA", [128, 2, 8]); t_lnA = P.T(const=True)
    P.dma("sp", lnA[:], D["lnA"], writes=[t_lnA])
    hT = P.sb("hT", [128, 8, WMAX]); hbf = P.sb("hbf", [128, 8, WMAX], BF16)
    NG = 3
    t_h = [P.T(f"h{g}") for g in range(NG)]
    t_hb = [P.T(f"hb{g}") for g in range(NG)]
    NSTG = 4
    stg = [P.sb(f"stg{i}", [128, 2048]) for i in range(NSTG)]
    t_stg = [P.T() for _ in range(NSTG)]
    WQ = ["sp", "act"]
    ps = [P.ps(f"ps{i}") for i in range(8)]
    t_ps = [P.T(f"psb{i}", excl=True) for i in range(8)]
    mean_sb = P.sb("mean_sb", [128, 512]); t_mean = P.T()
    rstd_sb = P.sb("rstd_sb", [128, 512]); t_rstd = P.T()
    tmp = [P.sb(f"tmp{i}", [128, 512]) for i in range(2)]
    t_tmp = [P.T(), P.T()]
    cnt = {"stg": 0, "tmp": 0, "ev": 0}

    def ln_group(gi, c0, w, lnp, t_lnp):
        PM, PX = 6, 7
        for k in range(8):
            s = cnt["tmp"] % 2; cnt["tmp"] += 1
            P.op("act", lambda e, k=k, s=s: e.activation(tmp[s][:, 0:w], hT[:, k, c0:c0 + w], AF.Square),
                 reads=[t_h[gi]], writes=[t_tmp[s]])
            P.op("pe", lambda e, k=k: e.matmul(ps[PM][:, 0:w], tc[:, 0, :], hT[:, k, c0:c0 + w], start=(k == 0), stop=(k == 7)),
                 reads=[t_tc, t_h[gi]], writes=[t_ps[PM]])
            P.op("pe", lambda e, k=k, s=s: e.matmul(ps[PX][:, 0:w], tc[:, 0, :], tmp[s][:, 0:w], start=(k == 0), stop=(k == 7)),
                 reads=[t_tc, t_tmp[s]], writes=[t_ps[PX]])
        P.op("act", lambda e: e.activation(mean_sb[:, 0:w], ps[PM][:, 0:w], AF.Identity), reads=[t_ps[PM]], writes=[t_mean])
        P.op("dve", lambda e: e.tensor_tensor(rstd_sb[:, 0:w], mean_sb[:, 0:w], mean_sb[:, 0:w], ALU.mult), reads=[t_mean], writes=[t_rstd])
        P.op("dve", lambda e: e.tensor_tensor(rstd_sb[:, 0:w], ps[PX][:, 0:w], rstd_sb[:, 0:w], ALU.subtract), reads=[t_ps[PX], t_rstd], writes=[t_rstd])
        P.op("dve", lambda e: e.tensor_scalar(rstd_sb[:, 0:w], rstd_sb[:, 0:w], LN_EPS, None, ALU.add), reads=[t_rstd], writes=[t_rstd])
        P.op("act", lambda e: e.activation(rstd_sb[:, 0:w], rstd_sb[:, 0:w], AF.Ln), reads=[t_rstd], writes=[t_rstd])
        P.op("act", lambda e: e.activation(rstd_sb[:, 0:w], rstd_sb[:, 0:w], AF.Exp, scale=-0.5), reads=[t_rstd], writes=[t_rstd])
        for k in range(8):
            s = cnt["tmp"] % 2; cnt["tmp"] += 1
            P.op("dve", lambda e, k=k, s=s: e.tensor_tensor(tmp[s][:, 0:w], hT[:, k, c0:c0 + w], mean_sb[:, 0:w], ALU.subtract),
                 reads=[t_h[gi], t_mean], writes=[t_tmp[s]])
            P.op("dve", lambda e, s=s: e.tensor_tensor(tmp[s][:, 0:w], tmp[s][:, 0:w], rstd_sb[:, 0:w], ALU.mult),
                 reads=[t_tmp[s], t_rstd], writes=[t_tmp[s]])
            P.op("act", lambda e, k=k, s=s: e.activation(hT[:, k, c0:c0 + w], tmp[s][:, 0:w], AF.Identity, bias=lnp[:, 1, k:k + 1], scale=lnp[:, 0, k:k + 1]),
                 reads=[t_tmp[s], t_lnp], writes=[t_h[gi]])
            P.op("act", lambda e, k=k, s=s: e.activation(hbf[:, k, c0:c0 + w], tmp[s][:, 0:w], AF.Identity, bias=lnp[:, 1, k:k + 1], scale=lnp[:, 0, k:k + 1]),
                 reads=[t_tmp[s], t_lnp], writes=[t_hb[gi]])

    if do_proj:
        wpj = [P.sb(f"wpj{i}", [128, 8, 128], BF16) for i in range(2)]
        t_wpj = [P.T(), P.T()]
        ost = [P.sb(f"ost{i}", [128, 512]) for i in range(4)]
        t_ost = [P.T() for _ in range(4)]
        w_in_v = D["w_in"].rearrange("(k p) c -> p k c", p=128)

    def proj(groups, tok0):
        for j in range(C_IN // 128):
            s = cnt["stg"] % NSTG; wq = WQ[cnt["stg"] % 2]; cnt["stg"] += 1
            wb = j % 2
            P.dma(wq, stg[s][:, 0:1024].rearrange("p (k c) -> p k c", k=8), w_in_v[:, :, 128 * j:128 * j + 128], writes=[t_stg[s]])
            P.op("act", lambda e, s=s, wb=wb: e.activation(wpj[wb][:], stg[s][:, 0:1024].rearrange("p (k c) -> p k c", k=8), AF.Identity),
                 reads=[t_stg[s]], writes=[t_wpj[wb]])
            for gi, (c0, w) in enumerate(groups):
                bk = cnt["ev"] % 4
                for k in range(8):
                    P.op("pe", lambda e, k=k, wb=wb, bk=bk, c0=c0, w=w: e.matmul(ps[bk][:, 0:w], wpj[wb][:, k, :], hbf[:, k, c0:c0 + w], start=(k == 0), stop=(k == 7)),
                         reads=[t_wpj[wb], t_hb[gi]], writes=[t_ps[bk]])
                eng = "dve"
                cnt["ev"] += 1
                if eng == "act":
                    P.op("act", lambda e, bk=bk, w=w: e.activation(ost[bk][:, 0:w], ps[bk][:, 0:w], AF.Identity), reads=[t_ps[bk]], writes=[t_ost[bk]])
                else:
                    P.op("dve", lambda e, bk=bk, w=w: e.tensor_copy(ost[bk][:, 0:w], ps[bk][:, 0:w]), reads=[t_ps[bk]], writes=[t_ost[bk]])
                P.dma("sp", D["pT"][128 * j:128 * j + 128, tok0 + c0:tok0 + c0 + w], ost[bk][:, 0:w], reads=[t_ost[bk]])

    if mode == "C":
        lnB = P.sb("lnB", [128, 2, 8]); t_lnB = P.T(const=True)
        P.dma("sp", lnB[:], D["lnB"], writes=[t_lnB])
        rw = P.sb("rw", [128, 8, 16]); t_rw = P.T(const=True)
        P.dma("sp", rw[:], D["router_w"].rearrange("(k p) e -> p k e", p=128), writes=[t_rw])
        rb = P.sb("rb", [128, 16]); t_rb = P.T(const=True)
        P.dma("sp", rb[:], D["rb"], writes=[t_rb])
        sel = P.sb("sel", [16, 16, 128]); t_sel = P.T(const=True)
        P.dma("sp", sel[:], D["sel"], writes=[t_sel])
        arena = [P.sb(f"arena{i}", [128, 8192], BF16) for i in range(2)]
        t_ar = [P.T("arena0"), P.T("arena1")]
        ob = [P.sb(f"ob{i}", [128, 4, 512], BF16) for i in range(2)]
        t_ob = [P.T(), P.T()]
        gts = [P.sb(f"gts{i}", [128, 512]) for i in range(4)]
        t_gts = [P.T() for _ in range(4)]
        merged = P.sb("merged", [128, 8, 512], BF16); t_merged = P.T()
        combT = P.sb("combT", [16, WMAX]); t_combT = P.T()
        cbc = [P.sb(f"cbc{i}", [128, WMAX]) for i in range(2)]
        t_cbc = [P.T(), P.T()]
        hid = [P.sb(f"hid{i}", [128, 2, 512], BF16) for i in range(2)]
        t_hid = [P.T(), P.T()]
        sgl = [P.sb(f"sgl{i}", [128, 512]) for i in range(2)]
        t_sgl = [P.T(), P.T()]
        rt = P.sb("rt", [128, 16, 16]); t_rt = P.T()
        rs = P.sb("rs", [128, 16]); t_rs = P.T()
        osb_v = D["osbT"].rearrange("(k p) t -> p k t", p=128)
        orw_v = D["orwT"].rearrange("(k p) t -> p k t", p=128)
        psb_v = D["p_sb"].rearrange("(k p) c -> p k c", p=128)
        prw_v = D["p_rw"].rearrange("(k p) c -> p k c", p=128)
        wout_v = D["w_out"].rearrange("(k p) c -> p k c", p=128)
        psb_bf = arena[0][:, 0:4096].rearrange("p (k c) -> p k c", k=4)
        prw_bf = arena[0][:, 4096:8192].rearrange("p (k c) -> p k c", k=4)
        wout_bf = arena[1][:, 0:8192].rearrange("p (k c) -> p k c", k=8)

    xT_v = D["xT"].rearrange("(k p) t -> p k t", p=128)
    hTo_v = D["hT"].rearrange("(k p) t -> p k t", p=128)

    for (tok0, groups, rtiles) in halves:
        for gi, (c0, w) in enumerate(groups):
            P.dma("sp", hT[:, :, c0:c0 + w], xT_v[:, :, tok0 + c0:tok0 + c0 + w], writes=[t_h[gi]])
        if mode == "A":
            for gi, (c0, w) in enumerate(groups):
                ln_group(gi, c0, w, lnA, t_lnA)
        else:
            for (src, dst, ai, nk) in ((psb_v, psb_bf, 0, 4), (prw_v, prw_bf, 0, 4), (wout_v, wout_bf, 1, 8)):
                for k0 in range(0, nk, 2):
                    s = cnt["stg"] % NSTG; wq = WQ[cnt["stg"] % 2]; cnt["stg"] += 1
                    P.dma(wq, stg[s][:, 0:2048].rearrange("p (k c) -> p k c", k=2), src[:, k0:k0 + 2, :], writes=[t_stg[s]])
                    P.op("act", lambda e, s=s, dst=dst, k0=k0: e.activation(dst[:, k0:k0 + 2, :], stg[s][:, 0:2048].rearrange("p (k c) -> p k c", k=2), AF.Identity),
                         reads=[t_stg[s]], writes=[t_ar[ai]])
            for gi, (c0, w) in enumerate(groups):
                for (src, oi) in ((osb_v, 0), (orw_v, 1)):
                    s = cnt["stg"] % NSTG; wq = WQ[cnt["stg"] % 2]; cnt["stg"] += 1
                    P.dma(wq, stg[s][:, 0:4 * w].rearrange("p (k c) -> p k c", k=4), src[:, :, tok0 + c0:tok0 + c0 + w], writes=[t_stg[s]])
                    P.op("dve", lambda e, s=s, oi=oi, w=w: e.tensor_copy(ob[oi][:, :, 0:w], stg[s][:, 0:4 * w].rearrange("p (k c) -> p k c", k=4)),
                         reads=[t_stg[s]], writes=[t_ob[oi]])
                for m in range(8):
                    ms = slice(128 * m, 128 * m + 128)
                    ba, bb = 0 + (m % 2) * 2, 1 + (m % 2) * 2
                    for k in range(4):
                        P.op("pe", lambda e, k=k, ms=ms, ba=ba, w=w: e.matmul(ps[ba][:, 0:w], psb_bf[:, k, ms], ob[0][:, k, 0:w], start=(k == 0), stop=(k == 3)),
                             reads=[t_ar[0], t_ob[0]], writes=[t_ps[ba]])
                    for k in range(4):
                        P.op("pe", lambda e, k=k, ms=ms, bb=bb, w=w: e.matmul(ps[bb][:, 0:w], prw_bf[:, k, ms], ob[1][:, k, 0:w], start=(k == 0), stop=(k == 3)),
                             reads=[t_ar[0], t_ob[1]], writes=[t_ps[bb]])
                    g0, g1 = (m % 2) * 2, (m % 2) * 2 + 1
                    P.dma("sp", gts[g0][:, 0:w], D["gT"][128 * m:128 * m + 128, tok0 + c0:tok0 + c0 + w], writes=[t_gts[g0]])
                    P.dma("sp", gts[g1][:, 0:w], D["gT"][1024 + 128 * m:1024 + 128 * m + 128, tok0 + c0:tok0 + c0 + w], writes=[t_gts[g1]])
                    P.op("act", lambda e, g0=g0, w=w: e.activation(gts[g0][:, 0:w], gts[g0][:, 0:w], AF.Sigmoid), reads=[t_gts[g0]], writes=[t_gts[g0]])
                    P.op("act", lambda e, g1=g1, w=w: e.activation(gts[g1][:, 0:w], gts[g1][:, 0:w], AF.Sigmoid), reads=[t_gts[g1]], writes=[t_gts[g1]])
                    P.op("dve", lambda e, g0=g0, ba=ba, w=w: e.tensor_tensor(gts[g0][:, 0:w], ps[ba][:, 0:w], gts[g0][:, 0:w], ALU.mult),
                         reads=[t_ps[ba], t_gts[g0]], writes=[t_gts[g0]])
                    P.op("dve", lambda e, g1=g1, bb=bb, w=w: e.tensor_tensor(gts[g1][:, 0:w], ps[bb][:, 0:w], gts[g1][:, 0:w], ALU.mult),
                         reads=[t_ps[bb], t_gts[g1]], writes=[t_gts[g1]])
                    P.op("pool", lambda e, g0=g0, g1=g1, m=m, w=w: e.tensor_tensor(merged[:, m, 0:w], gts[g0][:, 0:w], gts[g1][:, 0:w], ALU.add),
                         reads=[t_gts[g0], t_gts[g1]], writes=[t_merged])
                for m in range(8):
                    ms = slice(128 * m, 128 * m + 128)
                    bk = 4 + (m % 2)
                    for k in range(8):
                        P.op("pe", lambda e, k=k, ms=ms, bk=bk, w=w: e.matmul(ps[bk][:, 0:w], wout_bf[:, k, ms], merged[:, k, 0:w], start=(k == 0), stop=(k == 7)),
                             reads=[t_ar[1], t_merged], writes=[t_ps[bk]])
                    P.op("dve", lambda e, m=m, bk=bk, c0=c0, w=w: e.scalar_tensor_tensor(hT[:, m, c0:c0 + w], hT[:, m, c0:c0 + w], ALPHA, ps[bk][:, 0:w], ALU.mult, ALU.add),
                         reads=[t_ps[bk], t_h[gi]], writes=[t_h[gi]])
                ln_group(gi, c0, w, lnA, t_lnA)
            for (r0, nt) in rtiles:
                gi = [i for i, (c0, w) in enumerate(groups) if c0 <= r0 < c0 + w][0]
                RB = 5
                for k in range(8):
                    P.op("pe", lambda e, k=k, r0=r0, nt=nt: e.matmul(ps[RB][0:nt, 0:16], hT[:, k, r0:r0 + nt], rw[:, k, :], start=(k == 0), stop=(k == 7)),
                         reads=[t_h[gi], t_rw], writes=[t_ps[RB]])
                R = lambda i: rt[0:nt, i, :]
                S = lambda i: rs[0:nt, i:i + 1]

                def dv(fn, nt=nt):
                    P.op("dve", fn, reads=[t_rt, t_rs], writes=[t_rt, t_rs])
                P.op("dve", lambda e, nt=nt: e.tensor_tensor(rt[0:nt, 0, :], ps[RB][0:nt, 0:16], rb[0:nt, :], ALU.add),
                     reads=[t_ps[RB], t_rb], writes=[t_rt])
                dv(lambda e, nt=nt: e.reduce_max(rs[0:nt, 0:1], rt[0:nt, 0, :], axis=AX.X))
                dv(lambda e, nt=nt: e.tensor_scalar(rs[0:nt, 0:1], rs[0:nt, 0:1], -1.0, None, ALU.mult))
                P.op("act", lambda e, nt=nt: e.activation(rt[0:nt, 1, :], rt[0:nt, 0, :], AF.Exp, bias=rs[0:nt, 0:1]),
                     reads=[t_rt, t_rs], writes=[t_rt])
                dv(lambda e, nt=nt: e.reduce_sum(rs[0:nt, 1:2], rt[0:nt, 1, :], axis=AX.X))
                dv(lambda e, nt=nt: e.reciprocal(rs[0:nt, 1:2], rs[0:nt, 1:2]))
                dv(lambda e, nt=nt: e.tensor_scalar(rt[0:nt, 2, :], rt[0:nt, 1, :], rs[0:nt, 1:2], None, ALU.mult))
                for g in range(4):
                    dv(lambda e, nt=nt, g=g: e.reduce_max(rt[0:nt, 3, g:g + 1], rt[0:nt, 2, 4 * g:4 * g + 4], axis=AX.X))
                for g in range(4):
                    dv(lambda e, nt=nt, g=g: e.tensor_scalar(rt[0:nt, 4, 4 * g:4 * g + 4], rt[0:nt, 2, 4 * g:4 * g + 4], rt[0:nt, 3, g:g + 1], None, ALU.is_equal))
                dv(lambda e, nt=nt: e.scalar_tensor_tensor(rt[0:nt, 5, :], rt[0:nt, 4, :], -2.0, rt[0:nt, 2, :], ALU.mult, ALU.add))
                for g in range(4):
                    dv(lambda e, nt=nt, g=g: e.reduce_max(rt[0:nt, 3, 4 + g:5 + g], rt[0:nt, 5, 4 * g:4 * g + 4], axis=AX.X))
                dv(lambda e, nt=nt: e.tensor_tensor(rt[0:nt, 3, 8:12], rt[0:nt, 3, 0:4], rt[0:nt, 3, 4:8], ALU.add))
                dv(lambda e, nt=nt: e.reduce_max(rs[0:nt, 2:3], rt[0:nt, 3, 8:12], axis=AX.X))
                dv(lambda e, nt=nt: e.tensor_scalar(rt[0:nt, 3, 12:16], rt[0:nt, 3, 8:12], rs[0:nt, 2:3], None, ALU.is_equal))
                for g in range(4):
                    dv(lambda e, nt=nt, g=g: e.tensor_scalar(rt[0:nt, 6, 4 * g:4 * g + 4], rt[0:nt, 2, 4 * g:4 * g + 4], 1.0, rt[0:nt, 3, 12 + g:13 + g], ALU.add, ALU.mult))
                dv(lambda e, nt=nt: e.tensor_scalar(rt[0:nt, 6, :], rt[0:nt, 6, :], -1.0, None, ALU.add))
                dv(lambda e, nt=nt: e.reduce_max(rs[0:nt, 3:4], rt[0:nt, 6, :], axis=AX.X))
                dv(lambda e, nt=nt: e.tensor_scalar(rt[0:nt, 7, :], rt[0:nt, 6, :], rs[0:nt, 3:4], None, ALU.is_equal))
                dv(lambda e, nt=nt: e.scalar_tensor_tensor(rt[0:nt, 8, :], rt[0:nt, 7, :], -2.0, rt[0:nt, 6, :], ALU.mult, ALU.add))
                dv(lambda e, nt=nt: e.reduce_max(rs[0:nt, 4:5], rt[0:nt, 8, :], axis=AX.X))
                dv(lambda e, nt=nt: e.tensor_scalar(rt[0:nt, 9, :], rt[0:nt, 8, :], rs[0:nt, 4:5], None, ALU.is_equal))
                dv(lambda e, nt=nt: e.tensor_tensor(rs[0:nt, 5:6], rs[0:nt, 3:4], rs[0:nt, 4:5], ALU.add))
                dv(lambda e, nt=nt: e.reciprocal(rs[0:nt, 5:6], rs[0:nt, 5:6]))
                dv(lambda e, nt=nt: e.tensor_tensor(rs[0:nt, 6:7], rs[0:nt, 3:4], rs[0:nt, 5:6], ALU.mult))
                dv(lambda e, nt=nt: e.tensor_tensor(rs[0:nt, 7:8], rs[0:nt, 4:5], rs[0:nt, 5:6], ALU.mult))
                dv(lambda e, nt=nt: e.tensor_scalar(rt[0:nt, 10, :], rt[0:nt, 7, :], rs[0:nt, 6:7], None, ALU.mult))
                dv(lambda e, nt=nt: e.scalar_tensor_tensor(rt[0:nt, 11, :], rt[0:nt, 9, :], rs[0:nt, 7:8], rt[0:nt, 10, :], ALU.mult, ALU.add))
                P.op("pe", lambda e, nt=nt: e.transpose(ps[RB][0:16, 256:256 + nt], rt[0:nt, 11, :], tc[0:nt, 1, 0:nt]),
                     reads=[t_rt, t_tc], writes=[t_ps[RB]])
                P.op("act", lambda e, nt=nt, r0=r0: e.activation(combT[:, r0:r0 + nt], ps[RB][0:16, 256:256 + nt], AF.Identity),
                     reads=[t_ps[RB]], writes=[t_combT])
            for gi, (c0, w) in enumerate(groups):
                P.op("pool", lambda e, c0=c0, w=w: e.tensor_scalar(hT[:, :, c0:c0 + w], hT[:, :, c0:c0 + w], ALPHA, None, ALU.mult),
                     reads=[t_h[gi]], writes=[t_h[gi]])
            nhe = 0
            for ex in range(n_exp):
                cb_ = ex % 2
                for gi, (c0, w) in enumerate(groups):
                    P.op("pe", lambda e, ex=ex, c0=c0, w=w: e.matmul(ps[6][:, 0:w], sel[:, ex, :], combT[:, c0:c0 + w], start=True, stop=True),
                         reads=[t_sel, t_combT], writes=[t_ps[6]])
                    P.op("act", lambda e, cb_=cb_, c0=c0, w=w: e.activation(cbc[cb_][:, c0:c0 + w], ps[6][:, 0:w], AF.Identity),
                         reads=[t_ps[6]], writes=[t_cbc[cb_]])
                for hf in range(2):
                    ai = nhe % 2
                    nhe += 1
                    wg_bf = arena[ai][:, 0:2048].rearrange("p (k c) -> p k c", k=8)
                    wu_bf = arena[ai][:, 2048:4096].rearrange("p (k c) -> p k c", k=8)
                    wd_bf = arena[ai][:, 4096:6144].rearrange("p (k c) -> p k c", k=2)
                    fs = slice(256 * hf, 256 * hf + 256)
                    for (src, dst, kk_) in ((D["wg"][ex].rearrange("(k p) f -> p k f", p=128)[:, :, fs], wg_bf, 8),
                                            (D["wu"][ex].rearrange("(k p) f -> p k f", p=128)[:, :, fs], wu_bf, 8),
                                            (D["wd"][ex, 256 * hf:256 * hf + 256, :].rearrange("(k p) d -> p k d", p=128), wd_bf, 2)):
                        s = cnt["stg"] % NSTG; wq = WQ[cnt["stg"] % 2]; cnt["stg"] += 1
                        P.dma(wq, stg[s][:, 0:2048].rearrange("p (k c) -> p k c", k=kk_), src, writes=[t_stg[s]])
                        P.op("act", lambda e, s=s, dst=dst, kk_=kk_: e.activation(dst, stg[s][:, 0:2048].rearrange("p (k c) -> p k c", k=kk_), AF.Identity),
                             reads=[t_stg[s]], writes=[t_ar[ai]])
                    for gi, (c0, w) in enumerate(groups):
                        hb_ = cnt["ev"] % 2
                        cnt["ev"] += 1
                        for fc in range(2):
                            bg, bu = fc, 2 + fc
                            fcs = slice(128 * fc, 128 * fc + 128)
                            for k in range(8):
                                P.op("pe", lambda e, k=k, bg=bg, fcs=fcs, c0=c0, w=w, wg_bf=wg_bf: e.matmul(ps[bg][:, 0:w], wg_bf[:, k, fcs], hbf[:, k, c0:c0 + w], start=(k == 0), stop=(k == 7)),
                                     reads=[t_ar[ai], t_hb[gi]], writes=[t_ps[bg]])
                            for k in range(8):
                                P.op("pe", lambda e, k=k, bu=bu, fcs=fcs, c0=c0, w=w, wu_bf=wu_bf: e.matmul(ps[bu][:, 0:w], wu_bf[:, k, fcs], hbf[:, k, c0:c0 + w], start=(k == 0), stop=(k == 7)),
                                     reads=[t_ar[ai], t_hb[gi]], writes=[t_ps[bu]])
                            P.op("act", lambda e, fc=fc, bg=bg, w=w: e.activation(sgl[fc][:, 0:w], ps[bg][:, 0:w], AF.Silu),
                                 reads=[t_ps[bg]], writes=[t_sgl[fc]])
                            P.op("dve", lambda e, fc=fc, bu=bu, w=w: e.tensor_tensor(sgl[fc][:, 0:w], ps[bu][:, 0:w], sgl[fc][:, 0:w], ALU.mult),
                                 reads=[t_ps[bu], t_sgl[fc]], writes=[t_sgl[fc]])
                            P.op("dve", lambda e, fc=fc, hb_=hb_, cb_=cb_, c0=c0, w=w: e.tensor_tensor(hid[hb_][:, fc, 0:w], sgl[fc][:, 0:w], cbc[cb_][:, c0:c0 + w], ALU.mult),
                                 reads=[t_sgl[fc], t_cbc[cb_]], writes=[t_hid[hb_]])
                        for m in range(8):
                            bd = 4 + (m % 2)
                            ms = slice(128 * m, 128 * m + 128)
                            for fc in range(2):
                                P.op("pe", lambda e, fc=fc, bd=bd, ms=ms, hb_=hb_, w=w, wd_bf=wd_bf: e.matmul(ps[bd][:, 0:w], wd_bf[:, fc, ms], hid[hb_][:, fc, 0:w], start=(fc == 0), stop=(fc == 1)),
                                     reads=[t_ar[ai], t_hid[hb_]], writes=[t_ps[bd]])
                            P.op("dve", lambda e, m=m, bd=bd, c0=c0, w=w: e.tensor_tensor(hT[:, m, c0:c0 + w], ps[bd][:, 0:w], hT[:, m, c0:c0 + w], ALU.add),
                                 reads=[t_ps[bd], t_h[gi]], writes=[t_h[gi]])
            for gi, (c0, w) in enumerate(groups):
                ln_group(gi, c0, w, lnB, t_lnB)
        for gi, (c0, w) in enumerate(groups):
            P.dma("sp", hTo_v[:, :, tok0 + c0:tok0 + c0 + w], hT[:, :, c0:c0 + w], reads=[t_h[gi]])
        if do_proj:
            proj(groups, tok0)


NCORES = 8
SEQ = 8192
LFULL = N_META + SEQ


def _fm(v):
    return np.ascontiguousarray(np.asarray(v, np.float32).reshape(8, 128).T)


def _run(nc, in_maps):
    res = run_bass_kernel_spmd(nc, in_maps, core_ids=list(range(NCORES)))
    return res.results


def _new_nc():
    return bass.Bass("TRN2", target_bir_lowering=False)


def _mixers(pT_cores, prm):
    amask = attn_masks()
    attn_maps, rwkv_maps = [], []
    for c in range(NCORES):
        b, hp = c // 4, c % 4
        pb_ = np.concatenate([pT_cores[4 * b][:, 0:N_META]] + [pT_cores[4 * b + r][:, N_META:] for r in range(4)], axis=1)
        q = pb_[128 * hp:128 * hp + 128].reshape(2, 64, LFULL)
        k = pb_[512 + 128 * hp:512 + 128 * hp + 128].reshape(2, 64, LFULL)
        v = pb_[1024 + 128 * hp:1024 + 128 * hp + 128].reshape(2, 64, LFULL)
        vtm = v.transpose(0, 2, 1)
        vm = np.ascontiguousarray(vtm[:, 0:N_META])
        vx = np.ascontiguousarray(vtm[:, N_META:].reshape(2, SEQ // 128, 128, 64).transpose(0, 2, 1, 3))
        attn_maps.append({"qT": np.ascontiguousarray(q), "kT": np.ascontiguousarray(k), "vx": vx, "vm": vm, "msk": amask})
        p_rw = np.ascontiguousarray(pb_[1536:3328].T)
        rwkv_maps.append(rwkv_host_inputs(p_rw, hp, prm))
    nc = _new_nc()
    build_attn(nc, 2, SEQ // 512)
    ares = _run(nc, attn_maps)
    nc = _new_nc()
    build_rwkv(nc, SEQ // 128)
    rres = _run(nc, rwkv_maps)
    osbT, orwT = [], []
    for b in range(2):
        osbT.append(np.concatenate([ares[4 * b + hp]["oT"].reshape(128, LFULL) for hp in range(4)], axis=0))
        orwT.append(np.concatenate([rres[4 * b + hp]["o_rw"].T for hp in range(4)], axis=0))
    return osbT, orwT


def _core_cols(full, r):
    return np.ascontiguousarray(np.concatenate([full[:, 0:N_META], full[:, N_META + 2048 * r:N_META + 2048 * (r + 1)]], axis=1))


def kernel(**inputs):
    inp = {k: np.asarray(v) for k, v in inputs.items()}
    x, meta = inp["x"].astype(np.float32), inp["meta"].astype(np.float32)
    tcc, sel = tok_consts()
    maps = []
    for c in range(NCORES):
        b, r = c // 4, c % 4
        xT = np.ascontiguousarray(np.concatenate([meta.T, x[b, 2048 * r:2048 * (r + 1)].T], axis=1))
        maps.append({"xT": xT, "tc": tcc, "lnA": np.stack([_fm(inp["emb_ln_g"]), _fm(inp["emb_ln_b"])], axis=1),
                     "w_in": np.ascontiguousarray(inp["w_in"][0])})
    nc = _new_nc()
    build_tok(nc, "A", True)
    res = _run(nc, maps)
    hT_c = [r_["hT"] for r_ in res]
    pT_c = [r_["pT"] for r_ in res]
    for l in range(2):
        prm = {k: inp[k][l] for k in ["rwkv_mu", "w0", "w_up", "a0", "a_up", "g_up", "k_k", "k_a", "r_k", "lnx_g", "lnx_b"]}
        osbT, orwT = _mixers(pT_c, prm)
        last = (l == 1)
        maps = []
        for c in range(NCORES):
            b, r = c // 4, c % 4
            m = {"xT": hT_c[c], "tc": tcc, "lnA": np.stack([_fm(inp["ln1_g"][l]), _fm(inp["ln1_b"][l])], axis=1),
                 "osbT": _core_cols(osbT[b], r), "orwT": _core_cols(orwT[b], r),
                 "gT": np.ascontiguousarray(pT_c[c][3328:5376]),
                 "p_sb": np.ascontiguousarray(inp["p_sb"][l]), "p_rw": np.ascontiguousarray(inp["p_rwkv"][l]),
                 "w_out": np.ascontiguousarray(inp["w_out"][l]),
                 "lnB": np.stack([_fm(inp["ln2_g"][l]), _fm(inp["ln2_b"][l])], axis=1),
                 "router_w": np.ascontiguousarray(inp["router_w"]),
                 "rb": np.ascontiguousarray(np.broadcast_to(inp["router_b"][None].astype(np.float32), (128, 16))),
                 "sel": sel,
                 "wg": np.ascontiguousarray(inp["exp_w_gate"][l]), "wu": np.ascontiguousarray(inp["exp_w_up"][l]),
                 "wd": np.ascontiguousarray(inp["exp_w_down"][l])}
            if not last:
                m["w_in"] = np.ascontiguousarray(inp["w_in"][l + 1])
            maps.append(m)
        nc = _new_nc()
        build_tok(nc, "C", not last)
        res = _run(nc, maps)
        hT_c = [r_["hT"] for r_ in res]
        if not last:
            pT_c = [r_["pT"] for r_ in res]
    out = np.zeros((2, SEQ, 1024), np.float32)
    for c in range(NCORES):
        b, r = c // 4, c % 4
        out[b, 2048 * r:2048 * (r + 1)] = hT_c[c][:, N_META:].T
    return out
```

```python
import numpy as np
from contextlib import ExitStack
import concourse.bass as bass
import concourse.mybir as mybir
from concourse.bass_utils import run_bass_kernel_spmd

F32 = mybir.dt.float32
BF16 = mybir.dt.bfloat16
AF = mybir.ActivationFunctionType
ALU = mybir.AluOpType
AX = mybir.AxisListType

import os
BUDGET = int(os.environ["KBUDGET"]) if "KBUDGET" in os.environ else None
ENGS = ["pe", "act", "dve", "pool", "sp"]
DMA_R = 8


class T:
    __slots__ = ("name", "lw", "rd", "const", "excl")

    def __init__(self, name, const=False, excl=False):
        self.name = name
        self.lw = None
        self.rd = []
        self.const = const
        self.excl = excl


class Prog:
    def __init__(self, nc, es):
        self.nc = nc
        self.es = es
        self.items = {e: [] for e in ENGS}
        self.sem = {e: es.enter_context(nc.semaphore("s_" + e)) for e in ENGS}
        self.cnt = {e: 0 for e in ENGS}
        self.seen = {e: {} for e in ENGS}
        self.dsem = {}
        self.dma_n = {}
        self.dma_final = {}
        self.ntile = 0

    def sb(self, name, shape, dt=F32):
        return self.es.enter_context(self.nc.sbuf_tensor("sb_" + name, list(shape), dt))

    def ps(self, name, shape=(128, 512), dt=F32):
        return self.es.enter_context(self.nc.psum_tensor("pp_" + name, list(shape), dt))

    def T(self, name=None, const=False, excl=False):
        self.ntile += 1
        return T(name or f"t{self.ntile}", const, excl)

    def _deps(self, eng, reads, writes):
        deps = {}

        def add(p):
            if p is None:
                return
            k, v = p
            if deps.get(k, 0) < v:
                deps[k] = v

        for t in reads:
            add(t.lw)
        for t in writes:
            add(t.lw)
            for p in t.rd:
                add(p)
        waits = []
        for k, v in deps.items():
            if k == eng and eng == "pe":
                continue
            if self.seen[eng].get(k, 0) >= v:
                continue
            self.seen[eng][k] = v
            waits.append((k, v))
        return waits

    def _commit(self, pid, reads, writes):
        for t in writes:
            t.lw = pid
            t.rd = []
        for t in reads:
            if not t.const:
                t.rd.append(pid)
                if len(t.rd) > 64:
                    m = {}
                    for k, v in t.rd:
                        if m.get(k, 0) < v:
                            m[k] = v
                    t.rd = list(m.items())

    def begin_stream(self):
        self._rec = []

    def end_stream(self):
        r, self._rec = self._rec, None
        return r

    def mark(self):
        if getattr(self, "_rec", None) is not None:
            self._rec.append(("mark", ()))

    def run_interleaved(self, streams, max_active=2):
        streams = list(streams)
        active = []
        nxt = 0
        while nxt < len(streams) or active:
            if nxt < len(streams) and len(active) < max_active and (not active or active[0][2]):
                active.append([streams[nxt], 0, False])
                nxt += 1
            for idx, a in enumerate(list(active)):
                if a[1] >= len(a[0]):
                    continue
                kind, args = a[0][a[1]]
                if kind == "mark":
                    if idx > 0 and active[0] is not a and active[0][1] < len(active[0][0]):
                        continue
                    a[2] = True
                    a[1] += 1
                    continue
                a[1] += 1
                (self.op if kind == "op" else self.dma)(*args)
            active = [a for a in active if a[1] < len(a[0])]

    def op(self, eng, fn, reads=(), writes=()):
        if getattr(self, "_rec", None) is not None:
            self._rec.append(("op", (eng, fn, tuple(reads), tuple(writes))))
            return
        self.nops = getattr(self, "nops", 0) + 1
        if BUDGET is not None and self.nops > BUDGET:
            return
        ex = [t for t in reads if t.excl]
        if ex:
            reads = [t for t in reads if not t.excl]
            writes = list(writes) + ex
        waits = self._deps(eng, reads, writes)
        self.cnt[eng] += 1
        pid = (eng, self.cnt[eng])
        self.items[eng].append((waits, fn, True))
        self._commit(pid, reads, writes)

    def dma(self, q, out_ap, in_ap, reads=(), writes=()):
        if getattr(self, "_rec", None) is not None:
            self._rec.append(("dma", (q, out_ap, in_ap, tuple(reads), tuple(writes))))
            return
        self.nops = getattr(self, "nops", 0) + 1
        if BUDGET is not None and self.nops > BUDGET:
            return
        if q not in self.dsem:
            self.dsem[q] = [self.es.enter_context(self.nc.semaphore(f"d_{q}_{i}")) for i in range(DMA_R)]
            self.dma_n[q] = 0
        n = self.dma_n[q]
        self.dma_n[q] += 1
        i = n % DMA_R
        rnd = n // DMA_R
        key = ("d", q, i)
        waits = self._deps(q, reads, writes)
        if rnd > 0 and self.seen[q].get(key, 0) < 16 * rnd:
            self.seen[q][key] = 16 * rnd
            waits.append((key, 16 * rnd))
        sem = self.dsem[q][i]

        def fn(e, out_ap=out_ap, in_ap=in_ap, sem=sem):
            return e.dma_start(out=out_ap, in_=in_ap).then_inc(sem, 16)

        self.items[q].append((waits, fn, False))
        pid = (key, 16 * (rnd + 1))
        self.dma_final[key] = 16 * (rnd + 1)
        self._commit(pid, reads, writes)

    def _semobj(self, k):
        if isinstance(k, tuple):
            return self.dsem[k[1]][k[2]]
        return self.sem[k]

    def finish(self):
        waits = []
        for key, v in self.dma_final.items():
            waits.append((key, v))
        for e in ENGS:
            if e != "sp" and self.cnt[e] > 0:
                waits.append((e, self.cnt[e]))
        self.items["sp"].append((waits, None, False))
        nc = self.nc
        with nc.Block() as block:
            def replay(name, e):
                for waits, fn, inc in self.items[name]:
                    for k, v in waits:
                        e.wait_ge(self._semobj(k), v)
                    if fn is None:
                        continue
                    ins = fn(e)
                    if inc:
                        ins.then_inc(self.sem[name], 1)

            @block.tensor
            def _(e):
                replay("pe", e)

            @block.scalar
            def _(e):
                replay("act", e)

            @block.vector
            def _(e):
                replay("dve", e)

            @block.gpsimd
            def _(e):
                replay("pool", e)

            @block.sync
            def _(e):
                replay("sp", e)


N_META = 16


def build_attn(nc, NH, NG):
    NB = 4 * NG
    L = N_META + 512 * NG
    qT = nc.dram_tensor("qT", [NH, 64, L], F32, kind="ExternalInput").ap()
    kT = nc.dram_tensor("kT", [NH, 64, L], F32, kind="ExternalInput").ap()
    vx = nc.dram_tensor("vx", [NH, 128, NB, 64], F32, kind="ExternalInput").ap()
    vm = nc.dram_tensor("vm", [NH, 16, 64], F32, kind="ExternalInput").ap()
    msk = nc.dram_tensor("msk", [128, 3, 128], F32, kind="ExternalInput").ap()
    oT = nc.dram_tensor("oT", [NH, 64, L], F32, kind="ExternalOutput").ap()
    with ExitStack() as es:
        P = Prog(nc, es)
        emit_attn(P, NH, NG, qT, kT, vx, vm, msk, oT)
        P.finish()
    return nc


def emit_attn(P, NH, NG, qT, kT, vx, vm, msk, oT):
    NB = 4 * NG
    L = N_META + 512 * NG
    mstage = P.sb("mstage", [128, 3, 128], F32)
    t_mstage = P.T()
    P.dma("sp", mstage[:], msk, writes=[t_mstage])
    cm = P.sb("cm", [128, 3, 128], BF16)
    t_cm = P.T(const=True)
    P.op("dve", lambda e: e.tensor_copy(cm[:], mstage[:]), reads=[t_mstage], writes=[t_cm])
    zeros = P.sb("zeros", [128, 64], BF16)
    t_zeros = P.T(const=True)
    P.op("pool", lambda e: e.memset(zeros[:], 0.0), writes=[t_zeros])

    QT = P.sb("QT", [64, L], BF16)
    KT = P.sb("KT", [64, L], BF16)
    V = P.sb("V", [128, NB, 64], BF16)
    VM = P.sb("VM", [16, 64], BF16)
    t_Q, t_K, t_V = P.T(), P.T(), P.T()
    CH = 2064 if L > 2064 else L
    stg = [P.sb(f"stg{i}", [128, CH], F32) for i in range(2)]
    t_stg = [P.T(), P.T()]
    vstg = P.sb("vstg", [128, NB, 64], F32)
    t_vstg = P.T()
    vmstg = P.sb("vmstg", [16, 64], F32)
    t_vmstg = P.T()

    zA = [P.ps(f"zA{i}") for i in range(2)]
    zB = [P.ps(f"zB{i}") for i in range(2)]
    Ops = [P.ps(f"Ops{i}") for i in range(2)]
    t_zA = [P.T(excl=True), P.T(excl=True)]
    t_zB = [P.T(excl=True), P.T(excl=True)]
    t_O = [P.T(excl=True), P.T(excl=True)]
    e_sb = [P.sb(f"e{i}", [128, 512], F32) for i in range(2)]
    t_e = [P.T(), P.T()]
    sp_sb = [P.sb(f"sp{i}", [128, 512], BF16) for i in range(2)]
    t_sp = [P.T(), P.T()]
    A_sb = [P.sb(f"A{i}", [128, 512], BF16) for i in range(2)]
    t_A = [P.T(), P.T()]
    acc = P.sb("acc", [128, 512], F32)
    t_acc = P.T()
    accb = [P.sb(f"accb{i}", [128, 512], BF16) for i in range(3)]
    t_accb = [P.T(), P.T(), P.T()]
    ost = [P.sb(f"ost{i}", [64, 512], F32) for i in range(2)]
    t_ost = [P.T(), P.T()]

    nstg = 0
    gcount = 0
    for h in range(NH):
        for (src, dst, tt, scale) in ((qT, QT, t_Q, 0.125), (kT, KT, t_K, 1.0)):
            first = True
            for c0 in range(0, L, CH):
                w = min(CH, L - c0)
                s = nstg % 2
                nstg += 1
                P.dma("sp", stg[s][0:64, 0:w], src[h, :, c0:c0 + w], writes=[t_stg[s]])
                if scale != 1.0:
                    P.op("dve", lambda e, s=s, w=w, c0=c0, dst=dst, scale=scale:
                         e.tensor_scalar(dst[:, c0:c0 + w], stg[s][0:64, 0:w], scale, None, ALU.mult),
                         reads=[t_stg[s]], writes=[tt])
                else:
                    P.op("pool", lambda e, s=s, w=w, c0=c0, dst=dst:
                         e.tensor_copy(dst[:, c0:c0 + w], stg[s][0:64, 0:w]),
                         reads=[t_stg[s]], writes=[tt])
        P.dma("sp", vstg[:], vx[h], writes=[t_vstg])
        P.op("pool", lambda e: e.tensor_copy(V[:], vstg[:]), reads=[t_vstg], writes=[t_V])
        P.dma("sp", vmstg[:], vm[h], writes=[t_vmstg])
        P.op("pool", lambda e: e.tensor_copy(VM[:], vmstg[:]), reads=[t_vmstg], writes=[t_V])

        for g in [-1] + list(range(NG)):
            if g < 0:
                q0, QW = 0, N_META
                blocks = [("m", 0)]
            else:
                q0, QW = N_META + 512 * g, 512
                blocks = [("x", j) for j in range(4 * g + 3, -1, -1)] + [("m", 0)]
            ob = gcount % 2
            gcount += 1
            P.op("pe", lambda e, ob=ob, QW=QW, q0=q0:
                 e.matmul(Ops[ob][0:64, 0:QW], zeros[0:64, 0:64], QT[:, q0:q0 + QW], start=True, stop=False),
                 reads=[t_zeros, t_Q], writes=[t_O[ob]])
            P.op("pool", lambda e: e.memset(acc[:], 0.0), writes=[t_acc])
            nblk = len(blocks)
            info = []
            for it, (kind, j) in enumerate(blocks):
                if kind == "m":
                    kp, k0 = N_META, 0
                    c0 = 0
                    diag = (g < 0)
                else:
                    kp, k0 = 128, N_META + 128 * j
                    jl = j - 4 * g
                    diag = jl >= 0
                    c0 = 128 * jl if diag else 0
                info.append((kind, j, kp, k0, c0, diag))

            def prm(it, info=info, q0=q0, QW=QW):
                kind, j, kp, k0, c0, diag = info[it]
                W = QW - c0
                return kind, j, kp, k0, c0, diag, it % 2, W, slice(q0 + c0, q0 + QW), min(128, W)

            def pe_zA(it):
                kind, j, kp, k0, c0, diag, b, W, qs, dw = prm(it)
                P.op("pe", lambda e: e.matmul(zA[b][0:kp, 0:W], KT[:, k0:k0 + kp], QT[:, qs], start=True, stop=True),
                     reads=[t_K, t_Q], writes=[t_zA[b]])

            def act_e(it):
                kind, j, kp, k0, c0, diag, b, W, qs, dw = prm(it)
                P.op("act", lambda e: e.activation(e_sb[b][0:kp, 0:W], zA[b][0:kp, 0:W], AF.Exp),
                     reads=[t_zA[b]], writes=[t_e[b]])

            def act_sp(it, nblk=nblk, QW=QW):
                kind, j, kp, k0, c0, diag, b, W, qs, dw = prm(it)
                P.op("act", lambda e: e.activation(sp_sb[b][0:kp, 0:W], e_sb[b][0:kp, 0:W], AF.Ln, bias=1.0),
                     reads=[t_e[b]], writes=[t_sp[b]])
                if diag:
                    P.op("pool", lambda e: e.tensor_tensor(sp_sb[b][0:kp, 0:dw], sp_sb[b][0:kp, 0:dw],
                                                           cm[0:kp, 0, 0:dw], ALU.mult),
                         reads=[t_sp[b], t_cm], writes=[t_sp[b]])
                if it < nblk - 1:
                    a3 = it % 3
                    P.op("dve", lambda e: e.tensor_tensor(acc[0:kp, c0:QW], acc[0:kp, c0:QW], sp_sb[b][0:kp, 0:W], ALU.add),
                         reads=[t_sp[b], t_acc], writes=[t_acc])
                    P.op("dve", lambda e: e.tensor_copy(accb[a3][:, 0:QW], acc[:, 0:QW]),
                         reads=[t_acc], writes=[t_accb[a3]])

            def pe_zB(it, QW=QW):
                kind, j, kp, k0, c0, diag, b, W, qs, dw = prm(it)
                last = (it == 0)
                P.op("pe", lambda e: e.matmul(zB[b][0:kp, 0:W], KT[:, k0:k0 + kp], QT[:, qs], start=True, stop=False),
                     reads=[t_K, t_Q], writes=[t_zB[b]])
                P.op("pe", lambda e: e.matmul(zB[b][0:kp, 0:W], cm[0:kp, 1, 0:kp], sp_sb[b][0:kp, 0:W],
                                              start=False, stop=last),
                     reads=[t_cm, t_sp[b]], writes=[t_zB[b]])
                if not last:
                    ab = (it - 1) % 3
                    P.op("pe", lambda e: e.matmul(zB[b][0:kp, 0:W], cm[0:128, 2, 0:kp], accb[ab][0:128, c0:QW],
                                                  start=False, stop=True),
                         reads=[t_cm, t_accb[ab]], writes=[t_zB[b]])

            def act_A(it):
                kind, j, kp, k0, c0, diag, b, W, qs, dw = prm(it)
                P.op("act", lambda e: e.activation(A_sb[b][0:kp, 0:W], zB[b][0:kp, 0:W], AF.Exp),
                     reads=[t_zB[b]], writes=[t_A[b]])
                if diag:
                    P.op("pool", lambda e: e.tensor_tensor(A_sb[b][0:kp, 0:dw], A_sb[b][0:kp, 0:dw],
                                                           cm[0:kp, 0, 0:dw], ALU.mult),
                         reads=[t_A[b], t_cm], writes=[t_A[b]])

            def pe_AV(it, ob=ob, nblk=nblk, QW=QW):
                kind, j, kp, k0, c0, diag, b, W, qs, dw = prm(it)
                vop = (VM[0:kp, :] if kind == "m" else V[:, j, :])
                P.op("pe", lambda e: e.matmul(Ops[ob][0:64, c0:QW], vop, A_sb[b][0:kp, 0:W],
                                              start=False, stop=(it == nblk - 1)),
                     reads=[t_V, t_A[b]], writes=[t_O[ob]])

            for s_ in range(-2, nblk + 2):
                if 0 <= s_ + 2 < nblk:
                    pe_zA(s_ + 2)
                if 0 <= s_ < nblk:
                    pe_zB(s_)
                if 0 <= s_ - 2 < nblk:
                    pe_AV(s_ - 2)
                if 0 <= s_ + 1 < nblk:
                    act_e(s_ + 1)
                if 0 <= s_ - 1 < nblk:
                    act_A(s_ - 1)
                if 0 <= s_ + 1 < nblk:
                    act_sp(s_ + 1)
            P.op("dve", lambda e, ob=ob, QW=QW: e.tensor_copy(ost[ob][:, 0:QW], Ops[ob][0:64, 0:QW]),
                 reads=[t_O[ob]], writes=[t_ost[ob]])
            P.dma("sp", oT[h, :, q0:q0 + QW], ost[ob][:, 0:QW], reads=[t_ost[ob]])


def attn_masks():
    s = np.arange(128)[:, None]
    t = np.arange(128)[None, :]
    m = np.zeros((128, 3, 128), np.float32)
    m[:, 0, :] = (s < t)
    m[:, 1, :] = -1.0 * (s >= t)
    m[:, 2, :] = -1.0
    return m


DECAY_SCALE = float(np.exp(-0.5))
LNX_EPS = 64e-5


def rwkv_consts():
    idx = np.arange(128)
    ch = idx // 64
    same = ch[:, None] == ch[None, :]
    le = idx[:, None] <= idx[None, :]
    lt = idx[:, None] < idx[None, :]
    c = np.zeros((128, 9, 128), np.float32)
    c[:, 0] = -DECAY_SCALE * (same & le)
    c[:, 1] = -DECAY_SCALE * same
    c[:, 2] = same & lt
    c[:, 3] = same & le
    c[:, 4] = same & lt
    c[:, 5] = same & le
    c[:, 6] = (same & lt).T
    c[:, 7] = np.eye(128)
    c[:, 8, 0] = -DECAY_SCALE * (ch == 0)
    c[:, 8, 1] = -DECAY_SCALE * (ch == 1)
    c[:, 8, 2] = (ch == 0)
    c[:, 8, 3] = (ch == 1)
    return c


def build_rwkv(nc, NTX):
    L = N_META + 128 * NTX
    D = {}
    D["rkv"] = nc.dram_tensor("rkv", [L + 1, 384], F32, kind="ExternalInput").ap()
    D["lo"] = nc.dram_tensor("lo", [256, L + 1], F32, kind="ExternalInput").ap()
    D["mu_tm"] = nc.dram_tensor("mu_tm", [128, 384], F32, kind="ExternalInput").ap()
    D["mu_fm"] = nc.dram_tensor("mu_fm", [128, 3], F32, kind="ExternalInput").ap()
    D["bc"] = nc.dram_tensor("bc", [128, 7, 128], F32, kind="ExternalInput").ap()
    D["wa_up"] = nc.dram_tensor("wa_up", [128, 128], F32, kind="ExternalInput").ap()
    D["g_up"] = nc.dram_tensor("g_up", [128, 128], F32, kind="ExternalInput").ap()
    D["cst"] = nc.dram_tensor("cst", [128, 9, 128], F32, kind="ExternalInput").ap()
    D["o"] = nc.dram_tensor("o_rw", [L, 128], F32, kind="ExternalOutput").ap()
    with ExitStack() as es:
        P = Prog(nc, es)
        emit_rwkv(P, NTX, D)
        P.finish()
    return nc


def emit_rwkv(P, NTX, D, pfx="rw"):
    DS = DECAY_SCALE
    NCH = 1 + 2 * NTX

    def const_load(name, shape, src):
        t = P.sb(pfx + name, shape, F32)
        tt = P.T(const=True)
        P.dma("sp", t[:], src, writes=[tt])
        return t, tt

    cst, t_cst = const_load("cst", [128, 9, 128], D["cst"])
    bc, t_bc = const_load("bc", [128, 7, 128], D["bc"])
    mu_tm, t_mutm = const_load("mu_tm", [128, 384], D["mu_tm"])
    mu_fm, t_mufm = const_load("mu_fm", [128, 3], D["mu_fm"])
    wup, t_wup = const_load("wup", [64, 128], D["wa_up"][0:64, :])
    aup, t_aup = const_load("aup", [64, 128], D["wa_up"][64:128, :])
    gup, t_gup = const_load("gup", [128, 128], D["g_up"])

    S_all = [P.sb(pfx + f"S_all{h}", [64, NCH + 1, 64], BF16) for h in range(2)]
    ident_bf = P.sb(pfx + "ident_bf", [128, 128], BF16)
    t_idb = P.T(const=True)
    P.op("pool", lambda e: e.tensor_copy(ident_bf[:], cst[:, 7, :]), reads=[t_cst], writes=[t_idb])
    t_S = [[P.T() for _ in range(NCH + 1)] for h in range(2)]
    for h in range(2):
        P.op("pool", lambda e, h=h: e.memset(S_all[h][:, 0, :], 0.0), writes=[t_S[h][0]])

    pb = [P.ps(pfx + f"pb{i}", [128, 4, 128], BF16 if i == 2 else F32) for i in range(8)]
    t_bank = [P.T("bank%d" % i, excl=True) for i in range(8)]

    names_sb = {
        "xa": [128, 384], "xs": [128, 384], "x": [128, 384],
        "la": [64, 128], "ls": [64, 128], "la2": [64, 128], "ls2": [64, 128], "ga": [128, 128], "gs": [128, 128],
        "lx": [64, 128], "lx2": [64, 128], "gx": [128, 128], "tw": [64, 128], "sg": [128, 128],
        "sigw": [128, 128], "a": [128, 128], "g": [128, 128],
        "kk": [128, 128], "sq": [128, 128], "ss": [128, 2], "rn": [128, 2],
        "kp": [128, 128], "t1": [128, 128], "bs": [128, 2],
        "cum": [128, 128], "c1": [128, 128], "c2": [128, 128],
        "ep": [128, 128], "em": [128, 128], "epm": [128, 128], "ebar": [128, 128], "ebz": [128, 2, 128],
        "PC": [64, 2, 2],
        "TM": [128, 4, 128],
        "kka": [128, 128], "bbz": [128, 2, 128], "kbz": [128, 2, 128],
        "FM0": [64, 4, 128], "FM1": [64, 4, 128],
        "G0": [128, 4, 128], "G1": [128, 4, 128],
        "NP0": [128, 6, 2, 128], "NP1": [128, 6, 2, 128],
        "X0a": [128, 128], "X0b": [128, 128], "X1a": [128, 128], "X1b": [128, 128],
        "M": [64, 4, 64], "RHz0": [64, 2, 128], "RHz1": [64, 2, 128],
        "y": [128, 128], "ysq": [128, 128], "st": [128, 8], "yn": [128, 128], "o": [128, 128],
    }
    BF_NAMES = {"TM", "bbz", "kbz", "FM0", "FM1", "G0", "G1", "NP0", "NP1", "X0a", "X0b", "X1a", "X1b", "M", "RHz0", "RHz1", "xvb"}
    names_sb["xvb"] = [128, 128]
    bufs = []
    for p in range(2):
        d = {}
        for nm, shp in names_sb.items():
            d[nm] = (P.sb(f"{pfx}{nm}_{p}", shp, BF16 if nm in BF_NAMES else F32), P.T(f"{nm}{p}"))
        bufs.append(d)
        for nm in ("RHz0", "RHz1"):
            t, tt = d[nm]
            P.op("pool", lambda e, t=t: e.memset(t[:], 0.0), writes=[tt])

    rkv, lo, o_d = D["rkv"], D["lo"], D["o"]
    B_LORA, B_CUM, B_FM, B_G, B_H0, B_H1, B_PM, B_Y = range(8)
    B_HH = [B_H0, B_H1]

    def tile_body(ti):
        n = 16 if ti == 0 else 128
        t0 = 0 if ti == 0 else N_META + 128 * (ti - 1)
        nch = 1 if ti == 0 else 2
        cb = 0 if ti == 0 else 1 + 2 * (ti - 1)
        B = bufs[ti % 2]

        def b(nm):
            return B[nm]

        xa, t_xa = b("xa"); xs, t_xs = b("xs"); x, t_x = b("x")
        la, t_la = b("la"); ls, t_ls = b("ls"); la2, t_la2 = b("la2"); ls2, t_ls2 = b("ls2")
        ga, t_ga = b("ga"); gs, t_gs = b("gs")
        lx, t_lx = b("lx"); lx2, t_lx2 = b("lx2"); gx, t_gx = b("gx"); tw, t_tw = b("tw"); sg, t_sg = b("sg")
        sigw, t_sigw = b("sigw"); a_, t_a = b("a"); g_, t_g = b("g")
        kk, t_kk = b("kk"); sq, t_sq = b("sq"); ss, t_ss = b("ss"); rn, t_rn = b("rn")
        kp, t_kp = b("kp"); t1, t_t1 = b("t1"); bs, t_bs = b("bs")
        cum, t_cum = b("cum"); c1, t_c1 = b("c1"); c2, t_c2 = b("c2")
        ep, t_ep = b("ep"); em, t_em = b("em"); epm, t_epm = b("epm"); ebar, t_ebar = b("ebar")
        ebz, t_ebz = b("ebz")
        PC, t_PC = b("PC"); TM, t_TM = b("TM"); kka, t_kka = b("kka")
        bbz, t_bbz = b("bbz"); kbz, t_kbz = b("kbz")
        FMh = [b("FM0"), b("FM1")]
        M_, t_M = b("M"); RHz = [b("RHz0"), b("RHz1")]
        y_, t_y = b("y"); ysq, t_ysq = b("ysq"); st, t_st = b("st"); yn, t_yn = b("yn"); o_, t_o = b("o")

        P.dma("sp", xa[0:n, :], rkv[1 + t0:1 + t0 + n, :], writes=[t_xa])
        P.dma("sp", xs[0:n, :], rkv[t0:t0 + n, :], writes=[t_xs])
        P.dma("sp", la[:, 0:n], lo[0:64, 1 + t0:1 + t0 + n], writes=[t_la])
        P.dma("sp", ls[:, 0:n], lo[0:64, t0:t0 + n], writes=[t_ls])
        P.dma("sp", la2[:, 0:n], lo[64:128, 1 + t0:1 + t0 + n], writes=[t_la2])
        P.dma("sp", ls2[:, 0:n], lo[64:128, t0:t0 + n], writes=[t_ls2])
        P.dma("sp", ga[:, 0:n], lo[128:256, 1 + t0:1 + t0 + n], writes=[t_ga])
        P.dma("sp", gs[:, 0:n], lo[128:256, t0:t0 + n], writes=[t_gs])
        P.op("pool", lambda e: e.tensor_tensor(xs[0:n, :], xs[0:n, :], xa[0:n, :], ALU.subtract),
             reads=[t_xa, t_xs], writes=[t_xs])
        P.op("pool", lambda e: e.tensor_tensor(xs[0:n, :], xs[0:n, :], mu_tm[0:n, :], ALU.mult),
             reads=[t_xs, t_mutm], writes=[t_xs])
        P.op("pool", lambda e: e.tensor_tensor(x[0:n, :], xs[0:n, :], xa[0:n, :], ALU.add),
             reads=[t_xa, t_xs], writes=[t_x])
        for (A_, tA, S_, tS, O_, tO, col, np_) in ((la, t_la, ls, t_ls, lx, t_lx, 0, 64), (la2, t_la2, ls2, t_ls2, lx2, t_lx2, 1, 64),
                                                  (ga, t_ga, gs, t_gs, gx, t_gx, 2, 128)):
            P.op("pool", lambda e, A_=A_, S_=S_: e.tensor_tensor(S_[:, 0:n], S_[:, 0:n], A_[:, 0:n], ALU.subtract),
                 reads=[tA, tS], writes=[tS])
            P.op("dve", lambda e, A_=A_, S_=S_, O_=O_, col=col, np_=np_: e.scalar_tensor_tensor(O_[:, 0:n], S_[:, 0:n], mu_fm[0:np_, col:col + 1], A_[:, 0:n], ALU.mult, ALU.add),
                 reads=[tA, tS, t_mufm], writes=[tO])
        xr = x[0:n, 0:128]
        xk = x[0:n, 128:256]
        xvb, t_xvb = b("xvb")
        P.op("pool", lambda e: e.tensor_copy(xvb[0:n, :], x[0:n, 256:384]), reads=[t_x], writes=[t_xvb])
        P.op("act", lambda e: e.activation(tw[:, 0:n], lx[:, 0:n], AF.Exp, scale=-2.0), reads=[t_lx], writes=[t_tw])
        P.op("dve", lambda e: e.tensor_scalar(tw[:, 0:n], tw[:, 0:n], 1.0, None, ALU.add), reads=[t_tw], writes=[t_tw])
        P.op("dve", lambda e: e.reciprocal(tw[:, 0:n], tw[:, 0:n]), reads=[t_tw], writes=[t_tw])
        P.op("dve", lambda e: e.tensor_scalar(tw[:, 0:n], tw[:, 0:n], 2.0, -1.0, ALU.mult, ALU.add), reads=[t_tw], writes=[t_tw])
        P.op("act", lambda e: e.activation(sg[:, 0:n], gx[:, 0:n], AF.Exp, scale=-1.0), reads=[t_gx], writes=[t_sg])
        P.op("dve", lambda e: e.tensor_scalar(sg[:, 0:n], sg[:, 0:n], 1.0, None, ALU.add), reads=[t_sg], writes=[t_sg])
        P.op("dve", lambda e: e.reciprocal(sg[:, 0:n], sg[:, 0:n]), reads=[t_sg], writes=[t_sg])
        tb = t_bank[B_LORA]
        P.op("pe", lambda e: e.matmul(pb[B_LORA][0:n, 0, :], tw[:, 0:n], wup[:, :], start=True, stop=True),
             reads=[t_tw, t_wup], writes=[tb])
        P.op("pe", lambda e: e.matmul(pb[B_LORA][0:n, 1, :], lx2[:, 0:n], aup[:, :], start=True, stop=True),
             reads=[t_lx2, t_aup], writes=[tb])
        P.op("pe", lambda e: e.matmul(pb[B_LORA][0:n, 2, :], sg[:, 0:n], gup[:, :], start=True, stop=True),
             reads=[t_sg, t_gup], writes=[tb])
        P.op("dve", lambda e: e.tensor_tensor(sigw[0:n, :], pb[B_LORA][0:n, 0, :], bc[0:n, 0, :], ALU.add),
             reads=[tb, t_bc], writes=[t_sigw])
        P.op("dve", lambda e: e.tensor_tensor(a_[0:n, :], pb[B_LORA][0:n, 1, :], bc[0:n, 1, :], ALU.add),
             reads=[tb, t_bc], writes=[t_a])
        P.op("act", lambda e: e.activation(g_[0:n, :], pb[B_LORA][0:n, 2, :], AF.Identity), reads=[tb], writes=[t_g])
        for (Z, tZ) in ((sigw, t_sigw), (a_, t_a)):
            P.op("act", lambda e, Z=Z: e.activation(Z[0:n, :], Z[0:n, :], AF.Exp, scale=-1.0), reads=[tZ], writes=[tZ])
            P.op("dve", lambda e, Z=Z: e.tensor_scalar(Z[0:n, :], Z[0:n, :], 1.0, None, ALU.add), reads=[tZ], writes=[tZ])
            P.op("dve", lambda e, Z=Z: e.reciprocal(Z[0:n, :], Z[0:n, :]), reads=[tZ], writes=[tZ])
        P.op("pool", lambda e: e.tensor_tensor(kk[0:n, :], xk, bc[0:n, 2, :], ALU.mult), reads=[t_x, t_bc], writes=[t_kk])
        P.op("pool", lambda e: e.tensor_tensor(sq[0:n, :], kk[0:n, :], kk[0:n, :], ALU.mult), reads=[t_kk], writes=[t_sq])
        for h in range(2):
            P.op("dve", lambda e, h=h: e.reduce_sum(ss[0:n, h:h + 1], sq[0:n, 64 * h:64 * h + 64], axis=AX.X),
                 reads=[t_sq], writes=[t_ss])
        P.op("dve", lambda e: e.tensor_scalar(rn[0:n, :], ss[0:n, :], 1e-24, None, ALU.max), reads=[t_ss], writes=[t_rn])
        P.op("act", lambda e: e.activation(rn[0:n, :], rn[0:n, :], AF.Ln), reads=[t_rn], writes=[t_rn])
        P.op("act", lambda e: e.activation(rn[0:n, :], rn[0:n, :], AF.Exp, scale=-0.5), reads=[t_rn], writes=[t_rn])
        for h in range(2):
            P.op("dve", lambda e, h=h: e.tensor_scalar(kk[0:n, 64 * h:64 * h + 64], kk[0:n, 64 * h:64 * h + 64],
                                                       rn[0:n, h:h + 1], None, ALU.mult),
                 reads=[t_kk, t_rn], writes=[t_kk])
        P.op("dve", lambda e: e.scalar_tensor_tensor(t1[0:n, :], a_[0:n, :], -1.0, bc[0:n, 3, :], ALU.add, ALU.mult),
             reads=[t_a, t_bc], writes=[t_t1])
        P.op("dve", lambda e: e.scalar_tensor_tensor(kp[0:n, :], t1[0:n, :], 1.0, xk, ALU.add, ALU.mult),
             reads=[t_t1, t_x], writes=[t_kp])
        P.op("pool", lambda e: e.tensor_tensor(t1[0:n, :], xr, kp[0:n, :], ALU.mult), reads=[t_x, t_kp, t_t1], writes=[t_t1])
        P.op("pool", lambda e: e.tensor_tensor(t1[0:n, :], t1[0:n, :], bc[0:n, 4, :], ALU.mult), reads=[t_t1, t_bc], writes=[t_t1])
        for h in range(2):
            P.op("dve", lambda e, h=h: e.reduce_sum(bs[0:n, h:h + 1], t1[0:n, 64 * h:64 * h + 64], axis=AX.X),
                 reads=[t_t1], writes=[t_bs])
        tb = t_bank[B_CUM]
        P.op("pe", lambda e: e.matmul(pb[B_CUM][0:n, 0, :], cst[0:n, 0, 0:n], sigw[0:n, :], start=True, stop=True),
             reads=[t_cst, t_sigw], writes=[tb])
        P.op("pe", lambda e: e.matmul(pb[B_CUM][0:n, 1, :], cst[0:n, 1, 0:n], sigw[0:n, :], start=True, stop=True),
             reads=[t_cst, t_sigw], writes=[tb])
        for h in range(2):
            P.op("pe", lambda e, h=h: e.matmul(pb[B_CUM][0:64, 2 + h, 0:2], sigw[0:n, 64 * h:64 * h + 64], cst[0:n, 8, 0:2], start=True, stop=True),
                 reads=[t_cst, t_sigw], writes=[tb])
        P.op("dve", lambda e: e.tensor_copy(cum[0:n, :], pb[B_CUM][0:n, 0, :]), reads=[tb], writes=[t_cum])
        P.op("dve", lambda e: e.scalar_tensor_tensor(c1[0:n, :], sigw[0:n, :], DS, cum[0:n, :], ALU.mult, ALU.add),
             reads=[t_sigw, t_cum], writes=[t_c1])
        P.op("dve", lambda e: e.tensor_tensor(c2[0:n, :], pb[B_CUM][0:n, 1, :], cum[0:n, :], ALU.subtract),
             reads=[tb, t_cum], writes=[t_c2])
        P.op("act", lambda e: e.activation(ep[0:n, :], cum[0:n, :], AF.Exp), reads=[t_cum], writes=[t_ep])
        P.op("act", lambda e: e.activation(em[0:n, :], cum[0:n, :], AF.Exp, scale=-1.0), reads=[t_cum], writes=[t_em])
        P.op("act", lambda e: e.activation(epm[0:n, :], c1[0:n, :], AF.Exp), reads=[t_c1], writes=[t_epm])
        P.op("act", lambda e: e.activation(ebar[0:n, :], c2[0:n, :], AF.Exp), reads=[t_c2], writes=[t_ebar])
        P.op("act", lambda e: e.activation(PC[:, :, :], pb[B_CUM][0:64, 2:4, 0:2], AF.Exp), reads=[tb], writes=[t_PC])
        P.op("dve", lambda e: e.tensor_tensor(kka[0:n, :], kk[0:n, :], a_[0:n, :], ALU.mult), reads=[t_kk, t_a], writes=[t_kka])
        P.op("dve", lambda e: e.tensor_tensor(TM[0:n, 0, :], kka[0:n, :], em[0:n, :], ALU.mult), reads=[t_kka, t_em], writes=[t_TM])
        P.op("dve", lambda e: e.tensor_tensor(TM[0:n, 1, :], kp[0:n, :], em[0:n, :], ALU.mult), reads=[t_kp, t_em], writes=[t_TM])
        P.op("dve", lambda e: e.scalar_tensor_tensor(TM[0:n, 2, :], kk[0:n, :], -1.0, epm[0:n, :], ALU.mult, ALU.mult),
             reads=[t_kk, t_epm], writes=[t_TM])
        P.op("dve", lambda e: e.tensor_tensor(TM[0:n, 3, :], xr, ep[0:n, :], ALU.mult), reads=[t_x, t_ep], writes=[t_TM])
        for c in range(nch):
            P.op("pool", lambda e, c=c: e.tensor_scalar(ebz[0:n, c, :], ebar[0:n, :], cst[0:n, 8, 2 + c:3 + c], None, ALU.mult),
                 reads=[t_ebar, t_cst], writes=[t_ebz])
            P.op("pool", lambda e, c=c: e.tensor_tensor(bbz[0:n, c, :], kka[0:n, :], ebz[0:n, c, :], ALU.mult),
                 reads=[t_kka, t_ebz], writes=[t_bbz])
            P.op("pool", lambda e, c=c: e.tensor_tensor(kbz[0:n, c, :], kp[0:n, :], ebz[0:n, c, :], ALU.mult),
                 reads=[t_kp, t_ebz], writes=[t_kbz])
        for h in range(2):
            FM, t_FM = FMh[h]
            bk = B_FM
            for s_ in range(4):
                P.op("pe", lambda e, s_=s_, h=h, bk=bk: e.transpose(pb[bk][0:64, s_, 0:n], TM[0:n, s_, 64 * h:64 * h + 64], ident_bf[0:n, 0:n]),
                     reads=[t_TM, t_idb], writes=[t_bank[bk]])
            P.op("act", lambda e, FM=FM, bk=bk: e.activation(FM[:, :, 0:n], pb[bk][0:64, :, 0:n], AF.Identity),
                 reads=[t_bank[bk]], writes=[t_FM])

        G = [b("G0"), b("G1")]
        NP = [b("NP0"), b("NP1")]
        Xb = [[b("X0a"), b("X0b")], [b("X1a"), b("X1b")]]
        for h in range(2):
            FM, t_FM = FMh[h]
            Gh, t_G = G[h]
            NPh, t_NP = NP[h]
            gbk = B_G
            gb = pb[gbk]
            for (so, sl, sr) in ((0, 0, 2), (1, 0, 3), (2, 1, 2), (3, 1, 3)):
                P.op("pe", lambda e, gb=gb, so=so, sl=sl, sr=sr, FM=FM: e.matmul(gb[0:n, so, 0:n], FM[:, sl, 0:n], FM[:, sr, 0:n], start=True, stop=True),
                     reads=[t_FM], writes=[t_bank[gbk]])
            nbk = B_CUM
            P.op("pe", lambda e, nbk=nbk, FM=FM, h=h: e.matmul(pb[nbk][0:n, 2 + h, 0:n], FM[:, 2, 0:n], FM[:, 0, 0:n], start=True, stop=True),
                 reads=[t_FM], writes=[t_bank[nbk]])
            P.op("dve", lambda e, gb=gb, Gh=Gh: e.tensor_tensor(Gh[0:n, :, 0:n], gb[0:n, :, 0:n], cst[0:n, 2:6, 0:n], ALU.mult),
                 reads=[t_bank[gbk], t_cst], writes=[t_G])
            P.op("dve", lambda e, nbk=nbk, NPh=NPh, h=h: e.tensor_tensor(NPh[0:n, 0, 1, 0:n], pb[nbk][0:n, 2 + h, 0:n], cst[0:n, 6, 0:n], ALU.mult),
                 reads=[t_bank[nbk], t_cst], writes=[t_NP])
            P.op("pool", lambda e, Gh=Gh, NPh=NPh: e.tensor_copy(NPh[0:n, 0, 0, 0:n], Gh[0:n, 0, 0:n]),
                 reads=[t_G], writes=[t_NP])
        P.mark()
        for h in range(2):
            hs = slice(64 * h, 64 * h + 64)
            vs = slice(256 + 64 * h, 256 + 64 * h + 64)
            Gh, t_G = G[h]
            xbk = B_HH[h]
            P.op("pe", lambda e, hs=hs, xbk=xbk: e.matmul(pb[xbk][0:n, 2, 0:64], ident_bf[0:n, 0:n], TM[0:n, 2, hs], start=True, stop=True),
                 reads=[t_TM, t_idb], writes=[t_bank[xbk]])
            P.op("pe", lambda e, hs=hs, xbk=xbk, Gh=Gh: e.matmul(pb[xbk][0:n, 2, 64:128], Gh[0:n, 2, 0:n], xvb[0:n, hs], start=True, stop=True),
                 reads=[t_G, t_xvb], writes=[t_bank[xbk]])
            X0, t_X0 = Xb[h][0]
            P.op("act", lambda e, xbk=xbk, X0=X0: e.activation(X0[0:n, :], pb[xbk][0:n, 2, :], AF.Identity),
                 reads=[t_bank[xbk]], writes=[t_X0])
        NPW = 6
        cur = [0, 0]
        for i in range(NPW):
            for h in range(2):
                NPh, t_NP = NP[h]
                nbk = B_HH[h]
                xbk = B_HH[h]
                Xc, t_Xc = Xb[h][cur[h]]
                Xn, t_Xn = Xb[h][1 - cur[h]]
                P.op("pe", lambda e, i=i, NPh=NPh, Xc=Xc, xbk=xbk: e.matmul(pb[xbk][0:n, 2, :], NPh[0:n, i, 0, 0:n], Xc[0:n, :], start=True, stop=True),
                     reads=[t_NP, t_Xc], writes=[t_bank[xbk]])
                P.op("dve", lambda e, Xc=Xc, Xn=Xn, xbk=xbk: e.tensor_tensor(Xn[0:n, :], pb[xbk][0:n, 2, :], Xc[0:n, :], ALU.add),
                     reads=[t_bank[xbk], t_Xc], writes=[t_Xn])
                cur[h] = 1 - cur[h]
                if i < NPW - 1:
                    P.op("pe", lambda e, i=i, NPh=NPh, nbk=nbk: e.matmul(pb[nbk][0:n, 0, 0:n], NPh[0:n, i, 1, 0:n], NPh[0:n, i, 0, 0:n], start=True, stop=True),
                         reads=[t_NP], writes=[t_bank[nbk]])
                    P.op("pe", lambda e, i=i, NPh=NPh, nbk=nbk: e.matmul(pb[nbk][0:n, 1, 0:n], NPh[0:n, i, 0, 0:n], NPh[0:n, i, 1, 0:n], start=True, stop=True),
                         reads=[t_NP], writes=[t_bank[nbk]])
                    P.op("act", lambda e, i=i, NPh=NPh, nbk=nbk: e.activation(NPh[0:n, i + 1, :, 0:n], pb[nbk][0:n, 0:2, 0:n], AF.Identity),
                         reads=[t_bank[nbk]], writes=[t_NP])
        Xf = [Xb[h][cur[h]] for h in range(2)]
        tb = t_bank[B_PM]
        for h in range(2):
            hs = slice(64 * h, 64 * h + 64)
            Xh, t_Xh = Xf[h]
            for c in range(nch):
                P.op("pe", lambda e, hs=hs, c=c, h=h, Xh=Xh: e.matmul(pb[B_PM][0:64, h, 64 * c:64 * c + 64], Xh[0:n, 0:64], bbz[0:n, c, hs], start=True, stop=True),
                     reads=[t_Xh, t_bbz], writes=[tb])
        for h in range(2):
            for c in range(nch):
                P.op("dve", lambda e, h=h, c=c: e.scalar_tensor_tensor(M_[:, 2 * h + c, :], cst[0:64, 7, 0:64], PC[:, h, c:c + 1], pb[B_PM][0:64, h, 64 * c:64 * c + 64], ALU.mult, ALU.add),
                     reads=[tb, t_PC, t_cst], writes=[t_M])
        tb = t_bank[B_PM]
        for c in range(nch):
            ci = cb + c
            for h in range(2):
                hs = slice(64 * h, 64 * h + 64)
                vs = slice(256 + 64 * h, 256 + 64 * h + 64)
                Xh, t_Xh = Xf[h]
                P.op("pe", lambda e, h=h, c=c, ci=ci: e.matmul(pb[B_PM][0:64, 2 + h, 0:64], M_[:, 2 * h + c, :], S_all[h][:, ci, :], start=True, stop=False),
                     reads=[t_M, t_S[h][ci]], writes=[tb])
                P.op("pe", lambda e, h=h, hs=hs, c=c, Xh=Xh: e.matmul(pb[B_PM][0:64, 2 + h, 0:64], bbz[0:n, c, hs], Xh[0:n, 64:128], start=False, stop=False),
                     reads=[t_bbz, t_Xh], writes=[tb])
                P.op("pe", lambda e, h=h, hs=hs, vs=vs, c=c: e.matmul(pb[B_PM][0:64, 2 + h, 0:64], kbz[0:n, c, hs], xvb[0:n, hs], start=False, stop=True),
                     reads=[t_kbz, t_xvb], writes=[tb])
                P.op("act", lambda e, h=h, ci=ci: e.activation(S_all[h][:, ci + 1, :], pb[B_PM][0:64, 2 + h, 0:64], AF.Identity),
                     reads=[tb], writes=[t_S[h][ci + 1]])
        tb = t_bank[B_Y]
        for h in range(2):
            Xh, t_Xh = Xf[h]
            Gh, t_G = G[h]
            FM, t_FM = FMh[h]
            Rz, t_Rz = RHz[h]
            P.op("pe", lambda e, h=h, Xh=Xh, Gh=Gh: e.matmul(pb[B_Y][0:64, 1 + h, 0:n], Xh[0:n, 0:64], Gh[0:n, 1, 0:n], start=True, stop=True),
                 reads=[t_Xh, t_G], writes=[tb])
            for c in range(nch):
                cs = slice(64 * c, min(64 * c + 64, n))
                P.op("dve", lambda e, h=h, c=c, cs=cs, Rz=Rz, FM=FM: e.tensor_tensor(Rz[:, c, cs], pb[B_Y][0:64, 1 + h, cs], FM[:, 3, cs], ALU.add),
                     reads=[tb, t_FM], writes=[t_Rz])
        tb = t_bank[B_Y]
        for h in range(2):
            hs = slice(64 * h, 64 * h + 64)
            vs = slice(256 + 64 * h, 256 + 64 * h + 64)
            Xh, t_Xh = Xf[h]
            Gh, t_G = G[h]
            Rz, t_Rz = RHz[h]
            P.op("pe", lambda e, hs=hs, Xh=Xh, Gh=Gh: e.matmul(pb[B_Y][0:n, 0, hs], Gh[0:n, 1, 0:n], Xh[0:n, 64:128], start=True, stop=False),
                 reads=[t_G, t_Xh], writes=[tb])
            P.op("pe", lambda e, hs=hs, vs=vs, Gh=Gh: e.matmul(pb[B_Y][0:n, 0, hs], Gh[0:n, 3, 0:n], xvb[0:n, hs], start=False, stop=False),
                 reads=[t_G, t_xvb], writes=[tb])
            for c in range(nch):
                ci = cb + c
                P.op("pe", lambda e, h=h, hs=hs, ci=ci, c=c, Rz=Rz: e.matmul(pb[B_Y][0:n, 0, hs], Rz[:, c, 0:n], S_all[h][:, ci, :], start=False, stop=(c == nch - 1)),
                     reads=[t_Rz, t_S[h][ci]], writes=[tb])
        P.op("act", lambda e: e.activation(y_[0:n, :], pb[B_Y][0:n, 0, :], AF.Identity), reads=[tb], writes=[t_y])
        P.op("pool", lambda e: e.tensor_tensor(ysq[0:n, :], y_[0:n, :], y_[0:n, :], ALU.mult), reads=[t_y], writes=[t_ysq])
        for h in range(2):
            hs = slice(64 * h, 64 * h + 64)
            P.op("dve", lambda e, h=h, hs=hs: e.reduce_sum(st[0:n, h:h + 1], y_[0:n, hs], axis=AX.X), reads=[t_y], writes=[t_st])
            P.op("dve", lambda e, h=h, hs=hs: e.reduce_sum(st[0:n, 2 + h:3 + h], ysq[0:n, hs], axis=AX.X), reads=[t_ysq], writes=[t_st])
        P.op("dve", lambda e: e.tensor_scalar(st[0:n, 4:6], st[0:n, 0:2], 1.0 / 64, None, ALU.mult), reads=[t_st], writes=[t_st])
        P.op("dve", lambda e: e.tensor_tensor(st[0:n, 6:8], st[0:n, 4:6], st[0:n, 4:6], ALU.mult), reads=[t_st], writes=[t_st])
        P.op("dve", lambda e: e.scalar_tensor_tensor(st[0:n, 6:8], st[0:n, 2:4], 1.0 / 64, st[0:n, 6:8], ALU.mult, ALU.subtract), reads=[t_st], writes=[t_st])
        P.op("dve", lambda e: e.tensor_scalar(st[0:n, 6:8], st[0:n, 6:8], LNX_EPS, None, ALU.add), reads=[t_st], writes=[t_st])
        P.op("act", lambda e: e.activation(st[0:n, 6:8], st[0:n, 6:8], AF.Ln), reads=[t_st], writes=[t_st])
        P.op("act", lambda e: e.activation(st[0:n, 6:8], st[0:n, 6:8], AF.Exp, scale=-0.5), reads=[t_st], writes=[t_st])
        for h in range(2):
            hs = slice(64 * h, 64 * h + 64)
            P.op("dve", lambda e, h=h, hs=hs: e.tensor_scalar(yn[0:n, hs], y_[0:n, hs], st[0:n, 4 + h:5 + h], st[0:n, 6 + h:7 + h], ALU.subtract, ALU.mult),
                 reads=[t_y, t_st], writes=[t_yn])
        P.op("pool", lambda e: e.tensor_tensor(yn[0:n, :], yn[0:n, :], bc[0:n, 5, :], ALU.mult), reads=[t_yn, t_bc], writes=[t_yn])
        P.op("pool", lambda e: e.tensor_tensor(yn[0:n, :], yn[0:n, :], bc[0:n, 6, :], ALU.add), reads=[t_yn, t_bc], writes=[t_yn])
        for h in range(2):
            hs = slice(64 * h, 64 * h + 64)
            vs = slice(256 + 64 * h, 256 + 64 * h + 64)
            P.op("dve", lambda e, h=h, hs=hs, vs=vs: e.scalar_tensor_tensor(yn[0:n, hs], x[0:n, vs], bs[0:n, h:h + 1], yn[0:n, hs], ALU.mult, ALU.add),
                 reads=[t_x, t_bs, t_yn], writes=[t_yn])
        P.op("dve", lambda e: e.tensor_tensor(o_[0:n, :], yn[0:n, :], g_[0:n, :], ALU.mult), reads=[t_yn, t_g], writes=[t_o])
        P.dma("sp", o_d[t0:t0 + n, :], o_[0:n, :], reads=[t_o])

    streams = []
    for ti in range(NTX + 1):
        P.begin_stream()
        tile_body(ti)
        streams.append(P.end_stream())
    P.run_interleaved(streams, max_active=int(os.environ.get("RW_ACT", "2")))


def rwkv_host_inputs(p_rw, hp, prm):
    L = p_rw.shape[0]
    cs = slice(128 * hp, 128 * hp + 128)
    rkv = np.zeros((L + 1, 384), np.float32)
    rkv[1:, 0:128] = p_rw[:, 0:512][:, cs]
    rkv[1:, 128:256] = p_rw[:, 512:1024][:, cs]
    rkv[1:, 256:384] = p_rw[:, 1024:1536][:, cs]
    lo = np.zeros((256, L + 1), np.float32)
    lo[:, 1:] = p_rw[:, 1536:1792].T
    mu = prm["rwkv_mu"]
    mu_tm = np.concatenate([mu[0:512][cs], mu[512:1024][cs], mu[1024:1536][cs]])
    mu_tm = np.ascontiguousarray(np.broadcast_to(mu_tm[None, :], (128, 384)))
    mu_fm = np.zeros((128, 3), np.float32)
    mu_fm[0:64, 0] = mu[1536:1600]
    mu_fm[0:64, 1] = mu[1600:1664]
    mu_fm[:, 2] = mu[1664:1792]
    rows = [prm["w0"][cs], prm["a0"][cs], prm["k_k"][cs], prm["k_a"][cs], prm["r_k"].reshape(-1)[cs],
            prm["lnx_g"][cs], prm["lnx_b"][cs]]
    bc = np.ascontiguousarray(np.broadcast_to(np.stack(rows)[None], (128, 7, 128)))
    wa_up = np.ascontiguousarray(np.concatenate([prm["w_up"][:, cs], prm["a_up"][:, cs]], axis=0))
    g_up = np.ascontiguousarray(prm["g_up"][:, cs])
    return {"rkv": rkv, "lo": lo, "mu_tm": mu_tm, "mu_fm": mu_fm, "bc": bc, "wa_up": wa_up, "g_up": g_up,
            "cst": rwkv_consts()}


ALPHA = float((2 * 2) ** 0.25)
LN_EPS = 1e-5
NTOK = N_META + 2048
HALVES = [(0, [(0, 16), (16, 512), (528, 512)], [(0, 16)] + [(16 + 128 * i, 128) for i in range(8)]),
          (1040, [(0, 512), (512, 512)], [(128 * i, 128) for i in range(8)])]
C_IN = 5376


def tok_consts():
    c = np.zeros((128, 2, 128), np.float32)
    c[:, 0, :] = 1.0 / 1024
    c[:, 1, :] = np.eye(128)
    sel = np.zeros((16, 16, 128), np.float32)
    for e in range(16):
        sel[e, e, :] = 1.0
    return c, sel


def build_tok(nc, mode, do_proj, ntok=NTOK, halves=HALVES, n_exp=16):
    D = {}

    def inp(name, shape):
        D[name] = nc.dram_tensor(name, list(shape), F32, kind="ExternalInput").ap()

    def outp(name, shape):
        D[name] = nc.dram_tensor(name, list(shape), F32, kind="ExternalOutput").ap()

    inp("xT", [1024, ntok])
    inp("tc", [128, 2, 128])
    inp("lnA", [128, 2, 8])
    if mode == "C":
        inp("osbT", [512, ntok]); inp("orwT", [512, ntok]); inp("gT", [2048, ntok])
        inp("p_sb", [512, 1024]); inp("p_rw", [512, 1024]); inp("w_out", [1024, 1024])
        inp("lnB", [128, 2, 8])
        inp("router_w", [1024, 16]); inp("rb", [128, 16]); inp("sel", [16, 16, 128])
        inp("wg", [16, 1024, 512]); inp("wu", [16, 1024, 512]); inp("wd", [16, 512, 1024])
    if do_proj:
        inp("w_in", [1024, C_IN])
        outp("pT", [C_IN, ntok])
    outp("hT", [1024, ntok])
    with ExitStack() as es:
        P = Prog(nc, es)
        emit_tok(P, D, mode, do_proj, halves, n_exp)
        P.finish()
    return nc


def emit_tok(P, D, mode, do_proj, halves, n_exp=16):
    WMAX = 1040
    tc = P.sb("tc", [128, 2, 128]); t_tc = P.T(const=True)
    P.dma("sp", tc[:], D["tc"], writes=[t_tc])
    lnA = P.sb("lnA", [128, 2, 8]); t_lnA = P.T(const=True)
    P.dma("sp", lnA[:], D["lnA"], writes=[t_lnA])
    hT = P.sb("hT", [128, 8, WMAX]); hbf = P.sb("hbf", [128, 8, WMAX], BF16)
    NG = 3
    t_h = [P.T(f"h{g}") for g in range(NG)]
    t_hb = [P.T(f"hb{g}") for g in range(NG)]
    NSTG = 4
    stg = [P.sb(f"stg{i}", [128, 2048]) for i in range(NSTG)]
    t_stg = [P.T() for _ in range(NSTG)]
    WQ = ["sp", "act"]
    ps = [P.ps(f"ps{i}") for i in range(8)]
    t_ps = [P.T(f"psb{i}", excl=True) for i in range(8)]
    mean_sb = P.sb("mean_sb", [128, 512]); t_mean = P.T()
    rstd_sb = P.sb("rstd_sb", [128, 512]); t_rstd = P.T()
    tmp = [P.sb(f"tmp{i}", [128, 512]) for i in range(2)]
    t_tmp = [P.T(), P.T()]
    cnt = {"stg": 0, "tmp": 0, "ev": 0}

    def ln_group(gi, c0, w, lnp, t_lnp):
        PM, PX = 6, 7
        for k in range(8):
            s = cnt["tmp"] % 2; cnt["tmp"] += 1
            P.op("act", lambda e, k=k, s=s: e.activation(tmp[s][:, 0:w], hT[:, k, c0:c0 + w], AF.Square),
                 reads=[t_h[gi]], writes=[t_tmp[s]])
            P.op("pe", lambda e, k=k: e.matmul(ps[PM][:, 0:w], tc[:, 0, :], hT[:, k, c0:c0 + w], start=(k == 0), stop=(k == 7)),
                 reads=[t_tc, t_h[gi]], writes=[t_ps[PM]])
            P.op("pe", lambda e, k=k, s=s: e.matmul(ps[PX][:, 0:w], tc[:, 0, :], tmp[s][:, 0:w], start=(k == 0), stop=(k == 7)),
                 reads=[t_tc, t_tmp[s]], writes=[t_ps[PX]])
        P.op("act", lambda e: e.activation(mean_sb[:, 0:w], ps[PM][:, 0:w], AF.Identity), reads=[t_ps[PM]], writes=[t_mean])
        P.op("dve", lambda e: e.tensor_tensor(rstd_sb[:, 0:w], mean_sb[:, 0:w], mean_sb[:, 0:w], ALU.mult), reads=[t_mean], writes=[t_rstd])
        P.op("dve", lambda e: e.tensor_tensor(rstd_sb[:, 0:w], ps[PX][:, 0:w], rstd_sb[:, 0:w], ALU.subtract), reads=[t_ps[PX], t_rstd], writes=[t_rstd])
        P.op("dve", lambda e: e.tensor_scalar(rstd_sb[:, 0:w], rstd_sb[:, 0:w], LN_EPS, None, ALU.add), reads=[t_rstd], writes=[t_rstd])
        P.op("act", lambda e: e.activation(rstd_sb[:, 0:w], rstd_sb[:, 0:w], AF.Ln), reads=[t_rstd], writes=[t_rstd])
        P.op("act", lambda e: e.activation(rstd_sb[:, 0:w], rstd_sb[:, 0:w], AF.Exp, scale=-0.5), reads=[t_rstd], writes=[t_rstd])
        for k in range(8):
            s = cnt["tmp"] % 2; cnt["tmp"] += 1
            P.op("dve", lambda e, k=k, s=s: e.tensor_tensor(tmp[s][:, 0:w], hT[:, k, c0:c0 + w], mean_sb[:, 0:w], ALU.subtract),
                 reads=[t_h[gi], t_mean], writes=[t_tmp[s]])
            P.op("dve", lambda e, s=s: e.tensor_tensor(tmp[s][:, 0:w], tmp[s][:, 0:w], rstd_sb[:, 0:w], ALU.mult),
                 reads=[t_tmp[s], t_rstd], writes=[t_tmp[s]])
            P.op("act", lambda e, k=k, s=s: e.activation(hT[:, k, c0:c0 + w], tmp[s][:, 0:w], AF.Identity, bias=lnp[:, 1, k:k + 1], scale=lnp[:, 0, k:k + 1]),
                 reads=[t_tmp[s], t_lnp], writes=[t_h[gi]])
            P.op("act", lambda e, k=k, s=s: e.activation(hbf[:, k, c0:c0 + w], tmp[s][:, 0:w], AF.Identity, bias=lnp[:, 1, k:k + 1], scale=lnp[:, 0, k:k + 1]),
                 reads=[t_tmp[s], t_lnp], writes=[t_hb[gi]])

    if do_proj:
        wpj = [P.sb(f"wpj{i}", [128, 8, 128], BF16) for i in range(2)]
        t_wpj = [P.T(), P.T()]
        ost = [P.sb(f"ost{i}", [128, 512]) for i in range(4)]
        t_ost = [P.T() for _ in range(4)]
        w_in_v = D["w_in"].rearrange("(k p) c -> p k c", p=128)

    def proj(groups, tok0):
        for j in range(C_IN // 128):
            s = cnt["stg"] % NSTG; wq = WQ[cnt["stg"] % 2]; cnt["stg"] += 1
            wb = j % 2
            P.dma(wq, stg[s][:, 0:1024].rearrange("p (k c) -> p k c", k=8), w_in_v[:, :, 128 * j:128 * j + 128], writes=[t_stg[s]])
            P.op("act", lambda e, s=s, wb=wb: e.activation(wpj[wb][:], stg[s][:, 0:1024].rearrange("p (k c) -> p k c", k=8), AF.Identity),
                 reads=[t_stg[s]], writes=[t_wpj[wb]])
            for gi, (c0, w) in enumerate(groups):
                bk = cnt["ev"] % 4
                for k in range(8):
                    P.op("pe", lambda e, k=k, wb=wb, bk=bk, c0=c0, w=w: e.matmul(ps[bk][:, 0:w], wpj[wb][:, k, :], hbf[:, k, c0:c0 + w], start=(k == 0), stop=(k == 7)),
                         reads=[t_wpj[wb], t_hb[gi]], writes=[t_ps[bk]])
                eng = "dve"
                cnt["ev"] += 1
                if eng == "act":
                    P.op("act", lambda e, bk=bk, w=w: e.activation(ost[bk][:, 0:w], ps[bk][:, 0:w], AF.Identity), reads=[t_ps[bk]], writes=[t_ost[bk]])
                else:
                    P.op("dve", lambda e, bk=bk, w=w: e.tensor_copy(ost[bk][:, 0:w], ps[bk][:, 0:w]), reads=[t_ps[bk]], writes=[t_ost[bk]])
                P.dma("sp", D["pT"][128 * j:128 * j + 128, tok0 + c0:tok0 + c0 + w], ost[bk][:, 0:w], reads=[t_ost[bk]])

    if mode == "C":
        lnB = P.sb("lnB", [128, 2, 8]); t_lnB = P.T(const=True)
        P.dma("sp", lnB[:], D["lnB"], writes=[t_lnB])
        rw = P.sb("rw", [128, 8, 16]); t_rw = P.T(const=True)
        P.dma("sp", rw[:], D["router_w"].rearrange("(k p) e -> p k e", p=128), writes=[t_rw])
        rb = P.sb("rb", [128, 16]); t_rb = P.T(const=True)
        P.dma("sp", rb[:], D["rb"], writes=[t_rb])
        sel = P.sb("sel", [16, 16, 128]); t_sel = P.T(const=True)
        P.dma("sp", sel[:], D["sel"], writes=[t_sel])
        arena = [P.sb(f"arena{i}", [128, 8192], BF16) for i in range(2)]
        t_ar = [P.T("arena0"), P.T("arena1")]
        ob = [P.sb(f"ob{i}", [128, 4, 512], BF16) for i in range(2)]
        t_ob = [P.T(), P.T()]
        gts = [P.sb(f"gts{i}", [128, 512]) for i in range(4)]
        t_gts = [P.T() for _ in range(4)]
        merged = P.sb("merged", [128, 8, 512], BF16); t_merged = P.T()
        combT = P.sb("combT", [16, WMAX]); t_combT = P.T()
        cbc = [P.sb(f"cbc{i}", [128, WMAX]) for i in range(2)]
        t_cbc = [P.T(), P.T()]
        hid = [P.sb(f"hid{i}", [128, 2, 512], BF16) for i in range(2)]
        t_hid = [P.T(), P.T()]
        sgl = [P.sb(f"sgl{i}", [128, 512]) for i in range(2)]
        t_sgl = [P.T(), P.T()]
        rt = P.sb("rt", [128, 16, 16]); t_rt = P.T()
        rs = P.sb("rs", [128, 16]); t_rs = P.T()
        osb_v = D["osbT"].rearrange("(k p) t -> p k t", p=128)
        orw_v = D["orwT"].rearrange("(k p) t -> p k t", p=128)
        psb_v = D["p_sb"].rearrange("(k p) c -> p k c", p=128)
        prw_v = D["p_rw"].rearrange("(k p) c -> p k c", p=128)
        wout_v = D["w_out"].rearrange("(k p) c -> p k c", p=128)
        psb_bf = arena[0][:, 0:4096].rearrange("p (k c) -> p k c", k=4)
        prw_bf = arena[0][:, 4096:8192].rearrange("p (k c) -> p k c", k=4)
        wout_bf = arena[1][:, 0:8192].rearrange("p (k c) -> p k c", k=8)

    xT_v = D["xT"].rearrange("(k p) t -> p k t", p=128)
    hTo_v = D["hT"].rearrange("(k p) t -> p k t", p=128)

    for (tok0, groups, rtiles) in halves:
        for gi, (c0, w) in enumerate(groups):
            P.dma("sp", hT[:, :, c0:c0 + w], xT_v[:, :, tok0 + c0:tok0 + c0 + w], writes=[t_h[gi]])
        if mode == "A":
            for gi, (c0, w) in enumerate(groups):
                ln_group(gi, c0, w, lnA, t_lnA)
        else:
            for (src, dst, ai, nk) in ((psb_v, psb_bf, 0, 4), (prw_v, prw_bf, 0, 4), (wout_v, wout_bf, 1, 8)):
                for k0 in range(0, nk, 2):
                    s = cnt["stg"] % NSTG; wq = WQ[cnt["stg"] % 2]; cnt["stg"] += 1
                    P.dma(wq, stg[s][:, 0:2048].rearrange("p (k c) -> p k c", k=2), src[:, k0:k0 + 2, :], writes=[t_stg[s]])
                    P.op("act", lambda e, s=s, dst=dst, k0=k0: e.activation(dst[:, k0:k0 + 2, :], stg[s][:, 0:2048].rearrange("p (k c) -> p k c", k=2), AF.Identity),
                         reads=[t_stg[s]], writes=[t_ar[ai]])
            for gi, (c0, w) in enumerate(groups):
                for (src, oi) in ((osb_v, 0), (orw_v, 1)):
                    s = cnt["stg"] % NSTG; wq = WQ[cnt["stg"] % 2]; cnt["stg"] += 1
                    P.dma(wq, stg[s][:, 0:4 * w].rearrange("p (k c) -> p k c", k=4), src[:, :, tok0 + c0:tok0 + c0 + w], writes=[t_stg[s]])
                    P.op("dve", lambda e, s=s, oi=oi, w=w: e.tensor_copy(ob[oi][:, :, 0:w], stg[s][:, 0:4 * w].rearrange("p (k c) -> p k c", k=4)),
                         reads=[t_stg[s]], writes=[t_ob[oi]])
                for m in range(8):
                    ms = slice(128 * m, 128 * m + 128)
                    ba, bb = 0 + (m % 2) * 2, 1 + (m % 2) * 2
                    for k in range(4):
                        P.op("pe", lambda e, k=k, ms=ms, ba=ba, w=w: e.matmul(ps[ba][:, 0:w], psb_bf[:, k, ms], ob[0][:, k, 0:w], start=(k == 0), stop=(k == 3)),
                             reads=[t_ar[0], t_ob[0]], writes=[t_ps[ba]])
                    for k in range(4):
                        P.op("pe", lambda e, k=k, ms=ms, bb=bb, w=w: e.matmul(ps[bb][:, 0:w], prw_bf[:, k, ms], ob[1][:, k, 0:w], start=(k == 0), stop=(k == 3)),
                             reads=[t_ar[0], t_ob[1]], writes=[t_ps[bb]])
                    g0, g1 = (m % 2) * 2, (m % 2) * 2 + 1
                    P.dma("sp", gts[g0][:, 0:w], D["gT"][128 * m:128 * m + 128, tok0 + c0:tok0 + c0 + w], writes=[t_gts[g0]])
                    P.dma("sp", gts[g1][:, 0:w], D["gT"][1024 + 128 * m:1024 + 128 * m + 128, tok0 + c0:tok0 + c0 + w], writes=[t_gts[g1]])
                    P.op("act", lambda e, g0=g0, w=w: e.activation(gts[g0][:, 0:w], gts[g0][:, 0:w], AF.Sigmoid), reads=[t_gts[g0]], writes=[t_gts[g0]])
                    P.op("act", lambda e, g1=g1, w=w: e.activation(gts[g1][:, 0:w], gts[g1][:, 0:w], AF.Sigmoid), reads=[t_gts[g1]], writes=[t_gts[g1]])
                    P.op("dve", lambda e, g0=g0, ba=ba, w=w: e.tensor_tensor(gts[g0][:, 0:w], ps[ba][:, 0:w], gts[g0][:, 0:w], ALU.mult),
                         reads=[t_ps[ba], t_gts[g0]], writes=[t_gts[g0]])
                    P.op("dve", lambda e, g1=g1, bb=bb, w=w: e.tensor_tensor(gts[g1][:, 0:w], ps[bb][:, 0:w], gts[g1][:, 0:w], ALU.mult),
                         reads=[t_ps[bb], t_gts[g1]], writes=[t_gts[g1]])
                    P.op("pool", lambda e, g0=g0, g1=g1, m=m, w=w: e.tensor_tensor(merged[:, m, 0:w], gts[g0][:, 0:w], gts[g1][:, 0:w], ALU.add),
                         reads=[t_gts[g0], t_gts[g1]], writes=[t_merged])
                for m in range(8):
                    ms = slice(128 * m, 128 * m + 128)
                    bk = 4 + (m % 2)
                    for k in range(8):
                        P.op("pe", lambda e, k=k, ms=ms, bk=bk, w=w: e.matmul(ps[bk][:, 0:w], wout_bf[:, k, ms], merged[:, k, 0:w], start=(k == 0), stop=(k == 7)),
                             reads=[t_ar[1], t_merged], writes=[t_ps[bk]])
                    P.op("dve", lambda e, m=m, bk=bk, c0=c0, w=w: e.scalar_tensor_tensor(hT[:, m, c0:c0 + w], hT[:, m, c0:c0 + w], ALPHA, ps[bk][:, 0:w], ALU.mult, ALU.add),
                         reads=[t_ps[bk], t_h[gi]], writes=[t_h[gi]])
                ln_group(gi, c0, w, lnA, t_lnA)
            for (r0, nt) in rtiles:
                gi = [i for i, (c0, w) in enumerate(groups) if c0 <= r0 < c0 + w][0]
                RB = 5
                for k in range(8):
                    P.op("pe", lambda e, k=k, r0=r0, nt=nt: e.matmul(ps[RB][0:nt, 0:16], hT[:, k, r0:r0 + nt], rw[:, k, :], start=(k == 0), stop=(k == 7)),
                         reads=[t_h[gi], t_rw], writes=[t_ps[RB]])
                R = lambda i: rt[0:nt, i, :]
                S = lambda i: rs[0:nt, i:i + 1]

                def dv(fn, nt=nt):
                    P.op("dve", fn, reads=[t_rt, t_rs], writes=[t_rt, t_rs])
                P.op("dve", lambda e, nt=nt: e.tensor_tensor(rt[0:nt, 0, :], ps[RB][0:nt, 0:16], rb[0:nt, :], ALU.add),
                     reads=[t_ps[RB], t_rb], writes=[t_rt])
                dv(lambda e, nt=nt: e.reduce_max(rs[0:nt, 0:1], rt[0:nt, 0, :], axis=AX.X))
                dv(lambda e, nt=nt: e.tensor_scalar(rs[0:nt, 0:1], rs[0:nt, 0:1], -1.0, None, ALU.mult))
                P.op("act", lambda e, nt=nt: e.activation(rt[0:nt, 1, :], rt[0:nt, 0, :], AF.Exp, bias=rs[0:nt, 0:1]),
                     reads=[t_rt, t_rs], writes=[t_rt])
                dv(lambda e, nt=nt: e.reduce_sum(rs[0:nt, 1:2], rt[0:nt, 1, :], axis=AX.X))
                dv(lambda e, nt=nt: e.reciprocal(rs[0:nt, 1:2], rs[0:nt, 1:2]))
                dv(lambda e, nt=nt: e.tensor_scalar(rt[0:nt, 2, :], rt[0:nt, 1, :], rs[0:nt, 1:2], None, ALU.mult))
                for g in range(4):
                    dv(lambda e, nt=nt, g=g: e.reduce_max(rt[0:nt, 3, g:g + 1], rt[0:nt, 2, 4 * g:4 * g + 4], axis=AX.X))
                for g in range(4):
                    dv(lambda e, nt=nt, g=g: e.tensor_scalar(rt[0:nt, 4, 4 * g:4 * g + 4], rt[0:nt, 2, 4 * g:4 * g + 4], rt[0:nt, 3, g:g + 1], None, ALU.is_equal))
                dv(lambda e, nt=nt: e.scalar_tensor_tensor(rt[0:nt, 5, :], rt[0:nt, 4, :], -2.0, rt[0:nt, 2, :], ALU.mult, ALU.add))
                for g in range(4):
                    dv(lambda e, nt=nt, g=g: e.reduce_max(rt[0:nt, 3, 4 + g:5 + g], rt[0:nt, 5, 4 * g:4 * g + 4], axis=AX.X))
                dv(lambda e, nt=nt: e.tensor_tensor(rt[0:nt, 3, 8:12], rt[0:nt, 3, 0:4], rt[0:nt, 3, 4:8], ALU.add))
                dv(lambda e, nt=nt: e.reduce_max(rs[0:nt, 2:3], rt[0:nt, 3, 8:12], axis=AX.X))
                dv(lambda e, nt=nt: e.tensor_scalar(rt[0:nt, 3, 12:16], rt[0:nt, 3, 8:12], rs[0:nt, 2:3], None, ALU.is_equal))
                for g in range(4):
                    dv(lambda e, nt=nt, g=g: e.tensor_scalar(rt[0:nt, 6, 4 * g:4 * g + 4], rt[0:nt, 2, 4 * g:4 * g + 4], 1.0, rt[0:nt, 3, 12 + g:13 + g], ALU.add, ALU.mult))
                dv(lambda e, nt=nt: e.tensor_scalar(rt[0:nt, 6, :], rt[0:nt, 6, :], -1.0, None, ALU.add))
                dv(lambda e, nt=nt: e.reduce_max(rs[0:nt, 3:4], rt[0:nt, 6, :], axis=AX.X))
                dv(lambda e, nt=nt: e.tensor_scalar(rt[0:nt, 7, :], rt[0:nt, 6, :], rs[0:nt, 3:4], None, ALU.is_equal))
                dv(lambda e, nt=nt: e.scalar_tensor_tensor(rt[0:nt, 8, :], rt[0:nt, 7, :], -2.0, rt[0:nt, 6, :], ALU.mult, ALU.add))
                dv(lambda e, nt=nt: e.reduce_max(rs[0:nt, 4:5], rt[0:nt, 8, :], axis=AX.X))
                dv(lambda e, nt=nt: e.tensor_scalar(rt[0:nt, 9, :], rt[0:nt, 8, :], rs[0:nt, 4:5], None, ALU.is_equal))
                dv(lambda e, nt=nt: e.tensor_tensor(rs[0:nt, 5:6], rs[0:nt, 3:4], rs[0:nt, 4:5], ALU.add))
                dv(lambda e, nt=nt: e.reciprocal(rs[0:nt, 5:6], rs[0:nt, 5:6]))
                dv(lambda e, nt=nt: e.tensor_tensor(rs[0:nt, 6:7], rs[0:nt, 3:4], rs[0:nt, 5:6], ALU.mult))
                dv(lambda e, nt=nt: e.tensor_tensor(rs[0:nt, 7:8], rs[0:nt, 4:5], rs[0:nt, 5:6], ALU.mult))
                dv(lambda e, nt=nt: e.tensor_scalar(rt[0:nt, 10, :], rt[0:nt, 7, :], rs[0:nt, 6:7], None, ALU.mult))
                dv(lambda e, nt=nt: e.scalar_tensor_tensor(rt[0:nt, 11, :], rt[0:nt, 9, :], rs[0:nt, 7:8], rt[0:nt, 10, :], ALU.mult, ALU.add))
                P.op("pe", lambda e, nt=nt: e.transpose(ps[RB][0:16, 256:256 + nt], rt[0:nt, 11, :], tc[0:nt, 1, 0:nt]),
                     reads=[t_rt, t_tc], writes=[t_ps[RB]])
                P.op("act", lambda e, nt=nt, r0=r0: e.activation(combT[:, r0:r0 + nt], ps[RB][0:16, 256:256 + nt], AF.Identity),
                     reads=[t_ps[RB]], writes=[t_combT])
            for gi, (c0, w) in enumerate(groups):
                P.op("pool", lambda e, c0=c0, w=w: e.tensor_scalar(hT[:, :, c0:c0 + w], hT[:, :, c0:c0 + w], ALPHA, None, ALU.mult),
                     reads=[t_h[gi]], writes=[t_h[gi]])
            nhe = 0
            pending = []
            for ex in range(n_exp):
                cb_ = ex % 2
                for gi, (c0, w) in enumerate(groups):
                    P.op("pe", lambda e, ex=ex, c0=c0, w=w: e.matmul(ps[6][:, 0:w], sel[:, ex, :], combT[:, c0:c0 + w], start=True, stop=True),
                         reads=[t_sel, t_combT], writes=[t_ps[6]])
                    P.op("act", lambda e, cb_=cb_, c0=c0, w=w: e.activation(cbc[cb_][:, c0:c0 + w], ps[6][:, 0:w], AF.Identity),
                         reads=[t_ps[6]], writes=[t_cbc[cb_]])
                for hf in range(2):
                    ai = nhe % 2
                    nhe += 1
                    wg_bf = arena[ai][:, 0:2048].rearrange("p (k c) -> p k c", k=8)
                    wu_bf = arena[ai][:, 2048:4096].rearrange("p (k c) -> p k c", k=8)
                    wd_bf = arena[ai][:, 4096:6144].rearrange("p (k c) -> p k c", k=2)
                    fs = slice(256 * hf, 256 * hf + 256)
                    for (src, dst, kk_) in ((D["wg"][ex].rearrange("(k p) f -> p k f", p=128)[:, :, fs], wg_bf, 8),
                                            (D["wu"][ex].rearrange("(k p) f -> p k f", p=128)[:, :, fs], wu_bf, 8),
                                            (D["wd"][ex, 256 * hf:256 * hf + 256, :].rearrange("(k p) d -> p k d", p=128), wd_bf, 2)):
                        s = cnt["stg"] % NSTG; wq = WQ[cnt["stg"] % 2]; cnt["stg"] += 1
                        P.dma(wq, stg[s][:, 0:2048].rearrange("p (k c) -> p k c", k=kk_), src, writes=[t_stg[s]])
                        P.op("act", lambda e, s=s, dst=dst, kk_=kk_: e.activation(dst, stg[s][:, 0:2048].rearrange("p (k c) -> p k c", k=kk_), AF.Identity),
                             reads=[t_stg[s]], writes=[t_ar[ai]])
                    for gi, (c0, w) in enumerate(groups):
                        hb_ = cnt["ev"] % 2
                        cnt["ev"] += 1
                        for fc in range(2):
                            bg, bu = fc, 2 + fc
                            fcs = slice(128 * fc, 128 * fc + 128)
                            for k in range(8):
                                P.op("pe", lambda e, k=k, bg=bg, fcs=fcs, c0=c0, w=w, wg_bf=wg_bf: e.matmul(ps[bg][:, 0:w], wg_bf[:, k, fcs], hbf[:, k, c0:c0 + w], start=(k == 0), stop=(k == 7)),
                                     reads=[t_ar[ai], t_hb[gi]], writes=[t_ps[bg]])
                            for k in range(8):
                                P.op("pe", lambda e, k=k, bu=bu, fcs=fcs, c0=c0, w=w, wu_bf=wu_bf: e.matmul(ps[bu][:, 0:w], wu_bf[:, k, fcs], hbf[:, k, c0:c0 + w], start=(k == 0), stop=(k == 7)),
                                     reads=[t_ar[ai], t_hb[gi]], writes=[t_ps[bu]])
                            P.op("act", lambda e, fc=fc, bg=bg, w=w: e.activation(sgl[fc][:, 0:w], ps[bg][:, 0:w], AF.Silu),
                                 reads=[t_ps[bg]], writes=[t_sgl[fc]])
                            P.op("dve", lambda e, fc=fc, bu=bu, w=w: e.tensor_tensor(sgl[fc][:, 0:w], ps[bu][:, 0:w], sgl[fc][:, 0:w], ALU.mult),
                                 reads=[t_ps[bu], t_sgl[fc]], writes=[t_sgl[fc]])
                            P.op("dve", lambda e, fc=fc, hb_=hb_, cb_=cb_, c0=c0, w=w: e.tensor_tensor(hid[hb_][:, fc, 0:w], sgl[fc][:, 0:w], cbc[cb_][:, c0:c0 + w], ALU.mult),
                                 reads=[t_sgl[fc], t_cbc[cb_]], writes=[t_hid[hb_]])
                        if pending:
                            pending.pop()()

                        def down(gi=gi, c0=c0, w=w, hb_=hb_, ai=ai, wd_bf=wd_bf):
                            for m in range(8):
                                bd = 4 + (m % 2)
                                ms = slice(128 * m, 128 * m + 128)
                                for fc in range(2):
                                    P.op("pe", lambda e, fc=fc, bd=bd, ms=ms: e.matmul(ps[bd][:, 0:w], wd_bf[:, fc, ms], hid[hb_][:, fc, 0:w], start=(fc == 0), stop=(fc == 1)),
                                         reads=[t_ar[ai], t_hid[hb_]], writes=[t_ps[bd]])
                                P.op("dve", lambda e, m=m, bd=bd: e.tensor_tensor(hT[:, m, c0:c0 + w], ps[bd][:, 0:w], hT[:, m, c0:c0 + w], ALU.add),
                                     reads=[t_ps[bd], t_h[gi]], writes=[t_h[gi]])
                        pending.append(down)
            if pending:
                pending.pop()()
            for gi, (c0, w) in enumerate(groups):
                ln_group(gi, c0, w, lnB, t_lnB)
        for gi, (c0, w) in enumerate(groups):
            P.dma("sp", hTo_v[:, :, tok0 + c0:tok0 + c0 + w], hT[:, :, c0:c0 + w], reads=[t_h[gi]])
        if do_proj:
            proj(groups, tok0)


NCORES = 8
SEQ = 8192
LFULL = N_META + SEQ


def _fm(v):
    return np.ascontiguousarray(np.asarray(v, np.float32).reshape(8, 128).T)


def _run(nc, in_maps):
    res = run_bass_kernel_spmd(nc, in_maps, core_ids=list(range(NCORES)))
    return res.results


def _new_nc():
    return bass.Bass("TRN2", target_bir_lowering=False)


def _mixers(pT_cores, prm):
    amask = attn_masks()
    attn_maps, rwkv_maps = [], []
    for c in range(NCORES):
        b, hp = c // 4, c % 4
        pb_ = np.concatenate([pT_cores[4 * b][:, 0:N_META]] + [pT_cores[4 * b + r][:, N_META:] for r in range(4)], axis=1)
        q = pb_[128 * hp:128 * hp + 128].reshape(2, 64, LFULL)
        k = pb_[512 + 128 * hp:512 + 128 * hp + 128].reshape(2, 64, LFULL)
        v = pb_[1024 + 128 * hp:1024 + 128 * hp + 128].reshape(2, 64, LFULL)
        vtm = v.transpose(0, 2, 1)
        vm = np.ascontiguousarray(vtm[:, 0:N_META])
        vx = np.ascontiguousarray(vtm[:, N_META:].reshape(2, SEQ // 128, 128, 64).transpose(0, 2, 1, 3))
        attn_maps.append({"qT": np.ascontiguousarray(q), "kT": np.ascontiguousarray(k), "vx": vx, "vm": vm, "msk": amask})
        p_rw = np.ascontiguousarray(pb_[1536:3328].T)
        rwkv_maps.append(rwkv_host_inputs(p_rw, hp, prm))
    nc = _new_nc()
    build_attn(nc, 2, SEQ // 512)
    ares = _run(nc, attn_maps)
    nc = _new_nc()
    build_rwkv(nc, SEQ // 128)
    rres = _run(nc, rwkv_maps)
    osbT, orwT = [], []
    for b in range(2):
        osbT.append(np.concatenate([ares[4 * b + hp]["oT"].reshape(128, LFULL) for hp in range(4)], axis=0))
        orwT.append(np.concatenate([rres[4 * b + hp]["o_rw"].T for hp in range(4)], axis=0))
    return osbT, orwT


def _core_cols(full, r):
    return np.ascontiguousarray(np.concatenate([full[:, 0:N_META], full[:, N_META + 2048 * r:N_META + 2048 * (r + 1)]], axis=1))


def kernel(**inputs):
    inp = {k: np.asarray(v) for k, v in inputs.items()}
    x, meta = inp["x"].astype(np.float32), inp["meta"].astype(np.float32)
    tcc, sel = tok_consts()
    maps = []
    for c in range(NCORES):
        b, r = c // 4, c % 4
        xT = np.ascontiguousarray(np.concatenate([meta.T, x[b, 2048 * r:2048 * (r + 1)].T], axis=1))
        maps.append({"xT": xT, "tc": tcc, "lnA": np.stack([_fm(inp["emb_ln_g"]), _fm(inp["emb_ln_b"])], axis=1),
                     "w_in": np.ascontiguousarray(inp["w_in"][0])})
    nc = _new_nc()
    build_tok(nc, "A", True)
    res = _run(nc, maps)
    hT_c = [r_["hT"] for r_ in res]
    pT_c = [r_["pT"] for r_ in res]
    for l in range(2):
        prm = {k: inp[k][l] for k in ["rwkv_mu", "w0", "w_up", "a0", "a_up", "g_up", "k_k", "k_a", "r_k", "lnx_g", "lnx_b"]}
        osbT, orwT = _mixers(pT_c, prm)
        last = (l == 1)
        maps = []
        for c in range(NCORES):
            b, r = c // 4, c % 4
            m = {"xT": hT_c[c], "tc": tcc, "lnA": np.stack([_fm(inp["ln1_g"][l]), _fm(inp["ln1_b"][l])], axis=1),
                 "osbT": _core_cols(osbT[b], r), "orwT": _core_cols(orwT[b], r),
                 "gT": np.ascontiguousarray(pT_c[c][3328:5376]),
                 "p_sb": np.ascontiguousarray(inp["p_sb"][l]), "p_rw": np.ascontiguousarray(inp["p_rwkv"][l]),
                 "w_out": np.ascontiguousarray(inp["w_out"][l]),
                 "lnB": np.stack([_fm(inp["ln2_g"][l]), _fm(inp["ln2_b"][l])], axis=1),
                 "router_w": np.ascontiguousarray(inp["router_w"]),
                 "rb": np.ascontiguousarray(np.broadcast_to(inp["router_b"][None].astype(np.float32), (128, 16))),
                 "sel": sel,
                 "wg": np.ascontiguousarray(inp["exp_w_gate"][l]), "wu": np.ascontiguousarray(inp["exp_w_up"][l]),
                 "wd": np.ascontiguousarray(inp["exp_w_down"][l])}
            if not last:
                m["w_in"] = np.ascontiguousarray(inp["w_in"][l + 1])
            maps.append(m)
        nc = _new_nc()
        build_tok(nc, "C", not last)
        res = _run(nc, maps)
        hT_c = [r_["hT"] for r_ in res]
        if not last:
            pT_c = [r_["pT"] for r_ in res]
    out = np.zeros((2, SEQ, 1024), np.float32)
    for c in range(NCORES):
        b, r = c // 4, c % 4
        out[b, 2048 * r:2048 * (r + 1)] = hT_c[c][:, N_META:].T
    return out
```

```python
import numpy as np
from contextlib import ExitStack
import concourse.bass as bass
import concourse.mybir as mybir
from concourse.bass_utils import run_bass_kernel_spmd

F32 = mybir.dt.float32
BF16 = mybir.dt.bfloat16
AF = mybir.ActivationFunctionType
ALU = mybir.AluOpType
AX = mybir.AxisListType

import os
BUDGET = int(os.environ["KBUDGET"]) if "KBUDGET" in os.environ else None
ENGS = ["pe", "act", "dve", "pool", "sp"]
DMA_R = 8


class T:
    __slots__ = ("name", "lw", "rd", "const", "excl")

    def __init__(self, name, const=False, excl=False):
        self.name = name
        self.lw = None
        self.rd = []
        self.const = const
        self.excl = excl


class Prog:
    def __init__(self, nc, es):
        self.nc = nc
        self.es = es
        self.items = {e: [] for e in ENGS}
        self.sem = {e: es.enter_context(nc.semaphore("s_" + e)) for e in ENGS}
        self.cnt = {e: 0 for e in ENGS}
        self.seen = {e: {} for e in ENGS}
        self.dsem = {}
        self.dma_n = {}
        self.dma_final = {}
        self.ntile = 0

    def sb(self, name, shape, dt=F32):
        return self.es.enter_context(self.nc.sbuf_tensor("sb_" + name, list(shape), dt))

    def ps(self, name, shape=(128, 512), dt=F32):
        return self.es.enter_context(self.nc.psum_tensor("pp_" + name, list(shape), dt))

    def T(self, name=None, const=False, excl=False):
        self.ntile += 1
        return T(name or f"t{self.ntile}", const, excl)

    def _deps(self, eng, reads, writes):
        deps = {}

        def add(p):
            if p is None:
                return
            k, v = p
            if deps.get(k, 0) < v:
                deps[k] = v

        for t in reads:
            add(t.lw)
        for t in writes:
            add(t.lw)
            for p in t.rd:
                add(p)
        waits = []
        for k, v in deps.items():
            if k == eng and eng == "pe":
                continue
            if self.seen[eng].get(k, 0) >= v:
                continue
            self.seen[eng][k] = v
            waits.append((k, v))
        return waits

    def _commit(self, pid, reads, writes):
        for t in writes:
            t.lw = pid
            t.rd = []
        for t in reads:
            if not t.const:
                t.rd.append(pid)
                if len(t.rd) > 64:
                    m = {}
                    for k, v in t.rd:
                        if m.get(k, 0) < v:
                            m[k] = v
                    t.rd = list(m.items())

    def begin_stream(self):
        self._rec = []

    def end_stream(self):
        r, self._rec = self._rec, None
        return r

    def mark(self):
        if getattr(self, "_rec", None) is not None:
            self._rec.append(("mark", ()))

    def run_interleaved(self, streams, max_active=2):
        streams = list(streams)
        active = []
        nxt = 0
        while nxt < len(streams) or active:
            if nxt < len(streams) and len(active) < max_active and (not active or active[0][2]):
                active.append([streams[nxt], 0, False])
                nxt += 1
            for idx, a in enumerate(list(active)):
                if a[1] >= len(a[0]):
                    continue
                kind, args = a[0][a[1]]
                if kind == "mark":
                    if idx > 0 and active[0] is not a and active[0][1] < len(active[0][0]):
                        continue
                    a[2] = True
                    a[1] += 1
                    continue
                a[1] += 1
                (self.op if kind == "op" else self.dma)(*args)
            active = [a for a in active if a[1] < len(a[0])]

    def op(self, eng, fn, reads=(), writes=()):
        if getattr(self, "_rec", None) is not None:
            self._rec.append(("op", (eng, fn, tuple(reads), tuple(writes))))
            return
        self.nops = getattr(self, "nops", 0) + 1
        if BUDGET is not None and self.nops > BUDGET:
            return
        ex = [t for t in reads if t.excl]
        if ex:
            reads = [t for t in reads if not t.excl]
            writes = list(writes) + ex
        waits = self._deps(eng, reads, writes)
        self.cnt[eng] += 1
        pid = (eng, self.cnt[eng])
        self.items[eng].append((waits, fn, True))
        self._commit(pid, reads, writes)

    def dma(self, q, out_ap, in_ap, reads=(), writes=()):
        if getattr(self, "_rec", None) is not None:
            self._rec.append(("dma", (q, out_ap, in_ap, tuple(reads), tuple(writes))))
            return
        self.nops = getattr(self, "nops", 0) + 1
        if BUDGET is not None and self.nops > BUDGET:
            return
        if q not in self.dsem:
            self.dsem[q] = [self.es.enter_context(self.nc.semaphore(f"d_{q}_{i}")) for i in range(DMA_R)]
            self.dma_n[q] = 0
        n = self.dma_n[q]
        self.dma_n[q] += 1
        i = n % DMA_R
        rnd = n // DMA_R
        key = ("d", q, i)
        waits = self._deps(q, reads, writes)
        if rnd > 0 and self.seen[q].get(key, 0) < 16 * rnd:
            self.seen[q][key] = 16 * rnd
            waits.append((key, 16 * rnd))
        sem = self.dsem[q][i]

        def fn(e, out_ap=out_ap, in_ap=in_ap, sem=sem):
            return e.dma_start(out=out_ap, in_=in_ap).then_inc(sem, 16)

        self.items[q].append((waits, fn, False))
        pid = (key, 16 * (rnd + 1))
        self.dma_final[key] = 16 * (rnd + 1)
        self._commit(pid, reads, writes)

    def _semobj(self, k):
        if isinstance(k, tuple):
            return self.dsem[k[1]][k[2]]
        return self.sem[k]

    def finish(self):
        waits = []
        for key, v in self.dma_final.items():
            waits.append((key, v))
        for e in ENGS:
            if e != "sp" and self.cnt[e] > 0:
                waits.append((e, self.cnt[e]))
        self.items["sp"].append((waits, None, False))
        nc = self.nc
        with nc.Block() as block:
            def replay(name, e):
                for waits, fn, inc in self.items[name]:
                    for k, v in waits:
                        e.wait_ge(self._semobj(k), v)
                    if fn is None:
                        continue
                    ins = fn(e)
                    if inc:
                        ins.then_inc(self.sem[name], 1)

            @block.tensor
            def _(e):
                replay("pe", e)

            @block.scalar
            def _(e):
                replay("act", e)

            @block.vector
            def _(e):
                replay("dve", e)

            @block.gpsimd
            def _(e):
                replay("pool", e)

            @block.sync
            def _(e):
                replay("sp", e)


N_META = 16


def build_attn(nc, NH, NG):
    NB = 4 * NG
    L = N_META + 512 * NG
    qT = nc.dram_tensor("qT", [NH, 64, L], F32, kind="ExternalInput").ap()
    kT = nc.dram_tensor("kT", [NH, 64, L], F32, kind="ExternalInput").ap()
    vx = nc.dram_tensor("vx", [NH, 128, NB, 64], F32, kind="ExternalInput").ap()
    vm = nc.dram_tensor("vm", [NH, 16, 64], F32, kind="ExternalInput").ap()
    msk = nc.dram_tensor("msk", [128, 3, 128], F32, kind="ExternalInput").ap()
    oT = nc.dram_tensor("oT", [NH, 64, L], F32, kind="ExternalOutput").ap()
    with ExitStack() as es:
        P = Prog(nc, es)
        emit_attn(P, NH, NG, qT, kT, vx, vm, msk, oT)
        P.finish()
    return nc


def emit_attn(P, NH, NG, qT, kT, vx, vm, msk, oT):
    NB = 4 * NG
    L = N_META + 512 * NG
    mstage = P.sb("mstage", [128, 3, 128], F32)
    t_mstage = P.T()
    P.dma("sp", mstage[:], msk, writes=[t_mstage])
    cm = P.sb("cm", [128, 3, 128], BF16)
    t_cm = P.T(const=True)
    P.op("dve", lambda e: e.tensor_copy(cm[:], mstage[:]), reads=[t_mstage], writes=[t_cm])
    zeros = P.sb("zeros", [128, 64], BF16)
    t_zeros = P.T(const=True)
    P.op("pool", lambda e: e.memset(zeros[:], 0.0), writes=[t_zeros])

    QT = P.sb("QT", [64, L], BF16)
    KT = P.sb("KT", [64, L], BF16)
    V = P.sb("V", [128, NB, 64], BF16)
    VM = P.sb("VM", [16, 64], BF16)
    t_Q, t_K, t_V = P.T(), P.T(), P.T()
    CH = 2064 if L > 2064 else L
    stg = [P.sb(f"stg{i}", [128, CH], F32) for i in range(2)]
    t_stg = [P.T(), P.T()]
    vstg = P.sb("vstg", [128, NB, 64], F32)
    t_vstg = P.T()
    vmstg = P.sb("vmstg", [16, 64], F32)
    t_vmstg = P.T()

    zA = [P.ps(f"zA{i}") for i in range(2)]
    zB = [P.ps(f"zB{i}") for i in range(2)]
    Ops = [P.ps(f"Ops{i}") for i in range(2)]
    t_zA = [P.T(excl=True), P.T(excl=True)]
    t_zB = [P.T(excl=True), P.T(excl=True)]
    t_O = [P.T(excl=True), P.T(excl=True)]
    e_sb = [P.sb(f"e{i}", [128, 512], F32) for i in range(2)]
    t_e = [P.T(), P.T()]
    sp_sb = [P.sb(f"sp{i}", [128, 512], BF16) for i in range(2)]
    t_sp = [P.T(), P.T()]
    A_sb = [P.sb(f"A{i}", [128, 512], BF16) for i in range(2)]
    t_A = [P.T(), P.T()]
    acc = P.sb("acc", [128, 512], F32)
    t_acc = P.T()
    accb = [P.sb(f"accb{i}", [128, 512], BF16) for i in range(3)]
    t_accb = [P.T(), P.T(), P.T()]
    ost = [P.sb(f"ost{i}", [64, 512], F32) for i in range(2)]
    t_ost = [P.T(), P.T()]

    nstg = 0
    gcount = 0
    for h in range(NH):
        for (src, dst, tt, scale) in ((qT, QT, t_Q, 0.125), (kT, KT, t_K, 1.0)):
            first = True
            for c0 in range(0, L, CH):
                w = min(CH, L - c0)
                s = nstg % 2
                nstg += 1
                P.dma("sp", stg[s][0:64, 0:w], src[h, :, c0:c0 + w], writes=[t_stg[s]])
                if scale != 1.0:
                    P.op("dve", lambda e, s=s, w=w, c0=c0, dst=dst, scale=scale:
                         e.tensor_scalar(dst[:, c0:c0 + w], stg[s][0:64, 0:w], scale, None, ALU.mult),
                         reads=[t_stg[s]], writes=[tt])
                else:
                    P.op("pool", lambda e, s=s, w=w, c0=c0, dst=dst:
                         e.tensor_copy(dst[:, c0:c0 + w], stg[s][0:64, 0:w]),
                         reads=[t_stg[s]], writes=[tt])
        P.dma("sp", vstg[:], vx[h], writes=[t_vstg])
        P.op("pool", lambda e: e.tensor_copy(V[:], vstg[:]), reads=[t_vstg], writes=[t_V])
        P.dma("sp", vmstg[:], vm[h], writes=[t_vmstg])
        P.op("pool", lambda e: e.tensor_copy(VM[:], vmstg[:]), reads=[t_vmstg], writes=[t_V])

        for g in [-1] + list(range(NG)):
            if g < 0:
                q0, QW = 0, N_META
                blocks = [("m", 0)]
            else:
                q0, QW = N_META + 512 * g, 512
                blocks = [("x", j) for j in range(4 * g + 3, -1, -1)] + [("m", 0)]
            ob = gcount % 2
            gcount += 1
            P.op("pe", lambda e, ob=ob, QW=QW, q0=q0:
                 e.matmul(Ops[ob][0:64, 0:QW], zeros[0:64, 0:64], QT[:, q0:q0 + QW], start=True, stop=False),
                 reads=[t_zeros, t_Q], writes=[t_O[ob]])
            P.op("pool", lambda e: e.memset(acc[:], 0.0), writes=[t_acc])
            nblk = len(blocks)
            info = []
            for it, (kind, j) in enumerate(blocks):
                if kind == "m":
                    kp, k0 = N_META, 0
                    c0 = 0
                    diag = (g < 0)
                else:
                    kp, k0 = 128, N_META + 128 * j
                    jl = j - 4 * g
                    diag = jl >= 0
                    c0 = 128 * jl if diag else 0
                info.append((kind, j, kp, k0, c0, diag))

            def prm(it, info=info, q0=q0, QW=QW):
                kind, j, kp, k0, c0, diag = info[it]
                W = QW - c0
                return kind, j, kp, k0, c0, diag, it % 2, W, slice(q0 + c0, q0 + QW), min(128, W)

            def pe_zA(it):
                kind, j, kp, k0, c0, diag, b, W, qs, dw = prm(it)
                P.op("pe", lambda e: e.matmul(zA[b][0:kp, 0:W], KT[:, k0:k0 + kp], QT[:, qs], start=True, stop=True),
                     reads=[t_K, t_Q], writes=[t_zA[b]])

            def act_e(it):
                kind, j, kp, k0, c0, diag, b, W, qs, dw = prm(it)
                P.op("act", lambda e: e.activation(e_sb[b][0:kp, 0:W], zA[b][0:kp, 0:W], AF.Exp),
                     reads=[t_zA[b]], writes=[t_e[b]])

            def act_sp(it, nblk=nblk, QW=QW):
                kind, j, kp, k0, c0, diag, b, W, qs, dw = prm(it)
                P.op("act", lambda e: e.activation(sp_sb[b][0:kp, 0:W], e_sb[b][0:kp, 0:W], AF.Ln, bias=1.0),
                     reads=[t_e[b]], writes=[t_sp[b]])
                if diag:
                    P.op("pool", lambda e: e.tensor_tensor(sp_sb[b][0:kp, 0:dw], sp_sb[b][0:kp, 0:dw],
                                                           cm[0:kp, 0, 0:dw], ALU.mult),
                         reads=[t_sp[b], t_cm], writes=[t_sp[b]])
                if it < nblk - 1:
                    a3 = it % 3
                    P.op("dve", lambda e: e.tensor_tensor(acc[0:kp, c0:QW], acc[0:kp, c0:QW], sp_sb[b][0:kp, 0:W], ALU.add),
                         reads=[t_sp[b], t_acc], writes=[t_acc])
                    P.op("dve", lambda e: e.tensor_copy(accb[a3][:, 0:QW], acc[:, 0:QW]),
                         reads=[t_acc], writes=[t_accb[a3]])

            def pe_zB(it, QW=QW):
                kind, j, kp, k0, c0, diag, b, W, qs, dw = prm(it)
                last = (it == 0)
                P.op("pe", lambda e: e.matmul(zB[b][0:kp, 0:W], KT[:, k0:k0 + kp], QT[:, qs], start=True, stop=False),
                     reads=[t_K, t_Q], writes=[t_zB[b]])
                P.op("pe", lambda e: e.matmul(zB[b][0:kp, 0:W], cm[0:kp, 1, 0:kp], sp_sb[b][0:kp, 0:W],
                                              start=False, stop=last),
                     reads=[t_cm, t_sp[b]], writes=[t_zB[b]])
                if not last:
                    ab = (it - 1) % 3
                    P.op("pe", lambda e: e.matmul(zB[b][0:kp, 0:W], cm[0:128, 2, 0:kp], accb[ab][0:128, c0:QW],
                                                  start=False, stop=True),
                         reads=[t_cm, t_accb[ab]], writes=[t_zB[b]])

            def act_A(it):
                kind, j, kp, k0, c0, diag, b, W, qs, dw = prm(it)
                P.op("act", lambda e: e.activation(A_sb[b][0:kp, 0:W], zB[b][0:kp, 0:W], AF.Exp),
                     reads=[t_zB[b]], writes=[t_A[b]])
                if diag:
                    P.op("pool", lambda e: e.tensor_tensor(A_sb[b][0:kp, 0:dw], A_sb[b][0:kp, 0:dw],
                                                           cm[0:kp, 0, 0:dw], ALU.mult),
                         reads=[t_A[b], t_cm], writes=[t_A[b]])

            def pe_AV(it, ob=ob, nblk=nblk, QW=QW):
                kind, j, kp, k0, c0, diag, b, W, qs, dw = prm(it)
                vop = (VM[0:kp, :] if kind == "m" else V[:, j, :])
                P.op("pe", lambda e: e.matmul(Ops[ob][0:64, c0:QW], vop, A_sb[b][0:kp, 0:W],
                                              start=False, stop=(it == nblk - 1)),
                     reads=[t_V, t_A[b]], writes=[t_O[ob]])

            for s_ in range(-2, nblk + 2):
                if 0 <= s_ + 2 < nblk:
                    pe_zA(s_ + 2)
                if 0 <= s_ < nblk:
                    pe_zB(s_)
                if 0 <= s_ - 2 < nblk:
                    pe_AV(s_ - 2)
                if 0 <= s_ + 1 < nblk:
                    act_e(s_ + 1)
                if 0 <= s_ - 1 < nblk:
                    act_A(s_ - 1)
                if 0 <= s_ + 1 < nblk:
                    act_sp(s_ + 1)
            P.op("dve", lambda e, ob=ob, QW=QW: e.tensor_copy(ost[ob][:, 0:QW], Ops[ob][0:64, 0:QW]),
                 reads=[t_O[ob]], writes=[t_ost[ob]])
            P.dma("sp", oT[h, :, q0:q0 + QW], ost[ob][:, 0:QW], reads=[t_ost[ob]])


def attn_masks():
    s = np.arange(128)[:, None]
    t = np.arange(128)[None, :]
    m = np.zeros((128, 3, 128), np.float32)
    m[:, 0, :] = (s < t)
    m[:, 1, :] = -1.0 * (s >= t)
    m[:, 2, :] = -1.0
    return m


DECAY_SCALE = float(np.exp(-0.5))
LNX_EPS = 64e-5


def rwkv_consts():
    idx = np.arange(128)
    ch = idx // 64
    same = ch[:, None] == ch[None, :]
    le = idx[:, None] <= idx[None, :]
    lt = idx[:, None] < idx[None, :]
    c = np.zeros((128, 9, 128), np.float32)
    c[:, 0] = -DECAY_SCALE * (same & le)
    c[:, 1] = -DECAY_SCALE * same
    c[:, 2] = same & lt
    c[:, 3] = same & le
    c[:, 4] = same & lt
    c[:, 5] = same & le
    c[:, 6] = (same & lt).T
    c[:, 7] = np.eye(128)
    c[:, 8, 0] = -DECAY_SCALE * (ch == 0)
    c[:, 8, 1] = -DECAY_SCALE * (ch == 1)
    c[:, 8, 2] = (ch == 0)
    c[:, 8, 3] = (ch == 1)
    return c


def build_rwkv(nc, NTX):
    L = N_META + 128 * NTX
    D = {}
    D["rkv"] = nc.dram_tensor("rkv", [L + 1, 384], F32, kind="ExternalInput").ap()
    D["lo"] = nc.dram_tensor("lo", [256, L + 1], F32, kind="ExternalInput").ap()
    D["mu_tm"] = nc.dram_tensor("mu_tm", [128, 384], F32, kind="ExternalInput").ap()
    D["mu_fm"] = nc.dram_tensor("mu_fm", [128, 3], F32, kind="ExternalInput").ap()
    D["bc"] = nc.dram_tensor("bc", [128, 7, 128], F32, kind="ExternalInput").ap()
    D["wa_up"] = nc.dram_tensor("wa_up", [128, 128], F32, kind="ExternalInput").ap()
    D["g_up"] = nc.dram_tensor("g_up", [128, 128], F32, kind="ExternalInput").ap()
    D["cst"] = nc.dram_tensor("cst", [128, 9, 128], F32, kind="ExternalInput").ap()
    D["o"] = nc.dram_tensor("o_rw", [L, 128], F32, kind="ExternalOutput").ap()
    with ExitStack() as es:
        P = Prog(nc, es)
        emit_rwkv(P, NTX, D)
        P.finish()
    return nc


def emit_rwkv(P, NTX, D, pfx="rw"):
    DS = DECAY_SCALE
    NCH = 1 + 2 * NTX

    def const_load(name, shape, src):
        t = P.sb(pfx + name, shape, F32)
        tt = P.T(const=True)
        P.dma("sp", t[:], src, writes=[tt])
        return t, tt

    cst, t_cst = const_load("cst", [128, 9, 128], D["cst"])
    bc, t_bc = const_load("bc", [128, 7, 128], D["bc"])
    mu_tm, t_mutm = const_load("mu_tm", [128, 384], D["mu_tm"])
    mu_fm, t_mufm = const_load("mu_fm", [128, 3], D["mu_fm"])
    wup, t_wup = const_load("wup", [64, 128], D["wa_up"][0:64, :])
    aup, t_aup = const_load("aup", [64, 128], D["wa_up"][64:128, :])
    gup, t_gup = const_load("gup", [128, 128], D["g_up"])

    S_all = [P.sb(pfx + f"S_all{h}", [64, NCH + 1, 64], BF16) for h in range(2)]
    ident_bf = P.sb(pfx + "ident_bf", [128, 128], BF16)
    t_idb = P.T(const=True)
    P.op("pool", lambda e: e.tensor_copy(ident_bf[:], cst[:, 7, :]), reads=[t_cst], writes=[t_idb])
    t_S = [[P.T() for _ in range(NCH + 1)] for h in range(2)]
    for h in range(2):
        P.op("pool", lambda e, h=h: e.memset(S_all[h][:, 0, :], 0.0), writes=[t_S[h][0]])

    pb = [P.ps(pfx + f"pb{i}", [128, 4, 128], BF16 if i == 2 else F32) for i in range(8)]
    t_bank = [P.T("bank%d" % i, excl=True) for i in range(8)]

    names_sb = {
        "xa": [128, 384], "xs": [128, 384], "x": [128, 384],
        "la": [64, 128], "ls": [64, 128], "la2": [64, 128], "ls2": [64, 128], "ga": [128, 128], "gs": [128, 128],
        "lx": [64, 128], "lx2": [64, 128], "gx": [128, 128], "tw": [64, 128], "sg": [128, 128],
        "sigw": [128, 128], "a": [128, 128], "g": [128, 128],
        "kk": [128, 128], "sq": [128, 128], "ss": [128, 2], "rn": [128, 2],
        "kp": [128, 128], "t1": [128, 128], "bs": [128, 2],
        "cum": [128, 128], "c1": [128, 128], "c2": [128, 128],
        "ep": [128, 128], "em": [128, 128], "epm": [128, 128], "ebar": [128, 128], "ebz": [128, 2, 128],
        "PC": [64, 2, 2],
        "TM": [128, 4, 128],
        "kka": [128, 128], "bbz": [128, 2, 128], "kbz": [128, 2, 128],
        "FM0": [64, 4, 128], "FM1": [64, 4, 128],
        "G0": [128, 4, 128], "G1": [128, 4, 128],
        "NP0": [128, 6, 2, 128], "NP1": [128, 6, 2, 128],
        "X0a": [128, 128], "X0b": [128, 128], "X1a": [128, 128], "X1b": [128, 128],
        "M": [64, 4, 64], "RHz0": [64, 2, 128], "RHz1": [64, 2, 128],
        "y": [128, 128], "ysq": [128, 128], "st": [128, 8], "yn": [128, 128], "o": [128, 128],
    }
    BF_NAMES = {"TM", "bbz", "kbz", "FM0", "FM1", "G0", "G1", "NP0", "NP1", "X0a", "X0b", "X1a", "X1b", "M", "RHz0", "RHz1", "xvb"}
    names_sb["xvb"] = [128, 128]
    bufs = []
    for p in range(2):
        d = {}
        for nm, shp in names_sb.items():
            d[nm] = (P.sb(f"{pfx}{nm}_{p}", shp, BF16 if nm in BF_NAMES else F32), P.T(f"{nm}{p}"))
        bufs.append(d)
        for nm in ("RHz0", "RHz1"):
            t, tt = d[nm]
            P.op("pool", lambda e, t=t: e.memset(t[:], 0.0), writes=[tt])

    rkv, lo, o_d = D["rkv"], D["lo"], D["o"]
    B_LORA, B_CUM, B_FM, B_G, B_H0, B_H1, B_PM, B_Y = range(8)
    B_HH = [B_H0, B_H1]

    def tile_body(ti):
        n = 16 if ti == 0 else 128
        t0 = 0 if ti == 0 else N_META + 128 * (ti - 1)
        nch = 1 if ti == 0 else 2
        cb = 0 if ti == 0 else 1 + 2 * (ti - 1)
        B = bufs[ti % 2]

        def b(nm):
            return B[nm]

        xa, t_xa = b("xa"); xs, t_xs = b("xs"); x, t_x = b("x")
        la, t_la = b("la"); ls, t_ls = b("ls"); la2, t_la2 = b("la2"); ls2, t_ls2 = b("ls2")
        ga, t_ga = b("ga"); gs, t_gs = b("gs")
        lx, t_lx = b("lx"); lx2, t_lx2 = b("lx2"); gx, t_gx = b("gx"); tw, t_tw = b("tw"); sg, t_sg = b("sg")
        sigw, t_sigw = b("sigw"); a_, t_a = b("a"); g_, t_g = b("g")
        kk, t_kk = b("kk"); sq, t_sq = b("sq"); ss, t_ss = b("ss"); rn, t_rn = b("rn")
        kp, t_kp = b("kp"); t1, t_t1 = b("t1"); bs, t_bs = b("bs")
        cum, t_cum = b("cum"); c1, t_c1 = b("c1"); c2, t_c2 = b("c2")
        ep, t_ep = b("ep"); em, t_em = b("em"); epm, t_epm = b("epm"); ebar, t_ebar = b("ebar")
        ebz, t_ebz = b("ebz")
        PC, t_PC = b("PC"); TM, t_TM = b("TM"); kka, t_kka = b("kka")
        bbz, t_bbz = b("bbz"); kbz, t_kbz = b("kbz")
        FMh = [b("FM0"), b("FM1")]
        M_, t_M = b("M"); RHz = [b("RHz0"), b("RHz1")]
        y_, t_y = b("y"); ysq, t_ysq = b("ysq"); st, t_st = b("st"); yn, t_yn = b("yn"); o_, t_o = b("o")

        P.dma("sp", xa[0:n, :], rkv[1 + t0:1 + t0 + n, :], writes=[t_xa])
        P.dma("sp", xs[0:n, :], rkv[t0:t0 + n, :], writes=[t_xs])
        P.dma("sp", la[:, 0:n], lo[0:64, 1 + t0:1 + t0 + n], writes=[t_la])
        P.dma("sp", ls[:, 0:n], lo[0:64, t0:t0 + n], writes=[t_ls])
        P.dma("sp", la2[:, 0:n], lo[64:128, 1 + t0:1 + t0 + n], writes=[t_la2])
        P.dma("sp", ls2[:, 0:n], lo[64:128, t0:t0 + n], writes=[t_ls2])
        P.dma("sp", ga[:, 0:n], lo[128:256, 1 + t0:1 + t0 + n], writes=[t_ga])
        P.dma("sp", gs[:, 0:n], lo[128:256, t0:t0 + n], writes=[t_gs])
        P.op("pool", lambda e: e.tensor_tensor(xs[0:n, :], xs[0:n, :], xa[0:n, :], ALU.subtract),
             reads=[t_xa, t_xs], writes=[t_xs])
        P.op("pool", lambda e: e.tensor_tensor(xs[0:n, :], xs[0:n, :], mu_tm[0:n, :], ALU.mult),
             reads=[t_xs, t_mutm], writes=[t_xs])
        P.op("pool", lambda e: e.tensor_tensor(x[0:n, :], xs[0:n, :], xa[0:n, :], ALU.add),
             reads=[t_xa, t_xs], writes=[t_x])
        for (A_, tA, S_, tS, O_, tO, col, np_) in ((la, t_la, ls, t_ls, lx, t_lx, 0, 64), (la2, t_la2, ls2, t_ls2, lx2, t_lx2, 1, 64),
                                                  (ga, t_ga, gs, t_gs, gx, t_gx, 2, 128)):
            P.op("pool", lambda e, A_=A_, S_=S_: e.tensor_tensor(S_[:, 0:n], S_[:, 0:n], A_[:, 0:n], ALU.subtract),
                 reads=[tA, tS], writes=[tS])
            P.op("dve", lambda e, A_=A_, S_=S_, O_=O_, col=col, np_=np_: e.scalar_tensor_tensor(O_[:, 0:n], S_[:, 0:n], mu_fm[0:np_, col:col + 1], A_[:, 0:n], ALU.mult, ALU.add),
                 reads=[tA, tS, t_mufm], writes=[tO])
        xr = x[0:n, 0:128]
        xk = x[0:n, 128:256]
        xvb, t_xvb = b("xvb")
        P.op("pool", lambda e: e.tensor_copy(xvb[0:n, :], x[0:n, 256:384]), reads=[t_x], writes=[t_xvb])
        P.op("act", lambda e: e.activation(tw[:, 0:n], lx[:, 0:n], AF.Exp, scale=-2.0), reads=[t_lx], writes=[t_tw])
        P.op("dve", lambda e: e.tensor_scalar(tw[:, 0:n], tw[:, 0:n], 1.0, None, ALU.add), reads=[t_tw], writes=[t_tw])
        P.op("dve", lambda e: e.reciprocal(tw[:, 0:n], tw[:, 0:n]), reads=[t_tw], writes=[t_tw])
        P.op("dve", lambda e: e.tensor_scalar(tw[:, 0:n], tw[:, 0:n], 2.0, -1.0, ALU.mult, ALU.add), reads=[t_tw], writes=[t_tw])
        P.op("act", lambda e: e.activation(sg[:, 0:n], gx[:, 0:n], AF.Exp, scale=-1.0), reads=[t_gx], writes=[t_sg])
        P.op("dve", lambda e: e.tensor_scalar(sg[:, 0:n], sg[:, 0:n], 1.0, None, ALU.add), reads=[t_sg], writes=[t_sg])
        P.op("dve", lambda e: e.reciprocal(sg[:, 0:n], sg[:, 0:n]), reads=[t_sg], writes=[t_sg])
        tb = t_bank[B_LORA]
        P.op("pe", lambda e: e.matmul(pb[B_LORA][0:n, 0, :], tw[:, 0:n], wup[:, :], start=True, stop=True),
             reads=[t_tw, t_wup], writes=[tb])
        P.op("pe", lambda e: e.matmul(pb[B_LORA][0:n, 1, :], lx2[:, 0:n], aup[:, :], start=True, stop=True),
             reads=[t_lx2, t_aup], writes=[tb])
        P.op("pe", lambda e: e.matmul(pb[B_LORA][0:n, 2, :], sg[:, 0:n], gup[:, :], start=True, stop=True),
             reads=[t_sg, t_gup], writes=[tb])
        P.op("dve", lambda e: e.tensor_tensor(sigw[0:n, :], pb[B_LORA][0:n, 0, :], bc[0:n, 0, :], ALU.add),
             reads=[tb, t_bc], writes=[t_sigw])
        P.op("dve", lambda e: e.tensor_tensor(a_[0:n, :], pb[B_LORA][0:n, 1, :], bc[0:n, 1, :], ALU.add),
             reads=[tb, t_bc], writes=[t_a])
        P.op("act", lambda e: e.activation(g_[0:n, :], pb[B_LORA][0:n, 2, :], AF.Identity), reads=[tb], writes=[t_g])
        for (Z, tZ) in ((sigw, t_sigw), (a_, t_a)):
            P.op("act", lambda e, Z=Z: e.activation(Z[0:n, :], Z[0:n, :], AF.Exp, scale=-1.0), reads=[tZ], writes=[tZ])
            P.op("dve", lambda e, Z=Z: e.tensor_scalar(Z[0:n, :], Z[0:n, :], 1.0, None, ALU.add), reads=[tZ], writes=[tZ])
            P.op("dve", lambda e, Z=Z: e.reciprocal(Z[0:n, :], Z[0:n, :]), reads=[tZ], writes=[tZ])
        P.op("pool", lambda e: e.tensor_tensor(kk[0:n, :], xk, bc[0:n, 2, :], ALU.mult), reads=[t_x, t_bc], writes=[t_kk])
        P.op("pool", lambda e: e.tensor_tensor(sq[0:n, :], kk[0:n, :], kk[0:n, :], ALU.mult), reads=[t_kk], writes=[t_sq])
        for h in range(2):
            P.op("dve", lambda e, h=h: e.reduce_sum(ss[0:n, h:h + 1], sq[0:n, 64 * h:64 * h + 64], axis=AX.X),
                 reads=[t_sq], writes=[t_ss])
        P.op("dve", lambda e: e.tensor_scalar(rn[0:n, :], ss[0:n, :], 1e-24, None, ALU.max), reads=[t_ss], writes=[t_rn])
        P.op("act", lambda e: e.activation(rn[0:n, :], rn[0:n, :], AF.Ln), reads=[t_rn], writes=[t_rn])
        P.op("act", lambda e: e.activation(rn[0:n, :], rn[0:n, :], AF.Exp, scale=-0.5), reads=[t_rn], writes=[t_rn])
        for h in range(2):
            P.op("dve", lambda e, h=h: e.tensor_scalar(kk[0:n, 64 * h:64 * h + 64], kk[0:n, 64 * h:64 * h + 64],
                                                       rn[0:n, h:h + 1], None, ALU.mult),
                 reads=[t_kk, t_rn], writes=[t_kk])
        P.op("dve", lambda e: e.scalar_tensor_tensor(t1[0:n, :], a_[0:n, :], -1.0, bc[0:n, 3, :], ALU.add, ALU.mult),
             reads=[t_a, t_bc], writes=[t_t1])
        P.op("dve", lambda e: e.scalar_tensor_tensor(kp[0:n, :], t1[0:n, :], 1.0, xk, ALU.add, ALU.mult),
             reads=[t_t1, t_x], writes=[t_kp])
        P.op("pool", lambda e: e.tensor_tensor(t1[0:n, :], xr, kp[0:n, :], ALU.mult), reads=[t_x, t_kp, t_t1], writes=[t_t1])
        P.op("pool", lambda e: e.tensor_tensor(t1[0:n, :], t1[0:n, :], bc[0:n, 4, :], ALU.mult), reads=[t_t1, t_bc], writes=[t_t1])
        for h in range(2):
            P.op("dve", lambda e, h=h: e.reduce_sum(bs[0:n, h:h + 1], t1[0:n, 64 * h:64 * h + 64], axis=AX.X),
                 reads=[t_t1], writes=[t_bs])
        tb = t_bank[B_CUM]
        P.op("pe", lambda e: e.matmul(pb[B_CUM][0:n, 0, :], cst[0:n, 0, 0:n], sigw[0:n, :], start=True, stop=True),
             reads=[t_cst, t_sigw], writes=[tb])
        P.op("pe", lambda e: e.matmul(pb[B_CUM][0:n, 1, :], cst[0:n, 1, 0:n], sigw[0:n, :], start=True, stop=True),
             reads=[t_cst, t_sigw], writes=[tb])
        for h in range(2):
            P.op("pe", lambda e, h=h: e.matmul(pb[B_CUM][0:64, 2 + h, 0:2], sigw[0:n, 64 * h:64 * h + 64], cst[0:n, 8, 0:2], start=True, stop=True),
                 reads=[t_cst, t_sigw], writes=[tb])
        P.op("dve", lambda e: e.tensor_copy(cum[0:n, :], pb[B_CUM][0:n, 0, :]), reads=[tb], writes=[t_cum])
        P.op("dve", lambda e: e.scalar_tensor_tensor(c1[0:n, :], sigw[0:n, :], DS, cum[0:n, :], ALU.mult, ALU.add),
             reads=[t_sigw, t_cum], writes=[t_c1])
        P.op("dve", lambda e: e.tensor_tensor(c2[0:n, :], pb[B_CUM][0:n, 1, :], cum[0:n, :], ALU.subtract),
             reads=[tb, t_cum], writes=[t_c2])
        P.op("act", lambda e: e.activation(ep[0:n, :], cum[0:n, :], AF.Exp), reads=[t_cum], writes=[t_ep])
        P.op("act", lambda e: e.activation(em[0:n, :], cum[0:n, :], AF.Exp, scale=-1.0), reads=[t_cum], writes=[t_em])
        P.op("act", lambda e: e.activation(epm[0:n, :], c1[0:n, :], AF.Exp), reads=[t_c1], writes=[t_epm])
        P.op("act", lambda e: e.activation(ebar[0:n, :], c2[0:n, :], AF.Exp), reads=[t_c2], writes=[t_ebar])
        P.op("act", lambda e: e.activation(PC[:, :, :], pb[B_CUM][0:64, 2:4, 0:2], AF.Exp), reads=[tb], writes=[t_PC])
        P.op("dve", lambda e: e.tensor_tensor(kka[0:n, :], kk[0:n, :], a_[0:n, :], ALU.mult), reads=[t_kk, t_a], writes=[t_kka])
        P.op("dve", lambda e: e.tensor_tensor(TM[0:n, 0, :], kka[0:n, :], em[0:n, :], ALU.mult), reads=[t_kka, t_em], writes=[t_TM])
        P.op("dve", lambda e: e.tensor_tensor(TM[0:n, 1, :], kp[0:n, :], em[0:n, :], ALU.mult), reads=[t_kp, t_em], writes=[t_TM])
        P.op("dve", lambda e: e.scalar_tensor_tensor(TM[0:n, 2, :], kk[0:n, :], -1.0, epm[0:n, :], ALU.mult, ALU.mult),
             reads=[t_kk, t_epm], writes=[t_TM])
        P.op("dve", lambda e: e.tensor_tensor(TM[0:n, 3, :], xr, ep[0:n, :], ALU.mult), reads=[t_x, t_ep], writes=[t_TM])
        for c in range(nch):
            P.op("pool", lambda e, c=c: e.tensor_scalar(ebz[0:n, c, :], ebar[0:n, :], cst[0:n, 8, 2 + c:3 + c], None, ALU.mult),
                 reads=[t_ebar, t_cst], writes=[t_ebz])
            P.op("pool", lambda e, c=c: e.tensor_tensor(bbz[0:n, c, :], kka[0:n, :], ebz[0:n, c, :], ALU.mult),
                 reads=[t_kka, t_ebz], writes=[t_bbz])
            P.op("pool", lambda e, c=c: e.tensor_tensor(kbz[0:n, c, :], kp[0:n, :], ebz[0:n, c, :], ALU.mult),
                 reads=[t_kp, t_ebz], writes=[t_kbz])
        for h in range(2):
            FM, t_FM = FMh[h]
            bk = B_FM
            for s_ in range(4):
                P.op("pe", lambda e, s_=s_, h=h, bk=bk: e.transpose(pb[bk][0:64, s_, 0:n], TM[0:n, s_, 64 * h:64 * h + 64], ident_bf[0:n, 0:n]),
                     reads=[t_TM, t_idb], writes=[t_bank[bk]])
            P.op("act", lambda e, FM=FM, bk=bk: e.activation(FM[:, :, 0:n], pb[bk][0:64, :, 0:n], AF.Identity),
                 reads=[t_bank[bk]], writes=[t_FM])

        G = [b("G0"), b("G1")]
        NP = [b("NP0"), b("NP1")]
        Xb = [[b("X0a"), b("X0b")], [b("X1a"), b("X1b")]]
        for h in range(2):
            FM, t_FM = FMh[h]
            Gh, t_G = G[h]
            NPh, t_NP = NP[h]
            gbk = B_G
            gb = pb[gbk]
            for (so, sl, sr) in ((0, 0, 2), (1, 0, 3), (2, 1, 2), (3, 1, 3)):
                P.op("pe", lambda e, gb=gb, so=so, sl=sl, sr=sr, FM=FM: e.matmul(gb[0:n, so, 0:n], FM[:, sl, 0:n], FM[:, sr, 0:n], start=True, stop=True),
                     reads=[t_FM], writes=[t_bank[gbk]])
            nbk = B_CUM
            P.op("pe", lambda e, nbk=nbk, FM=FM, h=h: e.matmul(pb[nbk][0:n, 2 + h, 0:n], FM[:, 2, 0:n], FM[:, 0, 0:n], start=True, stop=True),
                 reads=[t_FM], writes=[t_bank[nbk]])
            P.op("dve", lambda e, gb=gb, Gh=Gh: e.tensor_tensor(Gh[0:n, :, 0:n], gb[0:n, :, 0:n], cst[0:n, 2:6, 0:n], ALU.mult),
                 reads=[t_bank[gbk], t_cst], writes=[t_G])
            P.op("dve", lambda e, nbk=nbk, NPh=NPh, h=h: e.tensor_tensor(NPh[0:n, 0, 1, 0:n], pb[nbk][0:n, 2 + h, 0:n], cst[0:n, 6, 0:n], ALU.mult),
                 reads=[t_bank[nbk], t_cst], writes=[t_NP])
            P.op("pool", lambda e, Gh=Gh, NPh=NPh: e.tensor_copy(NPh[0:n, 0, 0, 0:n], Gh[0:n, 0, 0:n]),
                 reads=[t_G], writes=[t_NP])
        P.mark()
        for h in range(2):
            hs = slice(64 * h, 64 * h + 64)
            vs = slice(256 + 64 * h, 256 + 64 * h + 64)
            Gh, t_G = G[h]
            xbk = B_HH[h]
            P.op("pe", lambda e, hs=hs, xbk=xbk: e.matmul(pb[xbk][0:n, 2, 0:64], ident_bf[0:n, 0:n], TM[0:n, 2, hs], start=True, stop=True),
                 reads=[t_TM, t_idb], writes=[t_bank[xbk]])
            P.op("pe", lambda e, hs=hs, xbk=xbk, Gh=Gh: e.matmul(pb[xbk][0:n, 2, 64:128], Gh[0:n, 2, 0:n], xvb[0:n, hs], start=True, stop=True),
                 reads=[t_G, t_xvb], writes=[t_bank[xbk]])
            X0, t_X0 = Xb[h][0]
            P.op("act", lambda e, xbk=xbk, X0=X0: e.activation(X0[0:n, :], pb[xbk][0:n, 2, :], AF.Identity),
                 reads=[t_bank[xbk]], writes=[t_X0])
        NPW = 6
        cur = [0, 0]
        for i in range(NPW):
            for h in range(2):
                NPh, t_NP = NP[h]
                nbk = B_HH[h]
                xbk = B_HH[h]
                Xc, t_Xc = Xb[h][cur[h]]
                Xn, t_Xn = Xb[h][1 - cur[h]]
                P.op("pe", lambda e, i=i, NPh=NPh, Xc=Xc, xbk=xbk: e.matmul(pb[xbk][0:n, 2, :], NPh[0:n, i, 0, 0:n], Xc[0:n, :], start=True, stop=True),
                     reads=[t_NP, t_Xc], writes=[t_bank[xbk]])
                P.op("dve", lambda e, Xc=Xc, Xn=Xn, xbk=xbk: e.tensor_tensor(Xn[0:n, :], pb[xbk][0:n, 2, :], Xc[0:n, :], ALU.add),
                     reads=[t_bank[xbk], t_Xc], writes=[t_Xn])
                cur[h] = 1 - cur[h]
                if i < NPW - 1:
                    P.op("pe", lambda e, i=i, NPh=NPh, nbk=nbk: e.matmul(pb[nbk][0:n, 0, 0:n], NPh[0:n, i, 1, 0:n], NPh[0:n, i, 0, 0:n], start=True, stop=True),
                         reads=[t_NP], writes=[t_bank[nbk]])
                    P.op("pe", lambda e, i=i, NPh=NPh, nbk=nbk: e.matmul(pb[nbk][0:n, 1, 0:n], NPh[0:n, i, 0, 0:n], NPh[0:n, i, 1, 0:n], start=True, stop=True),
                         reads=[t_NP], writes=[t_bank[nbk]])
                    P.op("act", lambda e, i=i, NPh=NPh, nbk=nbk: e.activation(NPh[0:n, i + 1, :, 0:n], pb[nbk][0:n, 0:2, 0:n], AF.Identity),
                         reads=[t_bank[nbk]], writes=[t_NP])
        Xf = [Xb[h][cur[h]] for h in range(2)]
        tb = t_bank[B_PM]
        for h in range(2):
            hs = slice(64 * h, 64 * h + 64)
            Xh, t_Xh = Xf[h]
            for c in range(nch):
                P.op("pe", lambda e, hs=hs, c=c, h=h, Xh=Xh: e.matmul(pb[B_PM][0:64, h, 64 * c:64 * c + 64], Xh[0:n, 0:64], bbz[0:n, c, hs], start=True, stop=True),
                     reads=[t_Xh, t_bbz], writes=[tb])
        for h in range(2):
            for c in range(nch):
                P.op("dve", lambda e, h=h, c=c: e.scalar_tensor_tensor(M_[:, 2 * h + c, :], cst[0:64, 7, 0:64], PC[:, h, c:c + 1], pb[B_PM][0:64, h, 64 * c:64 * c + 64], ALU.mult, ALU.add),
                     reads=[tb, t_PC, t_cst], writes=[t_M])
        tb = t_bank[B_PM]
        for c in range(nch):
            ci = cb + c
            for h in range(2):
                hs = slice(64 * h, 64 * h + 64)
                vs = slice(256 + 64 * h, 256 + 64 * h + 64)
                Xh, t_Xh = Xf[h]
                P.op("pe", lambda e, h=h, c=c, ci=ci: e.matmul(pb[B_PM][0:64, 2 + h, 0:64], M_[:, 2 * h + c, :], S_all[h][:, ci, :], start=True, stop=False),
                     reads=[t_M, t_S[h][ci]], writes=[tb])
                P.op("pe", lambda e, h=h, hs=hs, c=c, Xh=Xh: e.matmul(pb[B_PM][0:64, 2 + h, 0:64], bbz[0:n, c, hs], Xh[0:n, 64:128], start=False, stop=False),
                     reads=[t_bbz, t_Xh], writes=[tb])
                P.op("pe", lambda e, h=h, hs=hs, vs=vs, c=c: e.matmul(pb[B_PM][0:64, 2 + h, 0:64], kbz[0:n, c, hs], xvb[0:n, hs], start=False, stop=True),
                     reads=[t_kbz, t_xvb], writes=[tb])
                P.op("act", lambda e, h=h, ci=ci: e.activation(S_all[h][:, ci + 1, :], pb[B_PM][0:64, 2 + h, 0:64], AF.Identity),
                     reads=[tb], writes=[t_S[h][ci + 1]])
        tb = t_bank[B_Y]
        for h in range(2):
            Xh, t_Xh = Xf[h]
            Gh, t_G = G[h]
            FM, t_FM = FMh[h]
            Rz, t_Rz = RHz[h]
            P.op("pe", lambda e, h=h, Xh=Xh, Gh=Gh: e.matmul(pb[B_Y][0:64, 1 + h, 0:n], Xh[0:n, 0:64], Gh[0:n, 1, 0:n], start=True, stop=True),
                 reads=[t_Xh, t_G], writes=[tb])
            for c in range(nch):
                cs = slice(64 * c, min(64 * c + 64, n))
                P.op("dve", lambda e, h=h, c=c, cs=cs, Rz=Rz, FM=FM: e.tensor_tensor(Rz[:, c, cs], pb[B_Y][0:64, 1 + h, cs], FM[:, 3, cs], ALU.add),
                     reads=[tb, t_FM], writes=[t_Rz])
        tb = t_bank[B_Y]
        for h in range(2):
            hs = slice(64 * h, 64 * h + 64)
            vs = slice(256 + 64 * h, 256 + 64 * h + 64)
            Xh, t_Xh = Xf[h]
            Gh, t_G = G[h]
            Rz, t_Rz = RHz[h]
            P.op("pe", lambda e, hs=hs, Xh=Xh, Gh=Gh: e.matmul(pb[B_Y][0:n, 0, hs], Gh[0:n, 1, 0:n], Xh[0:n, 64:128], start=True, stop=False),
                 reads=[t_G, t_Xh], writes=[tb])
            P.op("pe", lambda e, hs=hs, vs=vs, Gh=Gh: e.matmul(pb[B_Y][0:n, 0, hs], Gh[0:n, 3, 0:n], xvb[0:n, hs], start=False, stop=False),
                 reads=[t_G, t_xvb], writes=[tb])
            for c in range(nch):
                ci = cb + c
                P.op("pe", lambda e, h=h, hs=hs, ci=ci, c=c, Rz=Rz: e.matmul(pb[B_Y][0:n, 0, hs], Rz[:, c, 0:n], S_all[h][:, ci, :], start=False, stop=(c == nch - 1)),
                     reads=[t_Rz, t_S[h][ci]], writes=[tb])
        P.op("act", lambda e: e.activation(y_[0:n, :], pb[B_Y][0:n, 0, :], AF.Identity), reads=[tb], writes=[t_y])
        P.op("pool", lambda e: e.tensor_tensor(ysq[0:n, :], y_[0:n, :], y_[0:n, :], ALU.mult), reads=[t_y], writes=[t_ysq])
        for h in range(2):
            hs = slice(64 * h, 64 * h + 64)
            P.op("dve", lambda e, h=h, hs=hs: e.reduce_sum(st[0:n, h:h + 1], y_[0:n, hs], axis=AX.X), reads=[t_y], writes=[t_st])
            P.op("dve", lambda e, h=h, hs=hs: e.reduce_sum(st[0:n, 2 + h:3 + h], ysq[0:n, hs], axis=AX.X), reads=[t_ysq], writes=[t_st])
        P.op("dve", lambda e: e.tensor_scalar(st[0:n, 4:6], st[0:n, 0:2], 1.0 / 64, None, ALU.mult), reads=[t_st], writes=[t_st])
        P.op("dve", lambda e: e.tensor_tensor(st[0:n, 6:8], st[0:n, 4:6], st[0:n, 4:6], ALU.mult), reads=[t_st], writes=[t_st])
        P.op("dve", lambda e: e.scalar_tensor_tensor(st[0:n, 6:8], st[0:n, 2:4], 1.0 / 64, st[0:n, 6:8], ALU.mult, ALU.subtract), reads=[t_st], writes=[t_st])
        P.op("dve", lambda e: e.tensor_scalar(st[0:n, 6:8], st[0:n, 6:8], LNX_EPS, None, ALU.add), reads=[t_st], writes=[t_st])
        P.op("act", lambda e: e.activation(st[0:n, 6:8], st[0:n, 6:8], AF.Ln), reads=[t_st], writes=[t_st])
        P.op("act", lambda e: e.activation(st[0:n, 6:8], st[0:n, 6:8], AF.Exp, scale=-0.5), reads=[t_st], writes=[t_st])
        for h in range(2):
            hs = slice(64 * h, 64 * h + 64)
            P.op("dve", lambda e, h=h, hs=hs: e.tensor_scalar(yn[0:n, hs], y_[0:n, hs], st[0:n, 4 + h:5 + h], st[0:n, 6 + h:7 + h], ALU.subtract, ALU.mult),
                 reads=[t_y, t_st], writes=[t_yn])
        P.op("pool", lambda e: e.tensor_tensor(yn[0:n, :], yn[0:n, :], bc[0:n, 5, :], ALU.mult), reads=[t_yn, t_bc], writes=[t_yn])
        P.op("pool", lambda e: e.tensor_tensor(yn[0:n, :], yn[0:n, :], bc[0:n, 6, :], ALU.add), reads=[t_yn, t_bc], writes=[t_yn])
        for h in range(2):
            hs = slice(64 * h, 64 * h + 64)
            vs = slice(256 + 64 * h, 256 + 64 * h + 64)
            P.op("dve", lambda e, h=h, hs=hs, vs=vs: e.scalar_tensor_tensor(yn[0:n, hs], x[0:n, vs], bs[0:n, h:h + 1], yn[0:n, hs], ALU.mult, ALU.add),
                 reads=[t_x, t_bs, t_yn], writes=[t_yn])
        P.op("dve", lambda e: e.tensor_tensor(o_[0:n, :], yn[0:n, :], g_[0:n, :], ALU.mult), reads=[t_yn, t_g], writes=[t_o])
        P.dma("sp", o_d[t0:t0 + n, :], o_[0:n, :], reads=[t_o])

    streams = []
    for ti in range(NTX + 1):
        P.begin_stream()
        tile_body(ti)
        streams.append(P.end_stream())
    P.run_interleaved(streams, max_active=int(os.environ.get("RW_ACT", "2")))


def rwkv_host_inputs(p_rw, hp, prm):
    L = p_rw.shape[0]
    cs = slice(128 * hp, 128 * hp + 128)
    rkv = np.zeros((L + 1, 384), np.float32)
    rkv[1:, 0:128] = p_rw[:, 0:512][:, cs]
    rkv[1:, 128:256] = p_rw[:, 512:1024][:, cs]
    rkv[1:, 256:384] = p_rw[:, 1024:1536][:, cs]
    lo = np.zeros((256, L + 1), np.float32)
    lo[:, 1:] = p_rw[:, 1536:1792].T
    mu = prm["rwkv_mu"]
    mu_tm = np.concatenate([mu[0:512][cs], mu[512:1024][cs], mu[1024:1536][cs]])
    mu_tm = np.ascontiguousarray(np.broadcast_to(mu_tm[None, :], (128, 384)))
    mu_fm = np.zeros((128, 3), np.float32)
    mu_fm[0:64, 0] = mu[1536:1600]
    mu_fm[0:64, 1] = mu[1600:1664]
    mu_fm[:, 2] = mu[1664:1792]
    rows = [prm["w0"][cs], prm["a0"][cs], prm["k_k"][cs], prm["k_a"][cs], prm["r_k"].reshape(-1)[cs],
            prm["lnx_g"][cs], prm["lnx_b"][cs]]
    bc = np.ascontiguousarray(np.broadcast_to(np.stack(rows)[None], (128, 7, 128)))
    wa_up = np.ascontiguousarray(np.concatenate([prm["w_up"][:, cs], prm["a_up"][:, cs]], axis=0))
    g_up = np.ascontiguousarray(prm["g_up"][:, cs])
    return {"rkv": rkv, "lo": lo, "mu_tm": mu_tm, "mu_fm": mu_fm, "bc": bc, "wa_up": wa_up, "g_up": g_up,
            "cst": rwkv_consts()}


ALPHA = float((2 * 2) ** 0.25)
LN_EPS = 1e-5
NTOK = N_META + 2048
HALVES = [(0, [(0, 16), (16, 512), (528, 512)], [(0, 16)] + [(16 + 128 * i, 128) for i in range(8)]),
          (1040, [(0, 512), (512, 512)], [(128 * i, 128) for i in range(8)])]
C_IN = 5376


def tok_consts():
    c = np.zeros((128, 2, 128), np.float32)
    c[:, 0, :] = 1.0 / 1024
    c[:, 1, :] = np.eye(128)
    sel = np.zeros((16, 16, 128), np.float32)
    for e in range(16):
        sel[e, e, :] = 1.0
    return c, sel


def build_tok(nc, mode, do_proj, ntok=NTOK, halves=HALVES, n_exp=16):
    D = {}

    def inp(name, shape):
        D[name] = nc.dram_tensor(name, list(shape), F32, kind="ExternalInput").ap()

    def outp(name, shape):
        D[name] = nc.dram_tensor(name, list(shape), F32, kind="ExternalOutput").ap()

    inp("xT", [1024, ntok])
    inp("tc", [128, 2, 128])
    inp("lnA", [128, 2, 8])
    if mode == "C":
        inp("osbT", [512, ntok]); inp("orwT", [512, ntok]); inp("gT", [2048, ntok])
        inp("p_sb", [512, 1024]); inp("p_rw", [512, 1024]); inp("w_out", [1024, 1024])
        inp("lnB", [128, 2, 8])
        inp("router_w", [1024, 16]); inp("rb", [128, 16]); inp("sel", [16, 16, 128])
        inp("wg", [16, 1024, 512]); inp("wu", [16, 1024, 512]); inp("wd", [16, 512, 1024])
    if do_proj:
        inp("w_in", [1024, C_IN])
        outp("pT", [C_IN, ntok])
    outp("hT", [1024, ntok])
    with ExitStack() as es:
        P = Prog(nc, es)
        emit_tok(P, D, mode, do_proj, halves, n_exp)
        P.finish()
    return nc


def emit_tok(P, D, mode, do_proj, halves, n_exp=16):
    WMAX = 1040
    tc = P.sb("tc", [128, 2, 128]); t_tc = P.T(const=True)
    P.dma("sp", tc[:], D["tc"], writes=[t_tc])
    lnA = P.sb("lnA", [128, 2, 8]); t_lnA = P.T(const=True)
    P.dma("sp", lnA[:], D["lnA"], writes=[t_lnA])
    hT = P.sb("hT", [128, 8, WMAX]); hbf = P.sb("hbf", [128, 8, WMAX], BF16)
    NG = 3
    t_h = [[P.T(f"h{g}_{m}") for m in range(8)] for g in range(NG)]
    t_hb = [P.T(f"hb{g}") for g in range(NG)]
    NSTG = 4
    stg = [P.sb(f"stg{i}", [128, 2048]) for i in range(NSTG)]
    t_stg = [P.T() for _ in range(NSTG)]
    WQ = ["sp", "act"]
    ps = [P.ps(f"ps{i}") for i in range(8)]
    t_ps = [P.T(f"psb{i}", excl=True) for i in range(8)]
    mean_sb = P.sb("mean_sb", [128, 512]); t_mean = P.T()
    rstd_sb = P.sb("rstd_sb", [128, 512]); t_rstd = P.T()
    tmp = [P.sb(f"tmp{i}", [128, 512]) for i in range(2)]
    t_tmp = [P.T(), P.T()]
    cnt = {"stg": 0, "tmp": 0, "ev": 0}

    def ln_group(gi, c0, w, lnp, t_lnp):
        PM, PX = 6, 7
        for k in range(8):
            s = cnt["tmp"] % 2; cnt["tmp"] += 1
            P.op("act", lambda e, k=k, s=s: e.activation(tmp[s][:, 0:w], hT[:, k, c0:c0 + w], AF.Square),
                 reads=[t_h[gi][k]], writes=[t_tmp[s]])
            P.op("pe", lambda e, k=k: e.matmul(ps[PM][:, 0:w], tc[:, 0, :], hT[:, k, c0:c0 + w], start=(k == 0), stop=(k == 7)),
                 reads=[t_tc, t_h[gi][k]], writes=[t_ps[PM]])
            P.op("pe", lambda e, k=k, s=s: e.matmul(ps[PX][:, 0:w], tc[:, 0, :], tmp[s][:, 0:w], start=(k == 0), stop=(k == 7)),
                 reads=[t_tc, t_tmp[s]], writes=[t_ps[PX]])
        P.op("act", lambda e: e.activation(mean_sb[:, 0:w], ps[PM][:, 0:w], AF.Identity), reads=[t_ps[PM]], writes=[t_mean])
        P.op("dve", lambda e: e.tensor_tensor(rstd_sb[:, 0:w], mean_sb[:, 0:w], mean_sb[:, 0:w], ALU.mult), reads=[t_mean], writes=[t_rstd])
        P.op("dve", lambda e: e.tensor_tensor(rstd_sb[:, 0:w], ps[PX][:, 0:w], rstd_sb[:, 0:w], ALU.subtract), reads=[t_ps[PX], t_rstd], writes=[t_rstd])
        P.op("dve", lambda e: e.tensor_scalar(rstd_sb[:, 0:w], rstd_sb[:, 0:w], LN_EPS, None, ALU.add), reads=[t_rstd], writes=[t_rstd])
        P.op("act", lambda e: e.activation(rstd_sb[:, 0:w], rstd_sb[:, 0:w], AF.Ln), reads=[t_rstd], writes=[t_rstd])
        P.op("act", lambda e: e.activation(rstd_sb[:, 0:w], rstd_sb[:, 0:w], AF.Exp, scale=-0.5), reads=[t_rstd], writes=[t_rstd])
        for k in range(8):
            s = cnt["tmp"] % 2; cnt["tmp"] += 1
            P.op("dve", lambda e, k=k, s=s: e.tensor_tensor(tmp[s][:, 0:w], hT[:, k, c0:c0 + w], mean_sb[:, 0:w], ALU.subtract),
                 reads=[t_h[gi][k], t_mean], writes=[t_tmp[s]])
            P.op("dve", lambda e, s=s: e.tensor_tensor(tmp[s][:, 0:w], tmp[s][:, 0:w], rstd_sb[:, 0:w], ALU.mult),
                 reads=[t_tmp[s], t_rstd], writes=[t_tmp[s]])
            P.op("act", lambda e, k=k, s=s: e.activation(hT[:, k, c0:c0 + w], tmp[s][:, 0:w], AF.Identity, bias=lnp[:, 1, k:k + 1], scale=lnp[:, 0, k:k + 1]),
                 reads=[t_tmp[s], t_lnp], writes=[t_h[gi][k]])
            P.op("act", lambda e, k=k, s=s: e.activation(hbf[:, k, c0:c0 + w], tmp[s][:, 0:w], AF.Identity, bias=lnp[:, 1, k:k + 1], scale=lnp[:, 0, k:k + 1]),
                 reads=[t_tmp[s], t_lnp], writes=[t_hb[gi]])

    if do_proj:
        wpj = [P.sb(f"wpj{i}", [128, 8, 128], BF16) for i in range(2)]
        t_wpj = [P.T(), P.T()]
        ost = [P.sb(f"ost{i}", [128, 512]) for i in range(4)]
        t_ost = [P.T() for _ in range(4)]
        w_in_v = D["w_in"].rearrange("(k p) c -> p k c", p=128)

    def proj(groups, tok0):
        for j in range(C_IN // 128):
            s = cnt["stg"] % NSTG; wq = WQ[cnt["stg"] % 2]; cnt["stg"] += 1
            wb = j % 2
            P.dma(wq, stg[s][:, 0:1024].rearrange("p (k c) -> p k c", k=8), w_in_v[:, :, 128 * j:128 * j + 128], writes=[t_stg[s]])
            P.op("act", lambda e, s=s, wb=wb: e.activation(wpj[wb][:], stg[s][:, 0:1024].rearrange("p (k c) -> p k c", k=8), AF.Identity),
                 reads=[t_stg[s]], writes=[t_wpj[wb]])
            for gi, (c0, w) in enumerate(groups):
                bk = cnt["ev"] % 4
                for k in range(8):
                    P.op("pe", lambda e, k=k, wb=wb, bk=bk, c0=c0, w=w: e.matmul(ps[bk][:, 0:w], wpj[wb][:, k, :], hbf[:, k, c0:c0 + w], start=(k == 0), stop=(k == 7)),
                         reads=[t_wpj[wb], t_hb[gi]], writes=[t_ps[bk]])
                eng = "dve"
                cnt["ev"] += 1
                if eng == "act":
                    P.op("act", lambda e, bk=bk, w=w: e.activation(ost[bk][:, 0:w], ps[bk][:, 0:w], AF.Identity), reads=[t_ps[bk]], writes=[t_ost[bk]])
                else:
                    P.op("dve", lambda e, bk=bk, w=w: e.tensor_copy(ost[bk][:, 0:w], ps[bk][:, 0:w]), reads=[t_ps[bk]], writes=[t_ost[bk]])
                P.dma("sp", D["pT"][128 * j:128 * j + 128, tok0 + c0:tok0 + c0 + w], ost[bk][:, 0:w], reads=[t_ost[bk]])

    if mode == "C":
        lnB = P.sb("lnB", [128, 2, 8]); t_lnB = P.T(const=True)
        P.dma("sp", lnB[:], D["lnB"], writes=[t_lnB])
        rw = P.sb("rw", [128, 8, 16]); t_rw = P.T(const=True)
        P.dma("sp", rw[:], D["router_w"].rearrange("(k p) e -> p k e", p=128), writes=[t_rw])
        rb = P.sb("rb", [128, 16]); t_rb = P.T(const=True)
        P.dma("sp", rb[:], D["rb"], writes=[t_rb])
        sel = P.sb("sel", [16, 16, 128]); t_sel = P.T(const=True)
        P.dma("sp", sel[:], D["sel"], writes=[t_sel])
        arena = [P.sb(f"arena{i}", [128, 8192], BF16) for i in range(2)]
        t_ar = [P.T("arena0"), P.T("arena1")]
        ob = [P.sb(f"ob{i}", [128, 4, 512], BF16) for i in range(2)]
        t_ob = [P.T(), P.T()]
        gts = [P.sb(f"gts{i}", [128, 512]) for i in range(4)]
        t_gts = [P.T() for _ in range(4)]
        merged = P.sb("merged", [128, 8, 512], BF16); t_merged = P.T()
        combT = P.sb("combT", [16, WMAX]); t_combT = P.T()
        cbc = [P.sb(f"cbc{i}", [128, WMAX]) for i in range(2)]
        t_cbc = [P.T(), P.T()]
        hid = [P.sb(f"hid{i}", [128, 2, 512], BF16) for i in range(2)]
        t_hid = [P.T(), P.T()]
        sgl = [P.sb(f"sgl{i}", [128, 512]) for i in range(2)]
        t_sgl = [P.T(), P.T()]
        rt = P.sb("rt", [128, 16, 16]); t_rt = P.T()
        rs = P.sb("rs", [128, 16]); t_rs = P.T()
        osb_v = D["osbT"].rearrange("(k p) t -> p k t", p=128)
        orw_v = D["orwT"].rearrange("(k p) t -> p k t", p=128)
        psb_v = D["p_sb"].rearrange("(k p) c -> p k c", p=128)
        prw_v = D["p_rw"].rearrange("(k p) c -> p k c", p=128)
        wout_v = D["w_out"].rearrange("(k p) c -> p k c", p=128)
        psb_bf = arena[0][:, 0:4096].rearrange("p (k c) -> p k c", k=4)
        prw_bf = arena[0][:, 4096:8192].rearrange("p (k c) -> p k c", k=4)
        wout_bf = arena[1][:, 0:8192].rearrange("p (k c) -> p k c", k=8)

    xT_v = D["xT"].rearrange("(k p) t -> p k t", p=128)
    hTo_v = D["hT"].rearrange("(k p) t -> p k t", p=128)

    for (tok0, groups, rtiles) in halves:
        for gi, (c0, w) in enumerate(groups):
            P.dma("sp", hT[:, :, c0:c0 + w], xT_v[:, :, tok0 + c0:tok0 + c0 + w], writes=t_h[gi])
        if mode == "A":
            for gi, (c0, w) in enumerate(groups):
                ln_group(gi, c0, w, lnA, t_lnA)
        else:
            for (src, dst, ai, nk) in ((psb_v, psb_bf, 0, 4), (prw_v, prw_bf, 0, 4), (wout_v, wout_bf, 1, 8)):
                for k0 in range(0, nk, 2):
                    s = cnt["stg"] % NSTG; wq = WQ[cnt["stg"] % 2]; cnt["stg"] += 1
                    P.dma(wq, stg[s][:, 0:2048].rearrange("p (k c) -> p k c", k=2), src[:, k0:k0 + 2, :], writes=[t_stg[s]])
                    P.op("act", lambda e, s=s, dst=dst, k0=k0: e.activation(dst[:, k0:k0 + 2, :], stg[s][:, 0:2048].rearrange("p (k c) -> p k c", k=2), AF.Identity),
                         reads=[t_stg[s]], writes=[t_ar[ai]])
            for gi, (c0, w) in enumerate(groups):
                for (src, oi) in ((osb_v, 0), (orw_v, 1)):
                    s = cnt["stg"] % NSTG; wq = WQ[cnt["stg"] % 2]; cnt["stg"] += 1
                    P.dma(wq, stg[s][:, 0:4 * w].rearrange("p (k c) -> p k c", k=4), src[:, :, tok0 + c0:tok0 + c0 + w], writes=[t_stg[s]])
                    P.op("dve", lambda e, s=s, oi=oi, w=w: e.tensor_copy(ob[oi][:, :, 0:w], stg[s][:, 0:4 * w].rearrange("p (k c) -> p k c", k=4)),
                         reads=[t_stg[s]], writes=[t_ob[oi]])
                for m in range(8):
                    ms = slice(128 * m, 128 * m + 128)
                    ba, bb = 0 + (m % 2) * 2, 1 + (m % 2) * 2
                    for k in range(4):
                        P.op("pe", lambda e, k=k, ms=ms, ba=ba, w=w: e.matmul(ps[ba][:, 0:w], psb_bf[:, k, ms], ob[0][:, k, 0:w], start=(k == 0), stop=(k == 3)),
                             reads=[t_ar[0], t_ob[0]], writes=[t_ps[ba]])
                    for k in range(4):
                        P.op("pe", lambda e, k=k, ms=ms, bb=bb, w=w: e.matmul(ps[bb][:, 0:w], prw_bf[:, k, ms], ob[1][:, k, 0:w], start=(k == 0), stop=(k == 3)),
                             reads=[t_ar[0], t_ob[1]], writes=[t_ps[bb]])
                    g0, g1 = (m % 2) * 2, (m % 2) * 2 + 1
                    P.dma("sp", gts[g0][:, 0:w], D["gT"][128 * m:128 * m + 128, tok0 + c0:tok0 + c0 + w], writes=[t_gts[g0]])
                    P.dma("sp", gts[g1][:, 0:w], D["gT"][1024 + 128 * m:1024 + 128 * m + 128, tok0 + c0:tok0 + c0 + w], writes=[t_gts[g1]])
                    P.op("act", lambda e, g0=g0, w=w: e.activation(gts[g0][:, 0:w], gts[g0][:, 0:w], AF.Sigmoid), reads=[t_gts[g0]], writes=[t_gts[g0]])
                    P.op("act", lambda e, g1=g1, w=w: e.activation(gts[g1][:, 0:w], gts[g1][:, 0:w], AF.Sigmoid), reads=[t_gts[g1]], writes=[t_gts[g1]])
                    P.op("dve", lambda e, g0=g0, ba=ba, w=w: e.tensor_tensor(gts[g0][:, 0:w], ps[ba][:, 0:w], gts[g0][:, 0:w], ALU.mult),
                         reads=[t_ps[ba], t_gts[g0]], writes=[t_gts[g0]])
                    P.op("dve", lambda e, g1=g1, bb=bb, w=w: e.tensor_tensor(gts[g1][:, 0:w], ps[bb][:, 0:w], gts[g1][:, 0:w], ALU.mult),
                         reads=[t_ps[bb], t_gts[g1]], writes=[t_gts[g1]])
                    P.op("pool", lambda e, g0=g0, g1=g1, m=m, w=w: e.tensor_tensor(merged[:, m, 0:w], gts[g0][:, 0:w], gts[g1][:, 0:w], ALU.add),
                         reads=[t_gts[g0], t_gts[g1]], writes=[t_merged])
                for m in range(8):
                    ms = slice(128 * m, 128 * m + 128)
                    bk = 4 + (m % 2)
                    for k in range(8):
                        P.op("pe", lambda e, k=k, ms=ms, bk=bk, w=w: e.matmul(ps[bk][:, 0:w], wout_bf[:, k, ms], merged[:, k, 0:w], start=(k == 0), stop=(k == 7)),
                             reads=[t_ar[1], t_merged], writes=[t_ps[bk]])
                    P.op("dve", lambda e, m=m, bk=bk, c0=c0, w=w: e.scalar_tensor_tensor(hT[:, m, c0:c0 + w], hT[:, m, c0:c0 + w], ALPHA, ps[bk][:, 0:w], ALU.mult, ALU.add),
                         reads=[t_ps[bk], t_h[gi][m]], writes=[t_h[gi][m]])
                ln_group(gi, c0, w, lnA, t_lnA)
            for (r0, nt) in rtiles:
                gi = [i for i, (c0, w) in enumerate(groups) if c0 <= r0 < c0 + w][0]
                RB = 5
                for k in range(8):
                    P.op("pe", lambda e, k=k, r0=r0, nt=nt: e.matmul(ps[RB][0:nt, 0:16], hT[:, k, r0:r0 + nt], rw[:, k, :], start=(k == 0), stop=(k == 7)),
                         reads=[t_h[gi][k], t_rw], writes=[t_ps[RB]])
                R = lambda i: rt[0:nt, i, :]
                S = lambda i: rs[0:nt, i:i + 1]

                def dv(fn, nt=nt):
                    P.op("dve", fn, reads=[t_rt, t_rs], writes=[t_rt, t_rs])
                P.op("dve", lambda e, nt=nt: e.tensor_tensor(rt[0:nt, 0, :], ps[RB][0:nt, 0:16], rb[0:nt, :], ALU.add),
                     reads=[t_ps[RB], t_rb], writes=[t_rt])
                dv(lambda e, nt=nt: e.reduce_max(rs[0:nt, 0:1], rt[0:nt, 0, :], axis=AX.X))
                dv(lambda e, nt=nt: e.tensor_scalar(rs[0:nt, 0:1], rs[0:nt, 0:1], -1.0, None, ALU.mult))
                P.op("act", lambda e, nt=nt: e.activation(rt[0:nt, 1, :], rt[0:nt, 0, :], AF.Exp, bias=rs[0:nt, 0:1]),
                     reads=[t_rt, t_rs], writes=[t_rt])
                dv(lambda e, nt=nt: e.reduce_sum(rs[0:nt, 1:2], rt[0:nt, 1, :], axis=AX.X))
                dv(lambda e, nt=nt: e.reciprocal(rs[0:nt, 1:2], rs[0:nt, 1:2]))
                dv(lambda e, nt=nt: e.tensor_scalar(rt[0:nt, 2, :], rt[0:nt, 1, :], rs[0:nt, 1:2], None, ALU.mult))
                for g in range(4):
                    dv(lambda e, nt=nt, g=g: e.reduce_max(rt[0:nt, 3, g:g + 1], rt[0:nt, 2, 4 * g:4 * g + 4], axis=AX.X))
                for g in range(4):
                    dv(lambda e, nt=nt, g=g: e.tensor_scalar(rt[0:nt, 4, 4 * g:4 * g + 4], rt[0:nt, 2, 4 * g:4 * g + 4], rt[0:nt, 3, g:g + 1], None, ALU.is_equal))
                dv(lambda e, nt=nt: e.scalar_tensor_tensor(rt[0:nt, 5, :], rt[0:nt, 4, :], -2.0, rt[0:nt, 2, :], ALU.mult, ALU.add))
                for g in range(4):
                    dv(lambda e, nt=nt, g=g: e.reduce_max(rt[0:nt, 3, 4 + g:5 + g], rt[0:nt, 5, 4 * g:4 * g + 4], axis=AX.X))
                dv(lambda e, nt=nt: e.tensor_tensor(rt[0:nt, 3, 8:12], rt[0:nt, 3, 0:4], rt[0:nt, 3, 4:8], ALU.add))
                dv(lambda e, nt=nt: e.reduce_max(rs[0:nt, 2:3], rt[0:nt, 3, 8:12], axis=AX.X))
                dv(lambda e, nt=nt: e.tensor_scalar(rt[0:nt, 3, 12:16], rt[0:nt, 3, 8:12], rs[0:nt, 2:3], None, ALU.is_equal))
                for g in range(4):
                    dv(lambda e, nt=nt, g=g: e.tensor_scalar(rt[0:nt, 6, 4 * g:4 * g + 4], rt[0:nt, 2, 4 * g:4 * g + 4], 1.0, rt[0:nt, 3, 12 + g:13 + g], ALU.add, ALU.mult))
                dv(lambda e, nt=nt: e.tensor_scalar(rt[0:nt, 6, :], rt[0:nt, 6, :], -1.0, None, ALU.add))
                dv(lambda e, nt=nt: e.reduce_max(rs[0:nt, 3:4], rt[0:nt, 6, :], axis=AX.X))
                dv(lambda e, nt=nt: e.tensor_scalar(rt[0:nt, 7, :], rt[0:nt, 6, :], rs[0:nt, 3:4], None, ALU.is_equal))
                dv(lambda e, nt=nt: e.scalar_tensor_tensor(rt[0:nt, 8, :], rt[0:nt, 7, :], -2.0, rt[0:nt, 6, :], ALU.mult, ALU.add))
                dv(lambda e, nt=nt: e.reduce_max(rs[0:nt, 4:5], rt[0:nt, 8, :], axis=AX.X))
                dv(lambda e, nt=nt: e.tensor_scalar(rt[0:nt, 9, :], rt[0:nt, 8, :], rs[0:nt, 4:5], None, ALU.is_equal))
                dv(lambda e, nt=nt: e.tensor_tensor(rs[0:nt, 5:6], rs[0:nt, 3:4], rs[0:nt, 4:5], ALU.add))
                dv(lambda e, nt=nt: e.reciprocal(rs[0:nt, 5:6], rs[0:nt, 5:6]))
                dv(lambda e, nt=nt: e.tensor_tensor(rs[0:nt, 6:7], rs[0:nt, 3:4], rs[0:nt, 5:6], ALU.mult))
                dv(lambda e, nt=nt: e.tensor_tensor(rs[0:nt, 7:8], rs[0:nt, 4:5], rs[0:nt, 5:6], ALU.mult))
                dv(lambda e, nt=nt: e.tensor_scalar(rt[0:nt, 10, :], rt[0:nt, 7, :], rs[0:nt, 6:7], None, ALU.mult))
                dv(lambda e, nt=nt: e.scalar_tensor_tensor(rt[0:nt, 11, :], rt[0:nt, 9, :], rs[0:nt, 7:8], rt[0:nt, 10, :], ALU.mult, ALU.add))
                P.op("pe", lambda e, nt=nt: e.transpose(ps[RB][0:16, 256:256 + nt], rt[0:nt, 11, :], tc[0:nt, 1, 0:nt]),
                     reads=[t_rt, t_tc], writes=[t_ps[RB]])
                P.op("act", lambda e, nt=nt, r0=r0: e.activation(combT[:, r0:r0 + nt], ps[RB][0:16, 256:256 + nt], AF.Identity),
                     reads=[t_ps[RB]], writes=[t_combT])
            for gi, (c0, w) in enumerate(groups):
                P.op("pool", lambda e, c0=c0, w=w: e.tensor_scalar(hT[:, :, c0:c0 + w], hT[:, :, c0:c0 + w], ALPHA, None, ALU.mult),
                     reads=t_h[gi], writes=t_h[gi])
            nhe = 0
            pending = []
            for ex in range(n_exp):
                cb_ = ex % 2
                for gi, (c0, w) in enumerate(groups):
                    P.op("pe", lambda e, ex=ex, c0=c0, w=w: e.matmul(ps[6][:, 0:w], sel[:, ex, :], combT[:, c0:c0 + w], start=True, stop=True),
                         reads=[t_sel, t_combT], writes=[t_ps[6]])
                    P.op("act", lambda e, cb_=cb_, c0=c0, w=w: e.activation(cbc[cb_][:, c0:c0 + w], ps[6][:, 0:w], AF.Identity),
                         reads=[t_ps[6]], writes=[t_cbc[cb_]])
                for hf in range(2):
                    ai = nhe % 2
                    nhe += 1
                    wg_bf = arena[ai][:, 0:2048].rearrange("p (k c) -> p k c", k=8)
                    wu_bf = arena[ai][:, 2048:4096].rearrange("p (k c) -> p k c", k=8)
                    wd_bf = arena[ai][:, 4096:6144].rearrange("p (k c) -> p k c", k=2)
                    fs = slice(256 * hf, 256 * hf + 256)
                    for (src, dst, kk_) in ((D["wg"][ex].rearrange("(k p) f -> p k f", p=128)[:, :, fs], wg_bf, 8),
                                            (D["wu"][ex].rearrange("(k p) f -> p k f", p=128)[:, :, fs], wu_bf, 8),
                                            (D["wd"][ex, 256 * hf:256 * hf + 256, :].rearrange("(k p) d -> p k d", p=128), wd_bf, 2)):
                        s = cnt["stg"] % NSTG; wq = WQ[cnt["stg"] % 2]; cnt["stg"] += 1
                        P.dma(wq, stg[s][:, 0:2048].rearrange("p (k c) -> p k c", k=kk_), src, writes=[t_stg[s]])
                        P.op("act", lambda e, s=s, dst=dst, kk_=kk_: e.activation(dst, stg[s][:, 0:2048].rearrange("p (k c) -> p k c", k=kk_), AF.Identity),
                             reads=[t_stg[s]], writes=[t_ar[ai]])
                    for gi, (c0, w) in enumerate(groups):
                        hb_ = cnt["ev"] % 2
                        cnt["ev"] += 1
                        for fc in range(2):
                            bg, bu = fc, 2 + fc
                            fcs = slice(128 * fc, 128 * fc + 128)
                            for k in range(8):
                                P.op("pe", lambda e, k=k, bg=bg, fcs=fcs, c0=c0, w=w, wg_bf=wg_bf: e.matmul(ps[bg][:, 0:w], wg_bf[:, k, fcs], hbf[:, k, c0:c0 + w], start=(k == 0), stop=(k == 7)),
                                     reads=[t_ar[ai], t_hb[gi]], writes=[t_ps[bg]])
                            for k in range(8):
                                P.op("pe", lambda e, k=k, bu=bu, fcs=fcs, c0=c0, w=w, wu_bf=wu_bf: e.matmul(ps[bu][:, 0:w], wu_bf[:, k, fcs], hbf[:, k, c0:c0 + w], start=(k == 0), stop=(k == 7)),
                                     reads=[t_ar[ai], t_hb[gi]], writes=[t_ps[bu]])
                            P.op("act", lambda e, fc=fc, bg=bg, w=w: e.activation(sgl[fc][:, 0:w], ps[bg][:, 0:w], AF.Silu),
                                 reads=[t_ps[bg]], writes=[t_sgl[fc]])
                            P.op("dve", lambda e, fc=fc, bu=bu, w=w: e.tensor_tensor(sgl[fc][:, 0:w], ps[bu][:, 0:w], sgl[fc][:, 0:w], ALU.mult),
                                 reads=[t_ps[bu], t_sgl[fc]], writes=[t_sgl[fc]])
                            P.op("dve", lambda e, fc=fc, hb_=hb_, cb_=cb_, c0=c0, w=w: e.tensor_tensor(hid[hb_][:, fc, 0:w], sgl[fc][:, 0:w], cbc[cb_][:, c0:c0 + w], ALU.mult),
                                 reads=[t_sgl[fc], t_cbc[cb_]], writes=[t_hid[hb_]])
                        if pending:
                            pending.pop()()

                        def down(gi=gi, c0=c0, w=w, hb_=hb_, ai=ai, wd_bf=wd_bf):
                            for m in range(8):
                                bd = 4 + (m % 4)
                                ms = slice(128 * m, 128 * m + 128)
                                for fc in range(2):
                                    P.op("pe", lambda e, fc=fc, bd=bd, ms=ms: e.matmul(ps[bd][:, 0:w], wd_bf[:, fc, ms], hid[hb_][:, fc, 0:w], start=(fc == 0), stop=(fc == 1)),
                                         reads=[t_ar[ai], t_hid[hb_]], writes=[t_ps[bd]])
                                P.op("dve", lambda e, m=m, bd=bd: e.tensor_tensor(hT[:, m, c0:c0 + w], ps[bd][:, 0:w], hT[:, m, c0:c0 + w], ALU.add),
                                     reads=[t_ps[bd], t_h[gi][m]], writes=[t_h[gi][m]])
                        pending.append(down)
            if pending:
                pending.pop()()
            for gi, (c0, w) in enumerate(groups):
                ln_group(gi, c0, w, lnB, t_lnB)
        for gi, (c0, w) in enumerate(groups):
            P.dma("sp", hTo_v[:, :, tok0 + c0:tok0 + c0 + w], hT[:, :, c0:c0 + w], reads=t_h[gi])
        if do_proj:
            proj(groups, tok0)


NCORES = 8
SEQ = 8192
LFULL = N_META + SEQ


def _fm(v):
    return np.ascontiguousarray(np.asarray(v, np.float32).reshape(8, 128).T)


def _run(nc, in_maps):
    res = run_bass_kernel_spmd(nc, in_maps, core_ids=list(range(NCORES)))
    return res.results


def _new_nc():
    return bass.Bass("TRN2", target_bir_lowering=False)


def _mixers(pT_cores, prm):
    amask = attn_masks()
    attn_maps, rwkv_maps = [], []
    for c in range(NCORES):
        b, hp = c // 4, c % 4
        pb_ = np.concatenate([pT_cores[4 * b][:, 0:N_META]] + [pT_cores[4 * b + r][:, N_META:] for r in range(4)], axis=1)
        q = pb_[128 * hp:128 * hp + 128].reshape(2, 64, LFULL)
        k = pb_[512 + 128 * hp:512 + 128 * hp + 128].reshape(2, 64, LFULL)
        v = pb_[1024 + 128 * hp:1024 + 128 * hp + 128].reshape(2, 64, LFULL)
        vtm = v.transpose(0, 2, 1)
        vm = np.ascontiguousarray(vtm[:, 0:N_META])
        vx = np.ascontiguousarray(vtm[:, N_META:].reshape(2, SEQ // 128, 128, 64).transpose(0, 2, 1, 3))
        attn_maps.append({"qT": np.ascontiguousarray(q), "kT": np.ascontiguousarray(k), "vx": vx, "vm": vm, "msk": amask})
        p_rw = np.ascontiguousarray(pb_[1536:3328].T)
        rwkv_maps.append(rwkv_host_inputs(p_rw, hp, prm))
    nc = _new_nc()
    build_attn(nc, 2, SEQ // 512)
    ares = _run(nc, attn_maps)
    nc = _new_nc()
    build_rwkv(nc, SEQ // 128)
    rres = _run(nc, rwkv_maps)
    osbT, orwT = [], []
    for b in range(2):
        osbT.append(np.concatenate([ares[4 * b + hp]["oT"].reshape(128, LFULL) for hp in range(4)], axis=0))
        orwT.append(np.concatenate([rres[4 * b + hp]["o_rw"].T for hp in range(4)], axis=0))
    return osbT, orwT


def _core_cols(full, r):
    return np.ascontiguousarray(np.concatenate([full[:, 0:N_META], full[:, N_META + 2048 * r:N_META + 2048 * (r + 1)]], axis=1))


def kernel(**inputs):
    inp = {k: np.asarray(v) for k, v in inputs.items()}
    x, meta = inp["x"].astype(np.float32), inp["meta"].astype(np.float32)
    tcc, sel = tok_consts()
    maps = []
    for c in range(NCORES):
        b, r = c // 4, c % 4
        xT = np.ascontiguousarray(np.concatenate([meta.T, x[b, 2048 * r:2048 * (r + 1)].T], axis=1))
        maps.append({"xT": xT, "tc": tcc, "lnA": np.stack([_fm(inp["emb_ln_g"]), _fm(inp["emb_ln_b"])], axis=1),
                     "w_in": np.ascontiguousarray(inp["w_in"][0])})
    nc = _new_nc()
    build_tok(nc, "A", True)
    res = _run(nc, maps)
    hT_c = [r_["hT"] for r_ in res]
    pT_c = [r_["pT"] for r_ in res]
    for l in range(2):
        prm = {k: inp[k][l] for k in ["rwkv_mu", "w0", "w_up", "a0", "a_up", "g_up", "k_k", "k_a", "r_k", "lnx_g", "lnx_b"]}
        osbT, orwT = _mixers(pT_c, prm)
        last = (l == 1)
        maps = []
        for c in range(NCORES):
            b, r = c // 4, c % 4
            m = {"xT": hT_c[c], "tc": tcc, "lnA": np.stack([_fm(inp["ln1_g"][l]), _fm(inp["ln1_b"][l])], axis=1),
                 "osbT": _core_cols(osbT[b], r), "orwT": _core_cols(orwT[b], r),
                 "gT": np.ascontiguousarray(pT_c[c][3328:5376]),
                 "p_sb": np.ascontiguousarray(inp["p_sb"][l]), "p_rw": np.ascontiguousarray(inp["p_rwkv"][l]),
                 "w_out": np.ascontiguousarray(inp["w_out"][l]),
                 "lnB": np.stack([_fm(inp["ln2_g"][l]), _fm(inp["ln2_b"][l])], axis=1),
                 "router_w": np.ascontiguousarray(inp["router_w"]),
                 "rb": np.ascontiguousarray(np.broadcast_to(inp["router_b"][None].astype(np.float32), (128, 16))),
                 "sel": sel,
                 "wg": np.ascontiguousarray(inp["exp_w_gate"][l]), "wu": np.ascontiguousarray(inp["exp_w_up"][l]),
                 "wd": np.ascontiguousarray(inp["exp_w_down"][l])}
            if not last:
                m["w_in"] = np.ascontiguousarray(inp["w_in"][l + 1])
            maps.append(m)
        nc = _new_nc()
        build_tok(nc, "C", not last)
        res = _run(nc, maps)
        hT_c = [r_["hT"] for r_ in res]
        if not last:
            pT_c = [r_["pT"] for r_ in res]
    out = np.zeros((2, SEQ, 1024), np.float32)
    for c in range(NCORES):
        b, r = c // 4, c % 4
        out[b, 2048 * r:2048 * (r + 1)] = hT_c[c][:, N_META:].T
    return out
```

```python
import numpy as np
from contextlib import ExitStack
import concourse.bass as bass
import concourse.mybir as mybir
from concourse.bass_utils import run_bass_kernel_spmd

F32 = mybir.dt.float32
BF16 = mybir.dt.bfloat16
AF = mybir.ActivationFunctionType
ALU = mybir.AluOpType
AX = mybir.AxisListType

import os
BUDGET = int(os.environ["KBUDGET"]) if "KBUDGET" in os.environ else None
ENGS = ["pe", "act", "dve", "pool", "sp"]
DMA_R = 8


class T:
    __slots__ = ("name", "lw", "rd", "const", "excl")

    def __init__(self, name, const=False, excl=False):
        self.name = name
        self.lw = None
        self.rd = []
        self.const = const
        self.excl = excl


class Prog:
    def __init__(self, nc, es):
        self.nc = nc
        self.es = es
        self.items = {e: [] for e in ENGS}
        self.sem = {e: es.enter_context(nc.semaphore("s_" + e)) for e in ENGS}
        self.cnt = {e: 0 for e in ENGS}
        self.seen = {e: {} for e in ENGS}
        self.dsem = {}
        self.dma_n = {}
        self.dma_final = {}
        self.ntile = 0

    def sb(self, name, shape, dt=F32):
        return self.es.enter_context(self.nc.sbuf_tensor("sb_" + name, list(shape), dt))

    def ps(self, name, shape=(128, 512), dt=F32):
        return self.es.enter_context(self.nc.psum_tensor("pp_" + name, list(shape), dt))

    def T(self, name=None, const=False, excl=False):
        self.ntile += 1
        return T(name or f"t{self.ntile}", const, excl)

    def _deps(self, eng, reads, writes):
        deps = {}

        def add(p):
            if p is None:
                return
            k, v = p
            if deps.get(k, 0) < v:
                deps[k] = v

        for t in reads:
            add(t.lw)
        for t in writes:
            add(t.lw)
            for p in t.rd:
                add(p)
        waits = []
        for k, v in deps.items():
            if k == eng and eng == "pe":
                continue
            if self.seen[eng].get(k, 0) >= v:
                continue
            self.seen[eng][k] = v
            waits.append((k, v))
        return waits

    def _commit(self, pid, reads, writes):
        for t in writes:
            t.lw = pid
            t.rd = []
        for t in reads:
            if not t.const:
                t.rd.append(pid)
                if len(t.rd) > 64:
                    m = {}
                    for k, v in t.rd:
                        if m.get(k, 0) < v:
                            m[k] = v
                    t.rd = list(m.items())

    def begin_stream(self):
        self._rec = []

    def end_stream(self):
        r, self._rec = self._rec, None
        return r

    def mark(self):
        if getattr(self, "_rec", None) is not None:
            self._rec.append(("mark", ()))

    def run_interleaved(self, streams, max_active=2):
        streams = list(streams)
        active = []
        nxt = 0
        while nxt < len(streams) or active:
            if nxt < len(streams) and len(active) < max_active and (not active or active[-1][2] >= 1):
                active.append([streams[nxt], 0, 0])
                nxt += 1
            for idx, a in enumerate(list(active)):
                if a[1] >= len(a[0]):
                    continue
                kind, args = a[0][a[1]]
                if kind == "mark":
                    if idx > 0:
                        older = active[idx - 1]
                        if older[1] < len(older[0]) and older[2] < a[2] + 2:
                            continue
                    a[2] += 1
                    a[1] += 1
                    continue
                a[1] += 1
                (self.op if kind == "op" else self.dma)(*args)
            active = [a for a in active if a[1] < len(a[0])]

    def op(self, eng, fn, reads=(), writes=()):
        if getattr(self, "_rec", None) is not None:
            self._rec.append(("op", (eng, fn, tuple(reads), tuple(writes))))
            return
        self.nops = getattr(self, "nops", 0) + 1
        if BUDGET is not None and self.nops > BUDGET:
            return
        ex = [t for t in reads if t.excl]
        if ex:
            reads = [t for t in reads if not t.excl]
            writes = list(writes) + ex
        waits = self._deps(eng, reads, writes)
        self.cnt[eng] += 1
        pid = (eng, self.cnt[eng])
        self.items[eng].append((waits, fn, True))
        self._commit(pid, reads, writes)

    def dma(self, q, out_ap, in_ap, reads=(), writes=()):
        if getattr(self, "_rec", None) is not None:
            self._rec.append(("dma", (q, out_ap, in_ap, tuple(reads), tuple(writes))))
            return
        self.nops = getattr(self, "nops", 0) + 1
        if BUDGET is not None and self.nops > BUDGET:
            return
        if q not in self.dsem:
            self.dsem[q] = [self.es.enter_context(self.nc.semaphore(f"d_{q}_{i}")) for i in range(DMA_R)]
            self.dma_n[q] = 0
        n = self.dma_n[q]
        self.dma_n[q] += 1
        i = n % DMA_R
        rnd = n // DMA_R
        key = ("d", q, i)
        waits = self._deps(q, reads, writes)
        if rnd > 0 and self.seen[q].get(key, 0) < 16 * rnd:
            self.seen[q][key] = 16 * rnd
            waits.append((key, 16 * rnd))
        sem = self.dsem[q][i]

        def fn(e, out_ap=out_ap, in_ap=in_ap, sem=sem):
            return e.dma_start(out=out_ap, in_=in_ap).then_inc(sem, 16)

        self.items[q].append((waits, fn, False))
        pid = (key, 16 * (rnd + 1))
        self.dma_final[key] = 16 * (rnd + 1)
        self._commit(pid, reads, writes)

    def _semobj(self, k):
        if isinstance(k, tuple):
            return self.dsem[k[1]][k[2]]
        return self.sem[k]

    def finish(self):
        waits = []
        for key, v in self.dma_final.items():
            waits.append((key, v))
        for e in ENGS:
            if e != "sp" and self.cnt[e] > 0:
                waits.append((e, self.cnt[e]))
        self.items["sp"].append((waits, None, False))
        nc = self.nc
        with nc.Block() as block:
            def replay(name, e):
                for waits, fn, inc in self.items[name]:
                    for k, v in waits:
                        e.wait_ge(self._semobj(k), v)
                    if fn is None:
                        continue
                    ins = fn(e)
                    if inc:
                        ins.then_inc(self.sem[name], 1)

            @block.tensor
            def _(e):
                replay("pe", e)

            @block.scalar
            def _(e):
                replay("act", e)

            @block.vector
            def _(e):
                replay("dve", e)

            @block.gpsimd
            def _(e):
                replay("pool", e)

            @block.sync
            def _(e):
                replay("sp", e)


N_META = 16


def build_attn(nc, NH, NG):
    NB = 4 * NG
    L = N_META + 512 * NG
    qT = nc.dram_tensor("qT", [NH, 64, L], F32, kind="ExternalInput").ap()
    kT = nc.dram_tensor("kT", [NH, 64, L], F32, kind="ExternalInput").ap()
    vx = nc.dram_tensor("vx", [NH, 128, NB, 64], F32, kind="ExternalInput").ap()
    vm = nc.dram_tensor("vm", [NH, 16, 64], F32, kind="ExternalInput").ap()
    msk = nc.dram_tensor("msk", [128, 3, 128], F32, kind="ExternalInput").ap()
    oT = nc.dram_tensor("oT", [NH, 64, L], F32, kind="ExternalOutput").ap()
    with ExitStack() as es:
        P = Prog(nc, es)
        emit_attn(P, NH, NG, qT, kT, vx, vm, msk, oT)
        P.finish()
    return nc


def emit_attn(P, NH, NG, qT, kT, vx, vm, msk, oT):
    NB = 4 * NG
    L = N_META + 512 * NG
    mstage = P.sb("mstage", [128, 3, 128], F32)
    t_mstage = P.T()
    P.dma("sp", mstage[:], msk, writes=[t_mstage])
    cm = P.sb("cm", [128, 3, 128], BF16)
    t_cm = P.T(const=True)
    P.op("dve", lambda e: e.tensor_copy(cm[:], mstage[:]), reads=[t_mstage], writes=[t_cm])
    zeros = P.sb("zeros", [128, 64], BF16)
    t_zeros = P.T(const=True)
    P.op("pool", lambda e: e.memset(zeros[:], 0.0), writes=[t_zeros])

    QT = P.sb("QT", [64, L], BF16)
    KT = P.sb("KT", [64, L], BF16)
    V = P.sb("V", [128, NB, 64], BF16)
    VM = P.sb("VM", [16, 64], BF16)
    t_Q, t_K, t_V = P.T(), P.T(), P.T()
    CH = 2064 if L > 2064 else L
    stg = [P.sb(f"stg{i}", [128, CH], F32) for i in range(2)]
    t_stg = [P.T(), P.T()]
    vstg = P.sb("vstg", [128, NB, 64], F32)
    t_vstg = P.T()
    vmstg = P.sb("vmstg", [16, 64], F32)
    t_vmstg = P.T()

    zA = [P.ps(f"zA{i}") for i in range(2)]
    zB = [P.ps(f"zB{i}") for i in range(2)]
    Ops = [P.ps(f"Ops{i}") for i in range(2)]
    t_zA = [P.T(excl=True), P.T(excl=True)]
    t_zB = [P.T(excl=True), P.T(excl=True)]
    t_O = [P.T(excl=True), P.T(excl=True)]
    e_sb = [P.sb(f"e{i}", [128, 512], F32) for i in range(2)]
    t_e = [P.T(), P.T()]
    sp_sb = [P.sb(f"sp{i}", [128, 512], BF16) for i in range(2)]
    t_sp = [P.T(), P.T()]
    A_sb = [P.sb(f"A{i}", [128, 512], BF16) for i in range(2)]
    t_A = [P.T(), P.T()]
    acc = P.sb("acc", [128, 512], F32)
    t_acc = P.T()
    accb = [P.sb(f"accb{i}", [128, 512], BF16) for i in range(3)]
    t_accb = [P.T(), P.T(), P.T()]
    ost = [P.sb(f"ost{i}", [64, 512], F32) for i in range(2)]
    t_ost = [P.T(), P.T()]

    nstg = 0
    gcount = 0
    for h in range(NH):
        for (src, dst, tt, scale) in ((qT, QT, t_Q, 0.125), (kT, KT, t_K, 1.0)):
            first = True
            for c0 in range(0, L, CH):
                w = min(CH, L - c0)
                s = nstg % 2
                nstg += 1
                P.dma("sp", stg[s][0:64, 0:w], src[h, :, c0:c0 + w], writes=[t_stg[s]])
                if scale != 1.0:
                    P.op("dve", lambda e, s=s, w=w, c0=c0, dst=dst, scale=scale:
                         e.tensor_scalar(dst[:, c0:c0 + w], stg[s][0:64, 0:w], scale, None, ALU.mult),
                         reads=[t_stg[s]], writes=[tt])
                else:
                    P.op("pool", lambda e, s=s, w=w, c0=c0, dst=dst:
                         e.tensor_copy(dst[:, c0:c0 + w], stg[s][0:64, 0:w]),
                         reads=[t_stg[s]], writes=[tt])
        P.dma("sp", vstg[:], vx[h], writes=[t_vstg])
        P.op("pool", lambda e: e.tensor_copy(V[:], vstg[:]), reads=[t_vstg], writes=[t_V])
        P.dma("sp", vmstg[:], vm[h], writes=[t_vmstg])
        P.op("pool", lambda e: e.tensor_copy(VM[:], vmstg[:]), reads=[t_vmstg], writes=[t_V])

        for g in [-1] + list(range(NG)):
            if g < 0:
                q0, QW = 0, N_META
                blocks = [("m", 0)]
            else:
                q0, QW = N_META + 512 * g, 512
                blocks = [("x", j) for j in range(4 * g + 3, -1, -1)] + [("m", 0)]
            ob = gcount % 2
            gcount += 1
            P.op("pe", lambda e, ob=ob, QW=QW, q0=q0:
                 e.matmul(Ops[ob][0:64, 0:QW], zeros[0:64, 0:64], QT[:, q0:q0 + QW], start=True, stop=False),
                 reads=[t_zeros, t_Q], writes=[t_O[ob]])
            P.op("pool", lambda e: e.memset(acc[:], 0.0), writes=[t_acc])
            nblk = len(blocks)
            info = []
            for it, (kind, j) in enumerate(blocks):
                if kind == "m":
                    kp, k0 = N_META, 0
                    c0 = 0
                    diag = (g < 0)
                else:
                    kp, k0 = 128, N_META + 128 * j
                    jl = j - 4 * g
                    diag = jl >= 0
                    c0 = 128 * jl if diag else 0
                info.append((kind, j, kp, k0, c0, diag))

            def prm(it, info=info, q0=q0, QW=QW):
                kind, j, kp, k0, c0, diag = info[it]
                W = QW - c0
                return kind, j, kp, k0, c0, diag, it % 2, W, slice(q0 + c0, q0 + QW), min(128, W)

            def pe_zA(it):
                kind, j, kp, k0, c0, diag, b, W, qs, dw = prm(it)
                P.op("pe", lambda e: e.matmul(zA[b][0:kp, 0:W], KT[:, k0:k0 + kp], QT[:, qs], start=True, stop=True),
                     reads=[t_K, t_Q], writes=[t_zA[b]])

            def act_e(it):
                kind, j, kp, k0, c0, diag, b, W, qs, dw = prm(it)
                P.op("act", lambda e: e.activation(e_sb[b][0:kp, 0:W], zA[b][0:kp, 0:W], AF.Exp),
                     reads=[t_zA[b]], writes=[t_e[b]])

            def act_sp(it, nblk=nblk, QW=QW):
                kind, j, kp, k0, c0, diag, b, W, qs, dw = prm(it)
                P.op("act", lambda e: e.activation(sp_sb[b][0:kp, 0:W], e_sb[b][0:kp, 0:W], AF.Ln, bias=1.0),
                     reads=[t_e[b]], writes=[t_sp[b]])
                if diag:
                    P.op("pool", lambda e: e.tensor_tensor(sp_sb[b][0:kp, 0:dw], sp_sb[b][0:kp, 0:dw],
                                                           cm[0:kp, 0, 0:dw], ALU.mult),
                         reads=[t_sp[b], t_cm], writes=[t_sp[b]])
                if it < nblk - 1:
                    a3 = it % 3
                    P.op("dve", lambda e: e.tensor_tensor(acc[0:kp, c0:QW], acc[0:kp, c0:QW], sp_sb[b][0:kp, 0:W], ALU.add),
                         reads=[t_sp[b], t_acc], writes=[t_acc])
                    P.op("dve", lambda e: e.tensor_copy(accb[a3][:, 0:QW], acc[:, 0:QW]),
                         reads=[t_acc], writes=[t_accb[a3]])

            def pe_zB(it, QW=QW):
                kind, j, kp, k0, c0, diag, b, W, qs, dw = prm(it)
                last = (it == 0)
                P.op("pe", lambda e: e.matmul(zB[b][0:kp, 0:W], KT[:, k0:k0 + kp], QT[:, qs], start=True, stop=False),
                     reads=[t_K, t_Q], writes=[t_zB[b]])
                P.op("pe", lambda e: e.matmul(zB[b][0:kp, 0:W], cm[0:kp, 1, 0:kp], sp_sb[b][0:kp, 0:W],
                                              start=False, stop=last),
                     reads=[t_cm, t_sp[b]], writes=[t_zB[b]])
                if not last:
                    ab = (it - 1) % 3
                    P.op("pe", lambda e: e.matmul(zB[b][0:kp, 0:W], cm[0:128, 2, 0:kp], accb[ab][0:128, c0:QW],
                                                  start=False, stop=True),
                         reads=[t_cm, t_accb[ab]], writes=[t_zB[b]])

            def act_A(it):
                kind, j, kp, k0, c0, diag, b, W, qs, dw = prm(it)
                P.op("act", lambda e: e.activation(A_sb[b][0:kp, 0:W], zB[b][0:kp, 0:W], AF.Exp),
                     reads=[t_zB[b]], writes=[t_A[b]])
                if diag:
                    P.op("pool", lambda e: e.tensor_tensor(A_sb[b][0:kp, 0:dw], A_sb[b][0:kp, 0:dw],
                                                           cm[0:kp, 0, 0:dw], ALU.mult),
                         reads=[t_A[b], t_cm], writes=[t_A[b]])

            def pe_AV(it, ob=ob, nblk=nblk, QW=QW):
                kind, j, kp, k0, c0, diag, b, W, qs, dw = prm(it)
                vop = (VM[0:kp, :] if kind == "m" else V[:, j, :])
                P.op("pe", lambda e: e.matmul(Ops[ob][0:64, c0:QW], vop, A_sb[b][0:kp, 0:W],
                                              start=False, stop=(it == nblk - 1)),
                     reads=[t_V, t_A[b]], writes=[t_O[ob]])

            for s_ in range(-2, nblk + 2):
                if 0 <= s_ + 2 < nblk:
                    pe_zA(s_ + 2)
                if 0 <= s_ < nblk:
                    pe_zB(s_)
                if 0 <= s_ - 2 < nblk:
                    pe_AV(s_ - 2)
                if 0 <= s_ + 1 < nblk:
                    act_e(s_ + 1)
                if 0 <= s_ - 1 < nblk:
                    act_A(s_ - 1)
                if 0 <= s_ + 1 < nblk:
                    act_sp(s_ + 1)
            P.op("dve", lambda e, ob=ob, QW=QW: e.tensor_copy(ost[ob][:, 0:QW], Ops[ob][0:64, 0:QW]),
                 reads=[t_O[ob]], writes=[t_ost[ob]])
            P.dma("sp", oT[h, :, q0:q0 + QW], ost[ob][:, 0:QW], reads=[t_ost[ob]])


def attn_masks():
    s = np.arange(128)[:, None]
    t = np.arange(128)[None, :]
    m = np.zeros((128, 3, 128), np.float32)
    m[:, 0, :] = (s < t)
    m[:, 1, :] = -1.0 * (s >= t)
    m[:, 2, :] = -1.0
    return m


DECAY_SCALE = float(np.exp(-0.5))
LNX_EPS = 64e-5


def rwkv_consts():
    idx = np.arange(128)
    ch = idx // 64
    same = ch[:, None] == ch[None, :]
    le = idx[:, None] <= idx[None, :]
    lt = idx[:, None] < idx[None, :]
    c = np.zeros((128, 9, 128), np.float32)
    c[:, 0] = -DECAY_SCALE * (same & le)
    c[:, 1] = -DECAY_SCALE * same
    c[:, 2] = same & lt
    c[:, 3] = same & le
    c[:, 4] = same & lt
    c[:, 5] = same & le
    c[:, 6] = (same & lt).T
    c[:, 7] = np.eye(128)
    c[:, 8, 0] = -DECAY_SCALE * (ch == 0)
    c[:, 8, 1] = -DECAY_SCALE * (ch == 1)
    c[:, 8, 2] = (ch == 0)
    c[:, 8, 3] = (ch == 1)
    return c


def build_rwkv(nc, NTX):
    L = N_META + 128 * NTX
    D = {}
    D["rkv"] = nc.dram_tensor("rkv", [L + 1, 384], F32, kind="ExternalInput").ap()
    D["lo"] = nc.dram_tensor("lo", [256, L + 1], F32, kind="ExternalInput").ap()
    D["mu_tm"] = nc.dram_tensor("mu_tm", [128, 384], F32, kind="ExternalInput").ap()
    D["mu_fm"] = nc.dram_tensor("mu_fm", [128, 3], F32, kind="ExternalInput").ap()
    D["bc"] = nc.dram_tensor("bc", [128, 7, 128], F32, kind="ExternalInput").ap()
    D["wa_up"] = nc.dram_tensor("wa_up", [128, 128], F32, kind="ExternalInput").ap()
    D["g_up"] = nc.dram_tensor("g_up", [128, 128], F32, kind="ExternalInput").ap()
    D["cst"] = nc.dram_tensor("cst", [128, 9, 128], F32, kind="ExternalInput").ap()
    D["o"] = nc.dram_tensor("o_rw", [L, 128], F32, kind="ExternalOutput").ap()
    with ExitStack() as es:
        P = Prog(nc, es)
        emit_rwkv(P, NTX, D)
        P.finish()
    return nc


def emit_rwkv(P, NTX, D, pfx="rw"):
    DS = DECAY_SCALE
    NCH = 1 + 2 * NTX

    def const_load(name, shape, src):
        t = P.sb(pfx + name, shape, F32)
        tt = P.T(const=True)
        P.dma("sp", t[:], src, writes=[tt])
        return t, tt

    cst, t_cst = const_load("cst", [128, 9, 128], D["cst"])
    bc, t_bc = const_load("bc", [128, 7, 128], D["bc"])
    mu_tm, t_mutm = const_load("mu_tm", [128, 384], D["mu_tm"])
    mu_fm, t_mufm = const_load("mu_fm", [128, 3], D["mu_fm"])
    wup, t_wup = const_load("wup", [64, 128], D["wa_up"][0:64, :])
    aup, t_aup = const_load("aup", [64, 128], D["wa_up"][64:128, :])
    gup, t_gup = const_load("gup", [128, 128], D["g_up"])

    S_all = [P.sb(pfx + f"S_all{h}", [64, NCH + 1, 64], BF16) for h in range(2)]
    ident_bf = P.sb(pfx + "ident_bf", [128, 128], BF16)
    t_idb = P.T(const=True)
    P.op("pool", lambda e: e.tensor_copy(ident_bf[:], cst[:, 7, :]), reads=[t_cst], writes=[t_idb])
    t_S = [[P.T() for _ in range(NCH + 1)] for h in range(2)]
    for h in range(2):
        P.op("pool", lambda e, h=h: e.memset(S_all[h][:, 0, :], 0.0), writes=[t_S[h][0]])

    pb = [P.ps(pfx + f"pb{i}", [128, 4, 128], BF16 if i == 2 else F32) for i in range(8)]
    t_bank = [P.T("bank%d" % i, excl=True) for i in range(8)]

    names_sb = {
        "xa": [128, 384], "xs": [128, 384], "x": [128, 384],
        "la": [64, 128], "ls": [64, 128], "la2": [64, 128], "ls2": [64, 128], "ga": [128, 128], "gs": [128, 128],
        "lx": [64, 128], "lx2": [64, 128], "gx": [128, 128], "tw": [64, 128], "sg": [128, 128],
        "sigw": [128, 128], "a": [128, 128], "g": [128, 128],
        "kk": [128, 128], "sq": [128, 128], "ss": [128, 2], "rn": [128, 2],
        "kp": [128, 128], "t1": [128, 128], "bs": [128, 2],
        "cum": [128, 128], "c1": [128, 128], "c2": [128, 128],
        "ep": [128, 128], "em": [128, 128], "epm": [128, 128], "ebar": [128, 128], "ebz": [128, 2, 128],
        "PC": [64, 2, 2],
        "TM": [128, 4, 128],
        "kka": [128, 128], "bbz": [128, 2, 128], "kbz": [128, 2, 128],
        "FM0": [64, 4, 128], "FM1": [64, 4, 128],
        "G0": [128, 4, 128], "G1": [128, 4, 128],
        "NP0": [128, 6, 2, 128], "NP1": [128, 6, 2, 128],
        "X0a": [128, 128], "X0b": [128, 128], "X1a": [128, 128], "X1b": [128, 128],
        "M": [64, 4, 64], "RHz0": [64, 2, 128], "RHz1": [64, 2, 128],
        "y": [128, 128], "ysq": [128, 128], "st": [128, 8], "yn": [128, 128], "o": [128, 128],
    }
    BF_NAMES = {"TM", "bbz", "kbz", "FM0", "FM1", "G0", "G1", "NP0", "NP1", "X0a", "X0b", "X1a", "X1b", "M", "RHz0", "RHz1", "xvb"}
    names_sb["xvb"] = [128, 128]
    bufs = []
    for p in range(2):
        d = {}
        for nm, shp in names_sb.items():
            d[nm] = (P.sb(f"{pfx}{nm}_{p}", shp, BF16 if nm in BF_NAMES else F32), P.T(f"{nm}{p}"))
        bufs.append(d)
        for nm in ("RHz0", "RHz1"):
            t, tt = d[nm]
            P.op("pool", lambda e, t=t: e.memset(t[:], 0.0), writes=[tt])

    rkv, lo, o_d = D["rkv"], D["lo"], D["o"]
    B_LORA, B_CUM, B_FM, B_G, B_H0, B_H1, B_PM, B_Y = range(8)
    B_HH = [B_H0, B_H1]

    def tile_body(ti):
        n = 16 if ti == 0 else 128
        t0 = 0 if ti == 0 else N_META + 128 * (ti - 1)
        nch = 1 if ti == 0 else 2
        cb = 0 if ti == 0 else 1 + 2 * (ti - 1)
        B = bufs[ti % 2]

        def b(nm):
            return B[nm]

        xa, t_xa = b("xa"); xs, t_xs = b("xs"); x, t_x = b("x")
        la, t_la = b("la"); ls, t_ls = b("ls"); la2, t_la2 = b("la2"); ls2, t_ls2 = b("ls2")
        ga, t_ga = b("ga"); gs, t_gs = b("gs")
        lx, t_lx = b("lx"); lx2, t_lx2 = b("lx2"); gx, t_gx = b("gx"); tw, t_tw = b("tw"); sg, t_sg = b("sg")
        sigw, t_sigw = b("sigw"); a_, t_a = b("a"); g_, t_g = b("g")
        kk, t_kk = b("kk"); sq, t_sq = b("sq"); ss, t_ss = b("ss"); rn, t_rn = b("rn")
        kp, t_kp = b("kp"); t1, t_t1 = b("t1"); bs, t_bs = b("bs")
        cum, t_cum = b("cum"); c1, t_c1 = b("c1"); c2, t_c2 = b("c2")
        ep, t_ep = b("ep"); em, t_em = b("em"); epm, t_epm = b("epm"); ebar, t_ebar = b("ebar")
        ebz, t_ebz = b("ebz")
        PC, t_PC = b("PC"); TM, t_TM = b("TM"); kka, t_kka = b("kka")
        bbz, t_bbz = b("bbz"); kbz, t_kbz = b("kbz")
        FMh = [b("FM0"), b("FM1")]
        M_, t_M = b("M"); RHz = [b("RHz0"), b("RHz1")]
        y_, t_y = b("y"); ysq, t_ysq = b("ysq"); st, t_st = b("st"); yn, t_yn = b("yn"); o_, t_o = b("o")

        P.dma("sp", xa[0:n, :], rkv[1 + t0:1 + t0 + n, :], writes=[t_xa])
        P.dma("sp", xs[0:n, :], rkv[t0:t0 + n, :], writes=[t_xs])
        P.dma("sp", la[:, 0:n], lo[0:64, 1 + t0:1 + t0 + n], writes=[t_la])
        P.dma("sp", ls[:, 0:n], lo[0:64, t0:t0 + n], writes=[t_ls])
        P.dma("sp", la2[:, 0:n], lo[64:128, 1 + t0:1 + t0 + n], writes=[t_la2])
        P.dma("sp", ls2[:, 0:n], lo[64:128, t0:t0 + n], writes=[t_ls2])
        P.dma("sp", ga[:, 0:n], lo[128:256, 1 + t0:1 + t0 + n], writes=[t_ga])
        P.dma("sp", gs[:, 0:n], lo[128:256, t0:t0 + n], writes=[t_gs])
        P.op("pool", lambda e: e.tensor_tensor(xs[0:n, :], xs[0:n, :], xa[0:n, :], ALU.subtract),
             reads=[t_xa, t_xs], writes=[t_xs])
        P.op("pool", lambda e: e.tensor_tensor(xs[0:n, :], xs[0:n, :], mu_tm[0:n, :], ALU.mult),
             reads=[t_xs, t_mutm], writes=[t_xs])
        P.op("pool", lambda e: e.tensor_tensor(x[0:n, :], xs[0:n, :], xa[0:n, :], ALU.add),
             reads=[t_xa, t_xs], writes=[t_x])
        for (A_, tA, S_, tS, O_, tO, col, np_) in ((la, t_la, ls, t_ls, lx, t_lx, 0, 64), (la2, t_la2, ls2, t_ls2, lx2, t_lx2, 1, 64),
                                                  (ga, t_ga, gs, t_gs, gx, t_gx, 2, 128)):
            P.op("pool", lambda e, A_=A_, S_=S_: e.tensor_tensor(S_[:, 0:n], S_[:, 0:n], A_[:, 0:n], ALU.subtract),
                 reads=[tA, tS], writes=[tS])
            P.op("dve", lambda e, A_=A_, S_=S_, O_=O_, col=col, np_=np_: e.scalar_tensor_tensor(O_[:, 0:n], S_[:, 0:n], mu_fm[0:np_, col:col + 1], A_[:, 0:n], ALU.mult, ALU.add),
                 reads=[tA, tS, t_mufm], writes=[tO])
        xr = x[0:n, 0:128]
        xk = x[0:n, 128:256]
        xvb, t_xvb = b("xvb")
        P.op("pool", lambda e: e.tensor_copy(xvb[0:n, :], x[0:n, 256:384]), reads=[t_x], writes=[t_xvb])
        P.op("act", lambda e: e.activation(tw[:, 0:n], lx[:, 0:n], AF.Exp, scale=-2.0), reads=[t_lx], writes=[t_tw])
        P.op("dve", lambda e: e.tensor_scalar(tw[:, 0:n], tw[:, 0:n], 1.0, None, ALU.add), reads=[t_tw], writes=[t_tw])
        P.op("dve", lambda e: e.reciprocal(tw[:, 0:n], tw[:, 0:n]), reads=[t_tw], writes=[t_tw])
        P.op("dve", lambda e: e.tensor_scalar(tw[:, 0:n], tw[:, 0:n], 2.0, -1.0, ALU.mult, ALU.add), reads=[t_tw], writes=[t_tw])
        P.op("act", lambda e: e.activation(sg[:, 0:n], gx[:, 0:n], AF.Exp, scale=-1.0), reads=[t_gx], writes=[t_sg])
        P.op("dve", lambda e: e.tensor_scalar(sg[:, 0:n], sg[:, 0:n], 1.0, None, ALU.add), reads=[t_sg], writes=[t_sg])
        P.op("dve", lambda e: e.reciprocal(sg[:, 0:n], sg[:, 0:n]), reads=[t_sg], writes=[t_sg])
        tb = t_bank[B_LORA]
        P.op("pe", lambda e: e.matmul(pb[B_LORA][0:n, 0, :], tw[:, 0:n], wup[:, :], start=True, stop=True),
             reads=[t_tw, t_wup], writes=[tb])
        P.op("pe", lambda e: e.matmul(pb[B_LORA][0:n, 1, :], lx2[:, 0:n], aup[:, :], start=True, stop=True),
             reads=[t_lx2, t_aup], writes=[tb])
        P.op("pe", lambda e: e.matmul(pb[B_LORA][0:n, 2, :], sg[:, 0:n], gup[:, :], start=True, stop=True),
             reads=[t_sg, t_gup], writes=[tb])
        P.op("dve", lambda e: e.tensor_tensor(sigw[0:n, :], pb[B_LORA][0:n, 0, :], bc[0:n, 0, :], ALU.add),
             reads=[tb, t_bc], writes=[t_sigw])
        P.op("dve", lambda e: e.tensor_tensor(a_[0:n, :], pb[B_LORA][0:n, 1, :], bc[0:n, 1, :], ALU.add),
             reads=[tb, t_bc], writes=[t_a])
        P.op("act", lambda e: e.activation(g_[0:n, :], pb[B_LORA][0:n, 2, :], AF.Identity), reads=[tb], writes=[t_g])
        for (Z, tZ) in ((sigw, t_sigw), (a_, t_a)):
            P.op("act", lambda e, Z=Z: e.activation(Z[0:n, :], Z[0:n, :], AF.Exp, scale=-1.0), reads=[tZ], writes=[tZ])
            P.op("dve", lambda e, Z=Z: e.tensor_scalar(Z[0:n, :], Z[0:n, :], 1.0, None, ALU.add), reads=[tZ], writes=[tZ])
            P.op("dve", lambda e, Z=Z: e.reciprocal(Z[0:n, :], Z[0:n, :]), reads=[tZ], writes=[tZ])
        P.op("pool", lambda e: e.tensor_tensor(kk[0:n, :], xk, bc[0:n, 2, :], ALU.mult), reads=[t_x, t_bc], writes=[t_kk])
        P.op("pool", lambda e: e.tensor_tensor(sq[0:n, :], kk[0:n, :], kk[0:n, :], ALU.mult), reads=[t_kk], writes=[t_sq])
        for h in range(2):
            P.op("dve", lambda e, h=h: e.reduce_sum(ss[0:n, h:h + 1], sq[0:n, 64 * h:64 * h + 64], axis=AX.X),
                 reads=[t_sq], writes=[t_ss])
        P.op("dve", lambda e: e.tensor_scalar(rn[0:n, :], ss[0:n, :], 1e-24, None, ALU.max), reads=[t_ss], writes=[t_rn])
        P.op("act", lambda e: e.activation(rn[0:n, :], rn[0:n, :], AF.Ln), reads=[t_rn], writes=[t_rn])
        P.op("act", lambda e: e.activation(rn[0:n, :], rn[0:n, :], AF.Exp, scale=-0.5), reads=[t_rn], writes=[t_rn])
        for h in range(2):
            P.op("dve", lambda e, h=h: e.tensor_scalar(kk[0:n, 64 * h:64 * h + 64], kk[0:n, 64 * h:64 * h + 64],
                                                       rn[0:n, h:h + 1], None, ALU.mult),
                 reads=[t_kk, t_rn], writes=[t_kk])
        P.op("dve", lambda e: e.scalar_tensor_tensor(t1[0:n, :], a_[0:n, :], -1.0, bc[0:n, 3, :], ALU.add, ALU.mult),
             reads=[t_a, t_bc], writes=[t_t1])
        P.op("dve", lambda e: e.scalar_tensor_tensor(kp[0:n, :], t1[0:n, :], 1.0, xk, ALU.add, ALU.mult),
             reads=[t_t1, t_x], writes=[t_kp])
        P.op("pool", lambda e: e.tensor_tensor(t1[0:n, :], xr, kp[0:n, :], ALU.mult), reads=[t_x, t_kp, t_t1], writes=[t_t1])
        P.op("pool", lambda e: e.tensor_tensor(t1[0:n, :], t1[0:n, :], bc[0:n, 4, :], ALU.mult), reads=[t_t1, t_bc], writes=[t_t1])
        for h in range(2):
            P.op("dve", lambda e, h=h: e.reduce_sum(bs[0:n, h:h + 1], t1[0:n, 64 * h:64 * h + 64], axis=AX.X),
                 reads=[t_t1], writes=[t_bs])
        tb = t_bank[B_CUM]
        P.op("pe", lambda e: e.matmul(pb[B_CUM][0:n, 0, :], cst[0:n, 0, 0:n], sigw[0:n, :], start=True, stop=True),
             reads=[t_cst, t_sigw], writes=[tb])
        P.op("pe", lambda e: e.matmul(pb[B_CUM][0:n, 1, :], cst[0:n, 1, 0:n], sigw[0:n, :], start=True, stop=True),
             reads=[t_cst, t_sigw], writes=[tb])
        for h in range(2):
            P.op("pe", lambda e, h=h: e.matmul(pb[B_CUM][0:64, 2 + h, 0:2], sigw[0:n, 64 * h:64 * h + 64], cst[0:n, 8, 0:2], start=True, stop=True),
                 reads=[t_cst, t_sigw], writes=[tb])
        P.op("dve", lambda e: e.tensor_copy(cum[0:n, :], pb[B_CUM][0:n, 0, :]), reads=[tb], writes=[t_cum])
        P.op("dve", lambda e: e.scalar_tensor_tensor(c1[0:n, :], sigw[0:n, :], DS, cum[0:n, :], ALU.mult, ALU.add),
             reads=[t_sigw, t_cum], writes=[t_c1])
        P.op("dve", lambda e: e.tensor_tensor(c2[0:n, :], pb[B_CUM][0:n, 1, :], cum[0:n, :], ALU.subtract),
             reads=[tb, t_cum], writes=[t_c2])
        P.op("act", lambda e: e.activation(ep[0:n, :], cum[0:n, :], AF.Exp), reads=[t_cum], writes=[t_ep])
        P.op("act", lambda e: e.activation(em[0:n, :], cum[0:n, :], AF.Exp, scale=-1.0), reads=[t_cum], writes=[t_em])
        P.op("act", lambda e: e.activation(epm[0:n, :], c1[0:n, :], AF.Exp), reads=[t_c1], writes=[t_epm])
        P.op("act", lambda e: e.activation(ebar[0:n, :], c2[0:n, :], AF.Exp), reads=[t_c2], writes=[t_ebar])
        P.op("act", lambda e: e.activation(PC[:, :, :], pb[B_CUM][0:64, 2:4, 0:2], AF.Exp), reads=[tb], writes=[t_PC])
        P.op("dve", lambda e: e.tensor_tensor(kka[0:n, :], kk[0:n, :], a_[0:n, :], ALU.mult), reads=[t_kk, t_a], writes=[t_kka])
        P.op("dve", lambda e: e.tensor_tensor(TM[0:n, 0, :], kka[0:n, :], em[0:n, :], ALU.mult), reads=[t_kka, t_em], writes=[t_TM])
        P.op("dve", lambda e: e.tensor_tensor(TM[0:n, 1, :], kp[0:n, :], em[0:n, :], ALU.mult), reads=[t_kp, t_em], writes=[t_TM])
        P.op("dve", lambda e: e.scalar_tensor_tensor(TM[0:n, 2, :], kk[0:n, :], -1.0, epm[0:n, :], ALU.mult, ALU.mult),
             reads=[t_kk, t_epm], writes=[t_TM])
        P.op("dve", lambda e: e.tensor_tensor(TM[0:n, 3, :], xr, ep[0:n, :], ALU.mult), reads=[t_x, t_ep], writes=[t_TM])
        for c in range(nch):
            P.op("pool", lambda e, c=c: e.tensor_scalar(ebz[0:n, c, :], ebar[0:n, :], cst[0:n, 8, 2 + c:3 + c], None, ALU.mult),
                 reads=[t_ebar, t_cst], writes=[t_ebz])
            P.op("pool", lambda e, c=c: e.tensor_tensor(bbz[0:n, c, :], kka[0:n, :], ebz[0:n, c, :], ALU.mult),
                 reads=[t_kka, t_ebz], writes=[t_bbz])
            P.op("pool", lambda e, c=c: e.tensor_tensor(kbz[0:n, c, :], kp[0:n, :], ebz[0:n, c, :], ALU.mult),
                 reads=[t_kp, t_ebz], writes=[t_kbz])
        for h in range(2):
            FM, t_FM = FMh[h]
            bk = B_FM
            for s_ in range(4):
                P.op("pe", lambda e, s_=s_, h=h, bk=bk: e.transpose(pb[bk][0:64, s_, 0:n], TM[0:n, s_, 64 * h:64 * h + 64], ident_bf[0:n, 0:n]),
                     reads=[t_TM, t_idb], writes=[t_bank[bk]])
            P.op("act", lambda e, FM=FM, bk=bk: e.activation(FM[:, :, 0:n], pb[bk][0:64, :, 0:n], AF.Identity),
                 reads=[t_bank[bk]], writes=[t_FM])

        G = [b("G0"), b("G1")]
        NP = [b("NP0"), b("NP1")]
        Xb = [[b("X0a"), b("X0b")], [b("X1a"), b("X1b")]]
        for h in range(2):
            FM, t_FM = FMh[h]
            Gh, t_G = G[h]
            NPh, t_NP = NP[h]
            gbk = B_G
            gb = pb[gbk]
            for (so, sl, sr) in ((0, 0, 2), (1, 0, 3), (2, 1, 2), (3, 1, 3)):
                P.op("pe", lambda e, gb=gb, so=so, sl=sl, sr=sr, FM=FM: e.matmul(gb[0:n, so, 0:n], FM[:, sl, 0:n], FM[:, sr, 0:n], start=True, stop=True),
                     reads=[t_FM], writes=[t_bank[gbk]])
            nbk = B_CUM
            P.op("pe", lambda e, nbk=nbk, FM=FM, h=h: e.matmul(pb[nbk][0:n, 2 + h, 0:n], FM[:, 2, 0:n], FM[:, 0, 0:n], start=True, stop=True),
                 reads=[t_FM], writes=[t_bank[nbk]])
            P.op("dve", lambda e, gb=gb, Gh=Gh: e.tensor_tensor(Gh[0:n, :, 0:n], gb[0:n, :, 0:n], cst[0:n, 2:6, 0:n], ALU.mult),
                 reads=[t_bank[gbk], t_cst], writes=[t_G])
            P.op("dve", lambda e, nbk=nbk, NPh=NPh, h=h: e.tensor_tensor(NPh[0:n, 0, 1, 0:n], pb[nbk][0:n, 2 + h, 0:n], cst[0:n, 6, 0:n], ALU.mult),
                 reads=[t_bank[nbk], t_cst], writes=[t_NP])
            P.op("pool", lambda e, Gh=Gh, NPh=NPh: e.tensor_copy(NPh[0:n, 0, 0, 0:n], Gh[0:n, 0, 0:n]),
                 reads=[t_G], writes=[t_NP])
        P.mark()
        for h in range(2):
            hs = slice(64 * h, 64 * h + 64)
            vs = slice(256 + 64 * h, 256 + 64 * h + 64)
            Gh, t_G = G[h]
            xbk = B_HH[h]
            P.op("pe", lambda e, hs=hs, xbk=xbk: e.matmul(pb[xbk][0:n, 2, 0:64], ident_bf[0:n, 0:n], TM[0:n, 2, hs], start=True, stop=True),
                 reads=[t_TM, t_idb], writes=[t_bank[xbk]])
            P.op("pe", lambda e, hs=hs, xbk=xbk, Gh=Gh: e.matmul(pb[xbk][0:n, 2, 64:128], Gh[0:n, 2, 0:n], xvb[0:n, hs], start=True, stop=True),
                 reads=[t_G, t_xvb], writes=[t_bank[xbk]])
            X0, t_X0 = Xb[h][0]
            P.op("act", lambda e, xbk=xbk, X0=X0: e.activation(X0[0:n, :], pb[xbk][0:n, 2, :], AF.Identity),
                 reads=[t_bank[xbk]], writes=[t_X0])
        NPW = 6
        cur = [0, 0]
        for i in range(NPW):
            for h in range(2):
                NPh, t_NP = NP[h]
                nbk = B_HH[h]
                xbk = B_HH[h]
                Xc, t_Xc = Xb[h][cur[h]]
                Xn, t_Xn = Xb[h][1 - cur[h]]
                P.op("pe", lambda e, i=i, NPh=NPh, Xc=Xc, xbk=xbk: e.matmul(pb[xbk][0:n, 2, :], NPh[0:n, i, 0, 0:n], Xc[0:n, :], start=True, stop=True),
                     reads=[t_NP, t_Xc], writes=[t_bank[xbk]])
                P.op("dve", lambda e, Xc=Xc, Xn=Xn, xbk=xbk: e.tensor_tensor(Xn[0:n, :], pb[xbk][0:n, 2, :], Xc[0:n, :], ALU.add),
                     reads=[t_bank[xbk], t_Xc], writes=[t_Xn])
                cur[h] = 1 - cur[h]
                if i < NPW - 1:
                    P.op("pe", lambda e, i=i, NPh=NPh, nbk=nbk: e.matmul(pb[nbk][0:n, 0, 0:n], NPh[0:n, i, 1, 0:n], NPh[0:n, i, 0, 0:n], start=True, stop=True),
                         reads=[t_NP], writes=[t_bank[nbk]])
                    P.op("pe", lambda e, i=i, NPh=NPh, nbk=nbk: e.matmul(pb[nbk][0:n, 1, 0:n], NPh[0:n, i, 0, 0:n], NPh[0:n, i, 1, 0:n], start=True, stop=True),
                         reads=[t_NP], writes=[t_bank[nbk]])
                    P.op("act", lambda e, i=i, NPh=NPh, nbk=nbk: e.activation(NPh[0:n, i + 1, :, 0:n], pb[nbk][0:n, 0:2, 0:n], AF.Identity),
                         reads=[t_bank[nbk]], writes=[t_NP])
        Xf = [Xb[h][cur[h]] for h in range(2)]
        P.mark()
        tb = t_bank[B_PM]
        for h in range(2):
            hs = slice(64 * h, 64 * h + 64)
            Xh, t_Xh = Xf[h]
            for c in range(nch):
                P.op("pe", lambda e, hs=hs, c=c, h=h, Xh=Xh: e.matmul(pb[B_PM][0:64, h, 64 * c:64 * c + 64], Xh[0:n, 0:64], bbz[0:n, c, hs], start=True, stop=True),
                     reads=[t_Xh, t_bbz], writes=[tb])
        for h in range(2):
            for c in range(nch):
                P.op("dve", lambda e, h=h, c=c: e.scalar_tensor_tensor(M_[:, 2 * h + c, :], cst[0:64, 7, 0:64], PC[:, h, c:c + 1], pb[B_PM][0:64, h, 64 * c:64 * c + 64], ALU.mult, ALU.add),
                     reads=[tb, t_PC, t_cst], writes=[t_M])
        tb = t_bank[B_PM]
        for c in range(nch):
            ci = cb + c
            for h in range(2):
                hs = slice(64 * h, 64 * h + 64)
                vs = slice(256 + 64 * h, 256 + 64 * h + 64)
                Xh, t_Xh = Xf[h]
                P.op("pe", lambda e, h=h, c=c, ci=ci: e.matmul(pb[B_PM][0:64, 2 + h, 0:64], M_[:, 2 * h + c, :], S_all[h][:, ci, :], start=True, stop=False),
                     reads=[t_M, t_S[h][ci]], writes=[tb])
                P.op("pe", lambda e, h=h, hs=hs, c=c, Xh=Xh: e.matmul(pb[B_PM][0:64, 2 + h, 0:64], bbz[0:n, c, hs], Xh[0:n, 64:128], start=False, stop=False),
                     reads=[t_bbz, t_Xh], writes=[tb])
                P.op("pe", lambda e, h=h, hs=hs, vs=vs, c=c: e.matmul(pb[B_PM][0:64, 2 + h, 0:64], kbz[0:n, c, hs], xvb[0:n, hs], start=False, stop=True),
                     reads=[t_kbz, t_xvb], writes=[tb])
                P.op("act", lambda e, h=h, ci=ci: e.activation(S_all[h][:, ci + 1, :], pb[B_PM][0:64, 2 + h, 0:64], AF.Identity),
                     reads=[tb], writes=[t_S[h][ci + 1]])
        tb = t_bank[B_Y]
        for h in range(2):
            Xh, t_Xh = Xf[h]
            Gh, t_G = G[h]
            FM, t_FM = FMh[h]
            Rz, t_Rz = RHz[h]
            P.op("pe", lambda e, h=h, Xh=Xh, Gh=Gh: e.matmul(pb[B_Y][0:64, 1 + h, 0:n], Xh[0:n, 0:64], Gh[0:n, 1, 0:n], start=True, stop=True),
                 reads=[t_Xh, t_G], writes=[tb])
            for c in range(nch):
                cs = slice(64 * c, min(64 * c + 64, n))
                P.op("dve", lambda e, h=h, c=c, cs=cs, Rz=Rz, FM=FM: e.tensor_tensor(Rz[:, c, cs], pb[B_Y][0:64, 1 + h, cs], FM[:, 3, cs], ALU.add),
                     reads=[tb, t_FM], writes=[t_Rz])
        tb = t_bank[B_Y]
        for h in range(2):
            hs = slice(64 * h, 64 * h + 64)
            vs = slice(256 + 64 * h, 256 + 64 * h + 64)
            Xh, t_Xh = Xf[h]
            Gh, t_G = G[h]
            Rz, t_Rz = RHz[h]
            P.op("pe", lambda e, hs=hs, Xh=Xh, Gh=Gh: e.matmul(pb[B_Y][0:n, 0, hs], Gh[0:n, 1, 0:n], Xh[0:n, 64:128], start=True, stop=False),
                 reads=[t_G, t_Xh], writes=[tb])
            P.op("pe", lambda e, hs=hs, vs=vs, Gh=Gh: e.matmul(pb[B_Y][0:n, 0, hs], Gh[0:n, 3, 0:n], xvb[0:n, hs], start=False, stop=False),
                 reads=[t_G, t_xvb], writes=[tb])
            for c in range(nch):
                ci = cb + c
                P.op("pe", lambda e, h=h, hs=hs, ci=ci, c=c, Rz=Rz: e.matmul(pb[B_Y][0:n, 0, hs], Rz[:, c, 0:n], S_all[h][:, ci, :], start=False, stop=(c == nch - 1)),
                     reads=[t_Rz, t_S[h][ci]], writes=[tb])
        P.op("act", lambda e: e.activation(y_[0:n, :], pb[B_Y][0:n, 0, :], AF.Identity), reads=[tb], writes=[t_y])
        P.op("pool", lambda e: e.tensor_tensor(ysq[0:n, :], y_[0:n, :], y_[0:n, :], ALU.mult), reads=[t_y], writes=[t_ysq])
        for h in range(2):
            hs = slice(64 * h, 64 * h + 64)
            P.op("dve", lambda e, h=h, hs=hs: e.reduce_sum(st[0:n, h:h + 1], y_[0:n, hs], axis=AX.X), reads=[t_y], writes=[t_st])
            P.op("dve", lambda e, h=h, hs=hs: e.reduce_sum(st[0:n, 2 + h:3 + h], ysq[0:n, hs], axis=AX.X), reads=[t_ysq], writes=[t_st])
        P.op("dve", lambda e: e.tensor_scalar(st[0:n, 4:6], st[0:n, 0:2], 1.0 / 64, None, ALU.mult), reads=[t_st], writes=[t_st])
        P.op("dve", lambda e: e.tensor_tensor(st[0:n, 6:8], st[0:n, 4:6], st[0:n, 4:6], ALU.mult), reads=[t_st], writes=[t_st])
        P.op("dve", lambda e: e.scalar_tensor_tensor(st[0:n, 6:8], st[0:n, 2:4], 1.0 / 64, st[0:n, 6:8], ALU.mult, ALU.subtract), reads=[t_st], writes=[t_st])
        P.op("dve", lambda e: e.tensor_scalar(st[0:n, 6:8], st[0:n, 6:8], LNX_EPS, None, ALU.add), reads=[t_st], writes=[t_st])
        P.op("act", lambda e: e.activation(st[0:n, 6:8], st[0:n, 6:8], AF.Ln), reads=[t_st], writes=[t_st])
        P.op("act", lambda e: e.activation(st[0:n, 6:8], st[0:n, 6:8], AF.Exp, scale=-0.5), reads=[t_st], writes=[t_st])
        for h in range(2):
            hs = slice(64 * h, 64 * h + 64)
            P.op("dve", lambda e, h=h, hs=hs: e.tensor_scalar(yn[0:n, hs], y_[0:n, hs], st[0:n, 4 + h:5 + h], st[0:n, 6 + h:7 + h], ALU.subtract, ALU.mult),
                 reads=[t_y, t_st], writes=[t_yn])
        P.op("pool", lambda e: e.tensor_tensor(yn[0:n, :], yn[0:n, :], bc[0:n, 5, :], ALU.mult), reads=[t_yn, t_bc], writes=[t_yn])
        P.op("pool", lambda e: e.tensor_tensor(yn[0:n, :], yn[0:n, :], bc[0:n, 6, :], ALU.add), reads=[t_yn, t_bc], writes=[t_yn])
        for h in range(2):
            hs = slice(64 * h, 64 * h + 64)
            vs = slice(256 + 64 * h, 256 + 64 * h + 64)
            P.op("dve", lambda e, h=h, hs=hs, vs=vs: e.scalar_tensor_tensor(yn[0:n, hs], x[0:n, vs], bs[0:n, h:h + 1], yn[0:n, hs], ALU.mult, ALU.add),
                 reads=[t_x, t_bs, t_yn], writes=[t_yn])
        P.op("dve", lambda e: e.tensor_tensor(o_[0:n, :], yn[0:n, :], g_[0:n, :], ALU.mult), reads=[t_yn, t_g], writes=[t_o])
        P.dma("sp", o_d[t0:t0 + n, :], o_[0:n, :], reads=[t_o])

    streams = []
    for ti in range(NTX + 1):
        P.begin_stream()
        tile_body(ti)
        streams.append(P.end_stream())
    P.run_interleaved(streams, max_active=int(os.environ.get("RW_ACT", "2")))


def rwkv_host_inputs(p_rw, hp, prm):
    L = p_rw.shape[0]
    cs = slice(128 * hp, 128 * hp + 128)
    rkv = np.zeros((L + 1, 384), np.float32)
    rkv[1:, 0:128] = p_rw[:, 0:512][:, cs]
    rkv[1:, 128:256] = p_rw[:, 512:1024][:, cs]
    rkv[1:, 256:384] = p_rw[:, 1024:1536][:, cs]
    lo = np.zeros((256, L + 1), np.float32)
    lo[:, 1:] = p_rw[:, 1536:1792].T
    mu = prm["rwkv_mu"]
    mu_tm = np.concatenate([mu[0:512][cs], mu[512:1024][cs], mu[1024:1536][cs]])
    mu_tm = np.ascontiguousarray(np.broadcast_to(mu_tm[None, :], (128, 384)))
    mu_fm = np.zeros((128, 3), np.float32)
    mu_fm[0:64, 0] = mu[1536:1600]
    mu_fm[0:64, 1] = mu[1600:1664]
    mu_fm[:, 2] = mu[1664:1792]
    rows = [prm["w0"][cs], prm["a0"][cs], prm["k_k"][cs], prm["k_a"][cs], prm["r_k"].reshape(-1)[cs],
            prm["lnx_g"][cs], prm["lnx_b"][cs]]
    bc = np.ascontiguousarray(np.broadcast_to(np.stack(rows)[None], (128, 7, 128)))
    wa_up = np.ascontiguousarray(np.concatenate([prm["w_up"][:, cs], prm["a_up"][:, cs]], axis=0))
    g_up = np.ascontiguousarray(prm["g_up"][:, cs])
    return {"rkv": rkv, "lo": lo, "mu_tm": mu_tm, "mu_fm": mu_fm, "bc": bc, "wa_up": wa_up, "g_up": g_up,
            "cst": rwkv_consts()}


ALPHA = float((2 * 2) ** 0.25)
LN_EPS = 1e-5
NTOK = N_META + 2048
HALVES = [(0, [(0, 16), (16, 512), (528, 512)], [(0, 16)] + [(16 + 128 * i, 128) for i in range(8)]),
          (1040, [(0, 512), (512, 512)], [(128 * i, 128) for i in range(8)])]
C_IN = 5376


def tok_consts():
    c = np.zeros((128, 2, 128), np.float32)
    c[:, 0, :] = 1.0 / 1024
    c[:, 1, :] = np.eye(128)
    sel = np.zeros((16, 16, 128), np.float32)
    for e in range(16):
        sel[e, e, :] = 1.0
    return c, sel


def build_tok(nc, mode, do_proj, ntok=NTOK, halves=HALVES, n_exp=16):
    D = {}

    def inp(name, shape):
        D[name] = nc.dram_tensor(name, list(shape), F32, kind="ExternalInput").ap()

    def outp(name, shape):
        D[name] = nc.dram_tensor(name, list(shape), F32, kind="ExternalOutput").ap()

    inp("xT", [1024, ntok])
    inp("tc", [128, 2, 128])
    inp("lnA", [128, 2, 8])
    if mode == "C":
        inp("osbT", [512, ntok]); inp("orwT", [512, ntok]); inp("gT", [2048, ntok])
        inp("p_sb", [512, 1024]); inp("p_rw", [512, 1024]); inp("w_out", [1024, 1024])
        inp("lnB", [128, 2, 8])
        inp("router_w", [1024, 16]); inp("rb", [128, 16]); inp("sel", [16, 16, 128])
        inp("wg", [16, 1024, 512]); inp("wu", [16, 1024, 512]); inp("wd", [16, 512, 1024])
    if do_proj:
        inp("w_in", [1024, C_IN])
        outp("pT", [C_IN, ntok])
    outp("hT", [1024, ntok])
    with ExitStack() as es:
        P = Prog(nc, es)
        emit_tok(P, D, mode, do_proj, halves, n_exp)
        P.finish()
    return nc


def emit_tok(P, D, mode, do_proj, halves, n_exp=16):
    WMAX = 1040
    tc = P.sb("tc", [128, 2, 128]); t_tc = P.T(const=True)
    P.dma("sp", tc[:], D["tc"], writes=[t_tc])
    lnA = P.sb("lnA", [128, 2, 8]); t_lnA = P.T(const=True)
    P.dma("sp", lnA[:], D["lnA"], writes=[t_lnA])
    hT = P.sb("hT", [128, 8, WMAX]); hbf = P.sb("hbf", [128, 8, WMAX], BF16)
    NG = 3
    t_h = [[P.T(f"h{g}_{m}") for m in range(8)] for g in range(NG)]
    t_hb = [P.T(f"hb{g}") for g in range(NG)]
    NSTG = 4
    stg = [P.sb(f"stg{i}", [128, 2048]) for i in range(NSTG)]
    t_stg = [P.T() for _ in range(NSTG)]
    WQ = ["sp", "act"]
    ps = [P.ps(f"ps{i}") for i in range(8)]
    t_ps = [P.T(f"psb{i}", excl=True) for i in range(8)]
    mean_sb = P.sb("mean_sb", [128, 512]); t_mean = P.T()
    rstd_sb = P.sb("rstd_sb", [128, 512]); t_rstd = P.T()
    tmp = [P.sb(f"tmp{i}", [128, 512]) for i in range(2)]
    t_tmp = [P.T(), P.T()]
    cnt = {"stg": 0, "tmp": 0, "ev": 0}

    def ln_group(gi, c0, w, lnp, t_lnp):
        PM, PX = 6, 7
        for k in range(8):
            s = cnt["tmp"] % 2; cnt["tmp"] += 1
            P.op("act", lambda e, k=k, s=s: e.activation(tmp[s][:, 0:w], hT[:, k, c0:c0 + w], AF.Square),
                 reads=[t_h[gi][k]], writes=[t_tmp[s]])
            P.op("pe", lambda e, k=k: e.matmul(ps[PM][:, 0:w], tc[:, 0, :], hT[:, k, c0:c0 + w], start=(k == 0), stop=(k == 7)),
                 reads=[t_tc, t_h[gi][k]], writes=[t_ps[PM]])
            P.op("pe", lambda e, k=k, s=s: e.matmul(ps[PX][:, 0:w], tc[:, 0, :], tmp[s][:, 0:w], start=(k == 0), stop=(k == 7)),
                 reads=[t_tc, t_tmp[s]], writes=[t_ps[PX]])
        P.op("act", lambda e: e.activation(mean_sb[:, 0:w], ps[PM][:, 0:w], AF.Identity), reads=[t_ps[PM]], writes=[t_mean])
        P.op("dve", lambda e: e.tensor_tensor(rstd_sb[:, 0:w], mean_sb[:, 0:w], mean_sb[:, 0:w], ALU.mult), reads=[t_mean], writes=[t_rstd])
        P.op("dve", lambda e: e.tensor_tensor(rstd_sb[:, 0:w], ps[PX][:, 0:w], rstd_sb[:, 0:w], ALU.subtract), reads=[t_ps[PX], t_rstd], writes=[t_rstd])
        P.op("dve", lambda e: e.tensor_scalar(rstd_sb[:, 0:w], rstd_sb[:, 0:w], LN_EPS, None, ALU.add), reads=[t_rstd], writes=[t_rstd])
        P.op("act", lambda e: e.activation(rstd_sb[:, 0:w], rstd_sb[:, 0:w], AF.Ln), reads=[t_rstd], writes=[t_rstd])
        P.op("act", lambda e: e.activation(rstd_sb[:, 0:w], rstd_sb[:, 0:w], AF.Exp, scale=-0.5), reads=[t_rstd], writes=[t_rstd])
        for k in range(8):
            s = cnt["tmp"] % 2; cnt["tmp"] += 1
            P.op("dve", lambda e, k=k, s=s: e.tensor_tensor(tmp[s][:, 0:w], hT[:, k, c0:c0 + w], mean_sb[:, 0:w], ALU.subtract),
                 reads=[t_h[gi][k], t_mean], writes=[t_tmp[s]])
            P.op("dve", lambda e, s=s: e.tensor_tensor(tmp[s][:, 0:w], tmp[s][:, 0:w], rstd_sb[:, 0:w], ALU.mult),
                 reads=[t_tmp[s], t_rstd], writes=[t_tmp[s]])
            P.op("act", lambda e, k=k, s=s: e.activation(hT[:, k, c0:c0 + w], tmp[s][:, 0:w], AF.Identity, bias=lnp[:, 1, k:k + 1], scale=lnp[:, 0, k:k + 1]),
                 reads=[t_tmp[s], t_lnp], writes=[t_h[gi][k]])
            P.op("act", lambda e, k=k, s=s: e.activation(hbf[:, k, c0:c0 + w], tmp[s][:, 0:w], AF.Identity, bias=lnp[:, 1, k:k + 1], scale=lnp[:, 0, k:k + 1]),
                 reads=[t_tmp[s], t_lnp], writes=[t_hb[gi]])

    if do_proj:
        wpj = [P.sb(f"wpj{i}", [128, 8, 128], BF16) for i in range(2)]
        t_wpj = [P.T(), P.T()]
        ost = [P.sb(f"ost{i}", [128, 512]) for i in range(4)]
        t_ost = [P.T() for _ in range(4)]
        w_in_v = D["w_in"].rearrange("(k p) c -> p k c", p=128)

    def proj(groups, tok0):
        for j in range(C_IN // 128):
            s = cnt["stg"] % NSTG; wq = WQ[cnt["stg"] % 2]; cnt["stg"] += 1
            wb = j % 2
            P.dma(wq, stg[s][:, 0:1024].rearrange("p (k c) -> p k c", k=8), w_in_v[:, :, 128 * j:128 * j + 128], writes=[t_stg[s]])
            P.op("act", lambda e, s=s, wb=wb: e.activation(wpj[wb][:], stg[s][:, 0:1024].rearrange("p (k c) -> p k c", k=8), AF.Identity),
                 reads=[t_stg[s]], writes=[t_wpj[wb]])
            for gi, (c0, w) in enumerate(groups):
                bk = cnt["ev"] % 4
                for k in range(8):
                    P.op("pe", lambda e, k=k, wb=wb, bk=bk, c0=c0, w=w: e.matmul(ps[bk][:, 0:w], wpj[wb][:, k, :], hbf[:, k, c0:c0 + w], start=(k == 0), stop=(k == 7)),
                         reads=[t_wpj[wb], t_hb[gi]], writes=[t_ps[bk]])
                eng = "dve"
                cnt["ev"] += 1
                if eng == "act":
                    P.op("act", lambda e, bk=bk, w=w: e.activation(ost[bk][:, 0:w], ps[bk][:, 0:w], AF.Identity), reads=[t_ps[bk]], writes=[t_ost[bk]])
                else:
                    P.op("dve", lambda e, bk=bk, w=w: e.tensor_copy(ost[bk][:, 0:w], ps[bk][:, 0:w]), reads=[t_ps[bk]], writes=[t_ost[bk]])
                P.dma("sp", D["pT"][128 * j:128 * j + 128, tok0 + c0:tok0 + c0 + w], ost[bk][:, 0:w], reads=[t_ost[bk]])

    if mode == "C":
        lnB = P.sb("lnB", [128, 2, 8]); t_lnB = P.T(const=True)
        P.dma("sp", lnB[:], D["lnB"], writes=[t_lnB])
        rw = P.sb("rw", [128, 8, 16]); t_rw = P.T(const=True)
        P.dma("sp", rw[:], D["router_w"].rearrange("(k p) e -> p k e", p=128), writes=[t_rw])
        rb = P.sb("rb", [128, 16]); t_rb = P.T(const=True)
        P.dma("sp", rb[:], D["rb"], writes=[t_rb])
        sel = P.sb("sel", [16, 16, 128]); t_sel = P.T(const=True)
        P.dma("sp", sel[:], D["sel"], writes=[t_sel])
        arena = [P.sb(f"arena{i}", [128, 8192], BF16) for i in range(2)]
        t_ar = [P.T("arena0"), P.T("arena1")]
        ob = [P.sb(f"ob{i}", [128, 4, 512], BF16) for i in range(2)]
        t_ob = [P.T(), P.T()]
        gts = [P.sb(f"gts{i}", [128, 512]) for i in range(4)]
        t_gts = [P.T() for _ in range(4)]
        merged = P.sb("merged", [128, 8, 512], BF16); t_merged = P.T()
        combT = P.sb("combT", [16, WMAX]); t_combT = P.T()
        cbc = [P.sb(f"cbc{i}", [128, WMAX]) for i in range(2)]
        t_cbc = [P.T(), P.T()]
        hid = [P.sb(f"hid{i}", [128, 2, 512], BF16) for i in range(2)]
        t_hid = [P.T(), P.T()]
        sgl = [P.sb(f"sgl{i}", [128, 512]) for i in range(2)]
        t_sgl = [P.T(), P.T()]
        rt = P.sb("rt", [128, 16, 16]); t_rt = P.T()
        rs = P.sb("rs", [128, 16]); t_rs = P.T()
        osb_v = D["osbT"].rearrange("(k p) t -> p k t", p=128)
        orw_v = D["orwT"].rearrange("(k p) t -> p k t", p=128)
        psb_v = D["p_sb"].rearrange("(k p) c -> p k c", p=128)
        prw_v = D["p_rw"].rearrange("(k p) c -> p k c", p=128)
        wout_v = D["w_out"].rearrange("(k p) c -> p k c", p=128)
        psb_bf = arena[0][:, 0:4096].rearrange("p (k c) -> p k c", k=4)
        prw_bf = arena[0][:, 4096:8192].rearrange("p (k c) -> p k c", k=4)
        wout_bf = arena[1][:, 0:8192].rearrange("p (k c) -> p k c", k=8)

    xT_v = D["xT"].rearrange("(k p) t -> p k t", p=128)
    hTo_v = D["hT"].rearrange("(k p) t -> p k t", p=128)

    for (tok0, groups, rtiles) in halves:
        for gi, (c0, w) in enumerate(groups):
            P.dma("sp", hT[:, :, c0:c0 + w], xT_v[:, :, tok0 + c0:tok0 + c0 + w], writes=t_h[gi])
        if mode == "A":
            for gi, (c0, w) in enumerate(groups):
                ln_group(gi, c0, w, lnA, t_lnA)
        else:
            for (src, dst, ai, nk) in ((psb_v, psb_bf, 0, 4), (prw_v, prw_bf, 0, 4), (wout_v, wout_bf, 1, 8)):
                for k0 in range(0, nk, 2):
                    s = cnt["stg"] % NSTG; wq = WQ[cnt["stg"] % 2]; cnt["stg"] += 1
                    P.dma(wq, stg[s][:, 0:2048].rearrange("p (k c) -> p k c", k=2), src[:, k0:k0 + 2, :], writes=[t_stg[s]])
                    P.op("act", lambda e, s=s, dst=dst, k0=k0: e.activation(dst[:, k0:k0 + 2, :], stg[s][:, 0:2048].rearrange("p (k c) -> p k c", k=2), AF.Identity),
                         reads=[t_stg[s]], writes=[t_ar[ai]])
            for gi, (c0, w) in enumerate(groups):
                for (src, oi) in ((osb_v, 0), (orw_v, 1)):
                    s = cnt["stg"] % NSTG; wq = WQ[cnt["stg"] % 2]; cnt["stg"] += 1
                    P.dma(wq, stg[s][:, 0:4 * w].rearrange("p (k c) -> p k c", k=4), src[:, :, tok0 + c0:tok0 + c0 + w], writes=[t_stg[s]])
                    P.op("dve", lambda e, s=s, oi=oi, w=w: e.tensor_copy(ob[oi][:, :, 0:w], stg[s][:, 0:4 * w].rearrange("p (k c) -> p k c", k=4)),
                         reads=[t_stg[s]], writes=[t_ob[oi]])
                for m in range(8):
                    ms = slice(128 * m, 128 * m + 128)
                    ba, bb = 0 + (m % 2) * 2, 1 + (m % 2) * 2
                    for k in range(4):
                        P.op("pe", lambda e, k=k, ms=ms, ba=ba, w=w: e.matmul(ps[ba][:, 0:w], psb_bf[:, k, ms], ob[0][:, k, 0:w], start=(k == 0), stop=(k == 3)),
                             reads=[t_ar[0], t_ob[0]], writes=[t_ps[ba]])
                    for k in range(4):
                        P.op("pe", lambda e, k=k, ms=ms, bb=bb, w=w: e.matmul(ps[bb][:, 0:w], prw_bf[:, k, ms], ob[1][:, k, 0:w], start=(k == 0), stop=(k == 3)),
                             reads=[t_ar[0], t_ob[1]], writes=[t_ps[bb]])
                    g0, g1 = (m % 2) * 2, (m % 2) * 2 + 1
                    P.dma("sp", gts[g0][:, 0:w], D["gT"][128 * m:128 * m + 128, tok0 + c0:tok0 + c0 + w], writes=[t_gts[g0]])
                    P.dma("sp", gts[g1][:, 0:w], D["gT"][1024 + 128 * m:1024 + 128 * m + 128, tok0 + c0:tok0 + c0 + w], writes=[t_gts[g1]])
                    P.op("act", lambda e, g0=g0, w=w: e.activation(gts[g0][:, 0:w], gts[g0][:, 0:w], AF.Sigmoid), reads=[t_gts[g0]], writes=[t_gts[g0]])
                    P.op("act", lambda e, g1=g1, w=w: e.activation(gts[g1][:, 0:w], gts[g1][:, 0:w], AF.Sigmoid), reads=[t_gts[g1]], writes=[t_gts[g1]])
                    P.op("dve", lambda e, g0=g0, ba=ba, w=w: e.tensor_tensor(gts[g0][:, 0:w], ps[ba][:, 0:w], gts[g0][:, 0:w], ALU.mult),
                         reads=[t_ps[ba], t_gts[g0]], writes=[t_gts[g0]])
                    P.op("dve", lambda e, g1=g1, bb=bb, w=w: e.tensor_tensor(gts[g1][:, 0:w], ps[bb][:, 0:w], gts[g1][:, 0:w], ALU.mult),
                         reads=[t_ps[bb], t_gts[g1]], writes=[t_gts[g1]])
                    P.op("pool", lambda e, g0=g0, g1=g1, m=m, w=w: e.tensor_tensor(merged[:, m, 0:w], gts[g0][:, 0:w], gts[g1][:, 0:w], ALU.add),
                         reads=[t_gts[g0], t_gts[g1]], writes=[t_merged])
                for m in range(8):
                    ms = slice(128 * m, 128 * m + 128)
                    bk = 4 + (m % 2)
                    for k in range(8):
                        P.op("pe", lambda e, k=k, ms=ms, bk=bk, w=w: e.matmul(ps[bk][:, 0:w], wout_bf[:, k, ms], merged[:, k, 0:w], start=(k == 0), stop=(k == 7)),
                             reads=[t_ar[1], t_merged], writes=[t_ps[bk]])
                    P.op("dve", lambda e, m=m, bk=bk, c0=c0, w=w: e.scalar_tensor_tensor(hT[:, m, c0:c0 + w], hT[:, m, c0:c0 + w], ALPHA, ps[bk][:, 0:w], ALU.mult, ALU.add),
                         reads=[t_ps[bk], t_h[gi][m]], writes=[t_h[gi][m]])
                ln_group(gi, c0, w, lnA, t_lnA)
            for (r0, nt) in rtiles:
                gi = [i for i, (c0, w) in enumerate(groups) if c0 <= r0 < c0 + w][0]
                RB = 5
                for k in range(8):
                    P.op("pe", lambda e, k=k, r0=r0, nt=nt: e.matmul(ps[RB][0:nt, 0:16], hT[:, k, r0:r0 + nt], rw[:, k, :], start=(k == 0), stop=(k == 7)),
                         reads=[t_h[gi][k], t_rw], writes=[t_ps[RB]])
                R = lambda i: rt[0:nt, i, :]
                S = lambda i: rs[0:nt, i:i + 1]

                def dv(fn, nt=nt):
                    P.op("dve", fn, reads=[t_rt, t_rs], writes=[t_rt, t_rs])
                P.op("dve", lambda e, nt=nt: e.tensor_tensor(rt[0:nt, 0, :], ps[RB][0:nt, 0:16], rb[0:nt, :], ALU.add),
                     reads=[t_ps[RB], t_rb], writes=[t_rt])
                dv(lambda e, nt=nt: e.reduce_max(rs[0:nt, 0:1], rt[0:nt, 0, :], axis=AX.X))
                dv(lambda e, nt=nt: e.tensor_scalar(rs[0:nt, 0:1], rs[0:nt, 0:1], -1.0, None, ALU.mult))
                P.op("act", lambda e, nt=nt: e.activation(rt[0:nt, 1, :], rt[0:nt, 0, :], AF.Exp, bias=rs[0:nt, 0:1]),
                     reads=[t_rt, t_rs], writes=[t_rt])
                dv(lambda e, nt=nt: e.reduce_sum(rs[0:nt, 1:2], rt[0:nt, 1, :], axis=AX.X))
                dv(lambda e, nt=nt: e.reciprocal(rs[0:nt, 1:2], rs[0:nt, 1:2]))
                dv(lambda e, nt=nt: e.tensor_scalar(rt[0:nt, 2, :], rt[0:nt, 1, :], rs[0:nt, 1:2], None, ALU.mult))
                for g in range(4):
                    dv(lambda e, nt=nt, g=g: e.reduce_max(rt[0:nt, 3, g:g + 1], rt[0:nt, 2, 4 * g:4 * g + 4], axis=AX.X))
                for g in range(4):
                    dv(lambda e, nt=nt, g=g: e.tensor_scalar(rt[0:nt, 4, 4 * g:4 * g + 4], rt[0:nt, 2, 4 * g:4 * g + 4], rt[0:nt, 3, g:g + 1], None, ALU.is_equal))
                dv(lambda e, nt=nt: e.scalar_tensor_tensor(rt[0:nt, 5, :], rt[0:nt, 4, :], -2.0, rt[0:nt, 2, :], ALU.mult, ALU.add))
                for g in range(4):
                    dv(lambda e, nt=nt, g=g: e.reduce_max(rt[0:nt, 3, 4 + g:5 + g], rt[0:nt, 5, 4 * g:4 * g + 4], axis=AX.X))
                dv(lambda e, nt=nt: e.tensor_tensor(rt[0:nt, 3, 8:12], rt[0:nt, 3, 0:4], rt[0:nt, 3, 4:8], ALU.add))
                dv(lambda e, nt=nt: e.reduce_max(rs[0:nt, 2:3], rt[0:nt, 3, 8:12], axis=AX.X))
                dv(lambda e, nt=nt: e.tensor_scalar(rt[0:nt, 3, 12:16], rt[0:nt, 3, 8:12], rs[0:nt, 2:3], None, ALU.is_equal))
                for g in range(4):
                    dv(lambda e, nt=nt, g=g: e.tensor_scalar(rt[0:nt, 6, 4 * g:4 * g + 4], rt[0:nt, 2, 4 * g:4 * g + 4], 1.0, rt[0:nt, 3, 12 + g:13 + g], ALU.add, ALU.mult))
                dv(lambda e, nt=nt: e.tensor_scalar(rt[0:nt, 6, :], rt[0:nt, 6, :], -1.0, None, ALU.add))
                dv(lambda e, nt=nt: e.reduce_max(rs[0:nt, 3:4], rt[0:nt, 6, :], axis=AX.X))
                dv(lambda e, nt=nt: e.tensor_scalar(rt[0:nt, 7, :], rt[0:nt, 6, :], rs[0:nt, 3:4], None, ALU.is_equal))
                dv(lambda e, nt=nt: e.scalar_tensor_tensor(rt[0:nt, 8, :], rt[0:nt, 7, :], -2.0, rt[0:nt, 6, :], ALU.mult, ALU.add))
                dv(lambda e, nt=nt: e.reduce_max(rs[0:nt, 4:5], rt[0:nt, 8, :], axis=AX.X))
                dv(lambda e, nt=nt: e.tensor_scalar(rt[0:nt, 9, :], rt[0:nt, 8, :], rs[0:nt, 4:5], None, ALU.is_equal))
                dv(lambda e, nt=nt: e.tensor_tensor(rs[0:nt, 5:6], rs[0:nt, 3:4], rs[0:nt, 4:5], ALU.add))
                dv(lambda e, nt=nt: e.reciprocal(rs[0:nt, 5:6], rs[0:nt, 5:6]))
                dv(lambda e, nt=nt: e.tensor_tensor(rs[0:nt, 6:7], rs[0:nt, 3:4], rs[0:nt, 5:6], ALU.mult))
                dv(lambda e, nt=nt: e.tensor_tensor(rs[0:nt, 7:8], rs[0:nt, 4:5], rs[0:nt, 5:6], ALU.mult))
                dv(lambda e, nt=nt: e.tensor_scalar(rt[0:nt, 10, :], rt[0:nt, 7, :], rs[0:nt, 6:7], None, ALU.mult))
                dv(lambda e, nt=nt: e.scalar_tensor_tensor(rt[0:nt, 11, :], rt[0:nt, 9, :], rs[0:nt, 7:8], rt[0:nt, 10, :], ALU.mult, ALU.add))
                P.op("pe", lambda e, nt=nt: e.transpose(ps[RB][0:16, 256:256 + nt], rt[0:nt, 11, :], tc[0:nt, 1, 0:nt]),
                     reads=[t_rt, t_tc], writes=[t_ps[RB]])
                P.op("act", lambda e, nt=nt, r0=r0: e.activation(combT[:, r0:r0 + nt], ps[RB][0:16, 256:256 + nt], AF.Identity),
                     reads=[t_ps[RB]], writes=[t_combT])
            for gi, (c0, w) in enumerate(groups):
                P.op("pool", lambda e, c0=c0, w=w: e.tensor_scalar(hT[:, :, c0:c0 + w], hT[:, :, c0:c0 + w], ALPHA, None, ALU.mult),
                     reads=t_h[gi], writes=t_h[gi])
            nhe = 0
            pending = []
            for ex in range(n_exp):
                cb_ = ex % 2
                for gi, (c0, w) in enumerate(groups):
                    P.op("pe", lambda e, ex=ex, c0=c0, w=w: e.matmul(ps[6][:, 0:w], sel[:, ex, :], combT[:, c0:c0 + w], start=True, stop=True),
                         reads=[t_sel, t_combT], writes=[t_ps[6]])
                    P.op("act", lambda e, cb_=cb_, c0=c0, w=w: e.activation(cbc[cb_][:, c0:c0 + w], ps[6][:, 0:w], AF.Identity),
                         reads=[t_ps[6]], writes=[t_cbc[cb_]])
                for hf in range(2):
                    ai = nhe % 2
                    nhe += 1
                    wg_bf = arena[ai][:, 0:2048].rearrange("p (k c) -> p k c", k=8)
                    wu_bf = arena[ai][:, 2048:4096].rearrange("p (k c) -> p k c", k=8)
                    wd_bf = arena[ai][:, 4096:6144].rearrange("p (k c) -> p k c", k=2)
                    fs = slice(256 * hf, 256 * hf + 256)
                    for (src, dst, kk_) in ((D["wg"][ex].rearrange("(k p) f -> p k f", p=128)[:, :, fs], wg_bf, 8),
                                            (D["wu"][ex].rearrange("(k p) f -> p k f", p=128)[:, :, fs], wu_bf, 8),
                                            (D["wd"][ex, 256 * hf:256 * hf + 256, :].rearrange("(k p) d -> p k d", p=128), wd_bf, 2)):
                        s = cnt["stg"] % NSTG; wq = WQ[cnt["stg"] % 2]; cnt["stg"] += 1
                        P.dma(wq, stg[s][:, 0:2048].rearrange("p (k c) -> p k c", k=kk_), src, writes=[t_stg[s]])
                        P.op("act", lambda e, s=s, dst=dst, kk_=kk_: e.activation(dst, stg[s][:, 0:2048].rearrange("p (k c) -> p k c", k=kk_), AF.Identity),
                             reads=[t_stg[s]], writes=[t_ar[ai]])
                    for gi, (c0, w) in enumerate(groups):
                        hb_ = cnt["ev"] % 2
                        cnt["ev"] += 1
                        dsteps = pending.pop() if pending else []
                        for fc in range(2):
                            bg, bu = fc, 2 + fc
                            fcs = slice(128 * fc, 128 * fc + 128)
                            for (bk_, wsrc) in ((bg, wg_bf), (bu, wu_bf)):
                                for k in range(8):
                                    P.op("pe", lambda e, k=k, bk_=bk_, fcs=fcs, c0=c0, w=w, wsrc=wsrc: e.matmul(ps[bk_][:, 0:w], wsrc[:, k, fcs], hbf[:, k, c0:c0 + w], start=(k == 0), stop=(k == 7)),
                                         reads=[t_ar[ai], t_hb[gi]], writes=[t_ps[bk_]])
                                    if k % 4 == 3 and dsteps:
                                        dsteps.pop(0)()
                            P.op("act", lambda e, fc=fc, bg=bg, w=w: e.activation(sgl[fc][:, 0:w], ps[bg][:, 0:w], AF.Silu),
                                 reads=[t_ps[bg]], writes=[t_sgl[fc]])
                            P.op("dve", lambda e, fc=fc, bu=bu, w=w: e.tensor_tensor(sgl[fc][:, 0:w], ps[bu][:, 0:w], sgl[fc][:, 0:w], ALU.mult),
                                 reads=[t_ps[bu], t_sgl[fc]], writes=[t_sgl[fc]])
                            P.op("dve", lambda e, fc=fc, hb_=hb_, cb_=cb_, c0=c0, w=w: e.tensor_tensor(hid[hb_][:, fc, 0:w], sgl[fc][:, 0:w], cbc[cb_][:, c0:c0 + w], ALU.mult),
                                 reads=[t_sgl[fc], t_cbc[cb_]], writes=[t_hid[hb_]])
                        while dsteps:
                            dsteps.pop(0)()

                        def mk_down(m, gi=gi, c0=c0, w=w, hb_=hb_, ai=ai, wd_bf=wd_bf):
                            def f():
                                bd = 4 + (m % 4)
                                ms = slice(128 * m, 128 * m + 128)
                                for fc in range(2):
                                    P.op("pe", lambda e, fc=fc: e.matmul(ps[bd][:, 0:w], wd_bf[:, fc, ms], hid[hb_][:, fc, 0:w], start=(fc == 0), stop=(fc == 1)),
                                         reads=[t_ar[ai], t_hid[hb_]], writes=[t_ps[bd]])
                                P.op("dve", lambda e: e.tensor_tensor(hT[:, m, c0:c0 + w], ps[bd][:, 0:w], hT[:, m, c0:c0 + w], ALU.add),
                                     reads=[t_ps[bd], t_h[gi][m]], writes=[t_h[gi][m]])
                            return f
                        pending.append([mk_down(m) for m in range(8)])
            if pending:
                for f_ in pending.pop():
                    f_()
            for gi, (c0, w) in enumerate(groups):
                ln_group(gi, c0, w, lnB, t_lnB)
        for gi, (c0, w) in enumerate(groups):
            P.dma("sp", hTo_v[:, :, tok0 + c0:tok0 + c0 + w], hT[:, :, c0:c0 + w], reads=t_h[gi])
        if do_proj:
            proj(groups, tok0)


NCORES = 8
SEQ = 8192
LFULL = N_META + SEQ


def _fm(v):
    return np.ascontiguousarray(np.asarray(v, np.float32).reshape(8, 128).T)


def _run(nc, in_maps):
    res = run_bass_kernel_spmd(nc, in_maps, core_ids=list(range(NCORES)))
    return res.results


def _new_nc():
    return bass.Bass("TRN2", target_bir_lowering=False)


def _mixers(pT_cores, prm):
    amask = attn_masks()
    attn_maps, rwkv_maps = [], []
    for c in range(NCORES):
        b, hp = c // 4, c % 4
        pb_ = np.concatenate([pT_cores[4 * b][:, 0:N_META]] + [pT_cores[4 * b + r][:, N_META:] for r in range(4)], axis=1)
        q = pb_[128 * hp:128 * hp + 128].reshape(2, 64, LFULL)
        k = pb_[512 + 128 * hp:512 + 128 * hp + 128].reshape(2, 64, LFULL)
        v = pb_[1024 + 128 * hp:1024 + 128 * hp + 128].reshape(2, 64, LFULL)
        vtm = v.transpose(0, 2, 1)
        vm = np.ascontiguousarray(vtm[:, 0:N_META])
        vx = np.ascontiguousarray(vtm[:, N_META:].reshape(2, SEQ // 128, 128, 64).transpose(0, 2, 1, 3))
        attn_maps.append({"qT": np.ascontiguousarray(q), "kT": np.ascontiguousarray(k), "vx": vx, "vm": vm, "msk": amask})
        p_rw = np.ascontiguousarray(pb_[1536:3328].T)
        rwkv_maps.append(rwkv_host_inputs(p_rw, hp, prm))
    nc = _new_nc()
    build_attn(nc, 2, SEQ // 512)
    ares = _run(nc, attn_maps)
    nc = _new_nc()
    build_rwkv(nc, SEQ // 128)
    rres = _run(nc, rwkv_maps)
    osbT, orwT = [], []
    for b in range(2):
        osbT.append(np.concatenate([ares[4 * b + hp]["oT"].reshape(128, LFULL) for hp in range(4)], axis=0))
        orwT.append(np.concatenate([rres[4 * b + hp]["o_rw"].T for hp in range(4)], axis=0))
    return osbT, orwT


def _core_cols(full, r):
    return np.ascontiguousarray(np.concatenate([full[:, 0:N_META], full[:, N_META + 2048 * r:N_META + 2048 * (r + 1)]], axis=1))


def kernel(**inputs):
    inp = {k: np.asarray(v) for k, v in inputs.items()}
    x, meta = inp["x"].astype(np.float32), inp["meta"].astype(np.float32)
    tcc, sel = tok_consts()
    maps = []
    for c in range(NCORES):
        b, r = c // 4, c % 4
        xT = np.ascontiguousarray(np.concatenate([meta.T, x[b, 2048 * r:2048 * (r + 1)].T], axis=1))
        maps.append({"xT": xT, "tc": tcc, "lnA": np.stack([_fm(inp["emb_ln_g"]), _fm(inp["emb_ln_b"])], axis=1),
                     "w_in": np.ascontiguousarray(inp["w_in"][0])})
    nc = _new_nc()
    build_tok(nc, "A", True)
    res = _run(nc, maps)
    hT_c = [r_["hT"] for r_ in res]
    pT_c = [r_["pT"] for r_ in res]
    for l in range(2):
        prm = {k: inp[k][l] for k in ["rwkv_mu", "w0", "w_up", "a0", "a_up", "g_up", "k_k", "k_a", "r_k", "lnx_g", "lnx_b"]}
        osbT, orwT = _mixers(pT_c, prm)
        last = (l == 1)
        maps = []
        for c in range(NCORES):
            b, r = c // 4, c % 4
            m = {"xT": hT_c[c], "tc": tcc, "lnA": np.stack([_fm(inp["ln1_g"][l]), _fm(inp["ln1_b"][l])], axis=1),
                 "osbT": _core_cols(osbT[b], r), "orwT": _core_cols(orwT[b], r),
                 "gT": np.ascontiguousarray(pT_c[c][3328:5376]),
                 "p_sb": np.ascontiguousarray(inp["p_sb"][l]), "p_rw": np.ascontiguousarray(inp["p_rwkv"][l]),
                 "w_out": np.ascontiguousarray(inp["w_out"][l]),
                 "lnB": np.stack([_fm(inp["ln2_g"][l]), _fm(inp["ln2_b"][l])], axis=1),
                 "router_w": np.ascontiguousarray(inp["router_w"]),
                 "rb": np.ascontiguousarray(np.broadcast_to(inp["router_b"][None].astype(np.float32), (128, 16))),
                 "sel": sel,
                 "wg": np.ascontiguousarray(inp["exp_w_gate"][l]), "wu": np.ascontiguousarray(inp["exp_w_up"][l]),
                 "wd": np.ascontiguousarray(inp["exp_w_down"][l])}
            if not last:
                m["w_in"] = np.ascontiguousarray(inp["w_in"][l + 1])
            maps.append(m)
        nc = _new_nc()
        build_tok(nc, "C", not last)
        res = _run(nc, maps)
        hT_c = [r_["hT"] for r_ in res]
        if not last:
            pT_c = [r_["pT"] for r_ in res]
    out = np.zeros((2, SEQ, 1024), np.float32)
    for c in range(NCORES):
        b, r = c // 4, c % 4
        out[b, 2048 * r:2048 * (r + 1)] = hT_c[c][:, N_META:].T
    return out
```

```python
import numpy as np
from contextlib import ExitStack
import concourse.bass as bass
import concourse.mybir as mybir
from concourse.bass_utils import run_bass_kernel_spmd

F32 = mybir.dt.float32
BF16 = mybir.dt.bfloat16
AF = mybir.ActivationFunctionType
ALU = mybir.AluOpType
AX = mybir.AxisListType

import os
BUDGET = int(os.environ["KBUDGET"]) if "KBUDGET" in os.environ else None
ENGS = ["pe", "act", "dve", "pool", "sp"]
DMA_R = 8


class T:
    __slots__ = ("name", "lw", "rd", "const", "excl")

    def __init__(self, name, const=False, excl=False):
        self.name = name
        self.lw = None
        self.rd = []
        self.const = const
        self.excl = excl


class Prog:
    def __init__(self, nc, es):
        self.nc = nc
        self.es = es
        self.items = {e: [] for e in ENGS}
        self.sem = {e: es.enter_context(nc.semaphore("s_" + e)) for e in ENGS}
        self.cnt = {e: 0 for e in ENGS}
        self.seen = {e: {} for e in ENGS}
        self.dsem = {}
        self.dma_n = {}
        self.dma_final = {}
        self.ntile = 0

    def sb(self, name, shape, dt=F32):
        return self.es.enter_context(self.nc.sbuf_tensor("sb_" + name, list(shape), dt))

    def ps(self, name, shape=(128, 512), dt=F32):
        return self.es.enter_context(self.nc.psum_tensor("pp_" + name, list(shape), dt))

    def T(self, name=None, const=False, excl=False):
        self.ntile += 1
        return T(name or f"t{self.ntile}", const, excl)

    def _deps(self, eng, reads, writes):
        deps = {}

        def add(p):
            if p is None:
                return
            k, v = p
            if deps.get(k, 0) < v:
                deps[k] = v

        for t in reads:
            add(t.lw)
        for t in writes:
            add(t.lw)
            for p in t.rd:
                add(p)
        waits = []
        for k, v in deps.items():
            if k == eng and eng == "pe":
                continue
            if self.seen[eng].get(k, 0) >= v:
                continue
            self.seen[eng][k] = v
            waits.append((k, v))
        return waits

    def _commit(self, pid, reads, writes):
        for t in writes:
            t.lw = pid
            t.rd = []
        for t in reads:
            if not t.const:
                t.rd.append(pid)
                if len(t.rd) > 64:
                    m = {}
                    for k, v in t.rd:
                        if m.get(k, 0) < v:
                            m[k] = v
                    t.rd = list(m.items())

    def begin_stream(self):
        self._rec = []

    def end_stream(self):
        r, self._rec = self._rec, None
        return r

    def mark(self):
        if getattr(self, "_rec", None) is not None:
            self._rec.append(("mark", ()))

    def run_interleaved(self, streams, max_active=2):
        streams = list(streams)
        active = []
        nxt = 0
        while nxt < len(streams) or active:
            if nxt < len(streams) and len(active) < max_active and (not active or active[-1][2] >= 1):
                active.append([streams[nxt], 0, 0])
                nxt += 1
            for idx, a in enumerate(list(active)):
                if a[1] >= len(a[0]):
                    continue
                kind, args = a[0][a[1]]
                if kind == "mark":
                    if idx > 0:
                        older = active[idx - 1]
                        if older[1] < len(older[0]) and older[2] < a[2] + 2:
                            continue
                    a[2] += 1
                    a[1] += 1
                    continue
                a[1] += 1
                (self.op if kind == "op" else self.dma)(*args)
            active = [a for a in active if a[1] < len(a[0])]

    def op(self, eng, fn, reads=(), writes=()):
        if getattr(self, "_rec", None) is not None:
            self._rec.append(("op", (eng, fn, tuple(reads), tuple(writes))))
            return
        self.nops = getattr(self, "nops", 0) + 1
        if BUDGET is not None and self.nops > BUDGET:
            return
        ex = [t for t in reads if t.excl]
        if ex:
            reads = [t for t in reads if not t.excl]
            writes = list(writes) + ex
        waits = self._deps(eng, reads, writes)
        self.cnt[eng] += 1
        pid = (eng, self.cnt[eng])
        self.items[eng].append((waits, fn, True))
        self._commit(pid, reads, writes)

    def dma(self, q, out_ap, in_ap, reads=(), writes=()):
        if getattr(self, "_rec", None) is not None:
            self._rec.append(("dma", (q, out_ap, in_ap, tuple(reads), tuple(writes))))
            return
        self.nops = getattr(self, "nops", 0) + 1
        if BUDGET is not None and self.nops > BUDGET:
            return
        if q not in self.dsem:
            self.dsem[q] = [self.es.enter_context(self.nc.semaphore(f"d_{q}_{i}")) for i in range(DMA_R)]
            self.dma_n[q] = 0
        n = self.dma_n[q]
        self.dma_n[q] += 1
        i = n % DMA_R
        rnd = n // DMA_R
        key = ("d", q, i)
        waits = self._deps(q, reads, writes)
        if rnd > 0 and self.seen[q].get(key, 0) < 16 * rnd:
            self.seen[q][key] = 16 * rnd
            waits.append((key, 16 * rnd))
        sem = self.dsem[q][i]

        def fn(e, out_ap=out_ap, in_ap=in_ap, sem=sem):
            return e.dma_start(out=out_ap, in_=in_ap).then_inc(sem, 16)

        self.items[q].append((waits, fn, False))
        pid = (key, 16 * (rnd + 1))
        self.dma_final[key] = 16 * (rnd + 1)
        self._commit(pid, reads, writes)

    def _semobj(self, k):
        if isinstance(k, tuple):
            return self.dsem[k[1]][k[2]]
        return self.sem[k]

    def finish(self):
        waits = []
        for key, v in self.dma_final.items():
            waits.append((key, v))
        for e in ENGS:
            if e != "sp" and self.cnt[e] > 0:
                waits.append((e, self.cnt[e]))
        self.items["sp"].append((waits, None, False))
        nc = self.nc
        with nc.Block() as block:
            def replay(name, e):
                for waits, fn, inc in self.items[name]:
                    for k, v in waits:
                        e.wait_ge(self._semobj(k), v)
                    if fn is None:
                        continue
                    ins = fn(e)
                    if inc:
                        ins.then_inc(self.sem[name], 1)

            @block.tensor
            def _(e):
                replay("pe", e)

            @block.scalar
            def _(e):
                replay("act", e)

            @block.vector
            def _(e):
                replay("dve", e)

            @block.gpsimd
            def _(e):
                replay("pool", e)

            @block.sync
            def _(e):
                replay("sp", e)


N_META = 16


def build_attn(nc, NH, NG):
    NB = 4 * NG
    L = N_META + 512 * NG
    qT = nc.dram_tensor("qT", [NH, 64, L], F32, kind="ExternalInput").ap()
    kT = nc.dram_tensor("kT", [NH, 64, L], F32, kind="ExternalInput").ap()
    vx = nc.dram_tensor("vx", [NH, 128, NB, 64], F32, kind="ExternalInput").ap()
    vm = nc.dram_tensor("vm", [NH, 16, 64], F32, kind="ExternalInput").ap()
    msk = nc.dram_tensor("msk", [128, 3, 128], F32, kind="ExternalInput").ap()
    oT = nc.dram_tensor("oT", [NH, 64, L], F32, kind="ExternalOutput").ap()
    with ExitStack() as es:
        P = Prog(nc, es)
        emit_attn(P, NH, NG, qT, kT, vx, vm, msk, oT)
        P.finish()
    return nc


def emit_attn(P, NH, NG, qT, kT, vx, vm, msk, oT):
    NB = 4 * NG
    L = N_META + 512 * NG
    mstage = P.sb("mstage", [128, 3, 128], F32)
    t_mstage = P.T()
    P.dma("sp", mstage[:], msk, writes=[t_mstage])
    cm = P.sb("cm", [128, 3, 128], BF16)
    t_cm = P.T(const=True)
    P.op("dve", lambda e: e.tensor_copy(cm[:], mstage[:]), reads=[t_mstage], writes=[t_cm])
    zeros = P.sb("zeros", [128, 64], BF16)
    t_zeros = P.T(const=True)
    P.op("pool", lambda e: e.memset(zeros[:], 0.0), writes=[t_zeros])

    QT = P.sb("QT", [64, L], BF16)
    KT = P.sb("KT", [64, L], BF16)
    V = P.sb("V", [128, NB, 64], BF16)
    VM = P.sb("VM", [16, 64], BF16)
    t_Q, t_K, t_V = P.T(), P.T(), P.T()
    CH = 2064 if L > 2064 else L
    stg = [P.sb(f"stg{i}", [128, CH], F32) for i in range(2)]
    t_stg = [P.T(), P.T()]
    vstg = P.sb("vstg", [128, NB, 64], F32)
    t_vstg = P.T()
    vmstg = P.sb("vmstg", [16, 64], F32)
    t_vmstg = P.T()

    zA = [P.ps(f"zA{i}") for i in range(2)]
    zB = [P.ps(f"zB{i}") for i in range(2)]
    Ops = [P.ps(f"Ops{i}") for i in range(2)]
    t_zA = [P.T(excl=True), P.T(excl=True)]
    t_zB = [P.T(excl=True), P.T(excl=True)]
    t_O = [P.T(excl=True), P.T(excl=True)]
    e_sb = [P.sb(f"e{i}", [128, 512], F32) for i in range(2)]
    t_e = [P.T(), P.T()]
    sp_sb = [P.sb(f"sp{i}", [128, 512], BF16) for i in range(2)]
    t_sp = [P.T(), P.T()]
    A_sb = [P.sb(f"A{i}", [128, 512], BF16) for i in range(2)]
    t_A = [P.T(), P.T()]
    acc = P.sb("acc", [128, 512], F32)
    t_acc = P.T()
    accb = [P.sb(f"accb{i}", [128, 512], BF16) for i in range(3)]
    t_accb = [P.T(), P.T(), P.T()]
    ost = [P.sb(f"ost{i}", [64, 512], F32) for i in range(2)]
    t_ost = [P.T(), P.T()]

    nstg = 0
    gcount = 0
    for h in range(NH):
        for (src, dst, tt, scale) in ((qT, QT, t_Q, 0.125), (kT, KT, t_K, 1.0)):
            first = True
            for c0 in range(0, L, CH):
                w = min(CH, L - c0)
                s = nstg % 2
                nstg += 1
                P.dma("sp", stg[s][0:64, 0:w], src[h, :, c0:c0 + w], writes=[t_stg[s]])
                if scale != 1.0:
                    P.op("dve", lambda e, s=s, w=w, c0=c0, dst=dst, scale=scale:
                         e.tensor_scalar(dst[:, c0:c0 + w], stg[s][0:64, 0:w], scale, None, ALU.mult),
                         reads=[t_stg[s]], writes=[tt])
                else:
                    P.op("pool", lambda e, s=s, w=w, c0=c0, dst=dst:
                         e.tensor_copy(dst[:, c0:c0 + w], stg[s][0:64, 0:w]),
                         reads=[t_stg[s]], writes=[tt])
        P.dma("sp", vstg[:], vx[h], writes=[t_vstg])
        P.op("pool", lambda e: e.tensor_copy(V[:], vstg[:]), reads=[t_vstg], writes=[t_V])
        P.dma("sp", vmstg[:], vm[h], writes=[t_vmstg])
        P.op("pool", lambda e: e.tensor_copy(VM[:], vmstg[:]), reads=[t_vmstg], writes=[t_V])

        for g in [-1] + list(range(NG)):
            if g < 0:
                q0, QW = 0, N_META
                blocks = [("m", 0)]
            else:
                q0, QW = N_META + 512 * g, 512
                blocks = [("x", j) for j in range(4 * g + 3, -1, -1)] + [("m", 0)]
            ob = gcount % 2
            gcount += 1
            P.op("pe", lambda e, ob=ob, QW=QW, q0=q0:
                 e.matmul(Ops[ob][0:64, 0:QW], zeros[0:64, 0:64], QT[:, q0:q0 + QW], start=True, stop=False),
                 reads=[t_zeros, t_Q], writes=[t_O[ob]])
            P.op("pool", lambda e: e.memset(acc[:], 0.0), writes=[t_acc])
            nblk = len(blocks)
            info = []
            for it, (kind, j) in enumerate(blocks):
                if kind == "m":
                    kp, k0 = N_META, 0
                    c0 = 0
                    diag = (g < 0)
                else:
                    kp, k0 = 128, N_META + 128 * j
                    jl = j - 4 * g
                    diag = jl >= 0
                    c0 = 128 * jl if diag else 0
                info.append((kind, j, kp, k0, c0, diag))

            def prm(it, info=info, q0=q0, QW=QW):
                kind, j, kp, k0, c0, diag = info[it]
                W = QW - c0
                return kind, j, kp, k0, c0, diag, it % 2, W, slice(q0 + c0, q0 + QW), min(128, W)

            def pe_zA(it):
                kind, j, kp, k0, c0, diag, b, W, qs, dw = prm(it)
                P.op("pe", lambda e: e.matmul(zA[b][0:kp, 0:W], KT[:, k0:k0 + kp], QT[:, qs], start=True, stop=True),
                     reads=[t_K, t_Q], writes=[t_zA[b]])

            def act_e(it):
                kind, j, kp, k0, c0, diag, b, W, qs, dw = prm(it)
                P.op("act", lambda e: e.activation(e_sb[b][0:kp, 0:W], zA[b][0:kp, 0:W], AF.Exp),
                     reads=[t_zA[b]], writes=[t_e[b]])

            def act_sp(it, nblk=nblk, QW=QW):
                kind, j, kp, k0, c0, diag, b, W, qs, dw = prm(it)
                P.op("act", lambda e: e.activation(sp_sb[b][0:kp, 0:W], e_sb[b][0:kp, 0:W], AF.Ln, bias=1.0),
                     reads=[t_e[b]], writes=[t_sp[b]])
                if diag:
                    P.op("pool", lambda e: e.tensor_tensor(sp_sb[b][0:kp, 0:dw], sp_sb[b][0:kp, 0:dw],
                                                           cm[0:kp, 0, 0:dw], ALU.mult),
                         reads=[t_sp[b], t_cm], writes=[t_sp[b]])
                if it < nblk - 1:
                    a3 = it % 3
                    P.op("dve", lambda e: e.tensor_tensor(acc[0:kp, c0:QW], acc[0:kp, c0:QW], sp_sb[b][0:kp, 0:W], ALU.add),
                         reads=[t_sp[b], t_acc], writes=[t_acc])
                    P.op("dve", lambda e: e.tensor_copy(accb[a3][:, 0:QW], acc[:, 0:QW]),
                         reads=[t_acc], writes=[t_accb[a3]])

            def pe_zB(it, QW=QW):
                kind, j, kp, k0, c0, diag, b, W, qs, dw = prm(it)
                last = (it == 0)
                P.op("pe", lambda e: e.matmul(zB[b][0:kp, 0:W], KT[:, k0:k0 + kp], QT[:, qs], start=True, stop=False),
                     reads=[t_K, t_Q], writes=[t_zB[b]])
                P.op("pe", lambda e: e.matmul(zB[b][0:kp, 0:W], cm[0:kp, 1, 0:kp], sp_sb[b][0:kp, 0:W],
                                              start=False, stop=last),
                     reads=[t_cm, t_sp[b]], writes=[t_zB[b]])
                if not last:
                    ab = (it - 1) % 3
                    P.op("pe", lambda e: e.matmul(zB[b][0:kp, 0:W], cm[0:128, 2, 0:kp], accb[ab][0:128, c0:QW],
                                                  start=False, stop=True),
                         reads=[t_cm, t_accb[ab]], writes=[t_zB[b]])

            def act_A(it):
                kind, j, kp, k0, c0, diag, b, W, qs, dw = prm(it)
                P.op("act", lambda e: e.activation(A_sb[b][0:kp, 0:W], zB[b][0:kp, 0:W], AF.Exp),
                     reads=[t_zB[b]], writes=[t_A[b]])
                if diag:
                    P.op("pool", lambda e: e.tensor_tensor(A_sb[b][0:kp, 0:dw], A_sb[b][0:kp, 0:dw],
                                                           cm[0:kp, 0, 0:dw], ALU.mult),
                         reads=[t_A[b], t_cm], writes=[t_A[b]])

            def pe_AV(it, ob=ob, nblk=nblk, QW=QW):
                kind, j, kp, k0, c0, diag, b, W, qs, dw = prm(it)
                vop = (VM[0:kp, :] if kind == "m" else V[:, j, :])
                P.op("pe", lambda e: e.matmul(Ops[ob][0:64, c0:QW], vop, A_sb[b][0:kp, 0:W],
                                              start=False, stop=(it == nblk - 1)),
                     reads=[t_V, t_A[b]], writes=[t_O[ob]])

            for s_ in range(-2, nblk + 2):
                if 0 <= s_ + 2 < nblk:
                    pe_zA(s_ + 2)
                if 0 <= s_ < nblk:
                    pe_zB(s_)
                if 0 <= s_ - 2 < nblk:
                    pe_AV(s_ - 2)
                if 0 <= s_ + 1 < nblk:
                    act_e(s_ + 1)
                if 0 <= s_ - 1 < nblk:
                    act_A(s_ - 1)
                if 0 <= s_ + 1 < nblk:
                    act_sp(s_ + 1)
            P.op("dve", lambda e, ob=ob, QW=QW: e.tensor_copy(ost[ob][:, 0:QW], Ops[ob][0:64, 0:QW]),
                 reads=[t_O[ob]], writes=[t_ost[ob]])
            P.dma("sp", oT[h, :, q0:q0 + QW], ost[ob][:, 0:QW], reads=[t_ost[ob]])


def attn_masks():
    s = np.arange(128)[:, None]
    t = np.arange(128)[None, :]
    m = np.zeros((128, 3, 128), np.float32)
    m[:, 0, :] = (s < t)
    m[:, 1, :] = -1.0 * (s >= t)
    m[:, 2, :] = -1.0
    return m


DECAY_SCALE = float(np.exp(-0.5))
LNX_EPS = 64e-5


def rwkv_consts():
    idx = np.arange(128)
    ch = idx // 64
    same = ch[:, None] == ch[None, :]
    le = idx[:, None] <= idx[None, :]
    lt = idx[:, None] < idx[None, :]
    c = np.zeros((128, 9, 128), np.float32)
    c[:, 0] = -DECAY_SCALE * (same & le)
    c[:, 1] = -DECAY_SCALE * same
    c[:, 2] = same & lt
    c[:, 3] = same & le
    c[:, 4] = same & lt
    c[:, 5] = same & le
    c[:, 6] = (same & lt).T
    c[:, 7] = np.eye(128)
    c[:, 8, 0] = -DECAY_SCALE * (ch == 0)
    c[:, 8, 1] = -DECAY_SCALE * (ch == 1)
    c[:, 8, 2] = (ch == 0)
    c[:, 8, 3] = (ch == 1)
    return c


def build_rwkv(nc, NTX):
    L = N_META + 128 * NTX
    D = {}
    D["rkv"] = nc.dram_tensor("rkv", [L + 1, 384], F32, kind="ExternalInput").ap()
    D["lo"] = nc.dram_tensor("lo", [256, L + 1], F32, kind="ExternalInput").ap()
    D["mu_tm"] = nc.dram_tensor("mu_tm", [128, 384], F32, kind="ExternalInput").ap()
    D["mu_fm"] = nc.dram_tensor("mu_fm", [128, 3], F32, kind="ExternalInput").ap()
    D["bc"] = nc.dram_tensor("bc", [128, 7, 128], F32, kind="ExternalInput").ap()
    D["wa_up"] = nc.dram_tensor("wa_up", [128, 128], F32, kind="ExternalInput").ap()
    D["g_up"] = nc.dram_tensor("g_up", [128, 128], F32, kind="ExternalInput").ap()
    D["cst"] = nc.dram_tensor("cst", [128, 9, 128], F32, kind="ExternalInput").ap()
    D["o"] = nc.dram_tensor("o_rw", [L, 128], F32, kind="ExternalOutput").ap()
    with ExitStack() as es:
        P = Prog(nc, es)
        emit_rwkv(P, NTX, D)
        P.finish()
    return nc


def emit_rwkv(P, NTX, D, pfx="rw"):
    DS = DECAY_SCALE
    NCH = 1 + 2 * NTX

    def const_load(name, shape, src):
        t = P.sb(pfx + name, shape, F32)
        tt = P.T(const=True)
        P.dma("sp", t[:], src, writes=[tt])
        return t, tt

    cst, t_cst = const_load("cst", [128, 9, 128], D["cst"])
    bc, t_bc = const_load("bc", [128, 7, 128], D["bc"])
    mu_tm, t_mutm = const_load("mu_tm", [128, 384], D["mu_tm"])
    mu_fm, t_mufm = const_load("mu_fm", [128, 3], D["mu_fm"])
    wup, t_wup = const_load("wup", [64, 128], D["wa_up"][0:64, :])
    aup, t_aup = const_load("aup", [64, 128], D["wa_up"][64:128, :])
    gup, t_gup = const_load("gup", [128, 128], D["g_up"])

    S_all = [P.sb(pfx + f"S_all{h}", [64, NCH + 1, 64], BF16) for h in range(2)]
    ident_bf = P.sb(pfx + "ident_bf", [128, 128], BF16)
    t_idb = P.T(const=True)
    P.op("pool", lambda e: e.tensor_copy(ident_bf[:], cst[:, 7, :]), reads=[t_cst], writes=[t_idb])
    t_S = [[P.T() for _ in range(NCH + 1)] for h in range(2)]
    for h in range(2):
        P.op("pool", lambda e, h=h: e.memset(S_all[h][:, 0, :], 0.0), writes=[t_S[h][0]])

    pb = [P.ps(pfx + f"pb{i}", [128, 4, 128], BF16 if i == 2 else F32) for i in range(8)]
    t_bank = [P.T("bank%d" % i, excl=True) for i in range(8)]

    names_sb = {
        "xa": [128, 384], "xs": [128, 384], "x": [128, 384],
        "la": [64, 128], "ls": [64, 128], "la2": [64, 128], "ls2": [64, 128], "ga": [128, 128], "gs": [128, 128],
        "lx": [64, 128], "lx2": [64, 128], "gx": [128, 128], "tw": [64, 128], "sg": [128, 128],
        "sigw": [128, 128], "a": [128, 128], "g": [128, 128],
        "kk": [128, 128], "sq": [128, 128], "ss": [128, 2], "rn": [128, 2],
        "kp": [128, 128], "t1": [128, 128], "bs": [128, 2],
        "cum": [128, 128], "c1": [128, 128], "c2": [128, 128],
        "ep": [128, 128], "em": [128, 128], "epm": [128, 128], "ebar": [128, 128], "ebz": [128, 2, 128],
        "PC": [64, 2, 2],
        "TM": [128, 4, 128],
        "kka": [128, 128], "bbz": [128, 2, 128], "kbz": [128, 2, 128],
        "FM0": [64, 4, 128], "FM1": [64, 4, 128],
        "G0": [128, 4, 128], "G1": [128, 4, 128],
        "NP0": [128, 6, 2, 128], "NP1": [128, 6, 2, 128],
        "X0a": [128, 128], "X0b": [128, 128], "X1a": [128, 128], "X1b": [128, 128],
        "M": [64, 4, 64], "RHz0": [64, 2, 128], "RHz1": [64, 2, 128],
        "y": [128, 128], "ysq": [128, 128], "st": [128, 8], "yn": [128, 128], "o": [128, 128],
    }
    BF_NAMES = {"TM", "bbz", "kbz", "FM0", "FM1", "G0", "G1", "NP0", "NP1", "X0a", "X0b", "X1a", "X1b", "M", "RHz0", "RHz1", "xvb"}
    names_sb["xvb"] = [128, 128]
    bufs = []
    for p in range(2):
        d = {}
        for nm, shp in names_sb.items():
            d[nm] = (P.sb(f"{pfx}{nm}_{p}", shp, BF16 if nm in BF_NAMES else F32), P.T(f"{nm}{p}"))
        bufs.append(d)
        for nm in ("RHz0", "RHz1"):
            t, tt = d[nm]
            P.op("pool", lambda e, t=t: e.memset(t[:], 0.0), writes=[tt])

    rkv, lo, o_d = D["rkv"], D["lo"], D["o"]
    B_LORA, B_CUM, B_FM, B_G, B_H0, B_H1, B_PM, B_Y = range(8)
    B_HH = [B_H0, B_H1]

    def tile_body(ti):
        n = 16 if ti == 0 else 128
        t0 = 0 if ti == 0 else N_META + 128 * (ti - 1)
        nch = 1 if ti == 0 else 2
        cb = 0 if ti == 0 else 1 + 2 * (ti - 1)
        B = bufs[ti % 2]

        def b(nm):
            return B[nm]

        xa, t_xa = b("xa"); xs, t_xs = b("xs"); x, t_x = b("x")
        la, t_la = b("la"); ls, t_ls = b("ls"); la2, t_la2 = b("la2"); ls2, t_ls2 = b("ls2")
        ga, t_ga = b("ga"); gs, t_gs = b("gs")
        lx, t_lx = b("lx"); lx2, t_lx2 = b("lx2"); gx, t_gx = b("gx"); tw, t_tw = b("tw"); sg, t_sg = b("sg")
        sigw, t_sigw = b("sigw"); a_, t_a = b("a"); g_, t_g = b("g")
        kk, t_kk = b("kk"); sq, t_sq = b("sq"); ss, t_ss = b("ss"); rn, t_rn = b("rn")
        kp, t_kp = b("kp"); t1, t_t1 = b("t1"); bs, t_bs = b("bs")
        cum, t_cum = b("cum"); c1, t_c1 = b("c1"); c2, t_c2 = b("c2")
        ep, t_ep = b("ep"); em, t_em = b("em"); epm, t_epm = b("epm"); ebar, t_ebar = b("ebar")
        ebz, t_ebz = b("ebz")
        PC, t_PC = b("PC"); TM, t_TM = b("TM"); kka, t_kka = b("kka")
        bbz, t_bbz = b("bbz"); kbz, t_kbz = b("kbz")
        FMh = [b("FM0"), b("FM1")]
        M_, t_M = b("M"); RHz = [b("RHz0"), b("RHz1")]
        y_, t_y = b("y"); ysq, t_ysq = b("ysq"); st, t_st = b("st"); yn, t_yn = b("yn"); o_, t_o = b("o")

        P.dma("sp", xa[0:n, :], rkv[1 + t0:1 + t0 + n, :], writes=[t_xa])
        P.dma("sp", xs[0:n, :], rkv[t0:t0 + n, :], writes=[t_xs])
        P.dma("sp", la[:, 0:n], lo[0:64, 1 + t0:1 + t0 + n], writes=[t_la])
        P.dma("sp", ls[:, 0:n], lo[0:64, t0:t0 + n], writes=[t_ls])
        P.dma("sp", la2[:, 0:n], lo[64:128, 1 + t0:1 + t0 + n], writes=[t_la2])
        P.dma("sp", ls2[:, 0:n], lo[64:128, t0:t0 + n], writes=[t_ls2])
        P.dma("sp", ga[:, 0:n], lo[128:256, 1 + t0:1 + t0 + n], writes=[t_ga])
        P.dma("sp", gs[:, 0:n], lo[128:256, t0:t0 + n], writes=[t_gs])
        P.op("pool", lambda e: e.tensor_tensor(xs[0:n, :], xs[0:n, :], xa[0:n, :], ALU.subtract),
             reads=[t_xa, t_xs], writes=[t_xs])
        P.op("pool", lambda e: e.tensor_tensor(xs[0:n, :], xs[0:n, :], mu_tm[0:n, :], ALU.mult),
             reads=[t_xs, t_mutm], writes=[t_xs])
        P.op("pool", lambda e: e.tensor_tensor(x[0:n, :], xs[0:n, :], xa[0:n, :], ALU.add),
             reads=[t_xa, t_xs], writes=[t_x])
        for (A_, tA, S_, tS, O_, tO, col, np_) in ((la, t_la, ls, t_ls, lx, t_lx, 0, 64), (la2, t_la2, ls2, t_ls2, lx2, t_lx2, 1, 64),
                                                  (ga, t_ga, gs, t_gs, gx, t_gx, 2, 128)):
            P.op("pool", lambda e, A_=A_, S_=S_: e.tensor_tensor(S_[:, 0:n], S_[:, 0:n], A_[:, 0:n], ALU.subtract),
                 reads=[tA, tS], writes=[tS])
            P.op("dve", lambda e, A_=A_, S_=S_, O_=O_, col=col, np_=np_: e.scalar_tensor_tensor(O_[:, 0:n], S_[:, 0:n], mu_fm[0:np_, col:col + 1], A_[:, 0:n], ALU.mult, ALU.add),
                 reads=[tA, tS, t_mufm], writes=[tO])
        xr = x[0:n, 0:128]
        xk = x[0:n, 128:256]
        xvb, t_xvb = b("xvb")
        P.op("pool", lambda e: e.tensor_copy(xvb[0:n, :], x[0:n, 256:384]), reads=[t_x], writes=[t_xvb])
        P.op("act", lambda e: e.activation(tw[:, 0:n], lx[:, 0:n], AF.Exp, scale=-2.0), reads=[t_lx], writes=[t_tw])
        P.op("dve", lambda e: e.tensor_scalar(tw[:, 0:n], tw[:, 0:n], 1.0, None, ALU.add), reads=[t_tw], writes=[t_tw])
        P.op("dve", lambda e: e.reciprocal(tw[:, 0:n], tw[:, 0:n]), reads=[t_tw], writes=[t_tw])
        P.op("dve", lambda e: e.tensor_scalar(tw[:, 0:n], tw[:, 0:n], 2.0, -1.0, ALU.mult, ALU.add), reads=[t_tw], writes=[t_tw])
        P.op("act", lambda e: e.activation(sg[:, 0:n], gx[:, 0:n], AF.Exp, scale=-1.0), reads=[t_gx], writes=[t_sg])
        P.op("dve", lambda e: e.tensor_scalar(sg[:, 0:n], sg[:, 0:n], 1.0, None, ALU.add), reads=[t_sg], writes=[t_sg])
        P.op("dve", lambda e: e.reciprocal(sg[:, 0:n], sg[:, 0:n]), reads=[t_sg], writes=[t_sg])
        tb = t_bank[B_LORA]
        P.op("pe", lambda e: e.matmul(pb[B_LORA][0:n, 0, :], tw[:, 0:n], wup[:, :], start=True, stop=True),
             reads=[t_tw, t_wup], writes=[tb])
        P.op("pe", lambda e: e.matmul(pb[B_LORA][0:n, 1, :], lx2[:, 0:n], aup[:, :], start=True, stop=True),
             reads=[t_lx2, t_aup], writes=[tb])
        P.op("pe", lambda e: e.matmul(pb[B_LORA][0:n, 2, :], sg[:, 0:n], gup[:, :], start=True, stop=True),
             reads=[t_sg, t_gup], writes=[tb])
        P.op("dve", lambda e: e.tensor_tensor(sigw[0:n, :], pb[B_LORA][0:n, 0, :], bc[0:n, 0, :], ALU.add),
             reads=[tb, t_bc], writes=[t_sigw])
        P.op("dve", lambda e: e.tensor_tensor(a_[0:n, :], pb[B_LORA][0:n, 1, :], bc[0:n, 1, :], ALU.add),
             reads=[tb, t_bc], writes=[t_a])
        P.op("act", lambda e: e.activation(g_[0:n, :], pb[B_LORA][0:n, 2, :], AF.Identity), reads=[tb], writes=[t_g])
        for (Z, tZ) in ((sigw, t_sigw), (a_, t_a)):
            P.op("act", lambda e, Z=Z: e.activation(Z[0:n, :], Z[0:n, :], AF.Exp, scale=-1.0), reads=[tZ], writes=[tZ])
            P.op("dve", lambda e, Z=Z: e.tensor_scalar(Z[0:n, :], Z[0:n, :], 1.0, None, ALU.add), reads=[tZ], writes=[tZ])
            P.op("dve", lambda e, Z=Z: e.reciprocal(Z[0:n, :], Z[0:n, :]), reads=[tZ], writes=[tZ])
        P.op("pool", lambda e: e.tensor_tensor(kk[0:n, :], xk, bc[0:n, 2, :], ALU.mult), reads=[t_x, t_bc], writes=[t_kk])
        P.op("pool", lambda e: e.tensor_tensor(sq[0:n, :], kk[0:n, :], kk[0:n, :], ALU.mult), reads=[t_kk], writes=[t_sq])
        for h in range(2):
            P.op("dve", lambda e, h=h: e.reduce_sum(ss[0:n, h:h + 1], sq[0:n, 64 * h:64 * h + 64], axis=AX.X),
                 reads=[t_sq], writes=[t_ss])
        P.op("dve", lambda e: e.tensor_scalar(rn[0:n, :], ss[0:n, :], 1e-24, None, ALU.max), reads=[t_ss], writes=[t_rn])
        P.op("act", lambda e: e.activation(rn[0:n, :], rn[0:n, :], AF.Ln), reads=[t_rn], writes=[t_rn])
        P.op("act", lambda e: e.activation(rn[0:n, :], rn[0:n, :], AF.Exp, scale=-0.5), reads=[t_rn], writes=[t_rn])
        for h in range(2):
            P.op("dve", lambda e, h=h: e.tensor_scalar(kk[0:n, 64 * h:64 * h + 64], kk[0:n, 64 * h:64 * h + 64],
                                                       rn[0:n, h:h + 1], None, ALU.mult),
                 reads=[t_kk, t_rn], writes=[t_kk])
        P.op("dve", lambda e: e.scalar_tensor_tensor(t1[0:n, :], a_[0:n, :], -1.0, bc[0:n, 3, :], ALU.add, ALU.mult),
             reads=[t_a, t_bc], writes=[t_t1])
        P.op("dve", lambda e: e.scalar_tensor_tensor(kp[0:n, :], t1[0:n, :], 1.0, xk, ALU.add, ALU.mult),
             reads=[t_t1, t_x], writes=[t_kp])
        P.op("pool", lambda e: e.tensor_tensor(t1[0:n, :], xr, kp[0:n, :], ALU.mult), reads=[t_x, t_kp, t_t1], writes=[t_t1])
        P.op("pool", lambda e: e.tensor_tensor(t1[0:n, :], t1[0:n, :], bc[0:n, 4, :], ALU.mult), reads=[t_t1, t_bc], writes=[t_t1])
        for h in range(2):
            P.op("dve", lambda e, h=h: e.reduce_sum(bs[0:n, h:h + 1], t1[0:n, 64 * h:64 * h + 64], axis=AX.X),
                 reads=[t_t1], writes=[t_bs])
        tb = t_bank[B_CUM]
        P.op("pe", lambda e: e.matmul(pb[B_CUM][0:n, 0, :], cst[0:n, 0, 0:n], sigw[0:n, :], start=True, stop=True),
             reads=[t_cst, t_sigw], writes=[tb])
        P.op("pe", lambda e: e.matmul(pb[B_CUM][0:n, 1, :], cst[0:n, 1, 0:n], sigw[0:n, :], start=True, stop=True),
             reads=[t_cst, t_sigw], writes=[tb])
        for h in range(2):
            P.op("pe", lambda e, h=h: e.matmul(pb[B_CUM][0:64, 2 + h, 0:2], sigw[0:n, 64 * h:64 * h + 64], cst[0:n, 8, 0:2], start=True, stop=True),
                 reads=[t_cst, t_sigw], writes=[tb])
        P.op("dve", lambda e: e.tensor_copy(cum[0:n, :], pb[B_CUM][0:n, 0, :]), reads=[tb], writes=[t_cum])
        P.op("dve", lambda e: e.scalar_tensor_tensor(c1[0:n, :], sigw[0:n, :], DS, cum[0:n, :], ALU.mult, ALU.add),
             reads=[t_sigw, t_cum], writes=[t_c1])
        P.op("dve", lambda e: e.tensor_tensor(c2[0:n, :], pb[B_CUM][0:n, 1, :], cum[0:n, :], ALU.subtract),
             reads=[tb, t_cum], writes=[t_c2])
        P.op("act", lambda e: e.activation(ep[0:n, :], cum[0:n, :], AF.Exp), reads=[t_cum], writes=[t_ep])
        P.op("act", lambda e: e.activation(em[0:n, :], cum[0:n, :], AF.Exp, scale=-1.0), reads=[t_cum], writes=[t_em])
        P.op("act", lambda e: e.activation(epm[0:n, :], c1[0:n, :], AF.Exp), reads=[t_c1], writes=[t_epm])
        P.op("act", lambda e: e.activation(ebar[0:n, :], c2[0:n, :], AF.Exp), reads=[t_c2], writes=[t_ebar])
        P.op("act", lambda e: e.activation(PC[:, :, :], pb[B_CUM][0:64, 2:4, 0:2], AF.Exp), reads=[tb], writes=[t_PC])
        P.op("dve", lambda e: e.tensor_tensor(kka[0:n, :], kk[0:n, :], a_[0:n, :], ALU.mult), reads=[t_kk, t_a], writes=[t_kka])
        P.op("dve", lambda e: e.tensor_tensor(TM[0:n, 0, :], kka[0:n, :], em[0:n, :], ALU.mult), reads=[t_kka, t_em], writes=[t_TM])
        P.op("dve", lambda e: e.tensor_tensor(TM[0:n, 1, :], kp[0:n, :], em[0:n, :], ALU.mult), reads=[t_kp, t_em], writes=[t_TM])
        P.op("dve", lambda e: e.scalar_tensor_tensor(TM[0:n, 2, :], kk[0:n, :], -1.0, epm[0:n, :], ALU.mult, ALU.mult),
             reads=[t_kk, t_epm], writes=[t_TM])
        P.op("dve", lambda e: e.tensor_tensor(TM[0:n, 3, :], xr, ep[0:n, :], ALU.mult), reads=[t_x, t_ep], writes=[t_TM])
        for c in range(nch):
            P.op("pool", lambda e, c=c: e.tensor_scalar(ebz[0:n, c, :], ebar[0:n, :], cst[0:n, 8, 2 + c:3 + c], None, ALU.mult),
                 reads=[t_ebar, t_cst], writes=[t_ebz])
            P.op("pool", lambda e, c=c: e.tensor_tensor(bbz[0:n, c, :], kka[0:n, :], ebz[0:n, c, :], ALU.mult),
                 reads=[t_kka, t_ebz], writes=[t_bbz])
            P.op("pool", lambda e, c=c: e.tensor_tensor(kbz[0:n, c, :], kp[0:n, :], ebz[0:n, c, :], ALU.mult),
                 reads=[t_kp, t_ebz], writes=[t_kbz])
        for h in range(2):
            FM, t_FM = FMh[h]
            bk = B_FM
            for s_ in range(4):
                P.op("pe", lambda e, s_=s_, h=h, bk=bk: e.transpose(pb[bk][0:64, s_, 0:n], TM[0:n, s_, 64 * h:64 * h + 64], ident_bf[0:n, 0:n]),
                     reads=[t_TM, t_idb], writes=[t_bank[bk]])
            P.op("act", lambda e, FM=FM, bk=bk: e.activation(FM[:, :, 0:n], pb[bk][0:64, :, 0:n], AF.Identity),
                 reads=[t_bank[bk]], writes=[t_FM])

        G = [b("G0"), b("G1")]
        NP = [b("NP0"), b("NP1")]
        Xb = [[b("X0a"), b("X0b")], [b("X1a"), b("X1b")]]
        for h in range(2):
            FM, t_FM = FMh[h]
            Gh, t_G = G[h]
            NPh, t_NP = NP[h]
            gbk = B_G
            gb = pb[gbk]
            for (so, sl, sr) in ((0, 0, 2), (1, 0, 3), (2, 1, 2), (3, 1, 3)):
                P.op("pe", lambda e, gb=gb, so=so, sl=sl, sr=sr, FM=FM: e.matmul(gb[0:n, so, 0:n], FM[:, sl, 0:n], FM[:, sr, 0:n], start=True, stop=True),
                     reads=[t_FM], writes=[t_bank[gbk]])
            nbk = B_CUM
            P.op("pe", lambda e, nbk=nbk, FM=FM, h=h: e.matmul(pb[nbk][0:n, 2 + h, 0:n], FM[:, 2, 0:n], FM[:, 0, 0:n], start=True, stop=True),
                 reads=[t_FM], writes=[t_bank[nbk]])
            P.op("dve", lambda e, gb=gb, Gh=Gh: e.tensor_tensor(Gh[0:n, :, 0:n], gb[0:n, :, 0:n], cst[0:n, 2:6, 0:n], ALU.mult),
                 reads=[t_bank[gbk], t_cst], writes=[t_G])
            P.op("dve", lambda e, nbk=nbk, NPh=NPh, h=h: e.tensor_tensor(NPh[0:n, 0, 1, 0:n], pb[nbk][0:n, 2 + h, 0:n], cst[0:n, 6, 0:n], ALU.mult),
                 reads=[t_bank[nbk], t_cst], writes=[t_NP])
            P.op("pool", lambda e, Gh=Gh, NPh=NPh: e.tensor_copy(NPh[0:n, 0, 0, 0:n], Gh[0:n, 0, 0:n]),
                 reads=[t_G], writes=[t_NP])
        P.mark()
        for h in range(2):
            hs = slice(64 * h, 64 * h + 64)
            vs = slice(256 + 64 * h, 256 + 64 * h + 64)
            Gh, t_G = G[h]
            xbk = B_HH[h]
            P.op("pe", lambda e, hs=hs, xbk=xbk: e.matmul(pb[xbk][0:n, 2, 0:64], ident_bf[0:n, 0:n], TM[0:n, 2, hs], start=True, stop=True),
                 reads=[t_TM, t_idb], writes=[t_bank[xbk]])
            P.op("pe", lambda e, hs=hs, xbk=xbk, Gh=Gh: e.matmul(pb[xbk][0:n, 2, 64:128], Gh[0:n, 2, 0:n], xvb[0:n, hs], start=True, stop=True),
                 reads=[t_G, t_xvb], writes=[t_bank[xbk]])
            X0, t_X0 = Xb[h][0]
            P.op("act", lambda e, xbk=xbk, X0=X0: e.activation(X0[0:n, :], pb[xbk][0:n, 2, :], AF.Identity),
                 reads=[t_bank[xbk]], writes=[t_X0])
        NPW = 6
        cur = [0, 0]
        for i in range(NPW):
            for h in range(2):
                NPh, t_NP = NP[h]
                nbk = B_HH[h]
                xbk = B_HH[h]
                Xc, t_Xc = Xb[h][cur[h]]
                Xn, t_Xn = Xb[h][1 - cur[h]]
                P.op("pe", lambda e, i=i, NPh=NPh, Xc=Xc, xbk=xbk: e.matmul(pb[xbk][0:n, 2, :], NPh[0:n, i, 0, 0:n], Xc[0:n, :], start=True, stop=True),
                     reads=[t_NP, t_Xc], writes=[t_bank[xbk]])
                P.op("dve", lambda e, Xc=Xc, Xn=Xn, xbk=xbk: e.tensor_tensor(Xn[0:n, :], pb[xbk][0:n, 2, :], Xc[0:n, :], ALU.add),
                     reads=[t_bank[xbk], t_Xc], writes=[t_Xn])
                cur[h] = 1 - cur[h]
                if i < NPW - 1:
                    P.op("pe", lambda e, i=i, NPh=NPh, nbk=nbk: e.matmul(pb[nbk][0:n, 0, 0:n], NPh[0:n, i, 1, 0:n], NPh[0:n, i, 0, 0:n], start=True, stop=True),
                         reads=[t_NP], writes=[t_bank[nbk]])
                    P.op("pe", lambda e, i=i, NPh=NPh, nbk=nbk: e.matmul(pb[nbk][0:n, 1, 0:n], NPh[0:n, i, 0, 0:n], NPh[0:n, i, 1, 0:n], start=True, stop=True),
                         reads=[t_NP], writes=[t_bank[nbk]])
                    P.op("act", lambda e, i=i, NPh=NPh, nbk=nbk: e.activation(NPh[0:n, i + 1, :, 0:n], pb[nbk][0:n, 0:2, 0:n], AF.Identity),
                         reads=[t_bank[nbk]], writes=[t_NP])
        Xf = [Xb[h][cur[h]] for h in range(2)]
        P.mark()
        tb = t_bank[B_PM]
        for h in range(2):
            hs = slice(64 * h, 64 * h + 64)
            Xh, t_Xh = Xf[h]
            for c in range(nch):
                P.op("pe", lambda e, hs=hs, c=c, h=h, Xh=Xh: e.matmul(pb[B_PM][0:64, h, 64 * c:64 * c + 64], Xh[0:n, 0:64], bbz[0:n, c, hs], start=True, stop=True),
                     reads=[t_Xh, t_bbz], writes=[tb])
        for h in range(2):
            for c in range(nch):
                P.op("dve", lambda e, h=h, c=c: e.scalar_tensor_tensor(M_[:, 2 * h + c, :], cst[0:64, 7, 0:64], PC[:, h, c:c + 1], pb[B_PM][0:64, h, 64 * c:64 * c + 64], ALU.mult, ALU.add),
                     reads=[tb, t_PC, t_cst], writes=[t_M])
        tb = t_bank[B_PM]
        for c in range(nch):
            ci = cb + c
            for h in range(2):
                hs = slice(64 * h, 64 * h + 64)
                vs = slice(256 + 64 * h, 256 + 64 * h + 64)
                Xh, t_Xh = Xf[h]
                P.op("pe", lambda e, h=h, c=c, ci=ci: e.matmul(pb[B_PM][0:64, 2 + h, 0:64], M_[:, 2 * h + c, :], S_all[h][:, ci, :], start=True, stop=False),
                     reads=[t_M, t_S[h][ci]], writes=[tb])
                P.op("pe", lambda e, h=h, hs=hs, c=c, Xh=Xh: e.matmul(pb[B_PM][0:64, 2 + h, 0:64], bbz[0:n, c, hs], Xh[0:n, 64:128], start=False, stop=False),
                     reads=[t_bbz, t_Xh], writes=[tb])
                P.op("pe", lambda e, h=h, hs=hs, vs=vs, c=c: e.matmul(pb[B_PM][0:64, 2 + h, 0:64], kbz[0:n, c, hs], xvb[0:n, hs], start=False, stop=True),
                     reads=[t_kbz, t_xvb], writes=[tb])
                P.op("act", lambda e, h=h, ci=ci: e.activation(S_all[h][:, ci + 1, :], pb[B_PM][0:64, 2 + h, 0:64], AF.Identity),
                     reads=[tb], writes=[t_S[h][ci + 1]])
        tb = t_bank[B_Y]
        for h in range(2):
            Xh, t_Xh = Xf[h]
            Gh, t_G = G[h]
            FM, t_FM = FMh[h]
            Rz, t_Rz = RHz[h]
            P.op("pe", lambda e, h=h, Xh=Xh, Gh=Gh: e.matmul(pb[B_Y][0:64, 1 + h, 0:n], Xh[0:n, 0:64], Gh[0:n, 1, 0:n], start=True, stop=True),
                 reads=[t_Xh, t_G], writes=[tb])
            for c in range(nch):
                cs = slice(64 * c, min(64 * c + 64, n))
                P.op("dve", lambda e, h=h, c=c, cs=cs, Rz=Rz, FM=FM: e.tensor_tensor(Rz[:, c, cs], pb[B_Y][0:64, 1 + h, cs], FM[:, 3, cs], ALU.add),
                     reads=[tb, t_FM], writes=[t_Rz])
        tb = t_bank[B_Y]
        for h in range(2):
            hs = slice(64 * h, 64 * h + 64)
            vs = slice(256 + 64 * h, 256 + 64 * h + 64)
            Xh, t_Xh = Xf[h]
            Gh, t_G = G[h]
            Rz, t_Rz = RHz[h]
            P.op("pe", lambda e, hs=hs, Xh=Xh, Gh=Gh: e.matmul(pb[B_Y][0:n, 0, hs], Gh[0:n, 1, 0:n], Xh[0:n, 64:128], start=True, stop=False),
                 reads=[t_G, t_Xh], writes=[tb])
            P.op("pe", lambda e, hs=hs, vs=vs, Gh=Gh: e.matmul(pb[B_Y][0:n, 0, hs], Gh[0:n, 3, 0:n], xvb[0:n, hs], start=False, stop=False),
                 reads=[t_G, t_xvb], writes=[tb])
            for c in range(nch):
                ci = cb + c
                P.op("pe", lambda e, h=h, hs=hs, ci=ci, c=c, Rz=Rz: e.matmul(pb[B_Y][0:n, 0, hs], Rz[:, c, 0:n], S_all[h][:, ci, :], start=False, stop=(c == nch - 1)),
                     reads=[t_Rz, t_S[h][ci]], writes=[tb])
        P.op("act", lambda e: e.activation(y_[0:n, :], pb[B_Y][0:n, 0, :], AF.Identity), reads=[tb], writes=[t_y])
        P.op("pool", lambda e: e.tensor_tensor(ysq[0:n, :], y_[0:n, :], y_[0:n, :], ALU.mult), reads=[t_y], writes=[t_ysq])
        for h in range(2):
            hs = slice(64 * h, 64 * h + 64)
            P.op("dve", lambda e, h=h, hs=hs: e.reduce_sum(st[0:n, h:h + 1], y_[0:n, hs], axis=AX.X), reads=[t_y], writes=[t_st])
            P.op("dve", lambda e, h=h, hs=hs: e.reduce_sum(st[0:n, 2 + h:3 + h], ysq[0:n, hs], axis=AX.X), reads=[t_ysq], writes=[t_st])
        P.op("dve", lambda e: e.tensor_scalar(st[0:n, 4:6], st[0:n, 0:2], 1.0 / 64, None, ALU.mult), reads=[t_st], writes=[t_st])
        P.op("dve", lambda e: e.tensor_tensor(st[0:n, 6:8], st[0:n, 4:6], st[0:n, 4:6], ALU.mult), reads=[t_st], writes=[t_st])
        P.op("dve", lambda e: e.scalar_tensor_tensor(st[0:n, 6:8], st[0:n, 2:4], 1.0 / 64, st[0:n, 6:8], ALU.mult, ALU.subtract), reads=[t_st], writes=[t_st])
        P.op("dve", lambda e: e.tensor_scalar(st[0:n, 6:8], st[0:n, 6:8], LNX_EPS, None, ALU.add), reads=[t_st], writes=[t_st])
        P.op("act", lambda e: e.activation(st[0:n, 6:8], st[0:n, 6:8], AF.Ln), reads=[t_st], writes=[t_st])
        P.op("act", lambda e: e.activation(st[0:n, 6:8], st[0:n, 6:8], AF.Exp, scale=-0.5), reads=[t_st], writes=[t_st])
        for h in range(2):
            hs = slice(64 * h, 64 * h + 64)
            P.op("dve", lambda e, h=h, hs=hs: e.tensor_scalar(yn[0:n, hs], y_[0:n, hs], st[0:n, 4 + h:5 + h], st[0:n, 6 + h:7 + h], ALU.subtract, ALU.mult),
                 reads=[t_y, t_st], writes=[t_yn])
        P.op("pool", lambda e: e.tensor_tensor(yn[0:n, :], yn[0:n, :], bc[0:n, 5, :], ALU.mult), reads=[t_yn, t_bc], writes=[t_yn])
        P.op("pool", lambda e: e.tensor_tensor(yn[0:n, :], yn[0:n, :], bc[0:n, 6, :], ALU.add), reads=[t_yn, t_bc], writes=[t_yn])
        for h in range(2):
            hs = slice(64 * h, 64 * h + 64)
            vs = slice(256 + 64 * h, 256 + 64 * h + 64)
            P.op("dve", lambda e, h=h, hs=hs, vs=vs: e.scalar_tensor_tensor(yn[0:n, hs], x[0:n, vs], bs[0:n, h:h + 1], yn[0:n, hs], ALU.mult, ALU.add),
                 reads=[t_x, t_bs, t_yn], writes=[t_yn])
        P.op("dve", lambda e: e.tensor_tensor(o_[0:n, :], yn[0:n, :], g_[0:n, :], ALU.mult), reads=[t_yn, t_g], writes=[t_o])
        P.dma("sp", o_d[t0:t0 + n, :], o_[0:n, :], reads=[t_o])

    streams = []
    for ti in range(NTX + 1):
        P.begin_stream()
        tile_body(ti)
        streams.append(P.end_stream())
    P.run_interleaved(streams, max_active=int(os.environ.get("RW_ACT", "2")))


def rwkv_host_inputs(p_rw, hp, prm):
    L = p_rw.shape[0]
    cs = slice(128 * hp, 128 * hp + 128)
    rkv = np.zeros((L + 1, 384), np.float32)
    rkv[1:, 0:128] = p_rw[:, 0:512][:, cs]
    rkv[1:, 128:256] = p_rw[:, 512:1024][:, cs]
    rkv[1:, 256:384] = p_rw[:, 1024:1536][:, cs]
    lo = np.zeros((256, L + 1), np.float32)
    lo[:, 1:] = p_rw[:, 1536:1792].T
    mu = prm["rwkv_mu"]
    mu_tm = np.concatenate([mu[0:512][cs], mu[512:1024][cs], mu[1024:1536][cs]])
    mu_tm = np.ascontiguousarray(np.broadcast_to(mu_tm[None, :], (128, 384)))
    mu_fm = np.zeros((128, 3), np.float32)
    mu_fm[0:64, 0] = mu[1536:1600]
    mu_fm[0:64, 1] = mu[1600:1664]
    mu_fm[:, 2] = mu[1664:1792]
    rows = [prm["w0"][cs], prm["a0"][cs], prm["k_k"][cs], prm["k_a"][cs], prm["r_k"].reshape(-1)[cs],
            prm["lnx_g"][cs], prm["lnx_b"][cs]]
    bc = np.ascontiguousarray(np.broadcast_to(np.stack(rows)[None], (128, 7, 128)))
    wa_up = np.ascontiguousarray(np.concatenate([prm["w_up"][:, cs], prm["a_up"][:, cs]], axis=0))
    g_up = np.ascontiguousarray(prm["g_up"][:, cs])
    return {"rkv": rkv, "lo": lo, "mu_tm": mu_tm, "mu_fm": mu_fm, "bc": bc, "wa_up": wa_up, "g_up": g_up,
            "cst": rwkv_consts()}


ALPHA = float((2 * 2) ** 0.25)
LN_EPS = 1e-5
NTOK = N_META + 2048
HALVES = [(0, [(0, 16), (16, 512), (528, 512)], [(0, 16)] + [(16 + 128 * i, 128) for i in range(8)]),
          (1040, [(0, 512), (512, 512)], [(128 * i, 128) for i in range(8)])]
C_IN = 5376


def tok_consts():
    c = np.zeros((128, 2, 128), np.float32)
    c[:, 0, :] = 1.0 / 1024
    c[:, 1, :] = np.eye(128)
    sel = np.zeros((16, 16, 128), np.float32)
    for e in range(16):
        sel[e, e, :] = 1.0
    return c, sel


def build_tok(nc, mode, do_proj, ntok=NTOK, halves=HALVES, n_exp=16):
    D = {}

    def inp(name, shape):
        D[name] = nc.dram_tensor(name, list(shape), F32, kind="ExternalInput").ap()

    def outp(name, shape):
        D[name] = nc.dram_tensor(name, list(shape), F32, kind="ExternalOutput").ap()

    inp("xT", [1024, ntok])
    inp("tc", [128, 2, 128])
    inp("lnA", [128, 2, 8])
    if mode == "C":
        inp("osbT", [512, ntok]); inp("orwT", [512, ntok]); inp("gT", [2048, ntok])
        inp("p_sb", [512, 1024]); inp("p_rw", [512, 1024]); inp("w_out", [1024, 1024])
        inp("lnB", [128, 2, 8])
        inp("router_w", [1024, 16]); inp("rb", [128, 16]); inp("sel", [16, 16, 128])
        inp("wg", [16, 1024, 512]); inp("wu", [16, 1024, 512]); inp("wd", [16, 512, 1024])
    if do_proj:
        inp("w_in", [1024, C_IN])
        outp("pT", [C_IN, ntok])
    outp("hT", [1024, ntok])
    with ExitStack() as es:
        P = Prog(nc, es)
        emit_tok(P, D, mode, do_proj, halves, n_exp)
        P.finish()
    return nc


def emit_tok(P, D, mode, do_proj, halves, n_exp=16):
    WMAX = 1040
    tc = P.sb("tc", [128, 2, 128]); t_tc = P.T(const=True)
    P.dma("sp", tc[:], D["tc"], writes=[t_tc])
    lnA = P.sb("lnA", [128, 2, 8]); t_lnA = P.T(const=True)
    P.dma("sp", lnA[:], D["lnA"], writes=[t_lnA])
    hT = P.sb("hT", [128, 8, WMAX]); hbf = P.sb("hbf", [128, 8, WMAX], BF16)
    NG = 3
    t_h = [[P.T(f"h{g}_{m}") for m in range(8)] for g in range(NG)]
    t_hb = [P.T(f"hb{g}") for g in range(NG)]
    NSTG = 4
    stg = [P.sb(f"stg{i}", [128, 2048]) for i in range(NSTG)]
    t_stg = [P.T() for _ in range(NSTG)]
    WQ = ["sp", "act"]
    ps = [P.ps(f"ps{i}") for i in range(8)]
    t_ps = [P.T(f"psb{i}", excl=True) for i in range(8)]
    mean_sb = P.sb("mean_sb", [128, 512]); t_mean = P.T()
    rstd_sb = P.sb("rstd_sb", [128, 512]); t_rstd = P.T()
    tmp = [P.sb(f"tmp{i}", [128, 512]) for i in range(2)]
    t_tmp = [P.T(), P.T()]
    cnt = {"stg": 0, "tmp": 0, "ev": 0}

    def ln_group(gi, c0, w, lnp, t_lnp):
        PM, PX = 6, 7
        for k in range(8):
            s = cnt["tmp"] % 2; cnt["tmp"] += 1
            P.op("act", lambda e, k=k, s=s: e.activation(tmp[s][:, 0:w], hT[:, k, c0:c0 + w], AF.Square),
                 reads=[t_h[gi][k]], writes=[t_tmp[s]])
            P.op("pe", lambda e, k=k: e.matmul(ps[PM][:, 0:w], tc[:, 0, :], hT[:, k, c0:c0 + w], start=(k == 0), stop=(k == 7)),
                 reads=[t_tc, t_h[gi][k]], writes=[t_ps[PM]])
            P.op("pe", lambda e, k=k, s=s: e.matmul(ps[PX][:, 0:w], tc[:, 0, :], tmp[s][:, 0:w], start=(k == 0), stop=(k == 7)),
                 reads=[t_tc, t_tmp[s]], writes=[t_ps[PX]])
        P.op("act", lambda e: e.activation(mean_sb[:, 0:w], ps[PM][:, 0:w], AF.Identity), reads=[t_ps[PM]], writes=[t_mean])
        P.op("dve", lambda e: e.tensor_tensor(rstd_sb[:, 0:w], mean_sb[:, 0:w], mean_sb[:, 0:w], ALU.mult), reads=[t_mean], writes=[t_rstd])
        P.op("dve", lambda e: e.tensor_tensor(rstd_sb[:, 0:w], ps[PX][:, 0:w], rstd_sb[:, 0:w], ALU.subtract), reads=[t_ps[PX], t_rstd], writes=[t_rstd])
        P.op("dve", lambda e: e.tensor_scalar(rstd_sb[:, 0:w], rstd_sb[:, 0:w], LN_EPS, None, ALU.add), reads=[t_rstd], writes=[t_rstd])
        P.op("act", lambda e: e.activation(rstd_sb[:, 0:w], rstd_sb[:, 0:w], AF.Ln), reads=[t_rstd], writes=[t_rstd])
        P.op("act", lambda e: e.activation(rstd_sb[:, 0:w], rstd_sb[:, 0:w], AF.Exp, scale=-0.5), reads=[t_rstd], writes=[t_rstd])
        for k in range(8):
            s = cnt["tmp"] % 2; cnt["tmp"] += 1
            P.op("dve", lambda e, k=k, s=s: e.tensor_tensor(tmp[s][:, 0:w], hT[:, k, c0:c0 + w], mean_sb[:, 0:w], ALU.subtract),
                 reads=[t_h[gi][k], t_mean], writes=[t_tmp[s]])
            P.op("dve", lambda e, s=s: e.tensor_tensor(tmp[s][:, 0:w], tmp[s][:, 0:w], rstd_sb[:, 0:w], ALU.mult),
                 reads=[t_tmp[s], t_rstd], writes=[t_tmp[s]])
            P.op("act", lambda e, k=k, s=s: e.activation(hT[:, k, c0:c0 + w], tmp[s][:, 0:w], AF.Identity, bias=lnp[:, 1, k:k + 1], scale=lnp[:, 0, k:k + 1]),
                 reads=[t_tmp[s], t_lnp], writes=[t_h[gi][k]])
            P.op("act", lambda e, k=k, s=s: e.activation(hbf[:, k, c0:c0 + w], tmp[s][:, 0:w], AF.Identity, bias=lnp[:, 1, k:k + 1], scale=lnp[:, 0, k:k + 1]),
                 reads=[t_tmp[s], t_lnp], writes=[t_hb[gi]])

    if do_proj:
        wpj = [P.sb(f"wpj{i}", [128, 8, 128], BF16) for i in range(2)]
        t_wpj = [P.T(), P.T()]
        ost = [P.sb(f"ost{i}", [128, 512]) for i in range(4)]
        t_ost = [P.T() for _ in range(4)]
        w_in_v = D["w_in"].rearrange("(k p) c -> p k c", p=128)

    def proj(groups, tok0):
        NJ = C_IN // 128

        def load_w(j):
            s = cnt["stg"] % NSTG; wq = "act"; cnt["stg"] += 1
            wb = j % 2
            P.dma(wq, stg[s][:, 0:1024].rearrange("p (k c) -> p k c", k=8), w_in_v[:, :, 128 * j:128 * j + 128], writes=[t_stg[s]])
            P.op("act", lambda e, s=s, wb=wb: e.activation(wpj[wb][:], stg[s][:, 0:1024].rearrange("p (k c) -> p k c", k=8), AF.Identity),
                 reads=[t_stg[s]], writes=[t_wpj[wb]])

        load_w(0)
        for j in range(NJ):
            wb = j % 2
            if j + 1 < NJ:
                load_w(j + 1)
            for gi, (c0, w) in enumerate(groups):
                bk = cnt["ev"] % 4
                for k in range(8):
                    P.op("pe", lambda e, k=k, wb=wb, bk=bk, c0=c0, w=w: e.matmul(ps[bk][:, 0:w], wpj[wb][:, k, :], hbf[:, k, c0:c0 + w], start=(k == 0), stop=(k == 7)),
                         reads=[t_wpj[wb], t_hb[gi]], writes=[t_ps[bk]])
                cnt["ev"] += 1
                P.op("dve", lambda e, bk=bk, w=w: e.tensor_copy(ost[bk][:, 0:w], ps[bk][:, 0:w]), reads=[t_ps[bk]], writes=[t_ost[bk]])
                P.dma("sp", D["pT"][128 * j:128 * j + 128, tok0 + c0:tok0 + c0 + w], ost[bk][:, 0:w], reads=[t_ost[bk]])

    if mode == "C":
        lnB = P.sb("lnB", [128, 2, 8]); t_lnB = P.T(const=True)
        P.dma("sp", lnB[:], D["lnB"], writes=[t_lnB])
        rw = P.sb("rw", [128, 8, 16]); t_rw = P.T(const=True)
        P.dma("sp", rw[:], D["router_w"].rearrange("(k p) e -> p k e", p=128), writes=[t_rw])
        rb = P.sb("rb", [128, 16]); t_rb = P.T(const=True)
        P.dma("sp", rb[:], D["rb"], writes=[t_rb])
        sel = P.sb("sel", [16, 16, 128]); t_sel = P.T(const=True)
        P.dma("sp", sel[:], D["sel"], writes=[t_sel])
        arena = [P.sb(f"arena{i}", [128, 8192], BF16) for i in range(2)]
        t_ar = [P.T("arena0"), P.T("arena1")]
        ob = [P.sb(f"ob{i}", [128, 4, 512], BF16) for i in range(2)]
        t_ob = [P.T(), P.T()]
        gts = [P.sb(f"gts{i}", [128, 512]) for i in range(4)]
        t_gts = [P.T() for _ in range(4)]
        merged = P.sb("merged", [128, 8, 512], BF16); t_merged = P.T()
        combT = P.sb("combT", [16, WMAX]); t_combT = P.T()
        cbc = [P.sb(f"cbc{i}", [128, WMAX]) for i in range(2)]
        t_cbc = [P.T(), P.T()]
        hid = [P.sb(f"hid{i}", [128, 2, 512], BF16) for i in range(2)]
        t_hid = [P.T(), P.T()]
        sgl = [P.sb(f"sgl{i}", [128, 512]) for i in range(2)]
        t_sgl = [P.T(), P.T()]
        rt = P.sb("rt", [128, 16, 16]); t_rt = P.T()
        rs = P.sb("rs", [128, 16]); t_rs = P.T()
        osb_v = D["osbT"].rearrange("(k p) t -> p k t", p=128)
        orw_v = D["orwT"].rearrange("(k p) t -> p k t", p=128)
        psb_v = D["p_sb"].rearrange("(k p) c -> p k c", p=128)
        prw_v = D["p_rw"].rearrange("(k p) c -> p k c", p=128)
        wout_v = D["w_out"].rearrange("(k p) c -> p k c", p=128)
        psb_bf = arena[0][:, 0:4096].rearrange("p (k c) -> p k c", k=4)
        prw_bf = arena[0][:, 4096:8192].rearrange("p (k c) -> p k c", k=4)
        wout_bf = arena[1][:, 0:8192].rearrange("p (k c) -> p k c", k=8)

    xT_v = D["xT"].rearrange("(k p) t -> p k t", p=128)
    hTo_v = D["hT"].rearrange("(k p) t -> p k t", p=128)

    for (tok0, groups, rtiles) in halves:
        for gi, (c0, w) in enumerate(groups):
            P.dma("sp", hT[:, :, c0:c0 + w], xT_v[:, :, tok0 + c0:tok0 + c0 + w], writes=t_h[gi])
        if mode == "A":
            for gi, (c0, w) in enumerate(groups):
                ln_group(gi, c0, w, lnA, t_lnA)
        else:
            for (src, dst, ai, nk) in ((psb_v, psb_bf, 0, 4), (prw_v, prw_bf, 0, 4), (wout_v, wout_bf, 1, 8)):
                for k0 in range(0, nk, 2):
                    s = cnt["stg"] % NSTG; wq = WQ[cnt["stg"] % 2]; cnt["stg"] += 1
                    P.dma(wq, stg[s][:, 0:2048].rearrange("p (k c) -> p k c", k=2), src[:, k0:k0 + 2, :], writes=[t_stg[s]])
                    P.op("act", lambda e, s=s, dst=dst, k0=k0: e.activation(dst[:, k0:k0 + 2, :], stg[s][:, 0:2048].rearrange("p (k c) -> p k c", k=2), AF.Identity),
                         reads=[t_stg[s]], writes=[t_ar[ai]])
            for gi, (c0, w) in enumerate(groups):
                for (src, oi) in ((osb_v, 0), (orw_v, 1)):
                    s = cnt["stg"] % NSTG; wq = WQ[cnt["stg"] % 2]; cnt["stg"] += 1
                    P.dma(wq, stg[s][:, 0:4 * w].rearrange("p (k c) -> p k c", k=4), src[:, :, tok0 + c0:tok0 + c0 + w], writes=[t_stg[s]])
                    P.op("dve", lambda e, s=s, oi=oi, w=w: e.tensor_copy(ob[oi][:, :, 0:w], stg[s][:, 0:4 * w].rearrange("p (k c) -> p k c", k=4)),
                         reads=[t_stg[s]], writes=[t_ob[oi]])
                for m in range(8):
                    ms = slice(128 * m, 128 * m + 128)
                    ba, bb = 0 + (m % 2) * 2, 1 + (m % 2) * 2
                    for k in range(4):
                        P.op("pe", lambda e, k=k, ms=ms, ba=ba, w=w: e.matmul(ps[ba][:, 0:w], psb_bf[:, k, ms], ob[0][:, k, 0:w], start=(k == 0), stop=(k == 3)),
                             reads=[t_ar[0], t_ob[0]], writes=[t_ps[ba]])
                    for k in range(4):
                        P.op("pe", lambda e, k=k, ms=ms, bb=bb, w=w: e.matmul(ps[bb][:, 0:w], prw_bf[:, k, ms], ob[1][:, k, 0:w], start=(k == 0), stop=(k == 3)),
                             reads=[t_ar[0], t_ob[1]], writes=[t_ps[bb]])
                    g0, g1 = (m % 2) * 2, (m % 2) * 2 + 1
                    P.dma("sp", gts[g0][:, 0:w], D["gT"][128 * m:128 * m + 128, tok0 + c0:tok0 + c0 + w], writes=[t_gts[g0]])
                    P.dma("sp", gts[g1][:, 0:w], D["gT"][1024 + 128 * m:1024 + 128 * m + 128, tok0 + c0:tok0 + c0 + w], writes=[t_gts[g1]])
                    P.op("act", lambda e, g0=g0, w=w: e.activation(gts[g0][:, 0:w], gts[g0][:, 0:w], AF.Sigmoid), reads=[t_gts[g0]], writes=[t_gts[g0]])
                    P.op("act", lambda e, g1=g1, w=w: e.activation(gts[g1][:, 0:w], gts[g1][:, 0:w], AF.Sigmoid), reads=[t_gts[g1]], writes=[t_gts[g1]])
                    P.op("dve", lambda e, g0=g0, ba=ba, w=w: e.tensor_tensor(gts[g0][:, 0:w], ps[ba][:, 0:w], gts[g0][:, 0:w], ALU.mult),
                         reads=[t_ps[ba], t_gts[g0]], writes=[t_gts[g0]])
                    P.op("dve", lambda e, g1=g1, bb=bb, w=w: e.tensor_tensor(gts[g1][:, 0:w], ps[bb][:, 0:w], gts[g1][:, 0:w], ALU.mult),
                         reads=[t_ps[bb], t_gts[g1]], writes=[t_gts[g1]])
                    P.op("pool", lambda e, g0=g0, g1=g1, m=m, w=w: e.tensor_tensor(merged[:, m, 0:w], gts[g0][:, 0:w], gts[g1][:, 0:w], ALU.add),
                         reads=[t_gts[g0], t_gts[g1]], writes=[t_merged])
                for m in range(8):
                    ms = slice(128 * m, 128 * m + 128)
                    bk = 4 + (m % 2)
                    for k in range(8):
                        P.op("pe", lambda e, k=k, ms=ms, bk=bk, w=w: e.matmul(ps[bk][:, 0:w], wout_bf[:, k, ms], merged[:, k, 0:w], start=(k == 0), stop=(k == 7)),
                             reads=[t_ar[1], t_merged], writes=[t_ps[bk]])
                    P.op("dve", lambda e, m=m, bk=bk, c0=c0, w=w: e.scalar_tensor_tensor(hT[:, m, c0:c0 + w], hT[:, m, c0:c0 + w], ALPHA, ps[bk][:, 0:w], ALU.mult, ALU.add),
                         reads=[t_ps[bk], t_h[gi][m]], writes=[t_h[gi][m]])
                ln_group(gi, c0, w, lnA, t_lnA)
            for (r0, nt) in rtiles:
                gi = [i for i, (c0, w) in enumerate(groups) if c0 <= r0 < c0 + w][0]
                RB = 5
                for k in range(8):
                    P.op("pe", lambda e, k=k, r0=r0, nt=nt: e.matmul(ps[RB][0:nt, 0:16], hT[:, k, r0:r0 + nt], rw[:, k, :], start=(k == 0), stop=(k == 7)),
                         reads=[t_h[gi][k], t_rw], writes=[t_ps[RB]])
                R = lambda i: rt[0:nt, i, :]
                S = lambda i: rs[0:nt, i:i + 1]

                def dv(fn, nt=nt):
                    P.op("dve", fn, reads=[t_rt, t_rs], writes=[t_rt, t_rs])
                P.op("dve", lambda e, nt=nt: e.tensor_tensor(rt[0:nt, 0, :], ps[RB][0:nt, 0:16], rb[0:nt, :], ALU.add),
                     reads=[t_ps[RB], t_rb], writes=[t_rt])
                dv(lambda e, nt=nt: e.reduce_max(rs[0:nt, 0:1], rt[0:nt, 0, :], axis=AX.X))
                dv(lambda e, nt=nt: e.tensor_scalar(rs[0:nt, 0:1], rs[0:nt, 0:1], -1.0, None, ALU.mult))
                P.op("act", lambda e, nt=nt: e.activation(rt[0:nt, 1, :], rt[0:nt, 0, :], AF.Exp, bias=rs[0:nt, 0:1]),
                     reads=[t_rt, t_rs], writes=[t_rt])
                dv(lambda e, nt=nt: e.reduce_sum(rs[0:nt, 1:2], rt[0:nt, 1, :], axis=AX.X))
                dv(lambda e, nt=nt: e.reciprocal(rs[0:nt, 1:2], rs[0:nt, 1:2]))
                dv(lambda e, nt=nt: e.tensor_scalar(rt[0:nt, 2, :], rt[0:nt, 1, :], rs[0:nt, 1:2], None, ALU.mult))
                for g in range(4):
                    dv(lambda e, nt=nt, g=g: e.reduce_max(rt[0:nt, 3, g:g + 1], rt[0:nt, 2, 4 * g:4 * g + 4], axis=AX.X))
                for g in range(4):
                    dv(lambda e, nt=nt, g=g: e.tensor_scalar(rt[0:nt, 4, 4 * g:4 * g + 4], rt[0:nt, 2, 4 * g:4 * g + 4], rt[0:nt, 3, g:g + 1], None, ALU.is_equal))
                dv(lambda e, nt=nt: e.scalar_tensor_tensor(rt[0:nt, 5, :], rt[0:nt, 4, :], -2.0, rt[0:nt, 2, :], ALU.mult, ALU.add))
                for g in range(4):
                    dv(lambda e, nt=nt, g=g: e.reduce_max(rt[0:nt, 3, 4 + g:5 + g], rt[0:nt, 5, 4 * g:4 * g + 4], axis=AX.X))
                dv(lambda e, nt=nt: e.tensor_tensor(rt[0:nt, 3, 8:12], rt[0:nt, 3, 0:4], rt[0:nt, 3, 4:8], ALU.add))
                dv(lambda e, nt=nt: e.reduce_max(rs[0:nt, 2:3], rt[0:nt, 3, 8:12], axis=AX.X))
                dv(lambda e, nt=nt: e.tensor_scalar(rt[0:nt, 3, 12:16], rt[0:nt, 3, 8:12], rs[0:nt, 2:3], None, ALU.is_equal))
                for g in range(4):
                    dv(lambda e, nt=nt, g=g: e.tensor_scalar(rt[0:nt, 6, 4 * g:4 * g + 4], rt[0:nt, 2, 4 * g:4 * g + 4], 1.0, rt[0:nt, 3, 12 + g:13 + g], ALU.add, ALU.mult))
                dv(lambda e, nt=nt: e.tensor_scalar(rt[0:nt, 6, :], rt[0:nt, 6, :], -1.0, None, ALU.add))
                dv(lambda e, nt=nt: e.reduce_max(rs[0:nt, 3:4], rt[0:nt, 6, :], axis=AX.X))
                dv(lambda e, nt=nt: e.tensor_scalar(rt[0:nt, 7, :], rt[0:nt, 6, :], rs[0:nt, 3:4], None, ALU.is_equal))
                dv(lambda e, nt=nt: e.scalar_tensor_tensor(rt[0:nt, 8, :], rt[0:nt, 7, :], -2.0, rt[0:nt, 6, :], ALU.mult, ALU.add))
                dv(lambda e, nt=nt: e.reduce_max(rs[0:nt, 4:5], rt[0:nt, 8, :], axis=AX.X))
                dv(lambda e, nt=nt: e.tensor_scalar(rt[0:nt, 9, :], rt[0:nt, 8, :], rs[0:nt, 4:5], None, ALU.is_equal))
                dv(lambda e, nt=nt: e.tensor_tensor(rs[0:nt, 5:6], rs[0:nt, 3:4], rs[0:nt, 4:5], ALU.add))
                dv(lambda e, nt=nt: e.reciprocal(rs[0:nt, 5:6], rs[0:nt, 5:6]))
                dv(lambda e, nt=nt: e.tensor_tensor(rs[0:nt, 6:7], rs[0:nt, 3:4], rs[0:nt, 5:6], ALU.mult))
                dv(lambda e, nt=nt: e.tensor_tensor(rs[0:nt, 7:8], rs[0:nt, 4:5], rs[0:nt, 5:6], ALU.mult))
                dv(lambda e, nt=nt: e.tensor_scalar(rt[0:nt, 10, :], rt[0:nt, 7, :], rs[0:nt, 6:7], None, ALU.mult))
                dv(lambda e, nt=nt: e.scalar_tensor_tensor(rt[0:nt, 11, :], rt[0:nt, 9, :], rs[0:nt, 7:8], rt[0:nt, 10, :], ALU.mult, ALU.add))
                P.op("pe", lambda e, nt=nt: e.transpose(ps[RB][0:16, 256:256 + nt], rt[0:nt, 11, :], tc[0:nt, 1, 0:nt]),
                     reads=[t_rt, t_tc], writes=[t_ps[RB]])
                P.op("act", lambda e, nt=nt, r0=r0: e.activation(combT[:, r0:r0 + nt], ps[RB][0:16, 256:256 + nt], AF.Identity),
                     reads=[t_ps[RB]], writes=[t_combT])
            for gi, (c0, w) in enumerate(groups):
                P.op("pool", lambda e, c0=c0, w=w: e.tensor_scalar(hT[:, :, c0:c0 + w], hT[:, :, c0:c0 + w], ALPHA, None, ALU.mult),
                     reads=t_h[gi], writes=t_h[gi])
            nhe = 0
            pending = []
            for ex in range(n_exp):
                cb_ = ex % 2
                for gi, (c0, w) in enumerate(groups):
                    P.op("pe", lambda e, ex=ex, c0=c0, w=w: e.matmul(ps[6][:, 0:w], sel[:, ex, :], combT[:, c0:c0 + w], start=True, stop=True),
                         reads=[t_sel, t_combT], writes=[t_ps[6]])
                    P.op("act", lambda e, cb_=cb_, c0=c0, w=w: e.activation(cbc[cb_][:, c0:c0 + w], ps[6][:, 0:w], AF.Identity),
                         reads=[t_ps[6]], writes=[t_cbc[cb_]])
                for hf in range(2):
                    ai = nhe % 2
                    nhe += 1
                    wg_bf = arena[ai][:, 0:2048].rearrange("p (k c) -> p k c", k=8)
                    wu_bf = arena[ai][:, 2048:4096].rearrange("p (k c) -> p k c", k=8)
                    wd_bf = arena[ai][:, 4096:6144].rearrange("p (k c) -> p k c", k=2)
                    def load_he(n_):
                        ex_, hf_ = n_ // 2, n_ % 2
                        a_ = n_ % 2
                        fs = slice(256 * hf_, 256 * hf_ + 256)
                        for (src, lo_, kk_) in ((D["wg"][ex_].rearrange("(k p) f -> p k f", p=128)[:, :, fs], 0, 8),
                                                (D["wu"][ex_].rearrange("(k p) f -> p k f", p=128)[:, :, fs], 2048, 8),
                                                (D["wd"][ex_, 256 * hf_:256 * hf_ + 256, :].rearrange("(k p) d -> p k d", p=128), 4096, 2)):
                            dst = arena[a_][:, lo_:lo_ + 2048].rearrange("p (k c) -> p k c", k=kk_)
                            s = cnt["stg"] % NSTG; wq = WQ[cnt["stg"] % 2]; cnt["stg"] += 1
                            P.dma(wq, stg[s][:, 0:2048].rearrange("p (k c) -> p k c", k=kk_), src, writes=[t_stg[s]])
                            P.op("act", lambda e, s=s, dst=dst, kk_=kk_: e.activation(dst, stg[s][:, 0:2048].rearrange("p (k c) -> p k c", k=kk_), AF.Identity),
                                 reads=[t_stg[s]], writes=[t_ar[a_]])

                    if nhe == 1:
                        load_he(0)
                    for gi, (c0, w) in enumerate(groups):
                        hb_ = cnt["ev"] % 2
                        cnt["ev"] += 1
                        dsteps = pending.pop() if pending else []
                        for fc in range(2):
                            bg, bu = fc, 2 + fc
                            fcs = slice(128 * fc, 128 * fc + 128)
                            for (bk_, wsrc) in ((bg, wg_bf), (bu, wu_bf)):
                                for k in range(8):
                                    P.op("pe", lambda e, k=k, bk_=bk_, fcs=fcs, c0=c0, w=w, wsrc=wsrc: e.matmul(ps[bk_][:, 0:w], wsrc[:, k, fcs], hbf[:, k, c0:c0 + w], start=(k == 0), stop=(k == 7)),
                                         reads=[t_ar[ai], t_hb[gi]], writes=[t_ps[bk_]])
                                    if k % 4 == 3 and dsteps:
                                        dsteps.pop(0)()
                            P.op("act", lambda e, fc=fc, bg=bg, w=w: e.activation(sgl[fc][:, 0:w], ps[bg][:, 0:w], AF.Silu),
                                 reads=[t_ps[bg]], writes=[t_sgl[fc]])
                            P.op("dve", lambda e, fc=fc, bu=bu, w=w: e.tensor_tensor(sgl[fc][:, 0:w], ps[bu][:, 0:w], sgl[fc][:, 0:w], ALU.mult),
                                 reads=[t_ps[bu], t_sgl[fc]], writes=[t_sgl[fc]])
                            P.op("dve", lambda e, fc=fc, hb_=hb_, cb_=cb_, c0=c0, w=w: e.tensor_tensor(hid[hb_][:, fc, 0:w], sgl[fc][:, 0:w], cbc[cb_][:, c0:c0 + w], ALU.mult),
                                 reads=[t_sgl[fc], t_cbc[cb_]], writes=[t_hid[hb_]])
                        while dsteps:
                            dsteps.pop(0)()
                        if gi == 0 and nhe < 2 * n_exp:
                            load_he(nhe)

                        def mk_down(m, gi=gi, c0=c0, w=w, hb_=hb_, ai=ai, wd_bf=wd_bf):
                            def f():
                                bd = 4 + (m % 4)
                                ms = slice(128 * m, 128 * m + 128)
                                for fc in range(2):
                                    P.op("pe", lambda e, fc=fc: e.matmul(ps[bd][:, 0:w], wd_bf[:, fc, ms], hid[hb_][:, fc, 0:w], start=(fc == 0), stop=(fc == 1)),
                                         reads=[t_ar[ai], t_hid[hb_]], writes=[t_ps[bd]])
                                P.op("dve", lambda e: e.tensor_tensor(hT[:, m, c0:c0 + w], ps[bd][:, 0:w], hT[:, m, c0:c0 + w], ALU.add),
                                     reads=[t_ps[bd], t_h[gi][m]], writes=[t_h[gi][m]])
                            return f
                        pending.append([mk_down(m) for m in range(8)])
            if pending:
                for f_ in pending.pop():
                    f_()
            for gi, (c0, w) in enumerate(groups):
                ln_group(gi, c0, w, lnB, t_lnB)
        for gi, (c0, w) in enumerate(groups):
            P.dma("sp", hTo_v[:, :, tok0 + c0:tok0 + c0 + w], hT[:, :, c0:c0 + w], reads=t_h[gi])
        if do_proj:
            proj(groups, tok0)


NCORES = 8
SEQ = 8192
LFULL = N_META + SEQ


def _fm(v):
    return np.ascontiguousarray(np.asarray(v, np.float32).reshape(8, 128).T)


def _run(nc, in_maps):
    res = run_bass_kernel_spmd(nc, in_maps, core_ids=list(range(NCORES)))
    return res.results


def _new_nc():
    return bass.Bass("TRN2", target_bir_lowering=False)


def _mixers(pT_cores, prm):
    amask = attn_masks()
    attn_maps, rwkv_maps = [], []
    for c in range(NCORES):
        b, hp = c // 4, c % 4
        pb_ = np.concatenate([pT_cores[4 * b][:, 0:N_META]] + [pT_cores[4 * b + r][:, N_META:] for r in range(4)], axis=1)
        q = pb_[128 * hp:128 * hp + 128].reshape(2, 64, LFULL)
        k = pb_[512 + 128 * hp:512 + 128 * hp + 128].reshape(2, 64, LFULL)
        v = pb_[1024 + 128 * hp:1024 + 128 * hp + 128].reshape(2, 64, LFULL)
        vtm = v.transpose(0, 2, 1)
        vm = np.ascontiguousarray(vtm[:, 0:N_META])
        vx = np.ascontiguousarray(vtm[:, N_META:].reshape(2, SEQ // 128, 128, 64).transpose(0, 2, 1, 3))
        attn_maps.append({"qT": np.ascontiguousarray(q), "kT": np.ascontiguousarray(k), "vx": vx, "vm": vm, "msk": amask})
        p_rw = np.ascontiguousarray(pb_[1536:3328].T)
        rwkv_maps.append(rwkv_host_inputs(p_rw, hp, prm))
    nc = _new_nc()
    build_attn(nc, 2, SEQ // 512)
    ares = _run(nc, attn_maps)
    nc = _new_nc()
    build_rwkv(nc, SEQ // 128)
    rres = _run(nc, rwkv_maps)
    osbT, orwT = [], []
    for b in range(2):
        osbT.append(np.concatenate([ares[4 * b + hp]["oT"].reshape(128, LFULL) for hp in range(4)], axis=0))
        orwT.append(np.concatenate([rres[4 * b + hp]["o_rw"].T for hp in range(4)], axis=0))
    return osbT, orwT


def _core_cols(full, r):
    return np.ascontiguousarray(np.concatenate([full[:, 0:N_META], full[:, N_META + 2048 * r:N_META + 2048 * (r + 1)]], axis=1))


def kernel(**inputs):
    inp = {k: np.asarray(v) for k, v in inputs.items()}
    x, meta = inp["x"].astype(np.float32), inp["meta"].astype(np.float32)
    tcc, sel = tok_consts()
    maps = []
    for c in range(NCORES):
        b, r = c // 4, c % 4
        xT = np.ascontiguousarray(np.concatenate([meta.T, x[b, 2048 * r:2048 * (r + 1)].T], axis=1))
        maps.append({"xT": xT, "tc": tcc, "lnA": np.stack([_fm(inp["emb_ln_g"]), _fm(inp["emb_ln_b"])], axis=1),
                     "w_in": np.ascontiguousarray(inp["w_in"][0])})
    nc = _new_nc()
    build_tok(nc, "A", True)
    res = _run(nc, maps)
    hT_c = [r_["hT"] for r_ in res]
    pT_c = [r_["pT"] for r_ in res]
    for l in range(2):
        prm = {k: inp[k][l] for k in ["rwkv_mu", "w0", "w_up", "a0", "a_up", "g_up", "k_k", "k_a", "r_k", "lnx_g", "lnx_b"]}
        osbT, orwT = _mixers(pT_c, prm)
        last = (l == 1)
        maps = []
        for c in range(NCORES):
            b, r = c // 4, c % 4
            m = {"xT": hT_c[c], "tc": tcc, "lnA": np.stack([_fm(inp["ln1_g"][l]), _fm(inp["ln1_b"][l])], axis=1),
                 "osbT": _core_cols(osbT[b], r), "orwT": _core_cols(orwT[b], r),
                 "gT": np.ascontiguousarray(pT_c[c][3328:5376]),
                 "p_sb": np.ascontiguousarray(inp["p_sb"][l]), "p_rw": np.ascontiguousarray(inp["p_rwkv"][l]),
                 "w_out": np.ascontiguousarray(inp["w_out"][l]),
                 "lnB": np.stack([_fm(inp["ln2_g"][l]), _fm(inp["ln2_b"][l])], axis=1),
                 "router_w": np.ascontiguousarray(inp["router_w"]),
                 "rb": np.ascontiguousarray(np.broadcast_to(inp["router_b"][None].astype(np.float32), (128, 16))),
                 "sel": sel,
                 "wg": np.ascontiguousarray(inp["exp_w_gate"][l]), "wu": np.ascontiguousarray(inp["exp_w_up"][l]),
                 "wd": np.ascontiguousarray(inp["exp_w_down"][l])}
            if not last:
                m["w_in"] = np.ascontiguousarray(inp["w_in"][l + 1])
            maps.append(m)
        nc = _new_nc()
        build_tok(nc, "C", not last)
        res = _run(nc, maps)
        hT_c = [r_["hT"] for r_ in res]
        if not last:
            pT_c = [r_["pT"] for r_ in res]
    out = np.zeros((2, SEQ, 1024), np.float32)
    for c in range(NCORES):
        b, r = c // 4, c % 4
        out[b, 2048 * r:2048 * (r + 1)] = hT_c[c][:, N_META:].T
    return out
```
